# Optimizing a Trainium2 kernel written in Bass

```python
import numpy as np
import jax
import jax.numpy as jnp
from jax import lax

D_MODEL = 1024
BATCH = 16
SEQ = 2048
DEPTH = 2

HEAD_DIM = 64
MIX_WIDTH = D_MODEL
N_GROUPS = 4
GROUP_WIDTH = MIX_WIDTH // N_GROUPS
NSA_HEADS = GROUP_WIDTH // HEAD_DIM
NSA_BRANCHES = 3
CMP_LEN = 32
CMP_STRIDE = 16
CMP_HIDDEN = 128
SLC_BLOCK = 64
SLC_TOP = 8
NSA_WINDOW = 512
CONV_K = 3
SWA_HEADS = GROUP_WIDTH // HEAD_DIM
SWA_KV_HEADS = 2
SWA_WINDOW = 128
POOL_WINDOWS = (2, 4, 8, 16)
POOL_DIM = GROUP_WIDTH // len(POOL_WINDOWS)
D_FF = 128 * ((8 * D_MODEL // 3 + 127) // 128)
Q_BLOCK = 128
EPS = 1e-6
NEG = -1e30
FORCE = 1e4
ATTN_SCALE = HEAD_DIM ** -0.5

SPLIT_SIZES = (
    NSA_HEADS * HEAD_DIM,
    HEAD_DIM, HEAD_DIM,
    HEAD_DIM, HEAD_DIM,
    HEAD_DIM, HEAD_DIM,
    NSA_HEADS * NSA_BRANCHES,
    GROUP_WIDTH, GROUP_WIDTH, GROUP_WIDTH,
    SWA_HEADS * HEAD_DIM,
    SWA_KV_HEADS * HEAD_DIM, SWA_KV_HEADS * HEAD_DIM,
    GROUP_WIDTH,
)
IN_WIDTH = sum(SPLIT_SIZES)

kernel_name = 'hymba_style_hybrid_nsa_conv_swa_pool'


def rms_norm(x, g):
    xf = x.astype(jnp.float32)
    y = xf * lax.rsqrt(jnp.mean(xf * xf, axis=-1, keepdims=True) + EPS)
    return (y * g.astype(jnp.float32)).astype(x.dtype)


def swiglu(h, w1, w3, w2):
    return (jax.nn.silu(h @ w1) * (h @ w3)) @ w2


def banded_attention(q, k, v, window, sinks=None):
    B, T, H, dh = q.shape
    G = k.shape[2]
    R = H // G
    nb = T // Q_BLOCK
    span = Q_BLOCK + window
    kp = jnp.pad(k, ((0, 0), (window, 0), (0, 0), (0, 0)))
    vp = jnp.pad(v, ((0, 0), (window, 0), (0, 0), (0, 0)))
    idx = np.arange(nb)[:, None] * Q_BLOCK + np.arange(span)[None, :]
    kb = kp[:, idx]
    vb = vp[:, idx]
    qb = q.reshape(B, nb, Q_BLOCK, G, R, dh)
    s = jnp.einsum('bnqgrd,bnkgd->bngrqk', qb, kb).astype(jnp.float32) * ATTN_SCALE
    qpos = np.arange(nb)[:, None, None] * Q_BLOCK + np.arange(Q_BLOCK)[None, :, None]
    kpos = idx[:, None, :] - window
    diff = qpos - kpos
    mask = (diff >= 0) & (diff < window) & (kpos >= 0)
    s = jnp.where(mask[None, :, None, None], s, NEG)
    if sinks is None:
        p = jax.nn.softmax(s, axis=-1)
    else:
        sink = sinks.astype(jnp.float32).reshape(1, 1, G, R, 1, 1)
        m = jnp.maximum(jnp.max(s, axis=-1, keepdims=True), sink)
        e = jnp.exp(s - m)
        p = e / (jnp.sum(e, axis=-1, keepdims=True) + jnp.exp(sink - m))
    o = jnp.einsum('bngrqk,bnkgd->bnqgrd', p.astype(v.dtype), vb)
    return o.reshape(B, T, H, dh)


def compress(kv, pos, w1, w2):
    B, T, dh = kv.shape
    n_c = (T - CMP_LEN) // CMP_STRIDE + 1
    idx = np.arange(n_c)[:, None] * CMP_STRIDE + np.arange(CMP_LEN)[None, :]
    blk = kv[:, idx] + pos.astype(kv.dtype)
    return jax.nn.gelu(blk.reshape(B, n_c, CMP_LEN * dh) @ w1) @ w2


def selected_attention(q, k, v, sel):
    B, T, H, dh = q.shape
    n = sel.shape[-1]
    kb = k.reshape(B, T // SLC_BLOCK, SLC_BLOCK, dh)
    vb = v.reshape(B, T // SLC_BLOCK, SLC_BLOCK, dh)
    gather = jax.vmap(lambda blocks, ix: blocks[ix])
    offs = jnp.arange(SLC_BLOCK)

    def one_block(i):
        t0 = i * Q_BLOCK
        qb = lax.dynamic_slice_in_dim(q, t0, Q_BLOCK, axis=1)
        ib = lax.dynamic_slice_in_dim(sel, t0, Q_BLOCK, axis=1)
        kg = gather(kb, ib)
        vg = gather(vb, ib)
        s = jnp.einsum('bqhd,bqnld->bqhnl', qb, kg).astype(jnp.float32) * ATTN_SCALE
        kpos = ib[..., None] * SLC_BLOCK + offs
        tpos = t0 + jnp.arange(Q_BLOCK)
        mask = (kpos <= tpos[None, :, None, None])[:, :, None]
        s = jnp.where(mask, s, NEG).reshape(B, Q_BLOCK, H, n * SLC_BLOCK)
        p = jax.nn.softmax(s, axis=-1).reshape(B, Q_BLOCK, H, n, SLC_BLOCK)
        return jnp.einsum('bqhnl,bqnld->bqhd', p.astype(vg.dtype), vg)

    out = lax.map(one_block, jnp.arange(T // Q_BLOCK))
    return jnp.moveaxis(out, 0, 1).reshape(B, T, H, dh)


def nsa(q, kc, vc, ks, vs, kw, vw, gates):
    B, T, H, dh = q.shape
    n_c = kc.shape[1]
    t = np.arange(T)
    s = jnp.einsum('bthd,bnd->bhtn', q, kc).astype(jnp.float32) * ATTN_SCALE
    c_valid = (np.arange(n_c)[None, :] * CMP_STRIDE + CMP_LEN - 1) <= t[:, None]
    p_cmp = jax.nn.softmax(jnp.where(c_valid, s, NEG), axis=-1) * c_valid
    o_cmp = jnp.einsum('bhtn,bnd->bthd', p_cmp.astype(vc.dtype), vc)
    n_sel = T // SLC_BLOCK
    ci = np.arange(n_c)[:, None] * CMP_STRIDE
    sj = np.arange(n_sel)[None, :] * SLC_BLOCK
    overlap = ((ci <= sj + SLC_BLOCK - 1) & (ci + CMP_LEN - 1 >= sj)).astype(np.float32)
    imp = jnp.einsum('bhtn,nj->btj', p_cmp, jnp.asarray(overlap))
    j = np.arange(n_sel)[None, :]
    cur = (t // SLC_BLOCK)[:, None]
    forced = (j == 0) | (j == cur) | (j == cur - 1)
    future = j * SLC_BLOCK > t[:, None]
    score = jnp.where(forced, FORCE, jnp.where(future, -1.0, imp))
    _, sel = lax.top_k(score, min(SLC_TOP, n_sel))
    o_slc = selected_attention(q, ks, vs, sel)
    o_win = banded_attention(q, kw[:, :, None], vw[:, :, None], NSA_WINDOW)
    return gates[..., 0:1] * o_cmp + gates[..., 1:2] * o_slc + gates[..., 2:3] * o_win


def causal_depthwise_conv(z, w):
    C = z.shape[-1]
    return lax.conv_general_dilated(
        z, w[:, None, :].astype(z.dtype), window_strides=(1,),
        padding=[(CONV_K - 1, 0)], dimension_numbers=('NWC', 'WIO', 'NWC'),
        feature_group_count=C)


def multiscale_pool(v, pool_w, pool_scale):
    B, T, _ = v.shape
    vf = v.astype(jnp.float32).reshape(B, T, len(POOL_WINDOWS), POOL_DIM)
    cs = jnp.cumsum(vf, axis=1)
    outs = []
    for g, w in enumerate(POOL_WINDOWS):
        c = cs[:, :, g]
        prev = jnp.pad(c, ((0, 0), (w, 0), (0, 0)))[:, :T]
        cnt = jnp.minimum(jnp.arange(1, T + 1), w).astype(jnp.float32)[None, :, None]
        outs.append((c - prev) / cnt - vf[:, :, g])
    d = jnp.stack(outs, axis=2)
    d = jnp.einsum('btgc,gcd->btgd', d, pool_w.astype(jnp.float32)).reshape(B, T, GROUP_WIDTH)
    return (d * pool_scale.astype(jnp.float32)).astype(v.dtype)


def token_mixing(h, w_in, nsa_q_norm, nsa_kc_norm, nsa_ks_norm, nsa_kw_norm,
                 cmp_pos_k, cmp_w1_k, cmp_w2_k, cmp_pos_v, cmp_w1_v, cmp_w2_v,
                 conv_w, swa_q_norm, swa_k_norm, swa_sinks, pool_w, pool_scale,
                 group_norm, w_out):
    B, T, _ = h.shape
    u = h @ w_in
    points = [int(p) for p in np.cumsum(SPLIT_SIZES)[:-1]]
    (a_q, a_kc, a_vc, a_ks, a_vs, a_kw, a_vw, a_g,
     b_b, b_c, b_x, c_q, c_k, c_v, d_v) = jnp.split(u, points, axis=-1)
    q = rms_norm(a_q.reshape(B, T, NSA_HEADS, HEAD_DIM), nsa_q_norm)
    kc = rms_norm(compress(a_kc, cmp_pos_k, cmp_w1_k, cmp_w2_k), nsa_kc_norm)
    vc = compress(a_vc, cmp_pos_v, cmp_w1_v, cmp_w2_v)
    ks = rms_norm(a_ks, nsa_ks_norm)
    kw = rms_norm(a_kw, nsa_kw_norm)
    gates = jax.nn.sigmoid(a_g.reshape(B, T, NSA_HEADS, NSA_BRANCHES))
    o_a = nsa(q, kc, vc, ks, a_vs, kw, a_vw, gates)
    o_b = b_b * causal_depthwise_conv(b_c * b_x, conv_w)
    cq = rms_norm(c_q.reshape(B, T, SWA_HEADS, HEAD_DIM), swa_q_norm)
    ck = rms_norm(c_k.reshape(B, T, SWA_KV_HEADS, HEAD_DIM), swa_k_norm)
    cv = c_v.reshape(B, T, SWA_KV_HEADS, HEAD_DIM)
    o_c = banded_attention(cq, ck, cv, SWA_WINDOW, swa_sinks)
    o_d = multiscale_pool(d_v, pool_w, pool_scale)
    y = jnp.concatenate([o_a.reshape(B, T, GROUP_WIDTH), o_b,
                         o_c.reshape(B, T, GROUP_WIDTH), o_d], axis=-1)
    y = rms_norm(y.reshape(B, T, N_GROUPS, GROUP_WIDTH),
                 group_norm.reshape(N_GROUPS, GROUP_WIDTH)).reshape(B, T, MIX_WIDTH)
    return y @ w_out


def setup_inputs(seed: int = 0) -> dict:
    key = jax.random.key(seed)
    keys = iter(jax.random.split(key, 40))
    L, D = DEPTH, D_MODEL

    def nrm(shape, scale):
        return scale * jax.random.normal(next(keys), shape, jnp.float32)

    def gain(shape):
        return 1.0 + nrm(shape, 0.1)

    return {
        'x': nrm((BATCH, SEQ, D), 1.0),
        'ffn1_norm': gain((L, D)),
        'ffn1_w1': nrm((L, D, D_FF), D ** -0.5),
        'ffn1_w3': nrm((L, D, D_FF), D ** -0.5),
        'ffn1_w2': nrm((L, D_FF, D), D_FF ** -0.5),
        'mix_norm': gain((L, D)),
        'w_in': nrm((L, D, IN_WIDTH), D ** -0.5),
        'nsa_q_norm': gain((L, HEAD_DIM)),
        'nsa_kc_norm': gain((L, HEAD_DIM)),
        'nsa_ks_norm': gain((L, HEAD_DIM)),
        'nsa_kw_norm': gain((L, HEAD_DIM)),
        'cmp_pos_k': nrm((L, CMP_LEN, HEAD_DIM), 0.1),
        'cmp_w1_k': nrm((L, CMP_LEN * HEAD_DIM, CMP_HIDDEN), (CMP_LEN * HEAD_DIM) ** -0.5),
        'cmp_w2_k': nrm((L, CMP_HIDDEN, HEAD_DIM), CMP_HIDDEN ** -0.5),
        'cmp_pos_v': nrm((L, CMP_LEN, HEAD_DIM), 0.1),
        'cmp_w1_v': nrm((L, CMP_LEN * HEAD_DIM, CMP_HIDDEN), (CMP_LEN * HEAD_DIM) ** -0.5),
        'cmp_w2_v': nrm((L, CMP_HIDDEN, HEAD_DIM), CMP_HIDDEN ** -0.5),
        'conv_w': nrm((L, CONV_K, GROUP_WIDTH), CONV_K ** -0.5),
        'swa_q_norm': gain((L, HEAD_DIM)),
        'swa_k_norm': gain((L, HEAD_DIM)),
        'swa_sinks': nrm((L, SWA_HEADS), 0.5),
        'pool_w': nrm((L, len(POOL_WINDOWS), POOL_DIM, POOL_DIM), POOL_DIM ** -0.5),
        'pool_scale': gain((L, GROUP_WIDTH)),
        'group_norm': gain((L, MIX_WIDTH)),
        'w_out': nrm((L, MIX_WIDTH, D), MIX_WIDTH ** -0.5),
        'ffn2_norm': gain((L, D)),
        'ffn2_w1': nrm((L, D, D_FF), D ** -0.5),
        'ffn2_w3': nrm((L, D, D_FF), D ** -0.5),
        'ffn2_w2': nrm((L, D_FF, D), D_FF ** -0.5),
    }


def reference(x, ffn1_norm, ffn1_w1, ffn1_w3, ffn1_w2, mix_norm, w_in,
              nsa_q_norm, nsa_kc_norm, nsa_ks_norm, nsa_kw_norm,
              cmp_pos_k, cmp_w1_k, cmp_w2_k, cmp_pos_v, cmp_w1_v, cmp_w2_v,
              conv_w, swa_q_norm, swa_k_norm, swa_sinks, pool_w, pool_scale,
              group_norm, w_out, ffn2_norm, ffn2_w1, ffn2_w3, ffn2_w2):
    for l in range(DEPTH):
        h = rms_norm(x, ffn1_norm[l])
        x = x + 0.5 * swiglu(h, ffn1_w1[l], ffn1_w3[l], ffn1_w2[l])
        h = rms_norm(x, mix_norm[l])
        x = x + token_mixing(h, w_in[l], nsa_q_norm[l], nsa_kc_norm[l], nsa_ks_norm[l],
                             nsa_kw_norm[l], cmp_pos_k[l], cmp_w1_k[l], cmp_w2_k[l],
                             cmp_pos_v[l], cmp_w1_v[l], cmp_w2_v[l], conv_w[l],
                             swa_q_norm[l], swa_k_norm[l], swa_sinks[l], pool_w[l],
                             pool_scale[l], group_norm[l], w_out[l])
        h = rms_norm(x, ffn2_norm[l])
        x = x + 0.5 * swiglu(h, ffn2_w1[l], ffn2_w3[l], ffn2_w2[l])
    return x
```

```python
import numpy as np
from contextlib import ExitStack
import concourse.bass as bass
import concourse.mybir as mybir
from concourse.bass_utils import run_bass_kernel_spmd

F32 = mybir.dt.float32
BF16 = mybir.dt.bfloat16
AF = mybir.ActivationFunctionType
ALU = mybir.AluOpType

NCORES = 8
L = 2
D = 1024
T = 2048
DFF = 2816
NFC = DFF // 128
NG = 2
GSZ = NFC // NG
SEQ_PER_CORE = 2
EPS = 1e-6
SELF_SYNC = True


class Prog:
    EPOCH = 2000

    def __init__(self, nc):
        self.nc = nc
        self.eng = dict(pe=nc.tensor, dve=nc.vector, act=nc.scalar,
                        pool=nc.gpsimd, sp=nc.sync)
        self.sems = {}
        self.epoch = {}
        self.cnt = {}
        self.seen = {k: {} for k in self.eng}
        self.lastw = {}
        self.reads = {}
        self.nwait = 0
        self.ninst = 0

    def _cur(self, base, step):
        ep = self.epoch.get(base, 0)
        sid = (base, ep)
        if sid in self.sems and self.cnt[sid] + step > self.EPOCH:
            ep += 1
            self.epoch[base] = ep
            sid = (base, ep)
        if sid not in self.sems:
            self.sems[sid] = self.nc.alloc_semaphore(name=f"s{len(self.sems)}")
            self.cnt[sid] = 0
        return sid

    def _deps(self, reads, writes):
        deps = []
        for r in reads:
            if r in self.lastw:
                deps.append(self.lastw[r])
        for w in writes:
            if w in self.lastw:
                deps.append(self.lastw[w])
            deps.extend(self.reads.get(w, {}).items())
        return deps

    def _wait(self, e, deps):
        need = {}
        for sid, v in deps:
            if sid[0] == e and (e in ("pe", "sp") or not SELF_SYNC):
                continue
            if self.seen[e].get(sid, 0) >= v:
                continue
            if need.get(sid, 0) < v:
                need[sid] = v
        for sid, v in need.items():
            self.eng[e].wait_ge(self.sems[sid], v)
            self.seen[e][sid] = v
            self.nwait += 1

    def _record(self, ev, reads, writes):
        for r in reads:
            d = self.reads.setdefault(r, {})
            if d.get(ev[0], 0) < ev[1]:
                d[ev[0]] = ev[1]
        for w in writes:
            self.lastw[w] = ev
            self.reads[w] = {}

    def op(self, e, fn, reads=(), writes=()):
        writes = list(writes) + [r for r in reads if isinstance(r, str) and r.startswith("ps")]
        self._wait(e, self._deps(reads, writes))
        inst = fn(self.eng[e])
        sid = self._cur(e, 1)
        self.cnt[sid] += 1
        inst.then_inc(self.sems[sid], 1)
        self.ninst += 1
        self._record((sid, self.cnt[sid]), reads, writes)

    def dma(self, q, out, in_, reads=(), writes=(), semkey=None, **kw):
        self._wait(q, self._deps(reads, writes))
        sid = self._cur(("d", semkey), 16)
        inst = self.eng[q].dma_start(out=out, in_=in_, **kw)
        self.cnt[sid] += 16
        inst.then_inc(self.sems[sid], 16)
        self.ninst += 1
        self._record((sid, self.cnt[sid]), reads, writes)

    def finish(self, e="sp"):
        deps = [(sid, v) for sid, v in self.cnt.items() if v > 0 and sid[0] != e]
        self._wait(e, deps)
        print("semaphores used", len(self.sems))


class WStream:
    def __init__(self, P, ring, nslots, plan, lookahead):
        self.P, self.ring, self.n, self.plan, self.la = P, ring, nslots, plan, lookahead
        self.pos = 0
        self.issued = 0

    def _issue(self, j):
        key, src, width = self.plan[j]
        slot = j % self.n
        self.P.dma("pool", self.ring[:, slot, 0:width], src,
                   writes=[("W", slot)], semkey=("W", slot))

    def get(self, key):
        k, src, width = self.plan[self.pos]
        assert k == key, (k, key)
        hi = min(len(self.plan), self.pos + self.la + 1)
        while self.issued < hi:
            self._issue(self.issued)
            self.issued += 1
        slot = self.pos % self.n
        self.pos += 1
        return self.ring[:, slot, 0:width], ("W", slot)


POOLWIN = (2, 4, 8, 16)
S_AQ0, S_AQ1, S_KS, S_KW, S_KVC, S_CQ0, S_CQ1, S_CK, S_YB0, S_YB1, S_YD0, S_YD1 = range(12)
NPRM = 537
NPRB = 480
NCF = 418
CB_IDB, CB_CAUS, CB_WINM, CB_BAND, CB_SELA, CB_SELB, CB_ZERO, CB_EXP, NCB = 0, 128, 1024, 1920, 2176, 2688, 3200, 3712, 5760
NEGB = -30000.0


def build_program(layers=(0, 1), nseq=SEQ_PER_CORE, do_mixer=True, do_ffn=True, stop_after=None):
    nc = bass.Bass("TRN2", target_bir_lowering=False)
    din = lambda n, s: nc.dram_tensor(n, list(s), F32, kind="ExternalInput").ap()
    xT = din("xT", [nseq, 128, 8, T])
    if do_ffn:
        w1r = din("w1r", [L, 2, NFC, 128, 1024])
        w3r = din("w3r", [L, 2, NFC, 128, 1024])
        w2r = din("w2r", [L, 2, NG, 8, 128, GSZ * 128])
    fnorm = din("fnorm", [128, L * 3 * 8])
    winr = din("winr", [L, 19, 128, 1024])
    cw1r = din("cw1r", [L, 4, 128, 1024])
    woutr = din("woutr", [L, 8, 128, 1024])
    prm = din("prm", [L, 128, NPRM])
    prb = din("prb", [L, 128, NPRB])
    cstf = din("cstf", [128, NCF])
    cstb = din("cstb", [128, NCB])
    covl = din("covl", [128, 33])
    yT = nc.dram_tensor("yT", [nseq, 128, 8, T], F32, kind="ExternalOutput").ap()

    with ExitStack() as es:
        sb = lambda n, s, d: es.enter_context(nc.sbuf_tensor(n, list(s), d))
        XT = sb("XT", [128, 8, T], F32)
        XN = sb("XN", [128, 8, T], BF16)
        Gf = sb("G", [128, 12 * T], BF16)
        NW = 6
        WR = sb("WR", [128, NW, 1024], BF16)
        SQ = sb("SQ", [128, 2, 512], F32)
        RS = sb("RS", [128, 2, 512], F32)
        SL = sb("SL", [128, 2, 512], BF16)
        GN = sb("GN", [128, L * 3 * 8], F32)
        CF = sb("CF", [128, NCF], F32)
        CB = sb("CB", [128, NCB], BF16)
        PRM = sb("PRM", [128, NPRM], F32)
        PRB = sb("PRB", [128, NPRB], BF16)
        EPSB = sb("EPSB", [128, 1], F32)
        VSW = sb("VSW", [128, 16, 2, 65], BF16)
        CV = sb("CV", [128, 16, 2, 65], BF16)
        GT = sb("GT", [128, 16, 12], F32)
        ET = sb("ET", [128, 3, 512], BF16)
        OA = sb("OA", [128, 4, 256], F32)
        YT = sb("YT", [128, 256], F32)
        SM = sb("SM", [128, 64], F32)
        SC = sb("SC", [128, 4, 32], F32)
        SELT = sb("SELT", [32, 512], BF16)
        KC2 = sb("KC2", [128, 128], BF16)
        VCA = sb("VCA", [128, 97], BF16)
        HKV = sb("HKV", [128, 2, 128], BF16)
        SINKE = sb("SINKE", [128, 4], F32)
        PS = [es.enter_context(nc.psum_tensor(f"ps{i}", [128, 512], F32)) for i in range(8)]
        ONESF = CF[:, 0:128]
        BD64 = CF[:, 128:256]
        IDF = CF[:, 256:384]
        IDB = CB[:, CB_IDB:CB_IDB + 128]

        P = Prog(nc)

        def Gs(slot, a=0, b=T, p0=0, p1=128):
            return Gf[p0:p1, slot * T + a: slot * T + b]

        def mm(out, lhsT, rhs, start, stop, reads, writes):
            P.op("pe", lambda e: e.matmul(out, lhsT=lhsT, rhs=rhs, start=start, stop=stop, skip_group_check=True),
                 reads, writes)

        def act(out, in_, func, reads, writes, **kw):
            P.op("act", lambda e: e.activation(out=out, in_=in_, func=func, **kw), reads, writes)

        def tt(out, in0, in1, op, reads, writes):
            P.op("dve", lambda e: e.tensor_tensor(out=out, in0=in0, in1=in1, op=op), reads, writes)

        def tsc(out, in0, s1, op0, reads, writes, s2=None, op1=None):
            if op1 is None:
                P.op("dve", lambda e: e.tensor_scalar(out=out, in0=in0, scalar1=s1, scalar2=None, op0=op0), reads, writes)
            else:
                P.op("dve", lambda e: e.tensor_scalar(out=out, in0=in0, scalar1=s1, scalar2=s2, op0=op0, op1=op1), reads, writes)

        def stt(out, in0, scalar, in1, op0, op1, reads, writes):
            P.op("dve", lambda e: e.scalar_tensor_tensor(out=out, in0=in0, scalar=scalar, in1=in1, op0=op0, op1=op1),
                 reads, writes)

        def recip(out, in_, reads, writes):
            P.op("dve", lambda e: e.reciprocal(out=out, in_=in_), reads, writes)

        def cpy(eng, out, in_, reads, writes):
            if eng == "act":
                P.op("act", lambda e: e.copy(out=out, in_=in_), reads, writes)
            else:
                P.op(eng, lambda e: e.tensor_copy(out=out, in_=in_), reads, writes)

        plan = []
        for s in range(nseq):
            for l in layers:
                for fi in range(2):
                    if fi == 1 and do_mixer:
                        for i in range(19):
                            plan.append((("win", s, l, i), winr[l, i], 1024))
                        for q in range(4):
                            plan.append((("cw1", s, l, q), cw1r[l, q], 1024))
                        for dc in range(8):
                            plan.append((("wout", s, l, dc), woutr[l, dc], 1024))
                    if not do_ffn:
                        continue
                    for g in range(NG):
                        for i in range(GSZ):
                            fc = g * GSZ + i
                            plan.append((("w1", s, l, fi, fc), w1r[l, fi, fc], 1024))
                            plan.append((("w3", s, l, fi, fc), w3r[l, fi, fc], 1024))
                        for dc in range(8):
                            plan.append((("w2", s, l, fi, g, dc, 0), w2r[l, fi, g, dc][:, 0:768], 768))
                            plan.append((("w2", s, l, fi, g, dc, 1), w2r[l, fi, g, dc][:, 768:GSZ * 128], GSZ * 128 - 768))
        WS = WStream(P, WR, NW, plan, NW - 3)

        P.dma("sp", GN[:], fnorm, writes=["GN"], semkey="GN")
        P.dma("sp", CF[:], cstf, writes=["CF"], semkey="CF")
        for i in range(0, NCB, 1920):
            P.dma("pool", CB[:, i:i + 1920], cstb[:, i:i + 1920], writes=[("CB", i)], semkey=("CB", i))
        CBK = [("CB", i) for i in range(0, NCB, 1920)]
        P.dma("pool", VCA[:, 64:97], covl, writes=["VCA"], semkey="VCA")
        P.op("dve", lambda e: e.memset(EPSB[:], EPS), writes=["EPSB"])
        P.op("pool", lambda e: e.memset(VSW[:, :, :, 64:65], 1.0), writes=["VSW"])
        P.op("pool", lambda e: e.memset(CV[:, :, :, 64:65], 1.0), writes=["CV"])

        cnt = {"h": 0, "o": 0, "sl": 0, "sq": 0, "pb": 0, "s": 0, "et": 0, "m": 0, "cp": 0}

        def sqbuf():
            j = cnt["sq"] % 2
            cnt["sq"] += 1
            return j

        def slbuf():
            j = cnt["sl"] % 2
            cnt["sl"] += 1
            return j

        def bank(lo=0, hi=8):
            b = lo + cnt["pb"] % (hi - lo)
            cnt["pb"] += 1
            return b

        def cpeng():
            cnt["cp"] += 1
            return "act" if cnt["cp"] % 2 else "dve"

        def rstd_from(pss_ap, pres, scale, n=512):
            act(RS[:, 0, 0:n], pss_ap, AF.Sqrt, [pres, "EPSB"], [("RS", 0)], scale=scale, bias=EPSB[:])
            recip(RS[:, 1, 0:n], RS[:, 0, 0:n], [("RS", 0)], [("RS", 1)])

        def rmsnorm(l, ni):
            for tc in range(4):
                ts = slice(tc * 512, (tc + 1) * 512)
                pb = bank()
                for kc in range(8):
                    j = sqbuf()
                    act(SQ[:, j, :], XT[:, kc, ts], AF.Square, [("XT", kc, tc)], [("SQ", j)])
                    mm(PS[pb][:], ONESF, SQ[:, j, :], kc == 0, kc == 7, [("SQ", j), "CF"], [f"ps{pb}"])
                rstd_from(PS[pb][:], f"ps{pb}", 1.0 / D)
                for kc in range(8):
                    gi = (l * 3 + ni) * 8 + kc
                    stt(XN[:, kc, ts], XT[:, kc, ts], GN[:, gi:gi + 1], RS[:, 1, :], ALU.mult, ALU.mult,
                        [("XT", kc, tc), ("RS", 1), "GN"], [("XN", kc, tc)])

        def ffn(s, l, fi):
            rmsnorm(l, 0 if fi == 0 else 2)
            for g in range(NG):
                for i in range(GSZ):
                    fc = g * GSZ + i
                    w1t, w1res = WS.get(("w1", s, l, fi, fc))
                    w3t, w3res = WS.get(("w3", s, l, fi, fc))
                    for tc in range(4):
                        ts = slice(tc * 512, (tc + 1) * 512)
                        hb = (cnt["h"] % 2) * 2
                        cnt["h"] += 1
                        p1, p3 = PS[hb], PS[hb + 1]
                        for kc in range(8):
                            mm(p1[:], w1t[:, kc * 128:(kc + 1) * 128], XN[:, kc, ts], kc == 0, kc == 7,
                               [w1res, ("XN", kc, tc)], [f"ps{hb}"])
                        for kc in range(8):
                            mm(p3[:], w3t[:, kc * 128:(kc + 1) * 128], XN[:, kc, ts], kc == 0, kc == 7,
                               [w3res, ("XN", kc, tc)], [f"ps{hb + 1}"])
                        j = slbuf()
                        act(SL[:, j, :], p1[:], AF.Silu, [f"ps{hb}"], [("SL", j)])
                        tt(Gs(i, tc * 512, (tc + 1) * 512), SL[:, j, :], p3[:], ALU.mult,
                           [("SL", j), f"ps{hb + 1}"], [("G", i, tc)])
                for dc in range(8):
                    w2a, w2ares = WS.get(("w2", s, l, fi, g, dc, 0))
                    w2b, w2bres = WS.get(("w2", s, l, fi, g, dc, 1))
                    for tc in range(4):
                        ts = slice(tc * 512, (tc + 1) * 512)
                        ob = 4 + cnt["o"] % 2
                        cnt["o"] += 1
                        po = PS[ob]
                        for i in range(GSZ):
                            w2t, w2res, ii = (w2a, w2ares, i) if i < 6 else (w2b, w2bres, i - 6)
                            mm(po[:], w2t[:, ii * 128:(ii + 1) * 128], Gs(i, tc * 512, (tc + 1) * 512), i == 0, i == GSZ - 1,
                               [w2res, ("G", i, tc)], [f"ps{ob}"])
                        stt(XT[:, dc, ts], po[:], 0.5, XT[:, dc, ts], ALU.mult, ALU.add,
                            [f"ps{ob}", ("XT", dc, tc)], [("XT", dc, tc)])

        ZB = Gf[:, 0:2 * T].bitcast(F32)

        def zkeys(tc):
            return [("G", tc // 2, 2 * (tc % 2)), ("G", tc // 2, 2 * (tc % 2) + 1)]

        def proj_fm(wt, wres, tc, pb):
            ts = slice(tc * 512, (tc + 1) * 512)
            for kc in range(8):
                mm(PS[pb][:], wt[:, kc * 128:(kc + 1) * 128], XN[:, kc, ts], kc == 0, kc == 7,
                   [wres, ("XN", kc, tc)], [f"ps{pb}"])

        def gn_fm(l, slots, gcols, dests):
            for tc in range(4):
                a, b = tc * 512, (tc + 1) * 512
                pb = bank()
                for i, sl in enumerate(slots):
                    j = sqbuf()
                    act(SQ[:, j, :], Gs(sl, a, b), AF.Square, [("G", sl, tc)], [("SQ", j)])
                    mm(PS[pb][:], ONESF, SQ[:, j, :], i == 0, i == 1, [("SQ", j), "CF"], [f"ps{pb}"])
                rstd_from(PS[pb][:], f"ps{pb}", 1.0 / 256)
                for i, sl in enumerate(slots):
                    stt(XN[:, dests[i], a:b], Gs(sl, a, b), PRM[:, gcols[i]:gcols[i] + 1], RS[:, 1, :], ALU.mult, ALU.mult,
                        [("G", sl, tc), ("RS", 1), "PRM"], [("XN", dests[i], tc)])

        def gn_tm(oav, oares, gain, dest, tt_):
            tc = tt_ // 4
            tt(YT[:], oav, oav, ALU.mult, oares, ["YT"])
            P.op("dve", lambda e: e.reduce_sum(out=SM[:, 40:41], in_=YT[:], axis=mybir.AxisListType.X), ["YT"], [("SM", 40)])
            act(SM[:, 41:42], SM[:, 40:41], AF.Sqrt, [("SM", 40), "EPSB"], [("SM", 41)], scale=1.0 / 256, bias=EPSB[:])
            recip(SM[:, 42:43], SM[:, 41:42], [("SM", 41)], [("SM", 42)])
            stt(YT[:], oav, SM[:, 42:43], gain, ALU.mult, ALU.mult, oares + [("SM", 42), "PRM"], ["YT"])
            for i in range(2):
                pb = 6 + cnt["m"] % 2
                cnt["m"] += 1
                P.op("pe", lambda e: e.transpose(out=PS[pb][:, 0:128], in_=YT[:, i * 128:(i + 1) * 128], identity=IDF),
                     ["YT", "CF"], [f"ps{pb}"])
                cpy("act", XN[:, dest + i, tt_ * 128:(tt_ + 1) * 128], PS[pb][:, 0:128], [f"ps{pb}"], [("XN", dest + i, tc)])

        def mixer(s, l):
            rmsnorm(l, 1)
            P.dma("sp", PRM[:], prm[l], writes=["PRM"], semkey="PRM")
            P.dma("pool", PRB[:], prb[l], writes=["PRB"], semkey="PRB")
            act(SINKE[:], PRM[:, 21:25], AF.Exp, ["PRM"], ["SINKE"])
            POSB = PRB[:, 0:32]
            CW2 = PRB[:, 32:224]
            PW = PRB[:, 224:480]
            wi = 0
            for cb in range(2):
                wc, rc = WS.get(("win", s, l, wi)); wx, rx = WS.get(("win", s, l, wi + 1)); wb, rb = WS.get(("win", s, l, wi + 2))
                wi += 3
                for tc in range(4):
                    a, b = tc * 512, (tc + 1) * 512
                    b1, b2, b3 = bank(), bank(), bank()
                    proj_fm(wc, rc, tc, b1); proj_fm(wx, rx, tc, b2); proj_fm(wb, rb, tc, b3)
                    j = sqbuf()
                    cpy("act", SQ[:, j, :], PS[b1][:], [f"ps{b1}"], [("SQ", j)])
                    tt(ZB[:, a:b], SQ[:, j, :], PS[b2][:], ALU.mult, [("SQ", j), f"ps{b2}"], zkeys(tc))
                    j2 = sqbuf()
                    zk = zkeys(tc) + (zkeys(tc - 1) if tc else [])
                    cw = lambda k: PRM[:, 9 + cb * 3 + k: 10 + cb * 3 + k]
                    tsc(SQ[:, j2, :], ZB[:, a:b], cw(2), ALU.mult, zk + ["PRM"], [("SQ", j2)])
                    for k, sh in ((1, 1), (0, 2)):
                        lo = max(a, sh)
                        stt(SQ[:, j2, lo - a:512], ZB[:, lo - sh:b - sh], cw(k), SQ[:, j2, lo - a:512], ALU.mult, ALU.add,
                            zk + ["PRM", ("SQ", j2)], [("SQ", j2)])
                    tt(Gs(S_YB0 + cb, a, b), SQ[:, j2, :], PS[b3][:], ALU.mult, [("SQ", j2), f"ps{b3}"], [("G", S_YB0 + cb, tc)])
            if stop_after == "B":
                return
            for cd in range(2):
                wd, rd = WS.get(("win", s, l, wi)); wi += 1
                for tc in range(4):
                    a, b = tc * 512, (tc + 1) * 512
                    b1, b2 = bank(), bank()
                    proj_fm(wd, rd, tc, b1)
                    zk = zkeys(tc) + (zkeys(tc - 1) if tc else [])
                    init = 0.0 if tc == 0 else ZB[:, a - 1:a]
                    P.op("dve", lambda e: e.tensor_tensor_scan(out=ZB[:, a:b], data0=CB[:, CB_ZERO:CB_ZERO + 512], data1=PS[b1][:],
                                                               initial=init, op0=ALU.add, op1=ALU.add),
                         [f"ps{b1}"] + CBK + zk, zkeys(tc))
                    j = sqbuf()
                    for half in range(2):
                        w = POOLWIN[2 * cd + half]
                        pr = slice(half * 64, half * 64 + 64)
                        lo = max(a, w)
                        tt(SQ[pr, j, lo - a:512], ZB[pr, lo:b], ZB[pr, lo - w:b - w], ALU.subtract, zk, [("SQ", j)])
                        if tc == 0:
                            cpy("dve", SQ[pr, j, 0:w], ZB[pr, 0:w], zk, [("SQ", j)])
                    if tc == 0:
                        tt(SQ[:, j, 0:16], SQ[:, j, 0:16], CF[:, 386 + cd * 16:386 + cd * 16 + 16], ALU.mult, [("SQ", j), "CF"], [("SQ", j)])
                    js = slbuf()
                    stt(SL[:, js, :], SQ[:, j, :], CF[:, 384 + cd:385 + cd], PS[b1][:], ALU.mult, ALU.subtract,
                        [("SQ", j), "CF", f"ps{b1}"], [("SL", js)])
                    mm(PS[b2][:], PW[:, cd * 128:(cd + 1) * 128], SL[:, js, :], True, True, ["PRB", ("SL", js)], [f"ps{b2}"])
                    tsc(Gs(S_YD0 + cd, a, b), PS[b2][:], PRM[:, 15 + cd:16 + cd], ALU.mult, [f"ps{b2}", "PRM"], [("G", S_YD0 + cd, tc)])
            if stop_after == "D":
                return
            for ct in range(8):
                wt, wres = WS.get(("win", s, l, wi)); wi += 1
                for tc in range(4):
                    a, b = tc * 512, (tc + 1) * 512
                    b1 = bank()
                    proj_fm(wt, wres, tc, b1)
                    if ct == S_KVC:
                        cpy(cpeng(), Gs(ct, a, b), PS[b1][:], [f"ps{b1}"], [("G", ct, tc)])
                        continue
                    j = sqbuf()
                    act(SQ[:, j, :], PS[b1][:], AF.Square, [f"ps{b1}"], [("SQ", j)])
                    b2 = bank()
                    mm(PS[b2][:], BD64, SQ[:, j, :], True, True, [("SQ", j), "CF"], [f"ps{b2}"])
                    rstd_from(PS[b2][:], f"ps{b2}", 1.0 / 64)
                    stt(Gs(ct, a, b), PS[b1][:], PRM[:, ct:ct + 1], RS[:, 1, :], ALU.mult, ALU.mult,
                        [f"ps{b1}", "PRM", ("RS", 1)], [("G", ct, tc)])
            if stop_after == "qk":
                return
            for ti in range(3):
                wt, wres = WS.get(("win", s, l, wi)); wi += 1
                for t16 in range(16):
                    tc = t16 // 4
                    pb = bank()
                    for kc in range(8):
                        mm(PS[pb][:, 0:128], XN[:, kc, t16 * 128:(t16 + 1) * 128], wt[:, kc * 128:(kc + 1) * 128], kc == 0, kc == 7,
                           [wres, ("XN", kc, tc)], [f"ps{pb}"])
                    if ti == 0:
                        cpy("act", VSW[:, t16, 0, 0:64], PS[pb][:, 0:64], [f"ps{pb}"], ["VSW"])
                        cpy("dve", VSW[:, t16, 1, 0:64], PS[pb][:, 64:128], [f"ps{pb}"], ["VSW"])
                    elif ti == 1:
                        cpy("act", CV[:, t16, 0, 0:64], PS[pb][:, 0:64], [f"ps{pb}"], ["CV"])
                        cpy("dve", CV[:, t16, 1, 0:64], PS[pb][:, 64:128], [f"ps{pb}"], ["CV"])
                    else:
                        act(GT[:, t16, :], PS[pb][:, 0:12], AF.Sigmoid, [f"ps{pb}"], ["GT"])
            if stop_after == "tm":
                return
            bA, bB = bank(), bank()
            kvr = [("G", S_KVC, tc) for tc in range(4)]
            for q in range(4):
                wt, wres = WS.get(("cw1", s, l, q))
                for l8 in range(8):
                    ll = 8 * q + l8
                    for pr, bk in ((slice(0, 64), bA), (slice(64, 128), bB)):
                        mm(PS[bk][:, 0:127], wt[pr, l8 * 128:(l8 + 1) * 128], Gf[pr, S_KVC * T + ll: S_KVC * T + ll + 2017: 16],
                           ll == 0, False, [wres] + kvr, [f"ps{bk}"])
                        mm(PS[bk][:, 127:128], wt[pr, l8 * 128:(l8 + 1) * 128], POSB[pr, ll:ll + 1],
                           False, ll == 31, [wres, "PRB"], [f"ps{bk}"])
            for i, bk in enumerate((bA, bB)):
                X, X2, X3 = SQ[:, 0, 0:127], SQ[:, 0, 128:255], SQ[:, 0, 256:383]
                cpy("act", SM[:, 50 + i:51 + i], PS[bk][:, 127:128], [f"ps{bk}"], [("SM", 50 + i)])
                tsc(X, PS[bk][:, 0:127], SM[:, 50 + i:51 + i], ALU.add, [f"ps{bk}", ("SM", 50 + i)], [("SQ", 0)])
                tt(X2, X, X, ALU.mult, [("SQ", 0)], [("SQ", 0)])
                tsc(X2, X2, 0.044715, ALU.mult, [("SQ", 0)], [("SQ", 0)], s2=1.0, op1=ALU.add)
                tt(X2, X2, X, ALU.mult, [("SQ", 0), ("SQ", 0)], [("SQ", 0)])
                act(X3, X2, AF.Sigmoid, [("SQ", 0)], [("SQ", 0)], scale=1.5957691216057308)
                tt(HKV[:, i, 0:127], X, X3, ALU.mult, [("SQ", 0), ("SQ", 0)], [("HKV", i)])
            b1, b2, b3 = bank(), bank(), bank()
            mm(PS[b1][:, 0:127], CW2[:, 0:128], HKV[:, 0, 0:127], True, True, ["PRB", ("HKV", 0)], [f"ps{b1}"])
            act(SQ[:, 1, 0:127], PS[b1][:, 0:127], AF.Square, [f"ps{b1}"], [("SQ", 1)])
            P.op("dve", lambda e: e.memset(SQ[:, 1, 127:128], 1.0), [("SQ", 1)], [("SQ", 1)])
            mm(PS[b2][:, 0:128], BD64, SQ[:, 1, 0:128], True, True, [("SQ", 1), "CF"], [f"ps{b2}"])
            rstd_from(PS[b2][:, 0:128], f"ps{b2}", 1.0 / 64, n=128)
            stt(KC2[:, 0:127], PS[b1][:, 0:127], PRM[:, 8:9], RS[:, 1, 0:127], ALU.mult, ALU.mult,
                [f"ps{b1}", "PRM", ("RS", 1)], ["KC2"])
            mm(PS[b3][0:127, 0:64], HKV[:, 1, 0:127], CW2[:, 128:192], True, True, ["PRB", ("HKV", 1)], [f"ps{b3}"])
            cpy("act", VCA[0:127, 0:64], PS[b3][0:127, 0:64], [f"ps{b3}"], ["VCA"])
            if stop_after == "cmp":
                return
            gn_fm(l, (S_YB0, S_YB1), (17, 18), (4, 5))
            gn_fm(l, (S_YD0, S_YD1), (19, 20), (6, 7))
            if stop_after == "gn":
                return
            GNA = PRM[:, 25:281]
            GNC = PRM[:, 281:537]

            def evac(qs, t16, ncol, br, first, last):
                ps = PS[qs]
                pr = f"ps{qs}"
                tsc(SM[:, 0:4], ps[:, 64:64 + 3 * ncol + 1:ncol], 1e-30, ALU.max, [pr], [("SM", 0)])
                recip(SM[:, 4:8], SM[:, 0:4], [("SM", 0)], [("SM", 4)])
                tt(SM[:, 8:12], SM[:, 4:8], GT[:, t16, br:12:3], ALU.mult, [("SM", 4), "GT"], [("SM", 8)])
                for h in range(4):
                    o = OA[:, qs, h * 64:(h + 1) * 64]
                    if first:
                        tsc(o, ps[:, h * ncol:h * ncol + 64], SM[:, 8 + h:9 + h], ALU.mult, [pr, ("SM", 8)], [("OA", qs)])
                    else:
                        stt(o, ps[:, h * ncol:h * ncol + 64], SM[:, 8 + h:9 + h], o, ALU.mult, ALU.add,
                            [pr, ("SM", 8), ("OA", qs)], [("OA", qs)])
                if br == 0:
                    IMP, SCR, SELM, M8 = SC[:, 0, :], SC[:, 1, :], SC[:, 2, :], SC[:, 3, 0:8]
                    tsc(IMP, ps[:, 65:97], SM[:, 4:5], ALU.mult, [pr, ("SM", 4)], ["IMP"])
                    for h in range(1, 4):
                        stt(IMP, ps[:, h * 97 + 65:(h + 1) * 97], SM[:, 4 + h:5 + h], IMP, ALU.mult, ALU.add,
                            [pr, ("SM", 4), "IMP"], ["IMP"])
                    tt(SCR, IMP, CB[:, CB_SELA + t16 * 32:CB_SELA + (t16 + 1) * 32], ALU.mult, ["IMP"] + CBK, ["SCR"])
                    tt(SCR, SCR, CB[:, CB_SELB + t16 * 32:CB_SELB + (t16 + 1) * 32], ALU.add, ["SCR"] + CBK, ["SCR"])
                    P.op("dve", lambda e: e.max(out=M8, in_=SCR), ["SCR"], ["M8"])
                    tsc(SELM, SCR, SC[:, 3, 7:8], ALU.is_ge, ["SCR", "M8"], ["SELM"], s2=-1.0, op1=ALU.add)
                    pb = 6 + cnt["m"] % 2
                    cnt["m"] += 1
                    P.op("pe", lambda e: e.transpose(out=PS[pb][0:32, 0:128], in_=SELM, identity=IDF), ["SELM", "CF"], [f"ps{pb}"])
                    cpy("act", SELT[0:32, qs * 128:(qs + 1) * 128], PS[pb][0:32, 0:128], [f"ps{pb}"], ["SELT"])
                if last:
                    gn_tm(OA[:, qs, :], [("OA", qs)], GNA, 0, t16)

            def sbank():
                b = 4 + cnt["s"] % 2
                cnt["s"] += 1
                return b

            def etbuf():
                j = cnt["et"] % 3
                cnt["et"] += 1
                return j

            for c in range(4):
                ca, cbb = c * 512, (c + 1) * 512
                for h in range(4):
                    aq, ph = S_AQ0 + h // 2, (h % 2) * 64
                    sbk = sbank()
                    mm(PS[sbk][0:127, :], KC2[ph:ph + 64, 0:127], Gs(aq, ca, cbb, ph, ph + 64), True, True,
                       ["KC2", ("G", aq, c)], [f"ps{sbk}"])
                    j = etbuf()
                    act(ET[0:127, j, :], PS[sbk][0:127, :], AF.Exp, [f"ps{sbk}"], [("ET", j)], scale=0.125)
                    P.op("pool", lambda e: e.affine_select(out=ET[0:127, j, :], in_=ET[0:127, j, :], pattern=[[1, 512]],
                                                           compare_op=ALU.is_ge, fill=0.0, base=512 * c - 31, channel_multiplier=-16),
                         [("ET", j)], [("ET", j)])
                    for qs in range(4):
                        mm(PS[qs][:, h * 97:(h + 1) * 97], ET[0:127, j, qs * 128:(qs + 1) * 128], VCA[0:127, 0:97], h == 0, True,
                           [("ET", j), "VCA"], [f"ps{qs}"])
                for qs in range(4):
                    evac(qs, 4 * c + qs, 97, 0, True, False)
                for h in range(4):
                    aq, ph = S_AQ0 + h // 2, (h % 2) * 64
                    for kt in range(4 * c + 4):
                        r = 4 * c - kt
                        sbk = sbank()
                        mm(PS[sbk][:], Gs(S_KS, kt * 128, (kt + 1) * 128, ph, ph + 64), Gs(aq, ca, cbb, ph, ph + 64), True, False,
                           [("G", S_KS, kt // 4), ("G", aq, c)], [f"ps{sbk}"])
                        mm(PS[sbk][:], CB[0:32, CB_EXP + kt * 128:CB_EXP + (kt + 1) * 128], SELT[0:32, :], False, r > 0,
                           CBK + ["SELT"], [f"ps{sbk}"])
                        if r <= 0:
                            mm(PS[sbk][:], IDB, CB[:, CB_CAUS + 384 + 128 * r:CB_CAUS + 384 + 128 * r + 512], False, True,
                               CBK, [f"ps{sbk}"])
                        j = etbuf()
                        act(ET[:, j, :], PS[sbk][:], AF.Exp, [f"ps{sbk}"], [("ET", j)], scale=0.125)
                        for qs in range(4):
                            if kt > 4 * c + qs:
                                continue
                            mm(PS[qs][:, h * 65:(h + 1) * 65], ET[:, j, qs * 128:(qs + 1) * 128], VSW[:, kt, 0, :],
                               h == 0 and kt == 0, True, [("ET", j), "VSW"], [f"ps{qs}"])
                for qs in range(4):
                    evac(qs, 4 * c + qs, 65, 1, False, False)
                for h in range(4):
                    aq, ph = S_AQ0 + h // 2, (h % 2) * 64
                    for kt in range(max(0, 4 * c - 4), 4 * c + 4):
                        r = 4 * c - kt
                        sbk = sbank()
                        mm(PS[sbk][:], Gs(S_KW, kt * 128, (kt + 1) * 128, ph, ph + 64), Gs(aq, ca, cbb, ph, ph + 64), True, False,
                           [("G", S_KW, kt // 4), ("G", aq, c)], [f"ps{sbk}"])
                        if r <= 0:
                            msk = CB[:, CB_CAUS + 384 + 128 * r:CB_CAUS + 384 + 128 * r + 512]
                        else:
                            msk = CB[:, CB_WINM + 128 * (r - 1):CB_WINM + 128 * (r - 1) + 512]
                        mm(PS[sbk][:], IDB, msk, False, True, CBK, [f"ps{sbk}"])
                        j = etbuf()
                        act(ET[:, j, :], PS[sbk][:], AF.Exp, [f"ps{sbk}"], [("ET", j)], scale=0.125)
                        for qs in range(4):
                            tq = 4 * c + qs
                            if kt > tq or kt < tq - 4:
                                continue
                            mm(PS[qs][:, h * 65:(h + 1) * 65], ET[:, j, qs * 128:(qs + 1) * 128], VSW[:, kt, 1, :],
                               h == 0 and kt == max(0, tq - 4), True, [("ET", j), "VSW"], [f"ps{qs}"])
                for qs in range(4):
                    evac(qs, 4 * c + qs, 65, 2, False, True)
            if stop_after == "nsa":
                return
            for kt in range(16):
                nq = 256 if kt < 15 else 128
                q0 = kt * 128
                qres = sorted(set([q0 // 512, (q0 + nq - 1) // 512]))
                firsthead = True
                for a in range(2):
                    for hf in range(2):
                        h = 2 * hf + a
                        ph = hf * 64
                        sbk = sbank()
                        mm(PS[sbk][:, 0:nq], Gs(S_CK, kt * 128, (kt + 1) * 128, ph, ph + 64), Gs(S_CQ0 + a, q0, q0 + nq, ph, ph + 64),
                           True, False, [("G", S_CK, kt // 4)] + [("G", S_CQ0 + a, t_) for t_ in qres], [f"ps{sbk}"])
                        mm(PS[sbk][:, 0:nq], IDB, CB[:, CB_BAND:CB_BAND + nq], False, True, CBK, [f"ps{sbk}"])
                        j = etbuf()
                        act(ET[:, j, 0:nq], PS[sbk][:, 0:nq], AF.Exp, [f"ps{sbk}"], [("ET", j)], scale=0.125)
                        b0, b1 = kt % 2, (kt + 1) % 2
                        mm(PS[b0][:, h * 65:(h + 1) * 65], ET[:, j, 0:128], CV[:, kt, hf, :], kt == 0 and firsthead, True,
                           [("ET", j), "CV"], [f"ps{b0}"])
                        if kt < 15:
                            mm(PS[b1][:, h * 65:(h + 1) * 65], ET[:, j, 128:256], CV[:, kt, hf, :], firsthead, False,
                               [("ET", j), "CV"], [f"ps{b1}"])
                        firsthead = False
                ps, pr = PS[kt % 2], f"ps{kt % 2}"
                tt(SM[:, 0:4], ps[:, 64:64 + 3 * 65 + 1:65], SINKE[:], ALU.add, [pr, "SINKE"], [("SM", 0)])
                recip(SM[:, 4:8], SM[:, 0:4], [("SM", 0)], [("SM", 4)])
                for h in range(4):
                    tsc(OA[:, 0, h * 64:(h + 1) * 64], ps[:, h * 65:h * 65 + 64], SM[:, 4 + h:5 + h], ALU.mult,
                        [pr, ("SM", 4)], [("OA", 0)])
                gn_tm(OA[:, 0, :], [("OA", 0)], GNC, 2, kt)
            if stop_after == "swa":
                return
            ysrc = (0, 1, 4, 5, 2, 3, 6, 7)
            for dc in range(8):
                wt, wres = WS.get(("wout", s, l, dc))
                for tc in range(4):
                    ts = slice(tc * 512, (tc + 1) * 512)
                    pb = bank()
                    for kc in range(8):
                        mm(PS[pb][:], wt[:, kc * 128:(kc + 1) * 128], XN[:, ysrc[kc], ts], kc == 0, kc == 7,
                           [wres, ("XN", ysrc[kc], tc)], [f"ps{pb}"])
                    tt(XT[:, dc, ts], PS[pb][:], XT[:, dc, ts], ALU.add, [f"ps{pb}", ("XT", dc, tc)], [("XT", dc, tc)])

        for s in range(nseq):
            for kc in range(8):
                P.dma("sp", XT[:, kc, :], xT[s, :, kc, :],
                      writes=[("XT", kc, tc) for tc in range(4)], semkey=("XT", kc))
            for l in layers:
                if do_ffn:
                    ffn(s, l, 0)
                if do_mixer:
                    mixer(s, l)
                if do_ffn:
                    ffn(s, l, 1)
            for kc in range(8):
                P.dma("sp", yT[s, :, kc, :], XT[:, kc, :],
                      reads=[("XT", kc, tc) for tc in range(4)], semkey=("XT", kc))
        P.finish()
        print("instructions", P.ninst, "waits", P.nwait)
    return nc


def prep_inputs(inp):
    f = lambda a: np.ascontiguousarray(np.asarray(a, dtype=np.float32))
    x = f(inp["x"])
    B = x.shape[0]
    xTh = np.ascontiguousarray(x.reshape(B, T, 8, 128).transpose(0, 3, 2, 1))

    def w13(w):
        w = f(w).reshape(L, 8, 128, NFC, 128)
        return np.ascontiguousarray(w.transpose(0, 3, 2, 1, 4)).reshape(L, NFC, 128, 1024)

    def w2(w):
        w = f(w).reshape(L, NG, GSZ, 128, 8, 128)
        return np.ascontiguousarray(w.transpose(0, 1, 4, 3, 2, 5)).reshape(L, NG, 8, 128, GSZ * 128)

    w1r = np.stack([w13(inp["ffn1_w1"]), w13(inp["ffn2_w1"])], axis=1)
    w3r = np.stack([w13(inp["ffn1_w3"]), w13(inp["ffn2_w3"])], axis=1)
    w2r = np.stack([w2(inp["ffn1_w2"]), w2(inp["ffn2_w2"])], axis=1)
    nrm = np.stack([f(inp["ffn1_norm"]), f(inp["mix_norm"]), f(inp["ffn2_norm"])], axis=1)
    fnorm = np.ascontiguousarray(nrm.reshape(L, 3, 8, 128).transpose(3, 0, 1, 2)).reshape(128, L * 3 * 8)

    o = {}
    off = 0
    for name, sz in (("a_q", 256), ("a_kc", 64), ("a_vc", 64), ("a_ks", 64), ("a_vs", 64), ("a_kw", 64), ("a_vw", 64),
                     ("a_g", 12), ("b_b", 256), ("b_c", 256), ("b_x", 256), ("c_q", 256), ("c_k", 128), ("c_v", 128), ("d_v", 256)):
        o[name] = off
        off += sz
    r = lambda name, a, n: list(range(o[name] + a, o[name] + a + n))
    tiles = [r("b_c", 0, 128), r("b_x", 0, 128), r("b_b", 0, 128), r("b_c", 128, 128), r("b_x", 128, 128), r("b_b", 128, 128),
             r("d_v", 0, 128), r("d_v", 128, 128),
             r("a_q", 0, 128), r("a_q", 128, 128), r("a_ks", 0, 64) * 2, r("a_kw", 0, 64) * 2,
             r("a_kc", 0, 64) + r("a_vc", 0, 64),
             r("c_q", 0, 64) + r("c_q", 128, 64), r("c_q", 64, 64) + r("c_q", 192, 64), r("c_k", 0, 128),
             r("a_vs", 0, 64) + r("a_vw", 0, 64), r("c_v", 0, 128), r("a_g", 0, 12) + [-1] * 116]
    w_in = f(inp["w_in"])
    w_in_p = np.concatenate([w_in, np.zeros((L, D, 1), np.float32)], axis=2)
    winr = np.stack([w_in_p[:, :, cols].reshape(L, 8, 128, 128).transpose(0, 2, 1, 3).reshape(L, 128, 1024)
                     for cols in tiles], axis=1)
    winr = np.ascontiguousarray(winr)
    ck = f(inp["cmp_w1_k"]).reshape(L, 4, 8, 64, 128)
    cvv = f(inp["cmp_w1_v"]).reshape(L, 4, 8, 64, 128)
    cw1r = np.ascontiguousarray(np.concatenate([ck, cvv], axis=3).transpose(0, 1, 3, 2, 4)).reshape(L, 4, 128, 1024)
    woutr = np.ascontiguousarray(f(inp["w_out"]).reshape(L, 8, 128, 8, 128).transpose(0, 3, 2, 1, 4)).reshape(L, 8, 128, 1024)
    prm = np.zeros((L, 128, NPRM), np.float32)
    p64 = np.arange(128) % 64
    qk_gain = {0: "nsa_q_norm", 1: "nsa_q_norm", 2: "nsa_ks_norm", 3: "nsa_kw_norm", 5: "swa_q_norm", 6: "swa_q_norm", 7: "swa_k_norm"}
    for ct, nm in qk_gain.items():
        prm[:, :, ct] = f(inp[nm])[:, p64]
    prm[:, :, 4] = 1.0
    prm[:, :, 8] = f(inp["nsa_kc_norm"])[:, p64]
    cwv = f(inp["conv_w"])
    for cb in range(2):
        for k in range(3):
            prm[:, :, 9 + cb * 3 + k] = cwv[:, k, cb * 128:(cb + 1) * 128]
    psc = f(inp["pool_scale"])
    gnv = f(inp["group_norm"])
    for c in range(2):
        prm[:, :, 15 + c] = psc[:, c * 128:(c + 1) * 128]
        prm[:, :, 17 + c] = gnv[:, 256 + c * 128:256 + (c + 1) * 128]
        prm[:, :, 19 + c] = gnv[:, 768 + c * 128:768 + (c + 1) * 128]
    prm[:, :, 21:25] = f(inp["swa_sinks"])[:, None, :]
    prm[:, :, 25:281] = gnv[:, None, 0:256]
    prm[:, :, 281:537] = gnv[:, None, 512:768]
    prb = np.zeros((L, 128, NPRB), np.float32)
    prb[:, 0:64, 0:32] = f(inp["cmp_pos_k"]).transpose(0, 2, 1)
    prb[:, 64:128, 0:32] = f(inp["cmp_pos_v"]).transpose(0, 2, 1)
    prb[:, :, 32:96] = f(inp["cmp_w2_k"])
    prb[:, :, 96:160] = f(inp["cmp_w2_k"])
    prb[:, :, 160:224] = f(inp["cmp_w2_v"])
    pw = f(inp["pool_w"])
    for c in range(2):
        prb[:, 0:64, 224 + c * 128:224 + c * 128 + 64] = pw[:, 2 * c]
        prb[:, 64:128, 224 + c * 128 + 64:224 + (c + 1) * 128] = pw[:, 2 * c + 1]
    cstf = np.zeros((128, NCF), np.float32)
    cstf[:, 0:128] = 1.0
    pp = np.arange(128)
    cstf[:, 128:256] = (pp[:, None] // 64 == pp[None, :] // 64)
    cstf[:, 256:384] = np.eye(128)
    for c in range(2):
        wp = np.where(pp < 64, POOLWIN[2 * c], POOLWIN[2 * c + 1]).astype(np.float32)
        cstf[:, 384 + c] = 1.0 / wp
        tcol = np.arange(16)[None, :]
        cstf[:, 386 + c * 16:386 + (c + 1) * 16] = wp[:, None] / np.minimum(tcol + 1, wp[:, None])
    cstb = np.zeros((128, NCB), np.float32)
    cstb[:, CB_IDB:CB_IDB + 128] = np.eye(128)
    k = pp[:, None]
    xx = np.arange(896)[None, :]
    cstb[:, CB_CAUS:CB_CAUS + 896] = np.where(xx - 384 - k >= 0, 0.0, NEGB)
    cstb[:, CB_WINM:CB_WINM + 896] = np.where(k - xx + 383 >= 0, 0.0, NEGB)
    xb = np.arange(256)[None, :]
    cstb[:, CB_BAND:CB_BAND + 256] = np.where((xb - k >= 0) & (xb - k < 128), 0.0, NEGB)
    tglob = (np.arange(16)[None, :, None] * 128 + pp[:, None, None])
    jj = np.arange(32)[None, None, :]
    cur = tglob // 64
    forced = (jj == 0) | (jj == cur) | (jj == cur - 1)
    future = jj * 64 > tglob
    cstb[:, CB_SELA:CB_SELA + 512] = np.where(forced | future, 0.0, 1.0).reshape(128, 512)
    cstb[:, CB_SELB:CB_SELB + 512] = np.where(forced, 1e4, np.where(future, -1.0, 0.0)).reshape(128, 512)
    ex = np.zeros((128, 16, 128), np.float32)
    for kt in range(16):
        ex[2 * kt, kt, 0:64] = -NEGB
        ex[2 * kt + 1, kt, 64:128] = -NEGB
    cstb[:, CB_EXP:CB_EXP + 2048] = ex.reshape(128, 2048)
    covl = np.zeros((128, 33), np.float32)
    covl[:, 0] = 1.0
    ci = np.arange(127)[:, None] * 16
    sj = np.arange(32)[None, :] * 64
    covl[0:127, 1:33] = ((ci <= sj + 63) & (ci + 31 >= sj))
    shared = dict(w1r=w1r, w3r=w3r, w2r=w2r, fnorm=fnorm, winr=winr, cw1r=cw1r, woutr=woutr, prm=prm, prb=prb,
                  cstf=cstf, cstb=cstb, covl=covl)
    return xTh, shared


def kernel(**inputs):
    xTh, shared = prep_inputs(inputs)
    nc = build_program()
    in_maps = []
    for c in range(NCORES):
        m = dict(shared)
        m["xT"] = np.ascontiguousarray(xTh[c * SEQ_PER_CORE:(c + 1) * SEQ_PER_CORE])
        in_maps.append(m)
    res = run_bass_kernel_spmd(nc, in_maps, core_ids=list(range(NCORES)))
    yT = np.concatenate([r["yT"] for r in res.results], axis=0)
    out = np.ascontiguousarray(yT.transpose(0, 3, 2, 1)).reshape(-1, T, D)
    return out.astype(np.float32)
```

```python
import numpy as np
from contextlib import ExitStack
import concourse.bass as bass
import concourse.mybir as mybir
from concourse.bass_utils import run_bass_kernel_spmd

F32 = mybir.dt.float32
BF16 = mybir.dt.bfloat16
AF = mybir.ActivationFunctionType
ALU = mybir.AluOpType

NCORES = 8
L = 2
D = 1024
T = 2048
DFF = 2816
NFC = DFF // 128
NG = 2
GSZ = NFC // NG
SEQ_PER_CORE = 2
EPS = 1e-6
SELF_SYNC = True


class Prog:
    EPOCH = 2000

    def __init__(self, nc):
        self.nc = nc
        self.eng = dict(pe=nc.tensor, dve=nc.vector, act=nc.scalar,
                        pool=nc.gpsimd, sp=nc.sync)
        self.sems = {}
        self.epoch = {}
        self.cnt = {}
        self.seen = {k: {} for k in self.eng}
        self.lastw = {}
        self.reads = {}
        self.nwait = 0
        self.ninst = 0

    def _cur(self, base, step):
        ep = self.epoch.get(base, 0)
        sid = (base, ep)
        if sid in self.sems and self.cnt[sid] + step > self.EPOCH:
            ep += 1
            self.epoch[base] = ep
            sid = (base, ep)
        if sid not in self.sems:
            self.sems[sid] = self.nc.alloc_semaphore(name=f"s{len(self.sems)}")
            self.cnt[sid] = 0
        return sid

    def _deps(self, reads, writes):
        deps = []
        for r in reads:
            if r in self.lastw:
                deps.append(self.lastw[r])
        for w in writes:
            if w in self.lastw:
                deps.append(self.lastw[w])
            deps.extend(self.reads.get(w, {}).items())
        return deps

    def _wait(self, e, deps):
        need = {}
        for sid, v in deps:
            if sid[0] == e and (e in ("pe", "sp") or not SELF_SYNC):
                continue
            if self.seen[e].get(sid, 0) >= v:
                continue
            if need.get(sid, 0) < v:
                need[sid] = v
        for sid, v in need.items():
            self.eng[e].wait_ge(self.sems[sid], v)
            self.seen[e][sid] = v
            self.nwait += 1

    def _record(self, ev, reads, writes):
        for r in reads:
            d = self.reads.setdefault(r, {})
            if d.get(ev[0], 0) < ev[1]:
                d[ev[0]] = ev[1]
        for w in writes:
            self.lastw[w] = ev
            self.reads[w] = {}

    def op(self, e, fn, reads=(), writes=()):
        writes = list(writes) + [r for r in reads if isinstance(r, str) and r.startswith("ps")]
        self._wait(e, self._deps(reads, writes))
        inst = fn(self.eng[e])
        sid = self._cur(e, 1)
        self.cnt[sid] += 1
        inst.then_inc(self.sems[sid], 1)
        self.ninst += 1
        self._record((sid, self.cnt[sid]), reads, writes)

    def dma(self, q, out, in_, reads=(), writes=(), semkey=None, **kw):
        self._wait(q, self._deps(reads, writes))
        sid = self._cur(("d", semkey), 16)
        inst = self.eng[q].dma_start(out=out, in_=in_, **kw)
        self.cnt[sid] += 16
        inst.then_inc(self.sems[sid], 16)
        self.ninst += 1
        self._record((sid, self.cnt[sid]), reads, writes)

    def finish(self, e="sp"):
        deps = [(sid, v) for sid, v in self.cnt.items() if v > 0 and sid[0] != e]
        self._wait(e, deps)
        print("semaphores used", len(self.sems))


class WStream:
    def __init__(self, P, ring, nslots, plan, lookahead):
        self.P, self.ring, self.n, self.plan, self.la = P, ring, nslots, plan, lookahead
        self.pos = 0
        self.issued = 0

    def _issue(self, j):
        key, src, width = self.plan[j]
        slot = j % self.n
        self.P.dma("pool", self.ring[:, slot, 0:width], src,
                   writes=[("W", slot)], semkey=("W", slot))

    def get(self, key):
        k, src, width = self.plan[self.pos]
        assert k == key, (k, key)
        hi = min(len(self.plan), self.pos + self.la + 1)
        while self.issued < hi:
            self._issue(self.issued)
            self.issued += 1
        slot = self.pos % self.n
        self.pos += 1
        return self.ring[:, slot, 0:width], ("W", slot)


POOLWIN = (2, 4, 8, 16)
S_AQ0, S_AQ1, S_KS, S_KW, S_KVC, S_CQ0, S_CQ1, S_CK, S_YB0, S_YB1, S_YD0, S_YD1 = range(12)
NPRM = 537
NPRB = 480
NCF = 418
CB_IDB, CB_CAUS, CB_WINM, CB_BAND, CB_SELA, CB_SELB, CB_ZERO, CB_EXP, NCB = 0, 128, 1024, 1920, 2176, 2688, 3200, 3712, 5760
NEGB = -30000.0


def build_program(layers=(0, 1), nseq=SEQ_PER_CORE, do_mixer=True, do_ffn=True, stop_after=None):
    nc = bass.Bass("TRN2", target_bir_lowering=False)
    din = lambda n, s: nc.dram_tensor(n, list(s), F32, kind="ExternalInput").ap()
    xT = din("xT", [nseq, 128, 8, T])
    if do_ffn:
        w1r = din("w1r", [L, 2, NFC, 128, 1024])
        w3r = din("w3r", [L, 2, NFC, 128, 1024])
        w2r = din("w2r", [L, 2, NG, 8, 128, GSZ * 128])
    fnorm = din("fnorm", [128, L * 3 * 8])
    winr = din("winr", [L, 19, 128, 1024])
    cw1r = din("cw1r", [L, 4, 128, 1024])
    woutr = din("woutr", [L, 8, 128, 1024])
    prm = din("prm", [L, 128, NPRM])
    prb = din("prb", [L, 128, NPRB])
    cstf = din("cstf", [128, NCF])
    cstb = din("cstb", [128, NCB])
    covl = din("covl", [128, 33])
    yT = nc.dram_tensor("yT", [nseq, 128, 8, T], F32, kind="ExternalOutput").ap()

    with ExitStack() as es:
        sb = lambda n, s, d: es.enter_context(nc.sbuf_tensor(n, list(s), d))
        XT = sb("XT", [128, 8, T], F32)
        XN = sb("XN", [128, 8, T], BF16)
        Gf = sb("G", [128, 12 * T], BF16)
        NW = 6
        WR = sb("WR", [128, NW, 1024], BF16)
        SQ = sb("SQ", [128, 2, 512], F32)
        RS = sb("RS", [128, 2, 512], F32)
        SL = sb("SL", [128, 2, 512], BF16)
        GN = sb("GN", [128, L * 3 * 8], F32)
        CF = sb("CF", [128, NCF], F32)
        CB = sb("CB", [128, NCB], BF16)
        PRM = sb("PRM", [128, NPRM], F32)
        PRB = sb("PRB", [128, NPRB], BF16)
        EPSB = sb("EPSB", [128, 1], F32)
        VSW = sb("VSW", [128, 16, 2, 65], BF16)
        CV = sb("CV", [128, 16, 2, 65], BF16)
        GT = sb("GT", [128, 16, 12], F32)
        ET = sb("ET", [128, 3, 512], BF16)
        OA = sb("OA", [128, 4, 256], F32)
        YT = sb("YT", [128, 256], F32)
        SM = sb("SM", [128, 64], F32)
        SC = sb("SC", [128, 4, 32], F32)
        SELT = sb("SELT", [128, 512], BF16)
        KCP = sb("KCP", [128, 2, 128], BF16)
        VCA = sb("VCA", [128, 97], BF16)
        HKV = sb("HKV", [128, 2, 128], BF16)
        SINKE = sb("SINKE", [128, 4], F32)
        PS = [es.enter_context(nc.psum_tensor(f"ps{i}", [128, 512], F32)) for i in range(8)]
        ONESF = CF[:, 0:128]
        BD64 = CF[:, 128:256]
        IDF = CF[:, 256:384]
        IDB = CB[:, CB_IDB:CB_IDB + 128]

        P = Prog(nc)

        def Gs(slot, a=0, b=T, p0=0, p1=128):
            return Gf[p0:p1, slot * T + a: slot * T + b]

        def mm(out, lhsT, rhs, start, stop, reads, writes):
            P.op("pe", lambda e: e.matmul(out, lhsT=lhsT, rhs=rhs, start=start, stop=stop, skip_group_check=True),
                 reads, writes)

        def act(out, in_, func, reads, writes, **kw):
            P.op("act", lambda e: e.activation(out=out, in_=in_, func=func, **kw), reads, writes)

        def tt(out, in0, in1, op, reads, writes):
            P.op("dve", lambda e: e.tensor_tensor(out=out, in0=in0, in1=in1, op=op), reads, writes)

        def tsc(out, in0, s1, op0, reads, writes, s2=None, op1=None):
            if op1 is None:
                P.op("dve", lambda e: e.tensor_scalar(out=out, in0=in0, scalar1=s1, scalar2=None, op0=op0), reads, writes)
            else:
                P.op("dve", lambda e: e.tensor_scalar(out=out, in0=in0, scalar1=s1, scalar2=s2, op0=op0, op1=op1), reads, writes)

        def stt(out, in0, scalar, in1, op0, op1, reads, writes):
            P.op("dve", lambda e: e.scalar_tensor_tensor(out=out, in0=in0, scalar=scalar, in1=in1, op0=op0, op1=op1),
                 reads, writes)

        def recip(out, in_, reads, writes):
            P.op("dve", lambda e: e.reciprocal(out=out, in_=in_), reads, writes)

        def cpy(eng, out, in_, reads, writes):
            if eng == "act":
                P.op("act", lambda e: e.copy(out=out, in_=in_), reads, writes)
            else:
                P.op(eng, lambda e: e.tensor_copy(out=out, in_=in_), reads, writes)

        plan = []
        for s in range(nseq):
            for l in layers:
                for fi in range(2):
                    if fi == 1 and do_mixer:
                        for i in range(19):
                            plan.append((("win", s, l, i), winr[l, i], 1024))
                        for q in range(4):
                            plan.append((("cw1", s, l, q), cw1r[l, q], 1024))
                        for dc in range(8):
                            plan.append((("wout", s, l, dc), woutr[l, dc], 1024))
                    if not do_ffn:
                        continue
                    for g in range(NG):
                        for i in range(GSZ):
                            fc = g * GSZ + i
                            plan.append((("w1", s, l, fi, fc), w1r[l, fi, fc], 1024))
                            plan.append((("w3", s, l, fi, fc), w3r[l, fi, fc], 1024))
                        for dc in range(8):
                            plan.append((("w2", s, l, fi, g, dc, 0), w2r[l, fi, g, dc][:, 0:768], 768))
                            plan.append((("w2", s, l, fi, g, dc, 1), w2r[l, fi, g, dc][:, 768:GSZ * 128], GSZ * 128 - 768))
        WS = WStream(P, WR, NW, plan, NW - 3)

        P.dma("sp", GN[:], fnorm, writes=["GN"], semkey="GN")
        P.dma("sp", CF[:], cstf, writes=["CF"], semkey="CF")
        for i in range(0, NCB, 1920):
            P.dma("pool", CB[:, i:i + 1920], cstb[:, i:i + 1920], writes=[("CB", i)], semkey=("CB", i))
        CBK = [("CB", i) for i in range(0, NCB, 1920)]
        P.dma("pool", VCA[:, 64:97], covl, writes=["VCA"], semkey="VCA")
        P.op("dve", lambda e: e.memset(EPSB[:], EPS), writes=["EPSB"])
        P.op("pool", lambda e: e.memset(VSW[:, :, :, 64:65], 1.0), writes=["VSW"])
        P.op("pool", lambda e: e.memset(CV[:, :, :, 64:65], 1.0), writes=["CV"])
        P.op("pool", lambda e: e.memset(SELT[:], 0.0), writes=["SELT"])
        P.op("pool", lambda e: e.memset(KCP[:], 0.0), writes=["KCP"])

        cnt = {"h": 0, "o": 0, "sl": 0, "sq": 0, "pb": 0, "s": 0, "et": 0, "m": 0, "cp": 0}

        def sqbuf():
            j = cnt["sq"] % 2
            cnt["sq"] += 1
            return j

        def slbuf():
            j = cnt["sl"] % 2
            cnt["sl"] += 1
            return j

        def bank(lo=0, hi=8):
            b = lo + cnt["pb"] % (hi - lo)
            cnt["pb"] += 1
            return b

        def cpeng():
            cnt["cp"] += 1
            return "act" if cnt["cp"] % 2 else "dve"

        def rstd_from(pss_ap, pres, scale, n=512):
            act(RS[:, 0, 0:n], pss_ap, AF.Sqrt, [pres, "EPSB"], [("RS", 0)], scale=scale, bias=EPSB[:])
            recip(RS[:, 1, 0:n], RS[:, 0, 0:n], [("RS", 0)], [("RS", 1)])

        def rmsnorm(l, ni):
            for tc in range(4):
                ts = slice(tc * 512, (tc + 1) * 512)
                pb = bank()
                for kc in range(8):
                    j = sqbuf()
                    act(SQ[:, j, :], XT[:, kc, ts], AF.Square, [("XT", kc, tc)], [("SQ", j)])
                    mm(PS[pb][:], ONESF, SQ[:, j, :], kc == 0, kc == 7, [("SQ", j), "CF"], [f"ps{pb}"])
                rstd_from(PS[pb][:], f"ps{pb}", 1.0 / D)
                for kc in range(8):
                    gi = (l * 3 + ni) * 8 + kc
                    stt(XN[:, kc, ts], XT[:, kc, ts], GN[:, gi:gi + 1], RS[:, 1, :], ALU.mult, ALU.mult,
                        [("XT", kc, tc), ("RS", 1), "GN"], [("XN", kc, tc)])

        def ffn(s, l, fi):
            rmsnorm(l, 0 if fi == 0 else 2)
            for g in range(NG):
                for i in range(GSZ):
                    fc = g * GSZ + i
                    w1t, w1res = WS.get(("w1", s, l, fi, fc))
                    w3t, w3res = WS.get(("w3", s, l, fi, fc))
                    for tc in range(4):
                        ts = slice(tc * 512, (tc + 1) * 512)
                        hb = (cnt["h"] % 2) * 2
                        cnt["h"] += 1
                        p1, p3 = PS[hb], PS[hb + 1]
                        for kc in range(8):
                            mm(p1[:], w1t[:, kc * 128:(kc + 1) * 128], XN[:, kc, ts], kc == 0, kc == 7,
                               [w1res, ("XN", kc, tc)], [f"ps{hb}"])
                        for kc in range(8):
                            mm(p3[:], w3t[:, kc * 128:(kc + 1) * 128], XN[:, kc, ts], kc == 0, kc == 7,
                               [w3res, ("XN", kc, tc)], [f"ps{hb + 1}"])
                        j = slbuf()
                        act(SL[:, j, :], p1[:], AF.Silu, [f"ps{hb}"], [("SL", j)])
                        tt(Gs(i, tc * 512, (tc + 1) * 512), SL[:, j, :], p3[:], ALU.mult,
                           [("SL", j), f"ps{hb + 1}"], [("G", i, tc)])
                for dc in range(8):
                    w2a, w2ares = WS.get(("w2", s, l, fi, g, dc, 0))
                    w2b, w2bres = WS.get(("w2", s, l, fi, g, dc, 1))
                    for tc in range(4):
                        ts = slice(tc * 512, (tc + 1) * 512)
                        ob = 4 + cnt["o"] % 2
                        cnt["o"] += 1
                        po = PS[ob]
                        for i in range(GSZ):
                            w2t, w2res, ii = (w2a, w2ares, i) if i < 6 else (w2b, w2bres, i - 6)
                            mm(po[:], w2t[:, ii * 128:(ii + 1) * 128], Gs(i, tc * 512, (tc + 1) * 512), i == 0, i == GSZ - 1,
                               [w2res, ("G", i, tc)], [f"ps{ob}"])
                        stt(XT[:, dc, ts], po[:], 0.5, XT[:, dc, ts], ALU.mult, ALU.add,
                            [f"ps{ob}", ("XT", dc, tc)], [("XT", dc, tc)])

        ZB = Gf[:, 0:2 * T].bitcast(F32)

        def zkeys(tc):
            return [("G", tc // 2, 2 * (tc % 2)), ("G", tc // 2, 2 * (tc % 2) + 1)]

        def proj_fm(wt, wres, tc, pb):
            ts = slice(tc * 512, (tc + 1) * 512)
            for kc in range(8):
                mm(PS[pb][:], wt[:, kc * 128:(kc + 1) * 128], XN[:, kc, ts], kc == 0, kc == 7,
                   [wres, ("XN", kc, tc)], [f"ps{pb}"])

        def gn_fm(l, slots, gcols, dests):
            for tc in range(4):
                a, b = tc * 512, (tc + 1) * 512
                pb = bank()
                for i, sl in enumerate(slots):
                    j = sqbuf()
                    act(SQ[:, j, :], Gs(sl, a, b), AF.Square, [("G", sl, tc)], [("SQ", j)])
                    mm(PS[pb][:], ONESF, SQ[:, j, :], i == 0, i == 1, [("SQ", j), "CF"], [f"ps{pb}"])
                rstd_from(PS[pb][:], f"ps{pb}", 1.0 / 256)
                for i, sl in enumerate(slots):
                    stt(XN[:, dests[i], a:b], Gs(sl, a, b), PRM[:, gcols[i]:gcols[i] + 1], RS[:, 1, :], ALU.mult, ALU.mult,
                        [("G", sl, tc), ("RS", 1), "PRM"], [("XN", dests[i], tc)])

        def gn_tm(oav, oares, gain, dest, tt_):
            tc = tt_ // 4
            tt(YT[:], oav, oav, ALU.mult, oares, ["YT"])
            P.op("dve", lambda e: e.reduce_sum(out=SM[:, 40:41], in_=YT[:], axis=mybir.AxisListType.X), ["YT"], [("SM", 40)])
            act(SM[:, 41:42], SM[:, 40:41], AF.Sqrt, [("SM", 40), "EPSB"], [("SM", 41)], scale=1.0 / 256, bias=EPSB[:])
            recip(SM[:, 42:43], SM[:, 41:42], [("SM", 41)], [("SM", 42)])
            stt(YT[:], oav, SM[:, 42:43], gain, ALU.mult, ALU.mult, oares + [("SM", 42), "PRM"], ["YT"])
            for i in range(2):
                pb = 6 + cnt["m"] % 2
                cnt["m"] += 1
                P.op("pe", lambda e: e.transpose(out=PS[pb][:, 0:128], in_=YT[:, i * 128:(i + 1) * 128], identity=IDF),
                     ["YT", "CF"], [f"ps{pb}"])
                cpy("act", XN[:, dest + i, tt_ * 128:(tt_ + 1) * 128], PS[pb][:, 0:128], [f"ps{pb}"], [("XN", dest + i, tc)])

        def mixer(s, l):
            rmsnorm(l, 1)
            P.dma("sp", PRM[:], prm[l], writes=["PRM"], semkey="PRM")
            P.dma("pool", PRB[:], prb[l], writes=["PRB"], semkey="PRB")
            act(SINKE[:], PRM[:, 21:25], AF.Exp, ["PRM"], ["SINKE"])
            POSB = PRB[:, 0:32]
            CW2 = PRB[:, 32:224]
            PW = PRB[:, 224:480]
            wi = 0
            for cb in range(2):
                wc, rc = WS.get(("win", s, l, wi)); wx, rx = WS.get(("win", s, l, wi + 1)); wb, rb = WS.get(("win", s, l, wi + 2))
                wi += 3
                for tc in range(4):
                    a, b = tc * 512, (tc + 1) * 512
                    b1, b2, b3 = bank(), bank(), bank()
                    proj_fm(wc, rc, tc, b1); proj_fm(wx, rx, tc, b2); proj_fm(wb, rb, tc, b3)
                    j = sqbuf()
                    cpy("act", SQ[:, j, :], PS[b1][:], [f"ps{b1}"], [("SQ", j)])
                    tt(ZB[:, a:b], SQ[:, j, :], PS[b2][:], ALU.mult, [("SQ", j), f"ps{b2}"], zkeys(tc))
                    j2 = sqbuf()
                    zk = zkeys(tc) + (zkeys(tc - 1) if tc else [])
                    cw = lambda k: PRM[:, 9 + cb * 3 + k: 10 + cb * 3 + k]
                    tsc(SQ[:, j2, :], ZB[:, a:b], cw(2), ALU.mult, zk + ["PRM"], [("SQ", j2)])
                    for k, sh in ((1, 1), (0, 2)):
                        lo = max(a, sh)
                        stt(SQ[:, j2, lo - a:512], ZB[:, lo - sh:b - sh], cw(k), SQ[:, j2, lo - a:512], ALU.mult, ALU.add,
                            zk + ["PRM", ("SQ", j2)], [("SQ", j2)])
                    tt(Gs(S_YB0 + cb, a, b), SQ[:, j2, :], PS[b3][:], ALU.mult, [("SQ", j2), f"ps{b3}"], [("G", S_YB0 + cb, tc)])
            if stop_after == "B":
                return
            for cd in range(2):
                wd, rd = WS.get(("win", s, l, wi)); wi += 1
                for tc in range(4):
                    a, b = tc * 512, (tc + 1) * 512
                    b1, b2 = bank(), bank()
                    proj_fm(wd, rd, tc, b1)
                    zk = zkeys(tc) + (zkeys(tc - 1) if tc else [])
                    init = 0.0 if tc == 0 else ZB[:, a - 1:a]
                    P.op("dve", lambda e: e.tensor_tensor_scan(out=ZB[:, a:b], data0=CB[:, CB_ZERO:CB_ZERO + 512], data1=PS[b1][:],
                                                               initial=init, op0=ALU.add, op1=ALU.add),
                         [f"ps{b1}"] + CBK + zk, zkeys(tc))
                    j = sqbuf()
                    for half in range(2):
                        w = POOLWIN[2 * cd + half]
                        pr = slice(half * 64, half * 64 + 64)
                        lo = max(a, w)
                        tt(SQ[pr, j, lo - a:512], ZB[pr, lo:b], ZB[pr, lo - w:b - w], ALU.subtract, zk, [("SQ", j)])
                        if tc == 0:
                            cpy("dve", SQ[pr, j, 0:w], ZB[pr, 0:w], zk, [("SQ", j)])
                    if tc == 0:
                        tt(SQ[:, j, 0:16], SQ[:, j, 0:16], CF[:, 386 + cd * 16:386 + cd * 16 + 16], ALU.mult, [("SQ", j), "CF"], [("SQ", j)])
                    js = slbuf()
                    stt(SL[:, js, :], SQ[:, j, :], CF[:, 384 + cd:385 + cd], PS[b1][:], ALU.mult, ALU.subtract,
                        [("SQ", j), "CF", f"ps{b1}"], [("SL", js)])
                    mm(PS[b2][:], PW[:, cd * 128:(cd + 1) * 128], SL[:, js, :], True, True, ["PRB", ("SL", js)], [f"ps{b2}"])
                    tsc(Gs(S_YD0 + cd, a, b), PS[b2][:], PRM[:, 15 + cd:16 + cd], ALU.mult, [f"ps{b2}", "PRM"], [("G", S_YD0 + cd, tc)])
            if stop_after == "D":
                return
            for ct in range(8):
                wt, wres = WS.get(("win", s, l, wi)); wi += 1
                for tc in range(4):
                    a, b = tc * 512, (tc + 1) * 512
                    b1 = bank()
                    proj_fm(wt, wres, tc, b1)
                    if ct == S_KVC:
                        cpy(cpeng(), Gs(ct, a, b), PS[b1][:], [f"ps{b1}"], [("G", ct, tc)])
                        continue
                    j = sqbuf()
                    act(SQ[:, j, :], PS[b1][:], AF.Square, [f"ps{b1}"], [("SQ", j)])
                    b2 = bank()
                    mm(PS[b2][:], BD64, SQ[:, j, :], True, True, [("SQ", j), "CF"], [f"ps{b2}"])
                    rstd_from(PS[b2][:], f"ps{b2}", 1.0 / 64)
                    stt(Gs(ct, a, b), PS[b1][:], PRM[:, ct:ct + 1], RS[:, 1, :], ALU.mult, ALU.mult,
                        [f"ps{b1}", "PRM", ("RS", 1)], [("G", ct, tc)])
            if stop_after == "qk":
                return
            for ti in range(3):
                wt, wres = WS.get(("win", s, l, wi)); wi += 1
                for t16 in range(16):
                    tc = t16 // 4
                    pb = bank()
                    for kc in range(8):
                        mm(PS[pb][:, 0:128], XN[:, kc, t16 * 128:(t16 + 1) * 128], wt[:, kc * 128:(kc + 1) * 128], kc == 0, kc == 7,
                           [wres, ("XN", kc, tc)], [f"ps{pb}"])
                    if ti == 0:
                        cpy("act", VSW[:, t16, 0, 0:64], PS[pb][:, 0:64], [f"ps{pb}"], ["VSW"])
                        cpy("dve", VSW[:, t16, 1, 0:64], PS[pb][:, 64:128], [f"ps{pb}"], ["VSW"])
                    elif ti == 1:
                        cpy("act", CV[:, t16, 0, 0:64], PS[pb][:, 0:64], [f"ps{pb}"], ["CV"])
                        cpy("dve", CV[:, t16, 1, 0:64], PS[pb][:, 64:128], [f"ps{pb}"], ["CV"])
                    else:
                        act(GT[:, t16, :], PS[pb][:, 0:12], AF.Sigmoid, [f"ps{pb}"], ["GT"])
            if stop_after == "tm":
                return
            bA, bB = bank(), bank()
            kvr = [("G", S_KVC, tc) for tc in range(4)]
            for q in range(4):
                wt, wres = WS.get(("cw1", s, l, q))
                for l8 in range(8):
                    ll = 8 * q + l8
                    for pr, bk in ((slice(0, 64), bA), (slice(64, 128), bB)):
                        mm(PS[bk][:, 0:127], wt[pr, l8 * 128:(l8 + 1) * 128], Gf[pr, S_KVC * T + ll: S_KVC * T + ll + 2017: 16],
                           ll == 0, False, [wres] + kvr, [f"ps{bk}"])
                        mm(PS[bk][:, 127:128], wt[pr, l8 * 128:(l8 + 1) * 128], POSB[pr, ll:ll + 1],
                           False, ll == 31, [wres, "PRB"], [f"ps{bk}"])
            for i, bk in enumerate((bA, bB)):
                X, X2, X3 = SQ[:, 0, 0:127], SQ[:, 0, 128:255], SQ[:, 0, 256:383]
                cpy("act", SM[:, 50 + i:51 + i], PS[bk][:, 127:128], [f"ps{bk}"], [("SM", 50 + i)])
                tsc(X, PS[bk][:, 0:127], SM[:, 50 + i:51 + i], ALU.add, [f"ps{bk}", ("SM", 50 + i)], [("SQ", 0)])
                tt(X2, X, X, ALU.mult, [("SQ", 0)], [("SQ", 0)])
                tsc(X2, X2, 0.044715, ALU.mult, [("SQ", 0)], [("SQ", 0)], s2=1.0, op1=ALU.add)
                tt(X2, X2, X, ALU.mult, [("SQ", 0), ("SQ", 0)], [("SQ", 0)])
                act(X3, X2, AF.Sigmoid, [("SQ", 0)], [("SQ", 0)], scale=1.5957691216057308)
                tt(HKV[:, i, 0:127], X, X3, ALU.mult, [("SQ", 0), ("SQ", 0)], [("HKV", i)])
            b1, b2, b3 = bank(), bank(), bank()
            mm(PS[b1][:, 0:127], CW2[:, 0:128], HKV[:, 0, 0:127], True, True, ["PRB", ("HKV", 0)], [f"ps{b1}"])
            act(SQ[:, 1, 0:127], PS[b1][:, 0:127], AF.Square, [f"ps{b1}"], [("SQ", 1)])
            P.op("dve", lambda e: e.memset(SQ[:, 1, 127:128], 1.0), [("SQ", 1)], [("SQ", 1)])
            mm(PS[b2][:, 0:128], BD64, SQ[:, 1, 0:128], True, True, [("SQ", 1), "CF"], [f"ps{b2}"])
            rstd_from(PS[b2][:, 0:128], f"ps{b2}", 1.0 / 64, n=128)
            for hf in range(2):
                pr = slice(hf * 64, hf * 64 + 64)
                stt(KCP[pr, hf, 0:127], PS[b1][pr, 0:127], PRM[pr, 8:9], RS[pr, 1, 0:127], ALU.mult, ALU.mult,
                    [f"ps{b1}", "PRM", ("RS", 1)], ["KCP"])
            mm(PS[b3][0:127, 0:64], HKV[:, 1, 0:127], CW2[:, 128:192], True, True, ["PRB", ("HKV", 1)], [f"ps{b3}"])
            cpy("act", VCA[0:127, 0:64], PS[b3][0:127, 0:64], [f"ps{b3}"], ["VCA"])
            if stop_after == "cmp":
                return
            gn_fm(l, (S_YB0, S_YB1), (17, 18), (4, 5))
            gn_fm(l, (S_YD0, S_YD1), (19, 20), (6, 7))
            if stop_after == "gn":
                return
            def zpad(dst, src, hf):
                pr, po = slice(hf * 64, hf * 64 + 64), slice((1 - hf) * 64, (1 - hf) * 64 + 64)
                allk = lambda sl: [("G", sl, t_) for t_ in range(4)]
                ZE = "pool"
                P.op(ZE, lambda e: e.memset(Gf[po, dst * T:(dst + 1) * T], 0.0), [], allk(dst))
                P.op(ZE, lambda e: e.tensor_copy(out=Gf[pr, dst * T:(dst + 1) * T], in_=Gf[pr, src * T:(src + 1) * T]),
                     allk(src), allk(dst))
            zpad(S_YB0, S_KS, 0); zpad(S_YB1, S_KS, 1)
            zpad(S_YD0, S_KW, 0); zpad(S_YD1, S_KW, 1)
            zpad(S_KS, S_CK, 0); zpad(S_KW, S_CK, 1)
            K_SLC, K_WIN, K_SWA = S_YB0, S_YD0, S_KS
            GNA = PRM[:, 25:281]
            GNC = PRM[:, 281:537]

            def evac(qs, t16, ncol, br, first, last):
                ps = PS[qs]
                pr = f"ps{qs}"
                tsc(SM[:, 0:4], ps[:, 64:64 + 3 * ncol + 1:ncol], 1e-30, ALU.max, [pr], [("SM", 0)])
                recip(SM[:, 4:8], SM[:, 0:4], [("SM", 0)], [("SM", 4)])
                tt(SM[:, 8:12], SM[:, 4:8], GT[:, t16, br:12:3], ALU.mult, [("SM", 4), "GT"], [("SM", 8)])
                for h in range(4):
                    o = OA[:, qs, h * 64:(h + 1) * 64]
                    if first:
                        tsc(o, ps[:, h * ncol:h * ncol + 64], SM[:, 8 + h:9 + h], ALU.mult, [pr, ("SM", 8)], [("OA", qs)])
                    else:
                        stt(o, ps[:, h * ncol:h * ncol + 64], SM[:, 8 + h:9 + h], o, ALU.mult, ALU.add,
                            [pr, ("SM", 8), ("OA", qs)], [("OA", qs)])
                if br == 0:
                    IMP, SCR, SELM, M8 = SC[:, 0, :], SC[:, 1, :], SC[:, 2, :], SC[:, 3, 0:8]
                    tsc(IMP, ps[:, 65:97], SM[:, 4:5], ALU.mult, [pr, ("SM", 4)], ["IMP"])
                    for h in range(1, 4):
                        stt(IMP, ps[:, h * 97 + 65:(h + 1) * 97], SM[:, 4 + h:5 + h], IMP, ALU.mult, ALU.add,
                            [pr, ("SM", 4), "IMP"], ["IMP"])
                    tt(SCR, IMP, CB[:, CB_SELA + t16 * 32:CB_SELA + (t16 + 1) * 32], ALU.mult, ["IMP"] + CBK, ["SCR"])
                    tt(SCR, SCR, CB[:, CB_SELB + t16 * 32:CB_SELB + (t16 + 1) * 32], ALU.add, ["SCR"] + CBK, ["SCR"])
                    P.op("dve", lambda e: e.max(out=M8, in_=SCR), ["SCR"], ["M8"])
                    tsc(SELM, SCR, SC[:, 3, 7:8], ALU.is_ge, ["SCR", "M8"], ["SELM"], s2=-1.0, op1=ALU.add)
                    pb = 6 + cnt["m"] % 2
                    cnt["m"] += 1
                    P.op("pe", lambda e: e.transpose(out=PS[pb][0:32, 0:128], in_=SELM, identity=IDF), ["SELM", "CF"], [f"ps{pb}"])
                    cpy("act", SELT[0:32, qs * 128:(qs + 1) * 128], PS[pb][0:32, 0:128], [f"ps{pb}"], ["SELT"])
                if last:
                    gn_tm(OA[:, qs, :], [("OA", qs)], GNA, 0, t16)

            def sbank():
                b = 4 + cnt["s"] % 2
                cnt["s"] += 1
                return b

            def etbuf():
                j = cnt["et"] % 3
                cnt["et"] += 1
                return j

            for c in range(4):
                ca, cbb = c * 512, (c + 1) * 512
                for h in range(4):
                    aq, ph = S_AQ0 + h // 2, (h % 2) * 64
                    sbk = sbank()
                    mm(PS[sbk][0:127, :], KCP[:, h % 2, 0:127], Gs(aq, ca, cbb), True, True,
                       ["KCP", ("G", aq, c)], [f"ps{sbk}"])
                    j = etbuf()
                    act(ET[0:127, j, :], PS[sbk][0:127, :], AF.Exp, [f"ps{sbk}"], [("ET", j)], scale=0.125)
                    P.op("pool", lambda e: e.affine_select(out=ET[0:127, j, :], in_=ET[0:127, j, :], pattern=[[1, 512]],
                                                           compare_op=ALU.is_ge, fill=0.0, base=512 * c - 31, channel_multiplier=-16),
                         [("ET", j)], [("ET", j)])
                    for qs in range(4):
                        mm(PS[qs][:, h * 97:(h + 1) * 97], ET[0:127, j, qs * 128:(qs + 1) * 128], VCA[0:127, 0:97], h == 0, True,
                           [("ET", j), "VCA"], [f"ps{qs}"])
                for qs in range(4):
                    evac(qs, 4 * c + qs, 97, 0, True, False)
                for h in range(4):
                    aq, ph = S_AQ0 + h // 2, (h % 2) * 64
                    for kt in range(4 * c + 4):
                        r = 4 * c - kt
                        sbk = sbank()
                        mm(PS[sbk][:], Gs(K_SLC + h % 2, kt * 128, (kt + 1) * 128), Gs(aq, ca, cbb), True, False,
                           [("G", K_SLC + h % 2, kt // 4), ("G", aq, c)], [f"ps{sbk}"])
                        mm(PS[sbk][:], CB[:, CB_EXP + kt * 128:CB_EXP + (kt + 1) * 128], SELT[:, :], False, r > 0,
                           CBK + ["SELT"], [f"ps{sbk}"])
                        if r <= 0:
                            mm(PS[sbk][:], IDB, CB[:, CB_CAUS + 384 + 128 * r:CB_CAUS + 384 + 128 * r + 512], False, True,
                               CBK, [f"ps{sbk}"])
                        j = etbuf()
                        act(ET[:, j, :], PS[sbk][:], AF.Exp, [f"ps{sbk}"], [("ET", j)], scale=0.125)
                        for qs in range(4):
                            if kt > 4 * c + qs:
                                continue
                            mm(PS[qs][:, h * 65:(h + 1) * 65], ET[:, j, qs * 128:(qs + 1) * 128], VSW[:, kt, 0, :],
                               h == 0 and kt == 0, True, [("ET", j), "VSW"], [f"ps{qs}"])
                for qs in range(4):
                    evac(qs, 4 * c + qs, 65, 1, False, False)
                for h in range(4):
                    aq, ph = S_AQ0 + h // 2, (h % 2) * 64
                    for kt in range(max(0, 4 * c - 4), 4 * c + 4):
                        r = 4 * c - kt
                        sbk = sbank()
                        mm(PS[sbk][:], Gs(K_WIN + h % 2, kt * 128, (kt + 1) * 128), Gs(aq, ca, cbb), True, False,
                           [("G", K_WIN + h % 2, kt // 4), ("G", aq, c)], [f"ps{sbk}"])
                        if r <= 0:
                            msk = CB[:, CB_CAUS + 384 + 128 * r:CB_CAUS + 384 + 128 * r + 512]
                        else:
                            msk = CB[:, CB_WINM + 128 * (r - 1):CB_WINM + 128 * (r - 1) + 512]
                        mm(PS[sbk][:], IDB, msk, False, True, CBK, [f"ps{sbk}"])
                        j = etbuf()
                        act(ET[:, j, :], PS[sbk][:], AF.Exp, [f"ps{sbk}"], [("ET", j)], scale=0.125)
                        for qs in range(4):
                            tq = 4 * c + qs
                            if kt > tq or kt < tq - 4:
                                continue
                            mm(PS[qs][:, h * 65:(h + 1) * 65], ET[:, j, qs * 128:(qs + 1) * 128], VSW[:, kt, 1, :],
                               h == 0 and kt == max(0, tq - 4), True, [("ET", j), "VSW"], [f"ps{qs}"])
                for qs in range(4):
                    evac(qs, 4 * c + qs, 65, 2, False, True)
            if stop_after == "nsa":
                return
            for kt in range(16):
                nq = 256 if kt < 15 else 128
                q0 = kt * 128
                qres = sorted(set([q0 // 512, (q0 + nq - 1) // 512]))
                firsthead = True
                for a in range(2):
                    for hf in range(2):
                        h = 2 * hf + a
                        ph = hf * 64
                        sbk = sbank()
                        mm(PS[sbk][:, 0:nq], Gs(K_SWA + hf, kt * 128, (kt + 1) * 128), Gs(S_CQ0 + a, q0, q0 + nq),
                           True, False, [("G", K_SWA + hf, kt // 4)] + [("G", S_CQ0 + a, t_) for t_ in qres], [f"ps{sbk}"])
                        mm(PS[sbk][:, 0:nq], IDB, CB[:, CB_BAND:CB_BAND + nq], False, True, CBK, [f"ps{sbk}"])
                        j = etbuf()
                        act(ET[:, j, 0:nq], PS[sbk][:, 0:nq], AF.Exp, [f"ps{sbk}"], [("ET", j)], scale=0.125)
                        b0, b1 = kt % 2, (kt + 1) % 2
                        mm(PS[b0][:, h * 65:(h + 1) * 65], ET[:, j, 0:128], CV[:, kt, hf, :], kt == 0 and firsthead, True,
                           [("ET", j), "CV"], [f"ps{b0}"])
                        if kt < 15:
                            mm(PS[b1][:, h * 65:(h + 1) * 65], ET[:, j, 128:256], CV[:, kt, hf, :], firsthead, False,
                               [("ET", j), "CV"], [f"ps{b1}"])
                        firsthead = False
                ps, pr = PS[kt % 2], f"ps{kt % 2}"
                tt(SM[:, 0:4], ps[:, 64:64 + 3 * 65 + 1:65], SINKE[:], ALU.add, [pr, "SINKE"], [("SM", 0)])
                recip(SM[:, 4:8], SM[:, 0:4], [("SM", 0)], [("SM", 4)])
                for h in range(4):
                    tsc(OA[:, 0, h * 64:(h + 1) * 64], ps[:, h * 65:h * 65 + 64], SM[:, 4 + h:5 + h], ALU.mult,
                        [pr, ("SM", 4)], [("OA", 0)])
                gn_tm(OA[:, 0, :], [("OA", 0)], GNC, 2, kt)
            if stop_after == "swa":
                return
            ysrc = (0, 1, 4, 5, 2, 3, 6, 7)
            for dc in range(8):
                wt, wres = WS.get(("wout", s, l, dc))
                for tc in range(4):
                    ts = slice(tc * 512, (tc + 1) * 512)
                    pb = bank()
                    for kc in range(8):
                        mm(PS[pb][:], wt[:, kc * 128:(kc + 1) * 128], XN[:, ysrc[kc], ts], kc == 0, kc == 7,
                           [wres, ("XN", ysrc[kc], tc)], [f"ps{pb}"])
                    tt(XT[:, dc, ts], PS[pb][:], XT[:, dc, ts], ALU.add, [f"ps{pb}", ("XT", dc, tc)], [("XT", dc, tc)])

        for s in range(nseq):
            for kc in range(8):
                P.dma("sp", XT[:, kc, :], xT[s, :, kc, :],
                      writes=[("XT", kc, tc) for tc in range(4)], semkey=("XT", kc))
            for l in layers:
                if do_ffn:
                    ffn(s, l, 0)
                if do_mixer:
                    mixer(s, l)
                if do_ffn:
                    ffn(s, l, 1)
            for kc in range(8):
                P.dma("sp", yT[s, :, kc, :], XT[:, kc, :],
                      reads=[("XT", kc, tc) for tc in range(4)], semkey=("XT", kc))
        P.finish()
        print("instructions", P.ninst, "waits", P.nwait)
    return nc


def prep_inputs(inp):
    f = lambda a: np.ascontiguousarray(np.asarray(a, dtype=np.float32))
    x = f(inp["x"])
    B = x.shape[0]
    xTh = np.ascontiguousarray(x.reshape(B, T, 8, 128).transpose(0, 3, 2, 1))

    def w13(w):
        w = f(w).reshape(L, 8, 128, NFC, 128)
        return np.ascontiguousarray(w.transpose(0, 3, 2, 1, 4)).reshape(L, NFC, 128, 1024)

    def w2(w):
        w = f(w).reshape(L, NG, GSZ, 128, 8, 128)
        return np.ascontiguousarray(w.transpose(0, 1, 4, 3, 2, 5)).reshape(L, NG, 8, 128, GSZ * 128)

    w1r = np.stack([w13(inp["ffn1_w1"]), w13(inp["ffn2_w1"])], axis=1)
    w3r = np.stack([w13(inp["ffn1_w3"]), w13(inp["ffn2_w3"])], axis=1)
    w2r = np.stack([w2(inp["ffn1_w2"]), w2(inp["ffn2_w2"])], axis=1)
    nrm = np.stack([f(inp["ffn1_norm"]), f(inp["mix_norm"]), f(inp["ffn2_norm"])], axis=1)
    fnorm = np.ascontiguousarray(nrm.reshape(L, 3, 8, 128).transpose(3, 0, 1, 2)).reshape(128, L * 3 * 8)

    o = {}
    off = 0
    for name, sz in (("a_q", 256), ("a_kc", 64), ("a_vc", 64), ("a_ks", 64), ("a_vs", 64), ("a_kw", 64), ("a_vw", 64),
                     ("a_g", 12), ("b_b", 256), ("b_c", 256), ("b_x", 256), ("c_q", 256), ("c_k", 128), ("c_v", 128), ("d_v", 256)):
        o[name] = off
        off += sz
    r = lambda name, a, n: list(range(o[name] + a, o[name] + a + n))
    tiles = [r("b_c", 0, 128), r("b_x", 0, 128), r("b_b", 0, 128), r("b_c", 128, 128), r("b_x", 128, 128), r("b_b", 128, 128),
             r("d_v", 0, 128), r("d_v", 128, 128),
             r("a_q", 0, 128), r("a_q", 128, 128), r("a_ks", 0, 64) * 2, r("a_kw", 0, 64) * 2,
             r("a_kc", 0, 64) + r("a_vc", 0, 64),
             r("c_q", 0, 64) + r("c_q", 128, 64), r("c_q", 64, 64) + r("c_q", 192, 64), r("c_k", 0, 128),
             r("a_vs", 0, 64) + r("a_vw", 0, 64), r("c_v", 0, 128), r("a_g", 0, 12) + [-1] * 116]
    w_in = f(inp["w_in"])
    w_in_p = np.concatenate([w_in, np.zeros((L, D, 1), np.float32)], axis=2)
    winr = np.stack([w_in_p[:, :, cols].reshape(L, 8, 128, 128).transpose(0, 2, 1, 3).reshape(L, 128, 1024)
                     for cols in tiles], axis=1)
    winr = np.ascontiguousarray(winr)
    ck = f(inp["cmp_w1_k"]).reshape(L, 4, 8, 64, 128)
    cvv = f(inp["cmp_w1_v"]).reshape(L, 4, 8, 64, 128)
    cw1r = np.ascontiguousarray(np.concatenate([ck, cvv], axis=3).transpose(0, 1, 3, 2, 4)).reshape(L, 4, 128, 1024)
    woutr = np.ascontiguousarray(f(inp["w_out"]).reshape(L, 8, 128, 8, 128).transpose(0, 3, 2, 1, 4)).reshape(L, 8, 128, 1024)
    prm = np.zeros((L, 128, NPRM), np.float32)
    p64 = np.arange(128) % 64
    qk_gain = {0: "nsa_q_norm", 1: "nsa_q_norm", 2: "nsa_ks_norm", 3: "nsa_kw_norm", 5: "swa_q_norm", 6: "swa_q_norm", 7: "swa_k_norm"}
    for ct, nm in qk_gain.items():
        prm[:, :, ct] = f(inp[nm])[:, p64]
    prm[:, :, 4] = 1.0
    prm[:, :, 8] = f(inp["nsa_kc_norm"])[:, p64]
    cwv = f(inp["conv_w"])
    for cb in range(2):
        for k in range(3):
            prm[:, :, 9 + cb * 3 + k] = cwv[:, k, cb * 128:(cb + 1) * 128]
    psc = f(inp["pool_scale"])
    gnv = f(inp["group_norm"])
    for c in range(2):
        prm[:, :, 15 + c] = psc[:, c * 128:(c + 1) * 128]
        prm[:, :, 17 + c] = gnv[:, 256 + c * 128:256 + (c + 1) * 128]
        prm[:, :, 19 + c] = gnv[:, 768 + c * 128:768 + (c + 1) * 128]
    prm[:, :, 21:25] = f(inp["swa_sinks"])[:, None, :]
    prm[:, :, 25:281] = gnv[:, None, 0:256]
    prm[:, :, 281:537] = gnv[:, None, 512:768]
    prb = np.zeros((L, 128, NPRB), np.float32)
    prb[:, 0:64, 0:32] = f(inp["cmp_pos_k"]).transpose(0, 2, 1)
    prb[:, 64:128, 0:32] = f(inp["cmp_pos_v"]).transpose(0, 2, 1)
    prb[:, :, 32:96] = f(inp["cmp_w2_k"])
    prb[:, :, 96:160] = f(inp["cmp_w2_k"])
    prb[:, :, 160:224] = f(inp["cmp_w2_v"])
    pw = f(inp["pool_w"])
    for c in range(2):
        prb[:, 0:64, 224 + c * 128:224 + c * 128 + 64] = pw[:, 2 * c]
        prb[:, 64:128, 224 + c * 128 + 64:224 + (c + 1) * 128] = pw[:, 2 * c + 1]
    cstf = np.zeros((128, NCF), np.float32)
    cstf[:, 0:128] = 1.0
    pp = np.arange(128)
    cstf[:, 128:256] = (pp[:, None] // 64 == pp[None, :] // 64)
    cstf[:, 256:384] = np.eye(128)
    for c in range(2):
        wp = np.where(pp < 64, POOLWIN[2 * c], POOLWIN[2 * c + 1]).astype(np.float32)
        cstf[:, 384 + c] = 1.0 / wp
        tcol = np.arange(16)[None, :]
        cstf[:, 386 + c * 16:386 + (c + 1) * 16] = wp[:, None] / np.minimum(tcol + 1, wp[:, None])
    cstb = np.zeros((128, NCB), np.float32)
    cstb[:, CB_IDB:CB_IDB + 128] = np.eye(128)
    k = pp[:, None]
    xx = np.arange(896)[None, :]
    cstb[:, CB_CAUS:CB_CAUS + 896] = np.where(xx - 384 - k >= 0, 0.0, NEGB)
    cstb[:, CB_WINM:CB_WINM + 896] = np.where(k - xx + 383 >= 0, 0.0, NEGB)
    xb = np.arange(256)[None, :]
    cstb[:, CB_BAND:CB_BAND + 256] = np.where((xb - k >= 0) & (xb - k < 128), 0.0, NEGB)
    tglob = (np.arange(16)[None, :, None] * 128 + pp[:, None, None])
    jj = np.arange(32)[None, None, :]
    cur = tglob // 64
    forced = (jj == 0) | (jj == cur) | (jj == cur - 1)
    future = jj * 64 > tglob
    cstb[:, CB_SELA:CB_SELA + 512] = np.where(forced | future, 0.0, 1.0).reshape(128, 512)
    cstb[:, CB_SELB:CB_SELB + 512] = np.where(forced, 1e4, np.where(future, -1.0, 0.0)).reshape(128, 512)
    ex = np.zeros((128, 16, 128), np.float32)
    for kt in range(16):
        ex[2 * kt, kt, 0:64] = -NEGB
        ex[2 * kt + 1, kt, 64:128] = -NEGB
    cstb[:, CB_EXP:CB_EXP + 2048] = ex.reshape(128, 2048)
    covl = np.zeros((128, 33), np.float32)
    covl[:, 0] = 1.0
    ci = np.arange(127)[:, None] * 16
    sj = np.arange(32)[None, :] * 64
    covl[0:127, 1:33] = ((ci <= sj + 63) & (ci + 31 >= sj))
    shared = dict(w1r=w1r, w3r=w3r, w2r=w2r, fnorm=fnorm, winr=winr, cw1r=cw1r, woutr=woutr, prm=prm, prb=prb,
                  cstf=cstf, cstb=cstb, covl=covl)
    return xTh, shared


def kernel(**inputs):
    xTh, shared = prep_inputs(inputs)
    nc = build_program()
    in_maps = []
    for c in range(NCORES):
        m = dict(shared)
        m["xT"] = np.ascontiguousarray(xTh[c * SEQ_PER_CORE:(c + 1) * SEQ_PER_CORE])
        in_maps.append(m)
    res = run_bass_kernel_spmd(nc, in_maps, core_ids=list(range(NCORES)))
    yT = np.concatenate([r["yT"] for r in res.results], axis=0)
    out = np.ascontiguousarray(yT.transpose(0, 3, 2, 1)).reshape(-1, T, D)
    return out.astype(np.float32)
```

```python
import numpy as np
from contextlib import ExitStack
import concourse.bass as bass
import concourse.mybir as mybir
from concourse.bass_utils import run_bass_kernel_spmd

F32 = mybir.dt.float32
BF16 = mybir.dt.bfloat16
AF = mybir.ActivationFunctionType
ALU = mybir.AluOpType

NCORES = 8
L = 2
D = 1024
T = 2048
DFF = 2816
NFC = DFF // 128
NG = 2
GSZ = NFC // NG
SEQ_PER_CORE = 2
EPS = 1e-6
SELF_SYNC = True


class Prog:
    EPOCH = 2000

    def __init__(self, nc):
        self.nc = nc
        self.eng = dict(pe=nc.tensor, dve=nc.vector, act=nc.scalar,
                        pool=nc.gpsimd, sp=nc.sync)
        self.sems = {}
        self.epoch = {}
        self.cnt = {}
        self.seen = {k: {} for k in self.eng}
        self.lastw = {}
        self.reads = {}
        self.nwait = 0
        self.ninst = 0

    def _cur(self, base, step):
        ep = self.epoch.get(base, 0)
        sid = (base, ep)
        if sid in self.sems and self.cnt[sid] + step > self.EPOCH:
            ep += 1
            self.epoch[base] = ep
            sid = (base, ep)
        if sid not in self.sems:
            self.sems[sid] = self.nc.alloc_semaphore(name=f"s{len(self.sems)}")
            self.cnt[sid] = 0
        return sid

    def _deps(self, reads, writes):
        deps = []
        for r in reads:
            if r in self.lastw:
                deps.append(self.lastw[r])
        for w in writes:
            if w in self.lastw:
                deps.append(self.lastw[w])
            deps.extend(self.reads.get(w, {}).items())
        return deps

    def _wait(self, e, deps):
        need = {}
        for sid, v in deps:
            if sid[0] == e and (e in ("pe", "sp") or not SELF_SYNC):
                continue
            if self.seen[e].get(sid, 0) >= v:
                continue
            if need.get(sid, 0) < v:
                need[sid] = v
        for sid, v in need.items():
            self.eng[e].wait_ge(self.sems[sid], v)
            self.seen[e][sid] = v
            self.nwait += 1

    def _record(self, ev, reads, writes):
        for r in reads:
            d = self.reads.setdefault(r, {})
            if d.get(ev[0], 0) < ev[1]:
                d[ev[0]] = ev[1]
        for w in writes:
            self.lastw[w] = ev
            self.reads[w] = {}

    def op(self, e, fn, reads=(), writes=()):
        writes = list(writes) + [r for r in reads if isinstance(r, str) and r.startswith("ps")]
        self._wait(e, self._deps(reads, writes))
        inst = fn(self.eng[e])
        sid = self._cur(e, 1)
        self.cnt[sid] += 1
        inst.then_inc(self.sems[sid], 1)
        self.ninst += 1
        self._record((sid, self.cnt[sid]), reads, writes)

    def dma(self, q, out, in_, reads=(), writes=(), semkey=None, **kw):
        self._wait(q, self._deps(reads, writes))
        sid = self._cur(("d", semkey), 16)
        inst = self.eng[q].dma_start(out=out, in_=in_, **kw)
        self.cnt[sid] += 16
        inst.then_inc(self.sems[sid], 16)
        self.ninst += 1
        self._record((sid, self.cnt[sid]), reads, writes)

    def finish(self, e="sp"):
        deps = [(sid, v) for sid, v in self.cnt.items() if v > 0 and sid[0] != e]
        self._wait(e, deps)
        print("semaphores used", len(self.sems))


class WStream:
    def __init__(self, P, ring, nslots, plan, lookahead):
        self.P, self.ring, self.n, self.plan, self.la = P, ring, nslots, plan, lookahead
        self.pos = 0
        self.issued = 0

    def _issue(self, j):
        key, src, width = self.plan[j]
        slot = j % self.n
        self.P.dma("pool", self.ring[:, slot, 0:width], src,
                   writes=[("W", slot)], semkey=("W", slot))

    def get(self, key):
        k, src, width = self.plan[self.pos]
        assert k == key, (k, key)
        hi = min(len(self.plan), self.pos + self.la + 1)
        while self.issued < hi:
            self._issue(self.issued)
            self.issued += 1
        slot = self.pos % self.n
        self.pos += 1
        return self.ring[:, slot, 0:width], ("W", slot)


POOLWIN = (2, 4, 8, 16)
S_AQ0, S_AQ1, S_KS, S_KW, S_KVC, S_CQ0, S_CQ1, S_CK, S_YB0, S_YB1, S_YD0, S_YD1 = range(12)
NPRM = 537
NPRB = 480
NCF = 418
CB_IDB, CB_CAUS, CB_WINM, CB_BAND, CB_SELA, CB_SELB, CB_ZERO, CB_EXP, NCB = 0, 128, 1024, 1920, 2176, 2688, 3200, 3712, 5760
NEGB = -30000.0


def build_program(layers=(0, 1), nseq=SEQ_PER_CORE, do_mixer=True, do_ffn=True, stop_after=None):
    nc = bass.Bass("TRN2", target_bir_lowering=False)
    din = lambda n, s: nc.dram_tensor(n, list(s), F32, kind="ExternalInput").ap()
    xT = din("xT", [nseq, 128, 8, T])
    if do_ffn:
        w1r = din("w1r", [L, 2, NFC, 128, 1024])
        w3r = din("w3r", [L, 2, NFC, 128, 1024])
        w2r = din("w2r", [L, 2, NG, 8, 128, GSZ * 128])
    fnorm = din("fnorm", [128, L * 3 * 8])
    winr = din("winr", [L, 19, 128, 1024])
    cw1r = din("cw1r", [L, 4, 128, 1024])
    woutr = din("woutr", [L, 8, 128, 1024])
    prm = din("prm", [L, 128, NPRM])
    prb = din("prb", [L, 128, NPRB])
    cstf = din("cstf", [128, NCF])
    cstb = din("cstb", [128, NCB])
    covl = din("covl", [128, 33])
    yT = nc.dram_tensor("yT", [nseq, 128, 8, T], F32, kind="ExternalOutput").ap()

    with ExitStack() as es:
        sb = lambda n, s, d: es.enter_context(nc.sbuf_tensor(n, list(s), d))
        XT = sb("XT", [128, 8, T], F32)
        XN = sb("XN", [128, 8, T], BF16)
        Gf = sb("G", [128, 12 * T], BF16)
        NW = 6
        WR = sb("WR", [128, NW, 1024], BF16)
        SQ = sb("SQ", [128, 2, 512], F32)
        RS = sb("RS", [128, 2, 512], F32)
        SL = sb("SL", [128, 2, 512], BF16)
        GN = sb("GN", [128, L * 3 * 8], F32)
        CF = sb("CF", [128, NCF], F32)
        CB = sb("CB", [128, NCB], BF16)
        PRM = sb("PRM", [128, NPRM], F32)
        PRB = sb("PRB", [128, NPRB], BF16)
        EPSB = sb("EPSB", [128, 1], F32)
        VSW = sb("VSW", [128, 16, 2, 65], BF16)
        CV = sb("CV", [128, 16, 2, 65], BF16)
        GT = sb("GT", [128, 16, 12], F32)
        ET = sb("ET", [128, 3, 512], BF16)
        OA = sb("OA", [128, 4, 256], F32)
        YT = sb("YT", [128, 256], F32)
        SM = sb("SM", [128, 64], F32)
        SC = sb("SC", [128, 4, 32], F32)
        SELT = sb("SELT", [128, 512], BF16)
        KCP = sb("KCP", [128, 2, 128], BF16)
        VCA = sb("VCA", [128, 97], BF16)
        HKV = sb("HKV", [128, 2, 128], BF16)
        SINKE = sb("SINKE", [128, 4], F32)
        PS = [es.enter_context(nc.psum_tensor(f"ps{i}", [128, 512], F32)) for i in range(8)]
        ONESF = CF[:, 0:128]
        BD64 = CF[:, 128:256]
        IDF = CF[:, 256:384]
        IDB = CB[:, CB_IDB:CB_IDB + 128]

        P = Prog(nc)

        def Gs(slot, a=0, b=T, p0=0, p1=128):
            return Gf[p0:p1, slot * T + a: slot * T + b]

        def mm(out, lhsT, rhs, start, stop, reads, writes):
            P.op("pe", lambda e: e.matmul(out, lhsT=lhsT, rhs=rhs, start=start, stop=stop, skip_group_check=True),
                 reads, writes)

        def act(out, in_, func, reads, writes, **kw):
            P.op("act", lambda e: e.activation(out=out, in_=in_, func=func, **kw), reads, writes)

        def tt(out, in0, in1, op, reads, writes):
            P.op("dve", lambda e: e.tensor_tensor(out=out, in0=in0, in1=in1, op=op), reads, writes)

        def tsc(out, in0, s1, op0, reads, writes, s2=None, op1=None):
            if op1 is None:
                P.op("dve", lambda e: e.tensor_scalar(out=out, in0=in0, scalar1=s1, scalar2=None, op0=op0), reads, writes)
            else:
                P.op("dve", lambda e: e.tensor_scalar(out=out, in0=in0, scalar1=s1, scalar2=s2, op0=op0, op1=op1), reads, writes)

        def stt(out, in0, scalar, in1, op0, op1, reads, writes):
            P.op("dve", lambda e: e.scalar_tensor_tensor(out=out, in0=in0, scalar=scalar, in1=in1, op0=op0, op1=op1),
                 reads, writes)

        def recip(out, in_, reads, writes):
            P.op("dve", lambda e: e.reciprocal(out=out, in_=in_), reads, writes)

        def cpy(eng, out, in_, reads, writes):
            if eng == "act":
                P.op("act", lambda e: e.copy(out=out, in_=in_), reads, writes)
            else:
                P.op(eng, lambda e: e.tensor_copy(out=out, in_=in_), reads, writes)

        plan = []
        for s in range(nseq):
            for l in layers:
                for fi in range(2):
                    if fi == 1 and do_mixer:
                        for i in range(19):
                            plan.append((("win", s, l, i), winr[l, i], 1024))
                        for q in range(4):
                            plan.append((("cw1", s, l, q), cw1r[l, q], 1024))
                        for dc in range(8):
                            plan.append((("wout", s, l, dc), woutr[l, dc], 1024))
                    if not do_ffn:
                        continue
                    for g in range(NG):
                        for i in range(GSZ):
                            fc = g * GSZ + i
                            plan.append((("w1", s, l, fi, fc), w1r[l, fi, fc], 1024))
                            plan.append((("w3", s, l, fi, fc), w3r[l, fi, fc], 1024))
                        for dc in range(8):
                            plan.append((("w2", s, l, fi, g, dc, 0), w2r[l, fi, g, dc][:, 0:768], 768))
                            plan.append((("w2", s, l, fi, g, dc, 1), w2r[l, fi, g, dc][:, 768:GSZ * 128], GSZ * 128 - 768))
        WS = WStream(P, WR, NW, plan, NW - 3)

        P.dma("sp", GN[:], fnorm, writes=["GN"], semkey="GN")
        P.dma("sp", CF[:], cstf, writes=["CF"], semkey="CF")
        for i in range(0, NCB, 1920):
            P.dma("pool", CB[:, i:i + 1920], cstb[:, i:i + 1920], writes=[("CB", i)], semkey=("CB", i))
        CBK = [("CB", i) for i in range(0, NCB, 1920)]
        P.dma("pool", VCA[:, 64:97], covl, writes=["VCA"], semkey="VCA")
        P.op("dve", lambda e: e.memset(EPSB[:], EPS), writes=["EPSB"])
        P.op("pool", lambda e: e.memset(VSW[:, :, :, 64:65], 1.0), writes=["VSW"])
        P.op("pool", lambda e: e.memset(CV[:, :, :, 64:65], 1.0), writes=["CV"])
        P.op("pool", lambda e: e.memset(SELT[:], 0.0), writes=["SELT"])
        P.op("pool", lambda e: e.memset(KCP[:], 0.0), writes=["KCP"])

        cnt = {"h": 0, "o": 0, "sl": 0, "sq": 0, "pb": 0, "s": 0, "et": 0, "m": 0, "cp": 0, "stg": 0}

        def sqbuf():
            j = cnt["sq"] % 2
            cnt["sq"] += 1
            return j

        def slbuf():
            j = cnt["sl"] % 2
            cnt["sl"] += 1
            return j

        def bank(lo=0, hi=8):
            b = lo + cnt["pb"] % (hi - lo)
            cnt["pb"] += 1
            return b

        def cpeng():
            cnt["cp"] += 1
            return "act" if cnt["cp"] % 2 else "dve"

        def rstd_from(pss_ap, pres, scale, n=512):
            act(RS[:, 0, 0:n], pss_ap, AF.Sqrt, [pres, "EPSB"], [("RS", 0)], scale=scale, bias=EPSB[:])
            recip(RS[:, 1, 0:n], RS[:, 0, 0:n], [("RS", 0)], [("RS", 1)])

        def rmsnorm(l, ni):
            for tc in range(4):
                ts = slice(tc * 512, (tc + 1) * 512)
                pb = bank()
                for kc in range(8):
                    j = sqbuf()
                    act(SQ[:, j, :], XT[:, kc, ts], AF.Square, [("XT", kc, tc)], [("SQ", j)])
                    mm(PS[pb][:], ONESF, SQ[:, j, :], kc == 0, kc == 7, [("SQ", j), "CF"], [f"ps{pb}"])
                rstd_from(PS[pb][:], f"ps{pb}", 1.0 / D)
                for kc in range(8):
                    gi = (l * 3 + ni) * 8 + kc
                    stt(XN[:, kc, ts], XT[:, kc, ts], GN[:, gi:gi + 1], RS[:, 1, :], ALU.mult, ALU.mult,
                        [("XT", kc, tc), ("RS", 1), "GN"], [("XN", kc, tc)])

        def ffn(s, l, fi):
            rmsnorm(l, 0 if fi == 0 else 2)
            for g in range(NG):
                for i in range(GSZ):
                    fc = g * GSZ + i
                    w1t, w1res = WS.get(("w1", s, l, fi, fc))
                    w3t, w3res = WS.get(("w3", s, l, fi, fc))
                    for tc in range(4):
                        ts = slice(tc * 512, (tc + 1) * 512)
                        hb = (cnt["h"] % 2) * 2
                        cnt["h"] += 1
                        p1, p3 = PS[hb], PS[hb + 1]
                        for kc in range(8):
                            mm(p1[:], w1t[:, kc * 128:(kc + 1) * 128], XN[:, kc, ts], kc == 0, kc == 7,
                               [w1res, ("XN", kc, tc)], [f"ps{hb}"])
                        for kc in range(8):
                            mm(p3[:], w3t[:, kc * 128:(kc + 1) * 128], XN[:, kc, ts], kc == 0, kc == 7,
                               [w3res, ("XN", kc, tc)], [f"ps{hb + 1}"])
                        j = slbuf()
                        act(SL[:, j, :], p1[:], AF.Silu, [f"ps{hb}"], [("SL", j)])
                        tt(Gs(i, tc * 512, (tc + 1) * 512), SL[:, j, :], p3[:], ALU.mult,
                           [("SL", j), f"ps{hb + 1}"], [("G", i, tc)])
                for dc in range(8):
                    w2a, w2ares = WS.get(("w2", s, l, fi, g, dc, 0))
                    w2b, w2bres = WS.get(("w2", s, l, fi, g, dc, 1))
                    for tc in range(4):
                        ts = slice(tc * 512, (tc + 1) * 512)
                        ob = 4 + cnt["o"] % 2
                        cnt["o"] += 1
                        po = PS[ob]
                        for i in range(GSZ):
                            w2t, w2res, ii = (w2a, w2ares, i) if i < 6 else (w2b, w2bres, i - 6)
                            mm(po[:], w2t[:, ii * 128:(ii + 1) * 128], Gs(i, tc * 512, (tc + 1) * 512), i == 0, i == GSZ - 1,
                               [w2res, ("G", i, tc)], [f"ps{ob}"])
                        stt(XT[:, dc, ts], po[:], 0.5, XT[:, dc, ts], ALU.mult, ALU.add,
                            [f"ps{ob}", ("XT", dc, tc)], [("XT", dc, tc)])

        ZB = Gf[:, 0:2 * T].bitcast(F32)

        def zkeys(tc):
            return [("G", tc // 2, 2 * (tc % 2)), ("G", tc // 2, 2 * (tc % 2) + 1)]

        def proj_fm(wt, wres, tc, pb):
            ts = slice(tc * 512, (tc + 1) * 512)
            for kc in range(8):
                mm(PS[pb][:], wt[:, kc * 128:(kc + 1) * 128], XN[:, kc, ts], kc == 0, kc == 7,
                   [wres, ("XN", kc, tc)], [f"ps{pb}"])

        def gn_fm(l, slots, gcols, dests):
            for tc in range(4):
                a, b = tc * 512, (tc + 1) * 512
                pb = bank()
                for i, sl in enumerate(slots):
                    j = sqbuf()
                    act(SQ[:, j, :], Gs(sl, a, b), AF.Square, [("G", sl, tc)], [("SQ", j)])
                    mm(PS[pb][:], ONESF, SQ[:, j, :], i == 0, i == 1, [("SQ", j), "CF"], [f"ps{pb}"])
                rstd_from(PS[pb][:], f"ps{pb}", 1.0 / 256)
                for i, sl in enumerate(slots):
                    stt(XN[:, dests[i], a:b], Gs(sl, a, b), PRM[:, gcols[i]:gcols[i] + 1], RS[:, 1, :], ALU.mult, ALU.mult,
                        [("G", sl, tc), ("RS", 1), "PRM"], [("XN", dests[i], tc)])

        def gn_tm(oav, oares, gain, dest, tt_):
            tc = tt_ // 4
            tt(YT[:], oav, oav, ALU.mult, oares, ["YT"])
            P.op("dve", lambda e: e.reduce_sum(out=SM[:, 40:41], in_=YT[:], axis=mybir.AxisListType.X), ["YT"], [("SM", 40)])
            act(SM[:, 41:42], SM[:, 40:41], AF.Sqrt, [("SM", 40), "EPSB"], [("SM", 41)], scale=1.0 / 256, bias=EPSB[:])
            recip(SM[:, 42:43], SM[:, 41:42], [("SM", 41)], [("SM", 42)])
            stt(YT[:], oav, SM[:, 42:43], gain, ALU.mult, ALU.mult, oares + [("SM", 42), "PRM"], ["YT"])
            for i in range(2):
                pb = 6 + cnt["m"] % 2
                cnt["m"] += 1
                P.op("pe", lambda e: e.transpose(out=PS[pb][:, 0:128], in_=YT[:, i * 128:(i + 1) * 128], identity=IDF),
                     ["YT", "CF"], [f"ps{pb}"])
                cpy("act", XN[:, dest + i, tt_ * 128:(tt_ + 1) * 128], PS[pb][:, 0:128], [f"ps{pb}"], [("XN", dest + i, tc)])

        def mixer(s, l):
            rmsnorm(l, 1)
            P.dma("sp", PRM[:], prm[l], writes=["PRM"], semkey="PRM")
            P.dma("pool", PRB[:], prb[l], writes=["PRB"], semkey="PRB")
            act(SINKE[:], PRM[:, 21:25], AF.Exp, ["PRM"], ["SINKE"])
            POSB = PRB[:, 0:32]
            CW2 = PRB[:, 32:224]
            PW = PRB[:, 224:480]
            wi = 0
            for cb in range(2):
                wc, rc = WS.get(("win", s, l, wi)); wx, rx = WS.get(("win", s, l, wi + 1)); wb, rb = WS.get(("win", s, l, wi + 2))
                wi += 3
                for tc in range(4):
                    a, b = tc * 512, (tc + 1) * 512
                    b1, b2, b3 = bank(), bank(), bank()
                    proj_fm(wc, rc, tc, b1); proj_fm(wx, rx, tc, b2); proj_fm(wb, rb, tc, b3)
                    j = sqbuf()
                    cpy("act", SQ[:, j, :], PS[b1][:], [f"ps{b1}"], [("SQ", j)])
                    tt(ZB[:, a:b], SQ[:, j, :], PS[b2][:], ALU.mult, [("SQ", j), f"ps{b2}"], zkeys(tc))
                    j2 = sqbuf()
                    zk = zkeys(tc) + (zkeys(tc - 1) if tc else [])
                    cw = lambda k: PRM[:, 9 + cb * 3 + k: 10 + cb * 3 + k]
                    tsc(SQ[:, j2, :], ZB[:, a:b], cw(2), ALU.mult, zk + ["PRM"], [("SQ", j2)])
                    for k, sh in ((1, 1), (0, 2)):
                        lo = max(a, sh)
                        stt(SQ[:, j2, lo - a:512], ZB[:, lo - sh:b - sh], cw(k), SQ[:, j2, lo - a:512], ALU.mult, ALU.add,
                            zk + ["PRM", ("SQ", j2)], [("SQ", j2)])
                    tt(Gs(S_YB0 + cb, a, b), SQ[:, j2, :], PS[b3][:], ALU.mult, [("SQ", j2), f"ps{b3}"], [("G", S_YB0 + cb, tc)])
            if stop_after == "B":
                return
            for cd in range(2):
                wd, rd = WS.get(("win", s, l, wi)); wi += 1
                for tc in range(4):
                    a, b = tc * 512, (tc + 1) * 512
                    b1, b2 = bank(), bank()
                    proj_fm(wd, rd, tc, b1)
                    zk = zkeys(tc) + (zkeys(tc - 1) if tc else [])
                    init = 0.0 if tc == 0 else ZB[:, a - 1:a]
                    P.op("dve", lambda e: e.tensor_tensor_scan(out=ZB[:, a:b], data0=CB[:, CB_ZERO:CB_ZERO + 512], data1=PS[b1][:],
                                                               initial=init, op0=ALU.add, op1=ALU.add),
                         [f"ps{b1}"] + CBK + zk, zkeys(tc))
                    j = sqbuf()
                    for half in range(2):
                        w = POOLWIN[2 * cd + half]
                        pr = slice(half * 64, half * 64 + 64)
                        lo = max(a, w)
                        tt(SQ[pr, j, lo - a:512], ZB[pr, lo:b], ZB[pr, lo - w:b - w], ALU.subtract, zk, [("SQ", j)])
                        if tc == 0:
                            cpy("dve", SQ[pr, j, 0:w], ZB[pr, 0:w], zk, [("SQ", j)])
                    if tc == 0:
                        tt(SQ[:, j, 0:16], SQ[:, j, 0:16], CF[:, 386 + cd * 16:386 + cd * 16 + 16], ALU.mult, [("SQ", j), "CF"], [("SQ", j)])
                    js = slbuf()
                    stt(SL[:, js, :], SQ[:, j, :], CF[:, 384 + cd:385 + cd], PS[b1][:], ALU.mult, ALU.subtract,
                        [("SQ", j), "CF", f"ps{b1}"], [("SL", js)])
                    mm(PS[b2][:], PW[:, cd * 128:(cd + 1) * 128], SL[:, js, :], True, True, ["PRB", ("SL", js)], [f"ps{b2}"])
                    tsc(Gs(S_YD0 + cd, a, b), PS[b2][:], PRM[:, 15 + cd:16 + cd], ALU.mult, [f"ps{b2}", "PRM"], [("G", S_YD0 + cd, tc)])
            if stop_after == "D":
                return
            for ct in range(8):
                wt, wres = WS.get(("win", s, l, wi)); wi += 1
                for tc in range(4):
                    a, b = tc * 512, (tc + 1) * 512
                    b1 = bank()
                    proj_fm(wt, wres, tc, b1)
                    if ct == S_KVC:
                        cpy(cpeng(), Gs(ct, a, b), PS[b1][:], [f"ps{b1}"], [("G", ct, tc)])
                        continue
                    j = sqbuf()
                    act(SQ[:, j, :], PS[b1][:], AF.Square, [f"ps{b1}"], [("SQ", j)])
                    b2 = bank()
                    mm(PS[b2][:], BD64, SQ[:, j, :], True, True, [("SQ", j), "CF"], [f"ps{b2}"])
                    rstd_from(PS[b2][:], f"ps{b2}", 1.0 / 64)
                    stt(Gs(ct, a, b), PS[b1][:], PRM[:, ct:ct + 1], RS[:, 1, :], ALU.mult, ALU.mult,
                        [f"ps{b1}", "PRM", ("RS", 1)], [("G", ct, tc)])
            if stop_after == "qk":
                return
            for ti in range(3):
                wt, wres = WS.get(("win", s, l, wi)); wi += 1
                for t16 in range(16):
                    tc = t16 // 4
                    pb = bank()
                    for kc in range(8):
                        mm(PS[pb][:, 0:128], XN[:, kc, t16 * 128:(t16 + 1) * 128], wt[:, kc * 128:(kc + 1) * 128], kc == 0, kc == 7,
                           [wres, ("XN", kc, tc)], [f"ps{pb}"])
                    if ti == 0:
                        cpy("act", VSW[:, t16, 0, 0:64], PS[pb][:, 0:64], [f"ps{pb}"], ["VSW"])
                        cpy("dve", VSW[:, t16, 1, 0:64], PS[pb][:, 64:128], [f"ps{pb}"], ["VSW"])
                    elif ti == 1:
                        cpy("act", CV[:, t16, 0, 0:64], PS[pb][:, 0:64], [f"ps{pb}"], ["CV"])
                        cpy("dve", CV[:, t16, 1, 0:64], PS[pb][:, 64:128], [f"ps{pb}"], ["CV"])
                    else:
                        act(GT[:, t16, :], PS[pb][:, 0:12], AF.Sigmoid, [f"ps{pb}"], ["GT"])
            if stop_after == "tm":
                return
            bA, bB = bank(), bank()
            kvr = [("G", S_KVC, tc) for tc in range(4)]
            for q in range(4):
                wt, wres = WS.get(("cw1", s, l, q))
                for l8 in range(8):
                    ll = 8 * q + l8
                    for pr, bk in ((slice(0, 64), bA), (slice(64, 128), bB)):
                        mm(PS[bk][:, 0:127], wt[pr, l8 * 128:(l8 + 1) * 128], Gf[pr, S_KVC * T + ll: S_KVC * T + ll + 2017: 16],
                           ll == 0, False, [wres] + kvr, [f"ps{bk}"])
                        mm(PS[bk][:, 127:128], wt[pr, l8 * 128:(l8 + 1) * 128], POSB[pr, ll:ll + 1],
                           False, ll == 31, [wres, "PRB"], [f"ps{bk}"])
            for i, bk in enumerate((bA, bB)):
                X, X2, X3 = SQ[:, 0, 0:127], SQ[:, 0, 128:255], SQ[:, 0, 256:383]
                cpy("act", SM[:, 50 + i:51 + i], PS[bk][:, 127:128], [f"ps{bk}"], [("SM", 50 + i)])
                tsc(X, PS[bk][:, 0:127], SM[:, 50 + i:51 + i], ALU.add, [f"ps{bk}", ("SM", 50 + i)], [("SQ", 0)])
                tt(X2, X, X, ALU.mult, [("SQ", 0)], [("SQ", 0)])
                tsc(X2, X2, 0.044715, ALU.mult, [("SQ", 0)], [("SQ", 0)], s2=1.0, op1=ALU.add)
                tt(X2, X2, X, ALU.mult, [("SQ", 0), ("SQ", 0)], [("SQ", 0)])
                act(X3, X2, AF.Sigmoid, [("SQ", 0)], [("SQ", 0)], scale=1.5957691216057308)
                tt(HKV[:, i, 0:127], X, X3, ALU.mult, [("SQ", 0), ("SQ", 0)], [("HKV", i)])
            b1, b2, b3 = bank(), bank(), bank()
            mm(PS[b1][:, 0:127], CW2[:, 0:128], HKV[:, 0, 0:127], True, True, ["PRB", ("HKV", 0)], [f"ps{b1}"])
            act(SQ[:, 1, 0:127], PS[b1][:, 0:127], AF.Square, [f"ps{b1}"], [("SQ", 1)])
            P.op("dve", lambda e: e.memset(SQ[:, 1, 127:128], 1.0), [("SQ", 1)], [("SQ", 1)])
            mm(PS[b2][:, 0:128], BD64, SQ[:, 1, 0:128], True, True, [("SQ", 1), "CF"], [f"ps{b2}"])
            rstd_from(PS[b2][:, 0:128], f"ps{b2}", 1.0 / 64, n=128)
            for hf in range(2):
                pr = slice(hf * 64, hf * 64 + 64)
                stt(KCP[pr, hf, 0:127], PS[b1][pr, 0:127], PRM[pr, 8:9], RS[pr, 1, 0:127], ALU.mult, ALU.mult,
                    [f"ps{b1}", "PRM", ("RS", 1)], ["KCP"])
            mm(PS[b3][0:127, 0:64], HKV[:, 1, 0:127], CW2[:, 128:192], True, True, ["PRB", ("HKV", 1)], [f"ps{b3}"])
            cpy("act", VCA[0:127, 0:64], PS[b3][0:127, 0:64], [f"ps{b3}"], ["VCA"])
            if stop_after == "cmp":
                return
            gn_fm(l, (S_YB0, S_YB1), (17, 18), (4, 5))
            gn_fm(l, (S_YD0, S_YD1), (19, 20), (6, 7))
            if stop_after == "gn":
                return
            def zpad(dst, src, hf):
                pr, po = slice(hf * 64, hf * 64 + 64), slice((1 - hf) * 64, (1 - hf) * 64 + 64)
                allk = lambda sl: [("G", sl, t_) for t_ in range(4)]
                P.op("act", lambda e: e.mul(Gf[po, dst * T:(dst + 1) * T], Gf[po, src * T:(src + 1) * T], 0.0), allk(src), allk(dst))
                P.op("act", lambda e: e.copy(out=Gf[pr, dst * T:(dst + 1) * T], in_=Gf[pr, src * T:(src + 1) * T]),
                     allk(src), allk(dst))
            zpad(S_YB0, S_KS, 0); zpad(S_YB1, S_KS, 1)
            zpad(S_YD0, S_KW, 0); zpad(S_YD1, S_KW, 1)
            zpad(S_KS, S_CK, 0); zpad(S_KW, S_CK, 1)
            K_SLC, K_WIN, K_SWA = S_YB0, S_YD0, S_KS
            GNA = PRM[:, 25:281]
            GNC = PRM[:, 281:537]

            def evac(qs, t16, ncol, br, first, last):
                ps = PS[qs]
                pr = f"ps{qs}"
                tsc(SM[:, 0:4], ps[:, 64:64 + 3 * ncol + 1:ncol], 1e-30, ALU.max, [pr], [("SM", 0)])
                recip(SM[:, 4:8], SM[:, 0:4], [("SM", 0)], [("SM", 4)])
                tt(SM[:, 8:12], SM[:, 4:8], GT[:, t16, br:12:3], ALU.mult, [("SM", 4), "GT"], [("SM", 8)])
                for h in range(4):
                    o = OA[:, qs, h * 64:(h + 1) * 64]
                    if first:
                        tsc(o, ps[:, h * ncol:h * ncol + 64], SM[:, 8 + h:9 + h], ALU.mult, [pr, ("SM", 8)], [("OA", qs)])
                    else:
                        stt(o, ps[:, h * ncol:h * ncol + 64], SM[:, 8 + h:9 + h], o, ALU.mult, ALU.add,
                            [pr, ("SM", 8), ("OA", qs)], [("OA", qs)])
                if br == 0:
                    IMP, SCR, SELM, M8 = SC[:, 0, :], SC[:, 1, :], SC[:, 2, :], SC[:, 3, 0:8]
                    tsc(IMP, ps[:, 65:97], SM[:, 4:5], ALU.mult, [pr, ("SM", 4)], ["IMP"])
                    for h in range(1, 4):
                        stt(IMP, ps[:, h * 97 + 65:(h + 1) * 97], SM[:, 4 + h:5 + h], IMP, ALU.mult, ALU.add,
                            [pr, ("SM", 4), "IMP"], ["IMP"])
                    tt(SCR, IMP, CB[:, CB_SELA + t16 * 32:CB_SELA + (t16 + 1) * 32], ALU.mult, ["IMP"] + CBK, ["SCR"])
                    tt(SCR, SCR, CB[:, CB_SELB + t16 * 32:CB_SELB + (t16 + 1) * 32], ALU.add, ["SCR"] + CBK, ["SCR"])
                    P.op("dve", lambda e: e.max(out=M8, in_=SCR), ["SCR"], ["M8"])
                    tsc(SELM, SCR, SC[:, 3, 7:8], ALU.is_ge, ["SCR", "M8"], ["SELM"], s2=-1.0, op1=ALU.add)
                    pb = 6 + cnt["m"] % 2
                    cnt["m"] += 1
                    P.op("pe", lambda e: e.transpose(out=PS[pb][0:32, 0:128], in_=SELM, identity=IDF), ["SELM", "CF"], [f"ps{pb}"])
                    cpy("act", SELT[0:32, qs * 128:(qs + 1) * 128], PS[pb][0:32, 0:128], [f"ps{pb}"], ["SELT"])
                if last:
                    gn_tm(OA[:, qs, :], [("OA", qs)], GNA, 0, t16)

            def sbank():
                b = 4 + cnt["s"] % 2
                cnt["s"] += 1
                return b

            def etbuf():
                j = cnt["et"] % 3
                cnt["et"] += 1
                return j

            for c in range(4):
                ca, cbb = c * 512, (c + 1) * 512
                def pipeline(units):
                    n = len(units)
                    bks = [sbank()]
                    units[0][0](bks[0])
                    for i in range(n):
                        if i + 1 < n:
                            bks.append(sbank())
                            units[i + 1][0](bks[i + 1])
                        j = etbuf()
                        units[i][1](bks[i], j)
                        units[i][2](j)

                units = []
                for h in range(4):
                    aq = S_AQ0 + h // 2

                    def sc(sbk, h=h, aq=aq):
                        mm(PS[sbk][0:127, :], KCP[:, h % 2, 0:127], Gs(aq, ca, cbb), True, True,
                           ["KCP", ("G", aq, c)], [f"ps{sbk}"])

                    def ex(sbk, j):
                        act(ET[0:127, j, :], PS[sbk][0:127, :], AF.Exp, [f"ps{sbk}"], [("ET", j)], scale=0.125)
                        P.op("pool", lambda e: e.affine_select(out=ET[0:127, j, :], in_=ET[0:127, j, :], pattern=[[1, 512]],
                                                               compare_op=ALU.is_ge, fill=0.0, base=512 * c - 31, channel_multiplier=-16),
                             [("ET", j)], [("ET", j)])

                    def pv(j, h=h):
                        for qs in range(4):
                            mm(PS[qs][:, h * 97:(h + 1) * 97], ET[0:127, j, qs * 128:(qs + 1) * 128], VCA[0:127, 0:97], h == 0, True,
                               [("ET", j), "VCA"], [f"ps{qs}"])
                    units.append((sc, ex, pv))
                pipeline(units)
                for qs in range(4):
                    evac(qs, 4 * c + qs, 97, 0, True, False)

                def ex_full(sbk, j):
                    act(ET[:, j, :], PS[sbk][:], AF.Exp, [f"ps{sbk}"], [("ET", j)], scale=0.125)
                units = []
                for h in range(4):
                    aq = S_AQ0 + h // 2
                    for kt in range(4 * c + 4):
                        r = 4 * c - kt

                        def sc(sbk, h=h, aq=aq, kt=kt, r=r):
                            mm(PS[sbk][:], Gs(K_SLC + h % 2, kt * 128, (kt + 1) * 128), Gs(aq, ca, cbb), True, False,
                               [("G", K_SLC + h % 2, kt // 4), ("G", aq, c)], [f"ps{sbk}"])
                            mm(PS[sbk][:], CB[:, CB_EXP + kt * 128:CB_EXP + (kt + 1) * 128], SELT[:, :], False, r > 0,
                               CBK + ["SELT"], [f"ps{sbk}"])
                            if r <= 0:
                                mm(PS[sbk][:], IDB, CB[:, CB_CAUS + 384 + 128 * r:CB_CAUS + 384 + 128 * r + 512], False, True,
                                   CBK, [f"ps{sbk}"])

                        def pv(j, h=h, kt=kt):
                            for qs in range(4):
                                if kt > 4 * c + qs:
                                    continue
                                mm(PS[qs][:, h * 65:(h + 1) * 65], ET[:, j, qs * 128:(qs + 1) * 128], VSW[:, kt, 0, :],
                                   h == 0 and kt == 0, True, [("ET", j), "VSW"], [f"ps{qs}"])
                        units.append((sc, ex_full, pv))
                pipeline(units)
                for qs in range(4):
                    evac(qs, 4 * c + qs, 65, 1, False, False)
                units = []
                for h in range(4):
                    aq = S_AQ0 + h // 2
                    for kt in range(max(0, 4 * c - 4), 4 * c + 4):
                        r = 4 * c - kt

                        def sc(sbk, h=h, aq=aq, kt=kt, r=r):
                            mm(PS[sbk][:], Gs(K_WIN + h % 2, kt * 128, (kt + 1) * 128), Gs(aq, ca, cbb), True, False,
                               [("G", K_WIN + h % 2, kt // 4), ("G", aq, c)], [f"ps{sbk}"])
                            if r <= 0:
                                msk = CB[:, CB_CAUS + 384 + 128 * r:CB_CAUS + 384 + 128 * r + 512]
                            else:
                                msk = CB[:, CB_WINM + 128 * (r - 1):CB_WINM + 128 * (r - 1) + 512]
                            mm(PS[sbk][:], IDB, msk, False, True, CBK, [f"ps{sbk}"])

                        def pv(j, h=h, kt=kt):
                            for qs in range(4):
                                tq = 4 * c + qs
                                if kt > tq or kt < tq - 4:
                                    continue
                                mm(PS[qs][:, h * 65:(h + 1) * 65], ET[:, j, qs * 128:(qs + 1) * 128], VSW[:, kt, 1, :],
                                   h == 0 and kt == max(0, tq - 4), True, [("ET", j), "VSW"], [f"ps{qs}"])
                        units.append((sc, ex_full, pv))
                pipeline(units)
                for qs in range(4):
                    evac(qs, 4 * c + qs, 65, 2, False, True)
            if stop_after == "nsa":
                return
            units = []
            for kt in range(16):
                nq = 256 if kt < 15 else 128
                q0 = kt * 128
                qres = sorted(set([q0 // 512, (q0 + nq - 1) // 512]))
                for ui, (a_, hf) in enumerate(((0, 0), (0, 1), (1, 0), (1, 1))):
                    h = 2 * hf + a_

                    def sc(sbk, kt=kt, nq=nq, q0=q0, qres=qres, a_=a_, hf=hf):
                        mm(PS[sbk][:, 0:nq], Gs(K_SWA + hf, kt * 128, (kt + 1) * 128), Gs(S_CQ0 + a_, q0, q0 + nq),
                           True, False, [("G", K_SWA + hf, kt // 4)] + [("G", S_CQ0 + a_, t_) for t_ in qres], [f"ps{sbk}"])
                        mm(PS[sbk][:, 0:nq], IDB, CB[:, CB_BAND:CB_BAND + nq], False, True, CBK, [f"ps{sbk}"])

                    def ex(sbk, j, nq=nq):
                        act(ET[:, j, 0:nq], PS[sbk][:, 0:nq], AF.Exp, [f"ps{sbk}"], [("ET", j)], scale=0.125)

                    def pv(j, kt=kt, h=h, hf=hf, ui=ui):
                        b0, b1 = kt % 4, (kt + 1) % 4
                        mm(PS[b0][:, h * 65:(h + 1) * 65], ET[:, j, 0:128], CV[:, kt, hf, :], kt == 0 and ui == 0, True,
                           [("ET", j), "CV"], [f"ps{b0}"])
                        if kt < 15:
                            mm(PS[b1][:, h * 65:(h + 1) * 65], ET[:, j, 128:256], CV[:, kt, hf, :], ui == 0, False,
                               [("ET", j), "CV"], [f"ps{b1}"])
                        if ui == 3:
                            ps, pr = PS[kt % 4], f"ps{kt % 4}"
                            tt(SM[:, 0:4], ps[:, 64:64 + 3 * 65 + 1:65], SINKE[:], ALU.add, [pr, "SINKE"], [("SM", 0)])
                            recip(SM[:, 4:8], SM[:, 0:4], [("SM", 0)], [("SM", 4)])
                            for hh in range(4):
                                tsc(OA[:, 0, hh * 64:(hh + 1) * 64], ps[:, hh * 65:hh * 65 + 64], SM[:, 4 + hh:5 + hh], ALU.mult,
                                    [pr, ("SM", 4)], [("OA", 0)])
                            gn_tm(OA[:, 0, :], [("OA", 0)], GNC, 2, kt)
                    units.append((sc, ex, pv))
            bks = [sbank()]
            units[0][0](bks[0])
            for i in range(len(units)):
                if i + 1 < len(units):
                    bks.append(sbank())
                    units[i + 1][0](bks[i + 1])
                j = etbuf()
                units[i][1](bks[i], j)
                units[i][2](j)
            if stop_after == "swa":
                return
            ysrc = (0, 1, 4, 5, 2, 3, 6, 7)
            for dc in range(8):
                wt, wres = WS.get(("wout", s, l, dc))
                for tc in range(4):
                    ts = slice(tc * 512, (tc + 1) * 512)
                    pb = bank()
                    for kc in range(8):
                        mm(PS[pb][:], wt[:, kc * 128:(kc + 1) * 128], XN[:, ysrc[kc], ts], kc == 0, kc == 7,
                           [wres, ("XN", ysrc[kc], tc)], [f"ps{pb}"])
                    tt(XT[:, dc, ts], PS[pb][:], XT[:, dc, ts], ALU.add, [f"ps{pb}", ("XT", dc, tc)], [("XT", dc, tc)])

        for s in range(nseq):
            for kc in range(8):
                P.dma("sp", XT[:, kc, :], xT[s, :, kc, :],
                      writes=[("XT", kc, tc) for tc in range(4)], semkey=("XT", kc))
            for l in layers:
                if do_ffn:
                    ffn(s, l, 0)
                if do_mixer:
                    mixer(s, l)
                if do_ffn:
                    ffn(s, l, 1)
            for kc in range(8):
                P.dma("sp", yT[s, :, kc, :], XT[:, kc, :],
                      reads=[("XT", kc, tc) for tc in range(4)], semkey=("XT", kc))
        P.finish()
        print("instructions", P.ninst, "waits", P.nwait)
    return nc


def prep_inputs(inp):
    f = lambda a: np.ascontiguousarray(np.asarray(a, dtype=np.float32))
    x = f(inp["x"])
    B = x.shape[0]
    xTh = np.ascontiguousarray(x.reshape(B, T, 8, 128).transpose(0, 3, 2, 1))

    def w13(w):
        w = f(w).reshape(L, 8, 128, NFC, 128)
        return np.ascontiguousarray(w.transpose(0, 3, 2, 1, 4)).reshape(L, NFC, 128, 1024)

    def w2(w):
        w = f(w).reshape(L, NG, GSZ, 128, 8, 128)
        return np.ascontiguousarray(w.transpose(0, 1, 4, 3, 2, 5)).reshape(L, NG, 8, 128, GSZ * 128)

    w1r = np.stack([w13(inp["ffn1_w1"]), w13(inp["ffn2_w1"])], axis=1)
    w3r = np.stack([w13(inp["ffn1_w3"]), w13(inp["ffn2_w3"])], axis=1)
    w2r = np.stack([w2(inp["ffn1_w2"]), w2(inp["ffn2_w2"])], axis=1)
    nrm = np.stack([f(inp["ffn1_norm"]), f(inp["mix_norm"]), f(inp["ffn2_norm"])], axis=1)
    fnorm = np.ascontiguousarray(nrm.reshape(L, 3, 8, 128).transpose(3, 0, 1, 2)).reshape(128, L * 3 * 8)

    o = {}
    off = 0
    for name, sz in (("a_q", 256), ("a_kc", 64), ("a_vc", 64), ("a_ks", 64), ("a_vs", 64), ("a_kw", 64), ("a_vw", 64),
                     ("a_g", 12), ("b_b", 256), ("b_c", 256), ("b_x", 256), ("c_q", 256), ("c_k", 128), ("c_v", 128), ("d_v", 256)):
        o[name] = off
        off += sz
    r = lambda name, a, n: list(range(o[name] + a, o[name] + a + n))
    tiles = [r("b_c", 0, 128), r("b_x", 0, 128), r("b_b", 0, 128), r("b_c", 128, 128), r("b_x", 128, 128), r("b_b", 128, 128),
             r("d_v", 0, 128), r("d_v", 128, 128),
             r("a_q", 0, 128), r("a_q", 128, 128), r("a_ks", 0, 64) * 2, r("a_kw", 0, 64) * 2,
             r("a_kc", 0, 64) + r("a_vc", 0, 64),
             r("c_q", 0, 64) + r("c_q", 128, 64), r("c_q", 64, 64) + r("c_q", 192, 64), r("c_k", 0, 128),
             r("a_vs", 0, 64) + r("a_vw", 0, 64), r("c_v", 0, 128), r("a_g", 0, 12) + [-1] * 116]
    w_in = f(inp["w_in"])
    w_in_p = np.concatenate([w_in, np.zeros((L, D, 1), np.float32)], axis=2)
    winr = np.stack([w_in_p[:, :, cols].reshape(L, 8, 128, 128).transpose(0, 2, 1, 3).reshape(L, 128, 1024)
                     for cols in tiles], axis=1)
    winr = np.ascontiguousarray(winr)
    ck = f(inp["cmp_w1_k"]).reshape(L, 4, 8, 64, 128)
    cvv = f(inp["cmp_w1_v"]).reshape(L, 4, 8, 64, 128)
    cw1r = np.ascontiguousarray(np.concatenate([ck, cvv], axis=3).transpose(0, 1, 3, 2, 4)).reshape(L, 4, 128, 1024)
    woutr = np.ascontiguousarray(f(inp["w_out"]).reshape(L, 8, 128, 8, 128).transpose(0, 3, 2, 1, 4)).reshape(L, 8, 128, 1024)
    prm = np.zeros((L, 128, NPRM), np.float32)
    p64 = np.arange(128) % 64
    qk_gain = {0: "nsa_q_norm", 1: "nsa_q_norm", 2: "nsa_ks_norm", 3: "nsa_kw_norm", 5: "swa_q_norm", 6: "swa_q_norm", 7: "swa_k_norm"}
    for ct, nm in qk_gain.items():
        prm[:, :, ct] = f(inp[nm])[:, p64]
    prm[:, :, 4] = 1.0
    prm[:, :, 8] = f(inp["nsa_kc_norm"])[:, p64]
    cwv = f(inp["conv_w"])
    for cb in range(2):
        for k in range(3):
            prm[:, :, 9 + cb * 3 + k] = cwv[:, k, cb * 128:(cb + 1) * 128]
    psc = f(inp["pool_scale"])
    gnv = f(inp["group_norm"])
    for c in range(2):
        prm[:, :, 15 + c] = psc[:, c * 128:(c + 1) * 128]
        prm[:, :, 17 + c] = gnv[:, 256 + c * 128:256 + (c + 1) * 128]
        prm[:, :, 19 + c] = gnv[:, 768 + c * 128:768 + (c + 1) * 128]
    prm[:, :, 21:25] = f(inp["swa_sinks"])[:, None, :]
    prm[:, :, 25:281] = gnv[:, None, 0:256]
    prm[:, :, 281:537] = gnv[:, None, 512:768]
    prb = np.zeros((L, 128, NPRB), np.float32)
    prb[:, 0:64, 0:32] = f(inp["cmp_pos_k"]).transpose(0, 2, 1)
    prb[:, 64:128, 0:32] = f(inp["cmp_pos_v"]).transpose(0, 2, 1)
    prb[:, :, 32:96] = f(inp["cmp_w2_k"])
    prb[:, :, 96:160] = f(inp["cmp_w2_k"])
    prb[:, :, 160:224] = f(inp["cmp_w2_v"])
    pw = f(inp["pool_w"])
    for c in range(2):
        prb[:, 0:64, 224 + c * 128:224 + c * 128 + 64] = pw[:, 2 * c]
        prb[:, 64:128, 224 + c * 128 + 64:224 + (c + 1) * 128] = pw[:, 2 * c + 1]
    cstf = np.zeros((128, NCF), np.float32)
    cstf[:, 0:128] = 1.0
    pp = np.arange(128)
    cstf[:, 128:256] = (pp[:, None] // 64 == pp[None, :] // 64)
    cstf[:, 256:384] = np.eye(128)
    for c in range(2):
        wp = np.where(pp < 64, POOLWIN[2 * c], POOLWIN[2 * c + 1]).astype(np.float32)
        cstf[:, 384 + c] = 1.0 / wp
        tcol = np.arange(16)[None, :]
        cstf[:, 386 + c * 16:386 + (c + 1) * 16] = wp[:, None] / np.minimum(tcol + 1, wp[:, None])
    cstb = np.zeros((128, NCB), np.float32)
    cstb[:, CB_IDB:CB_IDB + 128] = np.eye(128)
    k = pp[:, None]
    xx = np.arange(896)[None, :]
    cstb[:, CB_CAUS:CB_CAUS + 896] = np.where(xx - 384 - k >= 0, 0.0, NEGB)
    cstb[:, CB_WINM:CB_WINM + 896] = np.where(k - xx + 383 >= 0, 0.0, NEGB)
    xb = np.arange(256)[None, :]
    cstb[:, CB_BAND:CB_BAND + 256] = np.where((xb - k >= 0) & (xb - k < 128), 0.0, NEGB)
    tglob = (np.arange(16)[None, :, None] * 128 + pp[:, None, None])
    jj = np.arange(32)[None, None, :]
    cur = tglob // 64
    forced = (jj == 0) | (jj == cur) | (jj == cur - 1)
    future = jj * 64 > tglob
    cstb[:, CB_SELA:CB_SELA + 512] = np.where(forced | future, 0.0, 1.0).reshape(128, 512)
    cstb[:, CB_SELB:CB_SELB + 512] = np.where(forced, 1e4, np.where(future, -1.0, 0.0)).reshape(128, 512)
    ex = np.zeros((128, 16, 128), np.float32)
    for kt in range(16):
        ex[2 * kt, kt, 0:64] = -NEGB
        ex[2 * kt + 1, kt, 64:128] = -NEGB
    cstb[:, CB_EXP:CB_EXP + 2048] = ex.reshape(128, 2048)
    covl = np.zeros((128, 33), np.float32)
    covl[:, 0] = 1.0
    ci = np.arange(127)[:, None] * 16
    sj = np.arange(32)[None, :] * 64
    covl[0:127, 1:33] = ((ci <= sj + 63) & (ci + 31 >= sj))
    shared = dict(w1r=w1r, w3r=w3r, w2r=w2r, fnorm=fnorm, winr=winr, cw1r=cw1r, woutr=woutr, prm=prm, prb=prb,
                  cstf=cstf, cstb=cstb, covl=covl)
    return xTh, shared


def kernel(**inputs):
    xTh, shared = prep_inputs(inputs)
    nc = build_program()
    in_maps = []
    for c in range(NCORES):
        m = dict(shared)
        m["xT"] = np.ascontiguousarray(xTh[c * SEQ_PER_CORE:(c + 1) * SEQ_PER_CORE])
        in_maps.append(m)
    res = run_bass_kernel_spmd(nc, in_maps, core_ids=list(range(NCORES)))
    yT = np.concatenate([r["yT"] for r in res.results], axis=0)
    out = np.ascontiguousarray(yT.transpose(0, 3, 2, 1)).reshape(-1, T, D)
    return out.astype(np.float32)
```

```python
import numpy as np
from contextlib import ExitStack
import concourse.bass as bass
import concourse.mybir as mybir
from concourse.bass_utils import run_bass_kernel_spmd

F32 = mybir.dt.float32
BF16 = mybir.dt.bfloat16
AF = mybir.ActivationFunctionType
ALU = mybir.AluOpType

NCORES = 8
L = 2
D = 1024
T = 2048
DFF = 2816
NFC = DFF // 128
NG = 2
GSZ = NFC // NG
SEQ_PER_CORE = 2
EPS = 1e-6
SELF_SYNC = True


class Prog:
    EPOCH = 2000

    def __init__(self, nc):
        self.nc = nc
        self.eng = dict(pe=nc.tensor, dve=nc.vector, act=nc.scalar,
                        pool=nc.gpsimd, sp=nc.sync)
        self.sems = {}
        self.epoch = {}
        self.cnt = {}
        self.seen = {k: {} for k in self.eng}
        self.lastw = {}
        self.reads = {}
        self.nwait = 0
        self.ninst = 0

    def _cur(self, base, step):
        ep = self.epoch.get(base, 0)
        sid = (base, ep)
        if sid in self.sems and self.cnt[sid] + step > self.EPOCH:
            ep += 1
            self.epoch[base] = ep
            sid = (base, ep)
        if sid not in self.sems:
            self.sems[sid] = self.nc.alloc_semaphore(name=f"s{len(self.sems)}")
            self.cnt[sid] = 0
        return sid

    def _deps(self, reads, writes):
        deps = []
        for r in reads:
            if r in self.lastw:
                deps.append(self.lastw[r])
        for w in writes:
            if w in self.lastw:
                deps.append(self.lastw[w])
            deps.extend(self.reads.get(w, {}).items())
        return deps

    def _wait(self, e, deps):
        need = {}
        for sid, v in deps:
            if sid[0] == e and (e in ("pe", "sp") or not SELF_SYNC):
                continue
            if self.seen[e].get(sid, 0) >= v:
                continue
            if need.get(sid, 0) < v:
                need[sid] = v
        for sid, v in need.items():
            self.eng[e].wait_ge(self.sems[sid], v)
            self.seen[e][sid] = v
            self.nwait += 1

    def _record(self, ev, reads, writes):
        for r in reads:
            d = self.reads.setdefault(r, {})
            if d.get(ev[0], 0) < ev[1]:
                d[ev[0]] = ev[1]
        for w in writes:
            self.lastw[w] = ev
            self.reads[w] = {}

    def op(self, e, fn, reads=(), writes=()):
        writes = list(writes) + [r for r in reads if isinstance(r, str) and r.startswith("ps")]
        self._wait(e, self._deps(reads, writes))
        inst = fn(self.eng[e])
        sid = self._cur(e, 1)
        self.cnt[sid] += 1
        inst.then_inc(self.sems[sid], 1)
        self.ninst += 1
        self._record((sid, self.cnt[sid]), reads, writes)

    def dma(self, q, out, in_, reads=(), writes=(), semkey=None, **kw):
        self._wait(q, self._deps(reads, writes))
        sid = self._cur(("d", semkey), 16)
        inst = self.eng[q].dma_start(out=out, in_=in_, **kw)
        self.cnt[sid] += 16
        inst.then_inc(self.sems[sid], 16)
        self.ninst += 1
        self._record((sid, self.cnt[sid]), reads, writes)

    def finish(self, e="sp"):
        deps = [(sid, v) for sid, v in self.cnt.items() if v > 0 and sid[0] != e]
        self._wait(e, deps)
        print("semaphores used", len(self.sems))


class WStream:
    def __init__(self, P, ring, nslots, plan, lookahead):
        self.P, self.ring, self.n, self.plan, self.la = P, ring, nslots, plan, lookahead
        self.pos = 0
        self.issued = 0

    def _issue(self, j):
        key, src, width = self.plan[j]
        slot = j % self.n
        self.P.dma("pool", self.ring[:, slot, 0:width], src,
                   writes=[("W", slot)], semkey=("W", slot))

    def get(self, key):
        k, src, width = self.plan[self.pos]
        assert k == key, (k, key)
        hi = min(len(self.plan), self.pos + self.la + 1)
        while self.issued < hi:
            self._issue(self.issued)
            self.issued += 1
        slot = self.pos % self.n
        self.pos += 1
        return self.ring[:, slot, 0:width], ("W", slot)


POOLWIN = (2, 4, 8, 16)
S_AQ0, S_AQ1, S_KS, S_KW, S_KVC, S_CQ0, S_CQ1, S_CK, S_YB0, S_YB1, S_YD0, S_YD1 = range(12)
NPRM = 537
NPRB = 480
NCF = 418
CB_IDB, CB_CAUS, CB_WINM, CB_BAND, CB_SELA, CB_SELB, CB_ZERO, CB_EXP, NCB = 0, 128, 1024, 1920, 2176, 2688, 3200, 3712, 5760
NEGB = -30000.0


def build_program(layers=(0, 1), nseq=SEQ_PER_CORE, do_mixer=True, do_ffn=True, stop_after=None):
    nc = bass.Bass("TRN2", target_bir_lowering=False)
    din = lambda n, s: nc.dram_tensor(n, list(s), F32, kind="ExternalInput").ap()
    xT = din("xT", [nseq, 128, 8, T])
    if do_ffn:
        w1r = din("w1r", [L, 2, NFC, 128, 1024])
        w3r = din("w3r", [L, 2, NFC, 128, 1024])
        w2r = din("w2r", [L, 2, NG, 8, 128, GSZ * 128])
    fnorm = din("fnorm", [128, L * 3 * 8])
    winr = din("winr", [L, 19, 128, 1024])
    cw1r = din("cw1r", [L, 4, 128, 1024])
    woutr = din("woutr", [L, 8, 128, 1024])
    prm = din("prm", [L, 128, NPRM])
    prb = din("prb", [L, 128, NPRB])
    cstf = din("cstf", [128, NCF])
    cstb = din("cstb", [128, NCB])
    covl = din("covl", [128, 33])
    yT = nc.dram_tensor("yT", [nseq, 128, 8, T], F32, kind="ExternalOutput").ap()

    with ExitStack() as es:
        sb = lambda n, s, d: es.enter_context(nc.sbuf_tensor(n, list(s), d))
        XT = sb("XT", [128, 8, T], F32)
        XN = sb("XN", [128, 8, T], BF16)
        Gf = sb("G", [128, 12 * T], BF16)
        NW = 6
        WR = sb("WR", [128, NW, 1024], BF16)
        SQ = sb("SQ", [128, 2, 512], F32)
        RS = sb("RS", [128, 2, 512], F32)
        SL = sb("SL", [128, 2, 512], BF16)
        GN = sb("GN", [128, L * 3 * 8], F32)
        CF = sb("CF", [128, NCF], F32)
        CB = sb("CB", [128, NCB], BF16)
        PRM = sb("PRM", [128, NPRM], F32)
        PRB = sb("PRB", [128, NPRB], BF16)
        EPSB = sb("EPSB", [128, 1], F32)
        VSW = sb("VSW", [128, 16, 2, 65], BF16)
        CV = sb("CV", [128, 16, 2, 65], BF16)
        GT = sb("GT", [128, 16, 12], F32)
        ET = sb("ET", [128, 3, 512], BF16)
        OA = sb("OA", [128, 4, 256], F32)
        YT = sb("YT", [128, 256], F32)
        SM = sb("SM", [128, 64], F32)
        SC = sb("SC", [128, 4, 32], F32)
        SCI = sb("SCI", [128, 4, 32], F32)
        SM2 = sb("SM2", [128, 4, 12], F32)
        SELT = sb("SELT", [128, 512], BF16)
        KCP = sb("KCP", [128, 2, 128], BF16)
        VCA = sb("VCA", [128, 97], BF16)
        HKV = sb("HKV", [128, 2, 128], BF16)
        SINKE = sb("SINKE", [128, 4], F32)
        PS = [es.enter_context(nc.psum_tensor(f"ps{i}", [128, 512], F32)) for i in range(8)]
        ONESF = CF[:, 0:128]
        BD64 = CF[:, 128:256]
        IDF = CF[:, 256:384]
        IDB = CB[:, CB_IDB:CB_IDB + 128]

        P = Prog(nc)

        def Gs(slot, a=0, b=T, p0=0, p1=128):
            return Gf[p0:p1, slot * T + a: slot * T + b]

        def mm(out, lhsT, rhs, start, stop, reads, writes):
            P.op("pe", lambda e: e.matmul(out, lhsT=lhsT, rhs=rhs, start=start, stop=stop, skip_group_check=True),
                 reads, writes)

        def act(out, in_, func, reads, writes, **kw):
            P.op("act", lambda e: e.activation(out=out, in_=in_, func=func, **kw), reads, writes)

        def tt(out, in0, in1, op, reads, writes):
            P.op("dve", lambda e: e.tensor_tensor(out=out, in0=in0, in1=in1, op=op), reads, writes)

        def tsc(out, in0, s1, op0, reads, writes, s2=None, op1=None):
            if op1 is None:
                P.op("dve", lambda e: e.tensor_scalar(out=out, in0=in0, scalar1=s1, scalar2=None, op0=op0), reads, writes)
            else:
                P.op("dve", lambda e: e.tensor_scalar(out=out, in0=in0, scalar1=s1, scalar2=s2, op0=op0, op1=op1), reads, writes)

        def stt(out, in0, scalar, in1, op0, op1, reads, writes):
            P.op("dve", lambda e: e.scalar_tensor_tensor(out=out, in0=in0, scalar=scalar, in1=in1, op0=op0, op1=op1),
                 reads, writes)

        def recip(out, in_, reads, writes):
            P.op("dve", lambda e: e.reciprocal(out=out, in_=in_), reads, writes)

        def cpy(eng, out, in_, reads, writes):
            if eng == "act":
                P.op("act", lambda e: e.copy(out=out, in_=in_), reads, writes)
            else:
                P.op(eng, lambda e: e.tensor_copy(out=out, in_=in_), reads, writes)

        plan = []
        for s in range(nseq):
            for l in layers:
                for fi in range(2):
                    if fi == 1 and do_mixer:
                        for i in range(19):
                            plan.append((("win", s, l, i), winr[l, i], 1024))
                        for q in range(4):
                            plan.append((("cw1", s, l, q), cw1r[l, q], 1024))
                        for dc in range(8):
                            plan.append((("wout", s, l, dc), woutr[l, dc], 1024))
                    if not do_ffn:
                        continue
                    for g in range(NG):
                        for i in range(GSZ):
                            fc = g * GSZ + i
                            plan.append((("w1", s, l, fi, fc), w1r[l, fi, fc], 1024))
                            plan.append((("w3", s, l, fi, fc), w3r[l, fi, fc], 1024))
                        for dc in range(8):
                            plan.append((("w2", s, l, fi, g, dc, 0), w2r[l, fi, g, dc][:, 0:768], 768))
                            plan.append((("w2", s, l, fi, g, dc, 1), w2r[l, fi, g, dc][:, 768:GSZ * 128], GSZ * 128 - 768))
        WS = WStream(P, WR, NW, plan, NW - 3)

        P.dma("sp", GN[:], fnorm, writes=["GN"], semkey="GN")
        P.dma("sp", CF[:], cstf, writes=["CF"], semkey="CF")
        for i in range(0, NCB, 1920):
            P.dma("pool", CB[:, i:i + 1920], cstb[:, i:i + 1920], writes=[("CB", i)], semkey=("CB", i))
        CBK = [("CB", i) for i in range(0, NCB, 1920)]
        P.dma("pool", VCA[:, 64:97], covl, writes=["VCA"], semkey="VCA")
        P.op("dve", lambda e: e.memset(EPSB[:], EPS), writes=["EPSB"])
        P.op("pool", lambda e: e.memset(VSW[:, :, :, 64:65], 1.0), writes=["VSW"])
        P.op("pool", lambda e: e.memset(CV[:, :, :, 64:65], 1.0), writes=["CV"])
        P.op("pool", lambda e: e.memset(SELT[:], 0.0), writes=["SELT"])
        P.op("pool", lambda e: e.memset(KCP[:], 0.0), writes=["KCP"])

        cnt = {"h": 0, "o": 0, "sl": 0, "sq": 0, "pb": 0, "s": 0, "et": 0, "m": 0, "cp": 0, "stg": 0}

        def sqbuf():
            j = cnt["sq"] % 2
            cnt["sq"] += 1
            return j

        def slbuf():
            j = cnt["sl"] % 2
            cnt["sl"] += 1
            return j

        def bank(lo=0, hi=8):
            b = lo + cnt["pb"] % (hi - lo)
            cnt["pb"] += 1
            return b

        def cpeng():
            cnt["cp"] += 1
            return "act" if cnt["cp"] % 2 else "dve"

        def rstd_from(pss_ap, pres, scale, n=512):
            act(RS[:, 0, 0:n], pss_ap, AF.Sqrt, [pres, "EPSB"], [("RS", 0)], scale=scale, bias=EPSB[:])
            recip(RS[:, 1, 0:n], RS[:, 0, 0:n], [("RS", 0)], [("RS", 1)])

        def rmsnorm(l, ni):
            for tc in range(4):
                ts = slice(tc * 512, (tc + 1) * 512)
                pb = bank()
                for kc in range(8):
                    j = sqbuf()
                    act(SQ[:, j, :], XT[:, kc, ts], AF.Square, [("XT", kc, tc)], [("SQ", j)])
                    mm(PS[pb][:], ONESF, SQ[:, j, :], kc == 0, kc == 7, [("SQ", j), "CF"], [f"ps{pb}"])
                rstd_from(PS[pb][:], f"ps{pb}", 1.0 / D)
                for kc in range(8):
                    gi = (l * 3 + ni) * 8 + kc
                    stt(XN[:, kc, ts], XT[:, kc, ts], GN[:, gi:gi + 1], RS[:, 1, :], ALU.mult, ALU.mult,
                        [("XT", kc, tc), ("RS", 1), "GN"], [("XN", kc, tc)])

        def ffn(s, l, fi):
            rmsnorm(l, 0 if fi == 0 else 2)
            for g in range(NG):
                for i in range(GSZ):
                    fc = g * GSZ + i
                    w1t, w1res = WS.get(("w1", s, l, fi, fc))
                    w3t, w3res = WS.get(("w3", s, l, fi, fc))
                    for tc in range(4):
                        ts = slice(tc * 512, (tc + 1) * 512)
                        hb = (cnt["h"] % 2) * 2
                        cnt["h"] += 1
                        p1, p3 = PS[hb], PS[hb + 1]
                        for kc in range(8):
                            mm(p1[:], w1t[:, kc * 128:(kc + 1) * 128], XN[:, kc, ts], kc == 0, kc == 7,
                               [w1res, ("XN", kc, tc)], [f"ps{hb}"])
                        for kc in range(8):
                            mm(p3[:], w3t[:, kc * 128:(kc + 1) * 128], XN[:, kc, ts], kc == 0, kc == 7,
                               [w3res, ("XN", kc, tc)], [f"ps{hb + 1}"])
                        j = slbuf()
                        act(SL[:, j, :], p1[:], AF.Silu, [f"ps{hb}"], [("SL", j)])
                        tt(Gs(i, tc * 512, (tc + 1) * 512), SL[:, j, :], p3[:], ALU.mult,
                           [("SL", j), f"ps{hb + 1}"], [("G", i, tc)])
                for dc in range(8):
                    w2a, w2ares = WS.get(("w2", s, l, fi, g, dc, 0))
                    w2b, w2bres = WS.get(("w2", s, l, fi, g, dc, 1))
                    for tc in range(4):
                        ts = slice(tc * 512, (tc + 1) * 512)
                        ob = 4 + cnt["o"] % 2
                        cnt["o"] += 1
                        po = PS[ob]
                        for i in range(GSZ):
                            w2t, w2res, ii = (w2a, w2ares, i) if i < 6 else (w2b, w2bres, i - 6)
                            mm(po[:], w2t[:, ii * 128:(ii + 1) * 128], Gs(i, tc * 512, (tc + 1) * 512), i == 0, i == GSZ - 1,
                               [w2res, ("G", i, tc)], [f"ps{ob}"])
                        stt(XT[:, dc, ts], po[:], 0.5, XT[:, dc, ts], ALU.mult, ALU.add,
                            [f"ps{ob}", ("XT", dc, tc)], [("XT", dc, tc)])

        ZB = Gf[:, 0:2 * T].bitcast(F32)

        def zkeys(tc):
            return [("G", tc // 2, 2 * (tc % 2)), ("G", tc // 2, 2 * (tc % 2) + 1)]

        def proj_fm(wt, wres, tc, pb):
            ts = slice(tc * 512, (tc + 1) * 512)
            for kc in range(8):
                mm(PS[pb][:], wt[:, kc * 128:(kc + 1) * 128], XN[:, kc, ts], kc == 0, kc == 7,
                   [wres, ("XN", kc, tc)], [f"ps{pb}"])

        def gn_fm(l, slots, gcols, dests):
            for tc in range(4):
                a, b = tc * 512, (tc + 1) * 512
                pb = bank()
                for i, sl in enumerate(slots):
                    j = sqbuf()
                    act(SQ[:, j, :], Gs(sl, a, b), AF.Square, [("G", sl, tc)], [("SQ", j)])
                    mm(PS[pb][:], ONESF, SQ[:, j, :], i == 0, i == 1, [("SQ", j), "CF"], [f"ps{pb}"])
                rstd_from(PS[pb][:], f"ps{pb}", 1.0 / 256)
                for i, sl in enumerate(slots):
                    stt(XN[:, dests[i], a:b], Gs(sl, a, b), PRM[:, gcols[i]:gcols[i] + 1], RS[:, 1, :], ALU.mult, ALU.mult,
                        [("G", sl, tc), ("RS", 1), "PRM"], [("XN", dests[i], tc)])

        def gn_tm(oav, oares, gain, dest, tt_):
            tc = tt_ // 4
            tt(YT[:], oav, oav, ALU.mult, oares, ["YT"])
            P.op("dve", lambda e: e.reduce_sum(out=SM[:, 40:41], in_=YT[:], axis=mybir.AxisListType.X), ["YT"], [("SM", 40)])
            act(SM[:, 41:42], SM[:, 40:41], AF.Sqrt, [("SM", 40), "EPSB"], [("SM", 41)], scale=1.0 / 256, bias=EPSB[:])
            recip(SM[:, 42:43], SM[:, 41:42], [("SM", 41)], [("SM", 42)])
            stt(YT[:], oav, SM[:, 42:43], gain, ALU.mult, ALU.mult, oares + [("SM", 42), "PRM"], ["YT"])
            for i in range(2):
                pb = 6 + cnt["m"] % 2
                cnt["m"] += 1
                P.op("pe", lambda e: e.transpose(out=PS[pb][:, 0:128], in_=YT[:, i * 128:(i + 1) * 128], identity=IDF),
                     ["YT", "CF"], [f"ps{pb}"])
                cpy("act", XN[:, dest + i, tt_ * 128:(tt_ + 1) * 128], PS[pb][:, 0:128], [f"ps{pb}"], [("XN", dest + i, tc)])

        def mixer(s, l):
            rmsnorm(l, 1)
            P.dma("sp", PRM[:], prm[l], writes=["PRM"], semkey="PRM")
            P.dma("pool", PRB[:], prb[l], writes=["PRB"], semkey="PRB")
            act(SINKE[:], PRM[:, 21:25], AF.Exp, ["PRM"], ["SINKE"])
            POSB = PRB[:, 0:32]
            CW2 = PRB[:, 32:224]
            PW = PRB[:, 224:480]
            wi = 0
            for cb in range(2):
                wc, rc = WS.get(("win", s, l, wi)); wx, rx = WS.get(("win", s, l, wi + 1)); wb, rb = WS.get(("win", s, l, wi + 2))
                wi += 3
                for tc in range(4):
                    a, b = tc * 512, (tc + 1) * 512
                    b1, b2, b3 = bank(), bank(), bank()
                    proj_fm(wc, rc, tc, b1); proj_fm(wx, rx, tc, b2); proj_fm(wb, rb, tc, b3)
                    j = sqbuf()
                    cpy("act", SQ[:, j, :], PS[b1][:], [f"ps{b1}"], [("SQ", j)])
                    tt(ZB[:, a:b], SQ[:, j, :], PS[b2][:], ALU.mult, [("SQ", j), f"ps{b2}"], zkeys(tc))
                    j2 = sqbuf()
                    zk = zkeys(tc) + (zkeys(tc - 1) if tc else [])
                    cw = lambda k: PRM[:, 9 + cb * 3 + k: 10 + cb * 3 + k]
                    tsc(SQ[:, j2, :], ZB[:, a:b], cw(2), ALU.mult, zk + ["PRM"], [("SQ", j2)])
                    for k, sh in ((1, 1), (0, 2)):
                        lo = max(a, sh)
                        stt(SQ[:, j2, lo - a:512], ZB[:, lo - sh:b - sh], cw(k), SQ[:, j2, lo - a:512], ALU.mult, ALU.add,
                            zk + ["PRM", ("SQ", j2)], [("SQ", j2)])
                    tt(Gs(S_YB0 + cb, a, b), SQ[:, j2, :], PS[b3][:], ALU.mult, [("SQ", j2), f"ps{b3}"], [("G", S_YB0 + cb, tc)])
            if stop_after == "B":
                return
            for cd in range(2):
                wd, rd = WS.get(("win", s, l, wi)); wi += 1
                for tc in range(4):
                    a, b = tc * 512, (tc + 1) * 512
                    b1, b2 = bank(), bank()
                    proj_fm(wd, rd, tc, b1)
                    zk = zkeys(tc) + (zkeys(tc - 1) if tc else [])
                    init = 0.0 if tc == 0 else ZB[:, a - 1:a]
                    P.op("dve", lambda e: e.tensor_tensor_scan(out=ZB[:, a:b], data0=CB[:, CB_ZERO:CB_ZERO + 512], data1=PS[b1][:],
                                                               initial=init, op0=ALU.add, op1=ALU.add),
                         [f"ps{b1}"] + CBK + zk, zkeys(tc))
                    j = sqbuf()
                    for half in range(2):
                        w = POOLWIN[2 * cd + half]
                        pr = slice(half * 64, half * 64 + 64)
                        lo = max(a, w)
                        tt(SQ[pr, j, lo - a:512], ZB[pr, lo:b], ZB[pr, lo - w:b - w], ALU.subtract, zk, [("SQ", j)])
                        if tc == 0:
                            cpy("dve", SQ[pr, j, 0:w], ZB[pr, 0:w], zk, [("SQ", j)])
                    if tc == 0:
                        tt(SQ[:, j, 0:16], SQ[:, j, 0:16], CF[:, 386 + cd * 16:386 + cd * 16 + 16], ALU.mult, [("SQ", j), "CF"], [("SQ", j)])
                    js = slbuf()
                    stt(SL[:, js, :], SQ[:, j, :], CF[:, 384 + cd:385 + cd], PS[b1][:], ALU.mult, ALU.subtract,
                        [("SQ", j), "CF", f"ps{b1}"], [("SL", js)])
                    mm(PS[b2][:], PW[:, cd * 128:(cd + 1) * 128], SL[:, js, :], True, True, ["PRB", ("SL", js)], [f"ps{b2}"])
                    tsc(Gs(S_YD0 + cd, a, b), PS[b2][:], PRM[:, 15 + cd:16 + cd], ALU.mult, [f"ps{b2}", "PRM"], [("G", S_YD0 + cd, tc)])
            if stop_after == "D":
                return
            for ct in range(8):
                wt, wres = WS.get(("win", s, l, wi)); wi += 1
                for tc in range(4):
                    a, b = tc * 512, (tc + 1) * 512
                    b1 = bank()
                    proj_fm(wt, wres, tc, b1)
                    if ct == S_KVC:
                        cpy(cpeng(), Gs(ct, a, b), PS[b1][:], [f"ps{b1}"], [("G", ct, tc)])
                        continue
                    j = sqbuf()
                    act(SQ[:, j, :], PS[b1][:], AF.Square, [f"ps{b1}"], [("SQ", j)])
                    b2 = bank()
                    mm(PS[b2][:], BD64, SQ[:, j, :], True, True, [("SQ", j), "CF"], [f"ps{b2}"])
                    rstd_from(PS[b2][:], f"ps{b2}", 1.0 / 64)
                    stt(Gs(ct, a, b), PS[b1][:], PRM[:, ct:ct + 1], RS[:, 1, :], ALU.mult, ALU.mult,
                        [f"ps{b1}", "PRM", ("RS", 1)], [("G", ct, tc)])
            if stop_after == "qk":
                return
            for ti in range(3):
                wt, wres = WS.get(("win", s, l, wi)); wi += 1
                for t16 in range(16):
                    tc = t16 // 4
                    pb = bank()
                    for kc in range(8):
                        mm(PS[pb][:, 0:128], XN[:, kc, t16 * 128:(t16 + 1) * 128], wt[:, kc * 128:(kc + 1) * 128], kc == 0, kc == 7,
                           [wres, ("XN", kc, tc)], [f"ps{pb}"])
                    if ti == 0:
                        cpy("act", VSW[:, t16, 0, 0:64], PS[pb][:, 0:64], [f"ps{pb}"], ["VSW"])
                        cpy("dve", VSW[:, t16, 1, 0:64], PS[pb][:, 64:128], [f"ps{pb}"], ["VSW"])
                    elif ti == 1:
                        cpy("act", CV[:, t16, 0, 0:64], PS[pb][:, 0:64], [f"ps{pb}"], ["CV"])
                        cpy("dve", CV[:, t16, 1, 0:64], PS[pb][:, 64:128], [f"ps{pb}"], ["CV"])
                    else:
                        act(GT[:, t16, :], PS[pb][:, 0:12], AF.Sigmoid, [f"ps{pb}"], ["GT"])
            if stop_after == "tm":
                return
            bA, bB = bank(), bank()
            kvr = [("G", S_KVC, tc) for tc in range(4)]
            for q in range(4):
                wt, wres = WS.get(("cw1", s, l, q))
                for l8 in range(8):
                    ll = 8 * q + l8
                    for pr, bk in ((slice(0, 64), bA), (slice(64, 128), bB)):
                        mm(PS[bk][:, 0:127], wt[pr, l8 * 128:(l8 + 1) * 128], Gf[pr, S_KVC * T + ll: S_KVC * T + ll + 2017: 16],
                           ll == 0, False, [wres] + kvr, [f"ps{bk}"])
                        mm(PS[bk][:, 127:128], wt[pr, l8 * 128:(l8 + 1) * 128], POSB[pr, ll:ll + 1],
                           False, ll == 31, [wres, "PRB"], [f"ps{bk}"])
            for i, bk in enumerate((bA, bB)):
                X, X2, X3 = SQ[:, 0, 0:127], SQ[:, 0, 128:255], SQ[:, 0, 256:383]
                cpy("act", SM[:, 50 + i:51 + i], PS[bk][:, 127:128], [f"ps{bk}"], [("SM", 50 + i)])
                tsc(X, PS[bk][:, 0:127], SM[:, 50 + i:51 + i], ALU.add, [f"ps{bk}", ("SM", 50 + i)], [("SQ", 0)])
                tt(X2, X, X, ALU.mult, [("SQ", 0)], [("SQ", 0)])
                tsc(X2, X2, 0.044715, ALU.mult, [("SQ", 0)], [("SQ", 0)], s2=1.0, op1=ALU.add)
                tt(X2, X2, X, ALU.mult, [("SQ", 0), ("SQ", 0)], [("SQ", 0)])
                act(X3, X2, AF.Sigmoid, [("SQ", 0)], [("SQ", 0)], scale=1.5957691216057308)
                tt(HKV[:, i, 0:127], X, X3, ALU.mult, [("SQ", 0), ("SQ", 0)], [("HKV", i)])
            b1, b2, b3 = bank(), bank(), bank()
            mm(PS[b1][:, 0:127], CW2[:, 0:128], HKV[:, 0, 0:127], True, True, ["PRB", ("HKV", 0)], [f"ps{b1}"])
            act(SQ[:, 1, 0:127], PS[b1][:, 0:127], AF.Square, [f"ps{b1}"], [("SQ", 1)])
            P.op("dve", lambda e: e.memset(SQ[:, 1, 127:128], 1.0), [("SQ", 1)], [("SQ", 1)])
            mm(PS[b2][:, 0:128], BD64, SQ[:, 1, 0:128], True, True, [("SQ", 1), "CF"], [f"ps{b2}"])
            rstd_from(PS[b2][:, 0:128], f"ps{b2}", 1.0 / 64, n=128)
            for hf in range(2):
                pr = slice(hf * 64, hf * 64 + 64)
                stt(KCP[pr, hf, 0:127], PS[b1][pr, 0:127], PRM[pr, 8:9], RS[pr, 1, 0:127], ALU.mult, ALU.mult,
                    [f"ps{b1}", "PRM", ("RS", 1)], ["KCP"])
            mm(PS[b3][0:127, 0:64], HKV[:, 1, 0:127], CW2[:, 128:192], True, True, ["PRB", ("HKV", 1)], [f"ps{b3}"])
            cpy("act", VCA[0:127, 0:64], PS[b3][0:127, 0:64], [f"ps{b3}"], ["VCA"])
            if stop_after == "cmp":
                return
            gn_fm(l, (S_YB0, S_YB1), (17, 18), (4, 5))
            gn_fm(l, (S_YD0, S_YD1), (19, 20), (6, 7))
            if stop_after == "gn":
                return
            def zpad(dst, src, hf):
                pr, po = slice(hf * 64, hf * 64 + 64), slice((1 - hf) * 64, (1 - hf) * 64 + 64)
                allk = lambda sl: [("G", sl, t_) for t_ in range(4)]
                P.op("dve", lambda e: e.memset(Gf[po, dst * T:(dst + 1) * T], 0.0), [], allk(dst))
                P.op("act", lambda e: e.copy(out=Gf[pr, dst * T:(dst + 1) * T], in_=Gf[pr, src * T:(src + 1) * T]),
                     allk(src), allk(dst))
            zpad(S_YB0, S_KS, 0); zpad(S_YB1, S_KS, 1)
            zpad(S_YD0, S_KW, 0); zpad(S_YD1, S_KW, 1)
            K_SLC, K_WIN, K_SWA = S_YB0, S_YD0, S_KS
            GNA = PRM[:, 25:281]
            GNC = PRM[:, 281:537]

            def evac_a(qs, t16, ncol, br, first):
                ps = PS[qs]
                pr = f"ps{qs}"
                S = SM2[:, qs, :]
                sk = ("SM2", qs)
                tsc(S[:, 0:4], ps[:, 64:64 + 3 * ncol + 1:ncol], 1e-30, ALU.max, [pr], [sk])
                recip(S[:, 4:8], S[:, 0:4], [sk], [sk])
                tt(S[:, 8:12], S[:, 4:8], GT[:, t16, br:12:3], ALU.mult, [sk, "GT"], [sk])
                for h in range(4):
                    o = OA[:, qs, h * 64:(h + 1) * 64]
                    if first:
                        tsc(o, ps[:, h * ncol:h * ncol + 64], S[:, 8 + h:9 + h], ALU.mult, [pr, sk], [("OA", qs)])
                    else:
                        stt(o, ps[:, h * ncol:h * ncol + 64], S[:, 8 + h:9 + h], o, ALU.mult, ALU.add,
                            [pr, sk, ("OA", qs)], [("OA", qs)])
                if br == 0:
                    IMP = SCI[:, qs, :]
                    tsc(IMP, ps[:, 65:97], S[:, 4:5], ALU.mult, [pr, sk], [("IMP", qs)])
                    for h in range(1, 4):
                        stt(IMP, ps[:, h * 97 + 65:(h + 1) * 97], S[:, 4 + h:5 + h], IMP, ALU.mult, ALU.add,
                            [pr, sk, ("IMP", qs)], [("IMP", qs)])

            def evac_b(qs, t16, br, last):
                if br == 0:
                    IMP, SCR, SELM, M8 = SCI[:, qs, :], SC[:, 1, :], SC[:, 2, :], SC[:, 3, 0:8]
                    tt(SCR, IMP, CB[:, CB_SELA + t16 * 32:CB_SELA + (t16 + 1) * 32], ALU.mult, [("IMP", qs)] + CBK, ["SCR"])
                    tt(SCR, SCR, CB[:, CB_SELB + t16 * 32:CB_SELB + (t16 + 1) * 32], ALU.add, ["SCR"] + CBK, ["SCR"])
                    P.op("dve", lambda e: e.max(out=M8, in_=SCR), ["SCR"], ["M8"])
                    tsc(SELM, SCR, SC[:, 3, 7:8], ALU.is_ge, ["SCR", "M8"], ["SELM"], s2=-1.0, op1=ALU.add)
                    pb = 6 + cnt["m"] % 2
                    cnt["m"] += 1
                    P.op("pe", lambda e: e.transpose(out=PS[pb][0:32, 0:128], in_=SELM, identity=IDF), ["SELM", "CF"], [f"ps{pb}"])
                    cpy("act", SELT[0:32, qs * 128:(qs + 1) * 128], PS[pb][0:32, 0:128], [f"ps{pb}"], ["SELT"])
                if last:
                    gn_tm(OA[:, qs, :], [("OA", qs)], GNA, 0, t16)

            def evac_all(c, ncol, br, first, last):
                for qs in range(4):
                    evac_a(qs, 4 * c + qs, ncol, br, first)
                for qs in range(4):
                    evac_b(qs, 4 * c + qs, br, last)

            def sbank():
                b = 4 + cnt["s"] % 2
                cnt["s"] += 1
                return b

            def etbuf():
                j = cnt["et"] % 3
                cnt["et"] += 1
                return j

            for c in range(4):
                ca, cbb = c * 512, (c + 1) * 512
                def pipeline(units):
                    n = len(units)
                    bks = [sbank()]
                    units[0][0](bks[0])
                    for i in range(n):
                        if i + 1 < n:
                            bks.append(sbank())
                            units[i + 1][0](bks[i + 1])
                        j = etbuf()
                        units[i][1](bks[i], j)
                        units[i][2](j)

                units = []
                for h in range(4):
                    aq = S_AQ0 + h // 2

                    def sc(sbk, h=h, aq=aq):
                        mm(PS[sbk][0:127, :], KCP[:, h % 2, 0:127], Gs(aq, ca, cbb), True, True,
                           ["KCP", ("G", aq, c)], [f"ps{sbk}"])

                    def ex(sbk, j):
                        act(ET[0:127, j, :], PS[sbk][0:127, :], AF.Exp, [f"ps{sbk}"], [("ET", j)], scale=0.125)
                        P.op("pool", lambda e: e.affine_select(out=ET[0:127, j, :], in_=ET[0:127, j, :], pattern=[[1, 512]],
                                                               compare_op=ALU.is_ge, fill=0.0, base=512 * c - 31, channel_multiplier=-16),
                             [("ET", j)], [("ET", j)])

                    def pv(j, h=h):
                        for qs in range(4):
                            mm(PS[qs][:, h * 97:(h + 1) * 97], ET[0:127, j, qs * 128:(qs + 1) * 128], VCA[0:127, 0:97], h == 0, True,
                               [("ET", j), "VCA"], [f"ps{qs}"])
                    units.append((sc, ex, pv))
                pipeline(units)
                evac_all(c, 97, 0, True, False)

                def ex_full(sbk, j):
                    act(ET[:, j, :], PS[sbk][:], AF.Exp, [f"ps{sbk}"], [("ET", j)], scale=0.125)
                units = []
                for h in range(4):
                    aq = S_AQ0 + h // 2
                    for kt in range(4 * c + 4):
                        r = 4 * c - kt

                        def sc(sbk, h=h, aq=aq, kt=kt, r=r):
                            mm(PS[sbk][:], Gs(K_SLC + h % 2, kt * 128, (kt + 1) * 128), Gs(aq, ca, cbb), True, False,
                               [("G", K_SLC + h % 2, kt // 4), ("G", aq, c)], [f"ps{sbk}"])
                            mm(PS[sbk][:], CB[:, CB_EXP + kt * 128:CB_EXP + (kt + 1) * 128], SELT[:, :], False, r > 0,
                               CBK + ["SELT"], [f"ps{sbk}"])
                            if r <= 0:
                                mm(PS[sbk][:], IDB, CB[:, CB_CAUS + 384 + 128 * r:CB_CAUS + 384 + 128 * r + 512], False, True,
                                   CBK, [f"ps{sbk}"])

                        def pv(j, h=h, kt=kt):
                            for qs in range(4):
                                if kt > 4 * c + qs:
                                    continue
                                mm(PS[qs][:, h * 65:(h + 1) * 65], ET[:, j, qs * 128:(qs + 1) * 128], VSW[:, kt, 0, :],
                                   h == 0 and kt == 0, True, [("ET", j), "VSW"], [f"ps{qs}"])
                        units.append((sc, ex_full, pv))
                pipeline(units)
                evac_all(c, 65, 1, False, False)
                units = []
                for h in range(4):
                    aq = S_AQ0 + h // 2
                    for kt in range(max(0, 4 * c - 4), 4 * c + 4):
                        r = 4 * c - kt

                        def sc(sbk, h=h, aq=aq, kt=kt, r=r):
                            mm(PS[sbk][:], Gs(K_WIN + h % 2, kt * 128, (kt + 1) * 128), Gs(aq, ca, cbb), True, False,
                               [("G", K_WIN + h % 2, kt // 4), ("G", aq, c)], [f"ps{sbk}"])
                            if r <= 0:
                                msk = CB[:, CB_CAUS + 384 + 128 * r:CB_CAUS + 384 + 128 * r + 512]
                            else:
                                msk = CB[:, CB_WINM + 128 * (r - 1):CB_WINM + 128 * (r - 1) + 512]
                            mm(PS[sbk][:], IDB, msk, False, True, CBK, [f"ps{sbk}"])

                        def pv(j, h=h, kt=kt):
                            for qs in range(4):
                                tq = 4 * c + qs
                                if kt > tq or kt < tq - 4:
                                    continue
                                mm(PS[qs][:, h * 65:(h + 1) * 65], ET[:, j, qs * 128:(qs + 1) * 128], VSW[:, kt, 1, :],
                                   h == 0 and kt == max(0, tq - 4), True, [("ET", j), "VSW"], [f"ps{qs}"])
                        units.append((sc, ex_full, pv))
                pipeline(units)
                evac_all(c, 65, 2, False, True)
            if stop_after == "nsa":
                return
            zpad(S_KS, S_CK, 0); zpad(S_KW, S_CK, 1)
            deferred = []

            def swa_evac(kt):
                ps, pr = PS[kt % 4], f"ps{kt % 4}"
                tt(SM[:, 0:4], ps[:, 64:64 + 3 * 65 + 1:65], SINKE[:], ALU.add, [pr, "SINKE"], [("SM", 0)])
                recip(SM[:, 4:8], SM[:, 0:4], [("SM", 0)], [("SM", 4)])
                for hh in range(4):
                    tsc(OA[:, 0, hh * 64:(hh + 1) * 64], ps[:, hh * 65:hh * 65 + 64], SM[:, 4 + hh:5 + hh], ALU.mult,
                        [pr, ("SM", 4)], [("OA", 0)])
                gn_tm(OA[:, 0, :], [("OA", 0)], GNC, 2, kt)

            units = []
            for kt in range(16):
                nq = 256 if kt < 15 else 128
                q0 = kt * 128
                qres = sorted(set([q0 // 512, (q0 + nq - 1) // 512]))
                for ui, (a_, hf) in enumerate(((0, 0), (0, 1), (1, 0), (1, 1))):
                    h = 2 * hf + a_

                    def sc(sbk, kt=kt, nq=nq, q0=q0, qres=qres, a_=a_, hf=hf):
                        mm(PS[sbk][:, 0:nq], Gs(K_SWA + hf, kt * 128, (kt + 1) * 128), Gs(S_CQ0 + a_, q0, q0 + nq),
                           True, False, [("G", K_SWA + hf, kt // 4)] + [("G", S_CQ0 + a_, t_) for t_ in qres], [f"ps{sbk}"])
                        mm(PS[sbk][:, 0:nq], IDB, CB[:, CB_BAND:CB_BAND + nq], False, True, CBK, [f"ps{sbk}"])

                    def ex(sbk, j, nq=nq):
                        act(ET[:, j, 0:nq], PS[sbk][:, 0:nq], AF.Exp, [f"ps{sbk}"], [("ET", j)], scale=0.125)

                    def pv(j, kt=kt, h=h, hf=hf, ui=ui):
                        b0, b1 = kt % 4, (kt + 1) % 4
                        mm(PS[b0][:, h * 65:(h + 1) * 65], ET[:, j, 0:128], CV[:, kt, hf, :], kt == 0 and ui == 0, True,
                           [("ET", j), "CV"], [f"ps{b0}"])
                        if kt < 15:
                            mm(PS[b1][:, h * 65:(h + 1) * 65], ET[:, j, 128:256], CV[:, kt, hf, :], ui == 0, False,
                               [("ET", j), "CV"], [f"ps{b1}"])
                        if ui == 3:
                            deferred.append(lambda kt=kt: swa_evac(kt))
                    units.append((sc, ex, pv))
            bks = [sbank()]
            units[0][0](bks[0])
            for i in range(len(units)):
                if i + 1 < len(units):
                    bks.append(sbank())
                    units[i + 1][0](bks[i + 1])
                j = etbuf()
                units[i][1](bks[i], j)
                units[i][2](j)
                if i % 4 == 1 and len(deferred) and i > 4:
                    deferred.pop(0)()
            while deferred:
                deferred.pop(0)()
            if stop_after == "swa":
                return
            ysrc = (0, 1, 4, 5, 2, 3, 6, 7)
            for dc in range(8):
                wt, wres = WS.get(("wout", s, l, dc))
                for tc in range(4):
                    ts = slice(tc * 512, (tc + 1) * 512)
                    pb = bank()
                    for kc in range(8):
                        mm(PS[pb][:], wt[:, kc * 128:(kc + 1) * 128], XN[:, ysrc[kc], ts], kc == 0, kc == 7,
                           [wres, ("XN", ysrc[kc], tc)], [f"ps{pb}"])
                    tt(XT[:, dc, ts], PS[pb][:], XT[:, dc, ts], ALU.add, [f"ps{pb}", ("XT", dc, tc)], [("XT", dc, tc)])

        for s in range(nseq):
            for kc in range(8):
                P.dma("sp", XT[:, kc, :], xT[s, :, kc, :],
                      writes=[("XT", kc, tc) for tc in range(4)], semkey=("XT", kc))
            for l in layers:
                if do_ffn:
                    ffn(s, l, 0)
                if do_mixer:
                    mixer(s, l)
                if do_ffn:
                    ffn(s, l, 1)
            for kc in range(8):
                P.dma("sp", yT[s, :, kc, :], XT[:, kc, :],
                      reads=[("XT", kc, tc) for tc in range(4)], semkey=("XT", kc))
        P.finish()
        print("instructions", P.ninst, "waits", P.nwait)
    return nc


def prep_inputs(inp):
    f = lambda a: np.ascontiguousarray(np.asarray(a, dtype=np.float32))
    x = f(inp["x"])
    B = x.shape[0]
    xTh = np.ascontiguousarray(x.reshape(B, T, 8, 128).transpose(0, 3, 2, 1))

    def w13(w):
        w = f(w).reshape(L, 8, 128, NFC, 128)
        return np.ascontiguousarray(w.transpose(0, 3, 2, 1, 4)).reshape(L, NFC, 128, 1024)

    def w2(w):
        w = f(w).reshape(L, NG, GSZ, 128, 8, 128)
        return np.ascontiguousarray(w.transpose(0, 1, 4, 3, 2, 5)).reshape(L, NG, 8, 128, GSZ * 128)

    w1r = np.stack([w13(inp["ffn1_w1"]), w13(inp["ffn2_w1"])], axis=1)
    w3r = np.stack([w13(inp["ffn1_w3"]), w13(inp["ffn2_w3"])], axis=1)
    w2r = np.stack([w2(inp["ffn1_w2"]), w2(inp["ffn2_w2"])], axis=1)
    nrm = np.stack([f(inp["ffn1_norm"]), f(inp["mix_norm"]), f(inp["ffn2_norm"])], axis=1)
    fnorm = np.ascontiguousarray(nrm.reshape(L, 3, 8, 128).transpose(3, 0, 1, 2)).reshape(128, L * 3 * 8)

    o = {}
    off = 0
    for name, sz in (("a_q", 256), ("a_kc", 64), ("a_vc", 64), ("a_ks", 64), ("a_vs", 64), ("a_kw", 64), ("a_vw", 64),
                     ("a_g", 12), ("b_b", 256), ("b_c", 256), ("b_x", 256), ("c_q", 256), ("c_k", 128), ("c_v", 128), ("d_v", 256)):
        o[name] = off
        off += sz
    r = lambda name, a, n: list(range(o[name] + a, o[name] + a + n))
    tiles = [r("b_c", 0, 128), r("b_x", 0, 128), r("b_b", 0, 128), r("b_c", 128, 128), r("b_x", 128, 128), r("b_b", 128, 128),
             r("d_v", 0, 128), r("d_v", 128, 128),
             r("a_q", 0, 128), r("a_q", 128, 128), r("a_ks", 0, 64) * 2, r("a_kw", 0, 64) * 2,
             r("a_kc", 0, 64) + r("a_vc", 0, 64),
             r("c_q", 0, 64) + r("c_q", 128, 64), r("c_q", 64, 64) + r("c_q", 192, 64), r("c_k", 0, 128),
             r("a_vs", 0, 64) + r("a_vw", 0, 64), r("c_v", 0, 128), r("a_g", 0, 12) + [-1] * 116]
    w_in = f(inp["w_in"])
    w_in_p = np.concatenate([w_in, np.zeros((L, D, 1), np.float32)], axis=2)
    winr = np.stack([w_in_p[:, :, cols].reshape(L, 8, 128, 128).transpose(0, 2, 1, 3).reshape(L, 128, 1024)
                     for cols in tiles], axis=1)
    winr = np.ascontiguousarray(winr)
    ck = f(inp["cmp_w1_k"]).reshape(L, 4, 8, 64, 128)
    cvv = f(inp["cmp_w1_v"]).reshape(L, 4, 8, 64, 128)
    cw1r = np.ascontiguousarray(np.concatenate([ck, cvv], axis=3).transpose(0, 1, 3, 2, 4)).reshape(L, 4, 128, 1024)
    woutr = np.ascontiguousarray(f(inp["w_out"]).reshape(L, 8, 128, 8, 128).transpose(0, 3, 2, 1, 4)).reshape(L, 8, 128, 1024)
    prm = np.zeros((L, 128, NPRM), np.float32)
    p64 = np.arange(128) % 64
    qk_gain = {0: "nsa_q_norm", 1: "nsa_q_norm", 2: "nsa_ks_norm", 3: "nsa_kw_norm", 5: "swa_q_norm", 6: "swa_q_norm", 7: "swa_k_norm"}
    for ct, nm in qk_gain.items():
        prm[:, :, ct] = f(inp[nm])[:, p64]
    prm[:, :, 4] = 1.0
    prm[:, :, 8] = f(inp["nsa_kc_norm"])[:, p64]
    cwv = f(inp["conv_w"])
    for cb in range(2):
        for k in range(3):
            prm[:, :, 9 + cb * 3 + k] = cwv[:, k, cb * 128:(cb + 1) * 128]
    psc = f(inp["pool_scale"])
    gnv = f(inp["group_norm"])
    for c in range(2):
        prm[:, :, 15 + c] = psc[:, c * 128:(c + 1) * 128]
        prm[:, :, 17 + c] = gnv[:, 256 + c * 128:256 + (c + 1) * 128]
        prm[:, :, 19 + c] = gnv[:, 768 + c * 128:768 + (c + 1) * 128]
    prm[:, :, 21:25] = f(inp["swa_sinks"])[:, None, :]
    prm[:, :, 25:281] = gnv[:, None, 0:256]
    prm[:, :, 281:537] = gnv[:, None, 512:768]
    prb = np.zeros((L, 128, NPRB), np.float32)
    prb[:, 0:64, 0:32] = f(inp["cmp_pos_k"]).transpose(0, 2, 1)
    prb[:, 64:128, 0:32] = f(inp["cmp_pos_v"]).transpose(0, 2, 1)
    prb[:, :, 32:96] = f(inp["cmp_w2_k"])
    prb[:, :, 96:160] = f(inp["cmp_w2_k"])
    prb[:, :, 160:224] = f(inp["cmp_w2_v"])
    pw = f(inp["pool_w"])
    for c in range(2):
        prb[:, 0:64, 224 + c * 128:224 + c * 128 + 64] = pw[:, 2 * c]
        prb[:, 64:128, 224 + c * 128 + 64:224 + (c + 1) * 128] = pw[:, 2 * c + 1]
    cstf = np.zeros((128, NCF), np.float32)
    cstf[:, 0:128] = 1.0
    pp = np.arange(128)
    cstf[:, 128:256] = (pp[:, None] // 64 == pp[None, :] // 64)
    cstf[:, 256:384] = np.eye(128)
    for c in range(2):
        wp = np.where(pp < 64, POOLWIN[2 * c], POOLWIN[2 * c + 1]).astype(np.float32)
        cstf[:, 384 + c] = 1.0 / wp
        tcol = np.arange(16)[None, :]
        cstf[:, 386 + c * 16:386 + (c + 1) * 16] = wp[:, None] / np.minimum(tcol + 1, wp[:, None])
    cstb = np.zeros((128, NCB), np.float32)
    cstb[:, CB_IDB:CB_IDB + 128] = np.eye(128)
    k = pp[:, None]
    xx = np.arange(896)[None, :]
    cstb[:, CB_CAUS:CB_CAUS + 896] = np.where(xx - 384 - k >= 0, 0.0, NEGB)
    cstb[:, CB_WINM:CB_WINM + 896] = np.where(k - xx + 383 >= 0, 0.0, NEGB)
    xb = np.arange(256)[None, :]
    cstb[:, CB_BAND:CB_BAND + 256] = np.where((xb - k >= 0) & (xb - k < 128), 0.0, NEGB)
    tglob = (np.arange(16)[None, :, None] * 128 + pp[:, None, None])
    jj = np.arange(32)[None, None, :]
    cur = tglob // 64
    forced = (jj == 0) | (jj == cur) | (jj == cur - 1)
    future = jj * 64 > tglob
    cstb[:, CB_SELA:CB_SELA + 512] = np.where(forced | future, 0.0, 1.0).reshape(128, 512)
    cstb[:, CB_SELB:CB_SELB + 512] = np.where(forced, 1e4, np.where(future, -1.0, 0.0)).reshape(128, 512)
    ex = np.zeros((128, 16, 128), np.float32)
    for kt in range(16):
        ex[2 * kt, kt, 0:64] = -NEGB
        ex[2 * kt + 1, kt, 64:128] = -NEGB
    cstb[:, CB_EXP:CB_EXP + 2048] = ex.reshape(128, 2048)
    covl = np.zeros((128, 33), np.float32)
    covl[:, 0] = 1.0
    ci = np.arange(127)[:, None] * 16
    sj = np.arange(32)[None, :] * 64
    covl[0:127, 1:33] = ((ci <= sj + 63) & (ci + 31 >= sj))
    shared = dict(w1r=w1r, w3r=w3r, w2r=w2r, fnorm=fnorm, winr=winr, cw1r=cw1r, woutr=woutr, prm=prm, prb=prb,
                  cstf=cstf, cstb=cstb, covl=covl)
    return xTh, shared


def kernel(**inputs):
    xTh, shared = prep_inputs(inputs)
    nc = build_program()
    in_maps = []
    for c in range(NCORES):
        m = dict(shared)
        m["xT"] = np.ascontiguousarray(xTh[c * SEQ_PER_CORE:(c + 1) * SEQ_PER_CORE])
        in_maps.append(m)
    res = run_bass_kernel_spmd(nc, in_maps, core_ids=list(range(NCORES)))
    yT = np.concatenate([r["yT"] for r in res.results], axis=0)
    out = np.ascontiguousarray(yT.transpose(0, 3, 2, 1)).reshape(-1, T, D)
    return out.astype(np.float32)
```

```python
import numpy as np
from contextlib import ExitStack
import concourse.bass as bass
import concourse.mybir as mybir
from concourse.bass_utils import run_bass_kernel_spmd

F32 = mybir.dt.float32
BF16 = mybir.dt.bfloat16
AF = mybir.ActivationFunctionType
ALU = mybir.AluOpType

NCORES = 8
L = 2
D = 1024
T = 2048
DFF = 2816
NFC = DFF // 128
NG = 2
GSZ = NFC // NG
SEQ_PER_CORE = 2
EPS = 1e-6
SELF_SYNC = True


class Prog:
    EPOCH = 2000

    def __init__(self, nc):
        self.nc = nc
        self.eng = dict(pe=nc.tensor, dve=nc.vector, act=nc.scalar,
                        pool=nc.gpsimd, sp=nc.sync)
        self.sems = {}
        self.epoch = {}
        self.cnt = {}
        self.seen = {k: {} for k in self.eng}
        self.lastw = {}
        self.reads = {}
        self.nwait = 0
        self.ninst = 0

    def _cur(self, base, step):
        ep = self.epoch.get(base, 0)
        sid = (base, ep)
        if sid in self.sems and self.cnt[sid] + step > self.EPOCH:
            ep += 1
            self.epoch[base] = ep
            sid = (base, ep)
        if sid not in self.sems:
            self.sems[sid] = self.nc.alloc_semaphore(name=f"s{len(self.sems)}")
            self.cnt[sid] = 0
        return sid

    def _deps(self, reads, writes):
        deps = []
        for r in reads:
            if r in self.lastw:
                deps.append(self.lastw[r])
        for w in writes:
            if w in self.lastw:
                deps.append(self.lastw[w])
            deps.extend(self.reads.get(w, {}).items())
        return deps

    def _wait(self, e, deps):
        need = {}
        for sid, v in deps:
            if sid[0] == e and (e in ("pe", "sp") or not SELF_SYNC):
                continue
            if self.seen[e].get(sid, 0) >= v:
                continue
            if need.get(sid, 0) < v:
                need[sid] = v
        for sid, v in need.items():
            self.eng[e].wait_ge(self.sems[sid], v)
            self.seen[e][sid] = v
            self.nwait += 1

    def _record(self, ev, reads, writes):
        for r in reads:
            d = self.reads.setdefault(r, {})
            if d.get(ev[0], 0) < ev[1]:
                d[ev[0]] = ev[1]
        for w in writes:
            self.lastw[w] = ev
            self.reads[w] = {}

    def op(self, e, fn, reads=(), writes=()):
        writes = list(writes) + [r for r in reads if isinstance(r, str) and r.startswith("ps")]
        self._wait(e, self._deps(reads, writes))
        inst = fn(self.eng[e])
        sid = self._cur(e, 1)
        self.cnt[sid] += 1
        inst.then_inc(self.sems[sid], 1)
        self.ninst += 1
        self._record((sid, self.cnt[sid]), reads, writes)

    def dma(self, q, out, in_, reads=(), writes=(), semkey=None, **kw):
        self._wait(q, self._deps(reads, writes))
        sid = self._cur(("d", semkey), 16)
        inst = self.eng[q].dma_start(out=out, in_=in_, **kw)
        self.cnt[sid] += 16
        inst.then_inc(self.sems[sid], 16)
        self.ninst += 1
        self._record((sid, self.cnt[sid]), reads, writes)

    def finish(self, e="sp"):
        deps = [(sid, v) for sid, v in self.cnt.items() if v > 0 and sid[0] != e]
        self._wait(e, deps)
        print("semaphores used", len(self.sems))


class WStream:
    def __init__(self, P, ring, nslots, plan, lookahead):
        self.P, self.ring, self.n, self.plan, self.la = P, ring, nslots, plan, lookahead
        self.pos = 0
        self.issued = 0

    def _issue(self, j):
        key, src, width = self.plan[j]
        slot = j % self.n
        self.P.dma("pool", self.ring[:, slot, 0:width], src,
                   writes=[("W", slot)], semkey=("W", slot))

    def get(self, key):
        k, src, width = self.plan[self.pos]
        assert k == key, (k, key)
        hi = min(len(self.plan), self.pos + self.la + 1)
        while self.issued < hi:
            self._issue(self.issued)
            self.issued += 1
        slot = self.pos % self.n
        self.pos += 1
        return self.ring[:, slot, 0:width], ("W", slot)


POOLWIN = (2, 4, 8, 16)
S_AQ0, S_AQ1, S_KS, S_KW, S_KVC, S_CQ0, S_CQ1, S_CK, S_YB0, S_YB1, S_YD0, S_YD1 = range(12)
NPRM = 537
NPRB = 480
NCF = 418
CB_IDB, CB_CAUS, CB_WINM, CB_BAND, CB_SELA, CB_SELB, CB_ZERO, CB_EXP, NCB = 0, 128, 1024, 1920, 2176, 2688, 3200, 3712, 5760
NEGB = -30000.0


def build_program(layers=(0, 1), nseq=SEQ_PER_CORE, do_mixer=True, do_ffn=True, stop_after=None):
    nc = bass.Bass("TRN2", target_bir_lowering=False)
    din = lambda n, s: nc.dram_tensor(n, list(s), F32, kind="ExternalInput").ap()
    xT = din("xT", [nseq, 128, 8, T])
    if do_ffn:
        w1r = din("w1r", [L, 2, NFC, 128, 1024])
        w3r = din("w3r", [L, 2, NFC, 128, 1024])
        w2r = din("w2r", [L, 2, NG, 8, 128, GSZ * 128])
    fnorm = din("fnorm", [128, L * 3 * 8])
    winr = din("winr", [L, 19, 128, 1024])
    cw1r = din("cw1r", [L, 4, 128, 1024])
    woutr = din("woutr", [L, 8, 128, 1024])
    prm = din("prm", [L, 128, NPRM])
    prb = din("prb", [L, 128, NPRB])
    cstf = din("cstf", [128, NCF])
    cstb = din("cstb", [128, NCB])
    covl = din("covl", [128, 33])
    yT = nc.dram_tensor("yT", [nseq, 128, 8, T], F32, kind="ExternalOutput").ap()

    with ExitStack() as es:
        sb = lambda n, s, d: es.enter_context(nc.sbuf_tensor(n, list(s), d))
        XT = sb("XT", [128, 8, T], F32)
        XN = sb("XN", [128, 8, T], BF16)
        Gf = sb("G", [128, 12 * T], BF16)
        NW = 6
        WR = sb("WR", [128, NW, 1024], BF16)
        SQ = sb("SQ", [128, 2, 512], F32)
        RS = sb("RS", [128, 2, 512], F32)
        SL = sb("SL", [128, 2, 512], BF16)
        GN = sb("GN", [128, L * 3 * 8], F32)
        CF = sb("CF", [128, NCF], F32)
        CB = sb("CB", [128, NCB], BF16)
        PRM = sb("PRM", [128, NPRM], F32)
        PRB = sb("PRB", [128, NPRB], BF16)
        EPSB = sb("EPSB", [128, 1], F32)
        VSW = sb("VSW", [128, 16, 2, 65], BF16)
        CV = sb("CV", [128, 16, 2, 65], BF16)
        GT = sb("GT", [128, 16, 12], F32)
        ET = sb("ET", [128, 3, 512], BF16)
        OA = sb("OA", [128, 4, 256], F32)
        YT = sb("YT", [128, 256], F32)
        SM = sb("SM", [128, 64], F32)
        SC = sb("SC", [128, 4, 32], F32)
        SCI = sb("SCI", [128, 4, 32], F32)
        SM2 = sb("SM2", [128, 4, 12], F32)
        SELT = sb("SELT", [128, 512], BF16)
        KCP = sb("KCP", [128, 2, 128], BF16)
        VCA = sb("VCA", [128, 97], BF16)
        HKV = sb("HKV", [128, 2, 128], BF16)
        SINKE = sb("SINKE", [128, 4], F32)
        PS = [es.enter_context(nc.psum_tensor(f"ps{i}", [128, 512], F32)) for i in range(8)]
        ONESF = CF[:, 0:128]
        BD64 = CF[:, 128:256]
        IDF = CF[:, 256:384]
        IDB = CB[:, CB_IDB:CB_IDB + 128]

        P = Prog(nc)

        def Gs(slot, a=0, b=T, p0=0, p1=128):
            return Gf[p0:p1, slot * T + a: slot * T + b]

        def mm(out, lhsT, rhs, start, stop, reads, writes):
            P.op("pe", lambda e: e.matmul(out, lhsT=lhsT, rhs=rhs, start=start, stop=stop, skip_group_check=True),
                 reads, writes)

        def act(out, in_, func, reads, writes, **kw):
            P.op("act", lambda e: e.activation(out=out, in_=in_, func=func, **kw), reads, writes)

        def tt(out, in0, in1, op, reads, writes):
            P.op("dve", lambda e: e.tensor_tensor(out=out, in0=in0, in1=in1, op=op), reads, writes)

        def tsc(out, in0, s1, op0, reads, writes, s2=None, op1=None):
            if op1 is None:
                P.op("dve", lambda e: e.tensor_scalar(out=out, in0=in0, scalar1=s1, scalar2=None, op0=op0), reads, writes)
            else:
                P.op("dve", lambda e: e.tensor_scalar(out=out, in0=in0, scalar1=s1, scalar2=s2, op0=op0, op1=op1), reads, writes)

        def stt(out, in0, scalar, in1, op0, op1, reads, writes):
            P.op("dve", lambda e: e.scalar_tensor_tensor(out=out, in0=in0, scalar=scalar, in1=in1, op0=op0, op1=op1),
                 reads, writes)

        def recip(out, in_, reads, writes):
            P.op("dve", lambda e: e.reciprocal(out=out, in_=in_), reads, writes)

        def cpy(eng, out, in_, reads, writes):
            if eng == "act":
                P.op("act", lambda e: e.copy(out=out, in_=in_), reads, writes)
            else:
                P.op(eng, lambda e: e.tensor_copy(out=out, in_=in_), reads, writes)

        plan = []
        for s in range(nseq):
            for l in layers:
                for fi in range(2):
                    if fi == 1 and do_mixer:
                        for i in range(19):
                            plan.append((("win", s, l, i), winr[l, i], 1024))
                        for q in range(4):
                            plan.append((("cw1", s, l, q), cw1r[l, q], 1024))
                        for dc in range(8):
                            plan.append((("wout", s, l, dc), woutr[l, dc], 1024))
                    if not do_ffn:
                        continue
                    for g in range(NG):
                        for i in range(GSZ):
                            fc = g * GSZ + i
                            plan.append((("w1", s, l, fi, fc), w1r[l, fi, fc], 1024))
                            plan.append((("w3", s, l, fi, fc), w3r[l, fi, fc], 1024))
                        for dc in range(8):
                            plan.append((("w2", s, l, fi, g, dc, 0), w2r[l, fi, g, dc][:, 0:768], 768))
                            plan.append((("w2", s, l, fi, g, dc, 1), w2r[l, fi, g, dc][:, 768:GSZ * 128], GSZ * 128 - 768))
        WS = WStream(P, WR, NW, plan, NW - 3)

        P.dma("sp", GN[:], fnorm, writes=["GN"], semkey="GN")
        P.dma("sp", CF[:], cstf, writes=["CF"], semkey="CF")
        for i in range(0, NCB, 1920):
            P.dma("pool", CB[:, i:i + 1920], cstb[:, i:i + 1920], writes=[("CB", i)], semkey=("CB", i))
        CBK = [("CB", i) for i in range(0, NCB, 1920)]
        P.dma("pool", VCA[:, 64:97], covl, writes=["VCA"], semkey="VCA")
        P.op("dve", lambda e: e.memset(EPSB[:], EPS), writes=["EPSB"])
        P.op("pool", lambda e: e.memset(VSW[:, :, :, 64:65], 1.0), writes=["VSW"])
        P.op("pool", lambda e: e.memset(CV[:, :, :, 64:65], 1.0), writes=["CV"])
        P.op("pool", lambda e: e.memset(SELT[:], 0.0), writes=["SELT"])
        P.op("pool", lambda e: e.memset(KCP[:], 0.0), writes=["KCP"])

        cnt = {"h": 0, "o": 0, "sl": 0, "sq": 0, "pb": 0, "s": 0, "et": 0, "m": 0, "cp": 0, "stg": 0}

        def sqbuf():
            j = cnt["sq"] % 2
            cnt["sq"] += 1
            return j

        def slbuf():
            j = cnt["sl"] % 2
            cnt["sl"] += 1
            return j

        def bank(lo=0, hi=8):
            b = lo + cnt["pb"] % (hi - lo)
            cnt["pb"] += 1
            return b

        def cpeng():
            cnt["cp"] += 1
            return "act" if cnt["cp"] % 2 else "dve"

        def rstd_from(pss_ap, pres, scale, n=512):
            act(RS[:, 0, 0:n], pss_ap, AF.Ln, [pres, "EPSB"], [("RS", 0)], scale=scale, bias=EPSB[:])
            act(RS[:, 1, 0:n], RS[:, 0, 0:n], AF.Exp, [("RS", 0)], [("RS", 1)], scale=-0.5)

        def rmsnorm(l, ni):
            for tc in range(4):
                ts = slice(tc * 512, (tc + 1) * 512)
                pb = bank()
                for kc in range(8):
                    j = sqbuf()
                    act(SQ[:, j, :], XT[:, kc, ts], AF.Square, [("XT", kc, tc)], [("SQ", j)])
                    mm(PS[pb][:], ONESF, SQ[:, j, :], kc == 0, kc == 7, [("SQ", j), "CF"], [f"ps{pb}"])
                rstd_from(PS[pb][:], f"ps{pb}", 1.0 / D)
                for kc in range(8):
                    gi = (l * 3 + ni) * 8 + kc
                    stt(XN[:, kc, ts], XT[:, kc, ts], GN[:, gi:gi + 1], RS[:, 1, :], ALU.mult, ALU.mult,
                        [("XT", kc, tc), ("RS", 1), "GN"], [("XN", kc, tc)])

        def ffn(s, l, fi):
            rmsnorm(l, 0 if fi == 0 else 2)
            for g in range(NG):
                for i in range(GSZ):
                    fc = g * GSZ + i
                    w1t, w1res = WS.get(("w1", s, l, fi, fc))
                    w3t, w3res = WS.get(("w3", s, l, fi, fc))
                    for tc in range(4):
                        ts = slice(tc * 512, (tc + 1) * 512)
                        hb = (cnt["h"] % 2) * 2
                        cnt["h"] += 1
                        p1, p3 = PS[hb], PS[hb + 1]
                        for kc in range(8):
                            mm(p1[:], w1t[:, kc * 128:(kc + 1) * 128], XN[:, kc, ts], kc == 0, kc == 7,
                               [w1res, ("XN", kc, tc)], [f"ps{hb}"])
                        for kc in range(8):
                            mm(p3[:], w3t[:, kc * 128:(kc + 1) * 128], XN[:, kc, ts], kc == 0, kc == 7,
                               [w3res, ("XN", kc, tc)], [f"ps{hb + 1}"])
                        j = slbuf()
                        act(SL[:, j, :], p1[:], AF.Silu, [f"ps{hb}"], [("SL", j)])
                        tt(Gs(i, tc * 512, (tc + 1) * 512), SL[:, j, :], p3[:], ALU.mult,
                           [("SL", j), f"ps{hb + 1}"], [("G", i, tc)])
                for dc in range(8):
                    w2a, w2ares = WS.get(("w2", s, l, fi, g, dc, 0))
                    w2b, w2bres = WS.get(("w2", s, l, fi, g, dc, 1))
                    for tc in range(4):
                        ts = slice(tc * 512, (tc + 1) * 512)
                        ob = 4 + cnt["o"] % 2
                        cnt["o"] += 1
                        po = PS[ob]
                        for i in range(GSZ):
                            w2t, w2res, ii = (w2a, w2ares, i) if i < 6 else (w2b, w2bres, i - 6)
                            mm(po[:], w2t[:, ii * 128:(ii + 1) * 128], Gs(i, tc * 512, (tc + 1) * 512), i == 0, i == GSZ - 1,
                               [w2res, ("G", i, tc)], [f"ps{ob}"])
                        stt(XT[:, dc, ts], po[:], 0.5, XT[:, dc, ts], ALU.mult, ALU.add,
                            [f"ps{ob}", ("XT", dc, tc)], [("XT", dc, tc)])

        ZB = Gf[:, 0:2 * T].bitcast(F32)

        def zkeys(tc):
            return [("G", tc // 2, 2 * (tc % 2)), ("G", tc // 2, 2 * (tc % 2) + 1)]

        def proj_fm(wt, wres, tc, pb):
            ts = slice(tc * 512, (tc + 1) * 512)
            for kc in range(8):
                mm(PS[pb][:], wt[:, kc * 128:(kc + 1) * 128], XN[:, kc, ts], kc == 0, kc == 7,
                   [wres, ("XN", kc, tc)], [f"ps{pb}"])

        def gn_fm(l, slots, gcols, dests):
            for tc in range(4):
                a, b = tc * 512, (tc + 1) * 512
                pb = bank()
                for i, sl in enumerate(slots):
                    j = sqbuf()
                    act(SQ[:, j, :], Gs(sl, a, b), AF.Square, [("G", sl, tc)], [("SQ", j)])
                    mm(PS[pb][:], ONESF, SQ[:, j, :], i == 0, i == 1, [("SQ", j), "CF"], [f"ps{pb}"])
                rstd_from(PS[pb][:], f"ps{pb}", 1.0 / 256)
                for i, sl in enumerate(slots):
                    stt(XN[:, dests[i], a:b], Gs(sl, a, b), PRM[:, gcols[i]:gcols[i] + 1], RS[:, 1, :], ALU.mult, ALU.mult,
                        [("G", sl, tc), ("RS", 1), "PRM"], [("XN", dests[i], tc)])

        def gn_tm(oav, oares, gain, dest, tt_):
            tc = tt_ // 4
            tt(YT[:], oav, oav, ALU.mult, oares, ["YT"])
            P.op("dve", lambda e: e.reduce_sum(out=SM[:, 40:41], in_=YT[:], axis=mybir.AxisListType.X), ["YT"], [("SM", 40)])
            act(SM[:, 41:42], SM[:, 40:41], AF.Ln, [("SM", 40), "EPSB"], [("SM", 41)], scale=1.0 / 256, bias=EPSB[:])
            act(SM[:, 42:43], SM[:, 41:42], AF.Exp, [("SM", 41)], [("SM", 42)], scale=-0.5)
            stt(YT[:], oav, SM[:, 42:43], gain, ALU.mult, ALU.mult, oares + [("SM", 42), "PRM"], ["YT"])
            for i in range(2):
                pb = 6 + cnt["m"] % 2
                cnt["m"] += 1
                P.op("pe", lambda e: e.transpose(out=PS[pb][:, 0:128], in_=YT[:, i * 128:(i + 1) * 128], identity=IDF),
                     ["YT", "CF"], [f"ps{pb}"])
                cpy("dve", XN[:, dest + i, tt_ * 128:(tt_ + 1) * 128], PS[pb][:, 0:128], [f"ps{pb}"], [("XN", dest + i, tc)])

        def mixer(s, l):
            rmsnorm(l, 1)
            P.dma("sp", PRM[:], prm[l], writes=["PRM"], semkey="PRM")
            P.dma("pool", PRB[:], prb[l], writes=["PRB"], semkey="PRB")
            act(SINKE[:], PRM[:, 21:25], AF.Exp, ["PRM"], ["SINKE"])
            POSB = PRB[:, 0:32]
            CW2 = PRB[:, 32:224]
            PW = PRB[:, 224:480]
            wi = 0
            for cb in range(2):
                wc, rc = WS.get(("win", s, l, wi)); wx, rx = WS.get(("win", s, l, wi + 1)); wb, rb = WS.get(("win", s, l, wi + 2))
                wi += 3
                for tc in range(4):
                    a, b = tc * 512, (tc + 1) * 512
                    b1, b2, b3 = bank(), bank(), bank()
                    proj_fm(wc, rc, tc, b1); proj_fm(wx, rx, tc, b2); proj_fm(wb, rb, tc, b3)
                    j = sqbuf()
                    cpy("act", SQ[:, j, :], PS[b1][:], [f"ps{b1}"], [("SQ", j)])
                    tt(ZB[:, a:b], SQ[:, j, :], PS[b2][:], ALU.mult, [("SQ", j), f"ps{b2}"], zkeys(tc))
                    j2 = sqbuf()
                    zk = zkeys(tc) + (zkeys(tc - 1) if tc else [])
                    cw = lambda k: PRM[:, 9 + cb * 3 + k: 10 + cb * 3 + k]
                    tsc(SQ[:, j2, :], ZB[:, a:b], cw(2), ALU.mult, zk + ["PRM"], [("SQ", j2)])
                    for k, sh in ((1, 1), (0, 2)):
                        lo = max(a, sh)
                        stt(SQ[:, j2, lo - a:512], ZB[:, lo - sh:b - sh], cw(k), SQ[:, j2, lo - a:512], ALU.mult, ALU.add,
                            zk + ["PRM", ("SQ", j2)], [("SQ", j2)])
                    tt(Gs(S_YB0 + cb, a, b), SQ[:, j2, :], PS[b3][:], ALU.mult, [("SQ", j2), f"ps{b3}"], [("G", S_YB0 + cb, tc)])
            if stop_after == "B":
                return
            for cd in range(2):
                wd, rd = WS.get(("win", s, l, wi)); wi += 1
                for tc in range(4):
                    a, b = tc * 512, (tc + 1) * 512
                    b1, b2 = bank(), bank()
                    proj_fm(wd, rd, tc, b1)
                    zk = zkeys(tc) + (zkeys(tc - 1) if tc else [])
                    init = 0.0 if tc == 0 else ZB[:, a - 1:a]
                    P.op("dve", lambda e: e.tensor_tensor_scan(out=ZB[:, a:b], data0=CB[:, CB_ZERO:CB_ZERO + 512], data1=PS[b1][:],
                                                               initial=init, op0=ALU.add, op1=ALU.add),
                         [f"ps{b1}"] + CBK + zk, zkeys(tc))
                    j = sqbuf()
                    for half in range(2):
                        w = POOLWIN[2 * cd + half]
                        pr = slice(half * 64, half * 64 + 64)
                        lo = max(a, w)
                        tt(SQ[pr, j, lo - a:512], ZB[pr, lo:b], ZB[pr, lo - w:b - w], ALU.subtract, zk, [("SQ", j)])
                        if tc == 0:
                            cpy("dve", SQ[pr, j, 0:w], ZB[pr, 0:w], zk, [("SQ", j)])
                    if tc == 0:
                        tt(SQ[:, j, 0:16], SQ[:, j, 0:16], CF[:, 386 + cd * 16:386 + cd * 16 + 16], ALU.mult, [("SQ", j), "CF"], [("SQ", j)])
                    js = slbuf()
                    stt(SL[:, js, :], SQ[:, j, :], CF[:, 384 + cd:385 + cd], PS[b1][:], ALU.mult, ALU.subtract,
                        [("SQ", j), "CF", f"ps{b1}"], [("SL", js)])
                    mm(PS[b2][:], PW[:, cd * 128:(cd + 1) * 128], SL[:, js, :], True, True, ["PRB", ("SL", js)], [f"ps{b2}"])
                    tsc(Gs(S_YD0 + cd, a, b), PS[b2][:], PRM[:, 15 + cd:16 + cd], ALU.mult, [f"ps{b2}", "PRM"], [("G", S_YD0 + cd, tc)])
            if stop_after == "D":
                return
            for ct in range(8):
                wt, wres = WS.get(("win", s, l, wi)); wi += 1
                for tc in range(4):
                    a, b = tc * 512, (tc + 1) * 512
                    b1 = bank()
                    proj_fm(wt, wres, tc, b1)
                    if ct == S_KVC:
                        cpy(cpeng(), Gs(ct, a, b), PS[b1][:], [f"ps{b1}"], [("G", ct, tc)])
                        continue
                    j = sqbuf()
                    act(SQ[:, j, :], PS[b1][:], AF.Square, [f"ps{b1}"], [("SQ", j)])
                    b2 = bank()
                    mm(PS[b2][:], BD64, SQ[:, j, :], True, True, [("SQ", j), "CF"], [f"ps{b2}"])
                    rstd_from(PS[b2][:], f"ps{b2}", 1.0 / 64)
                    stt(Gs(ct, a, b), PS[b1][:], PRM[:, ct:ct + 1], RS[:, 1, :], ALU.mult, ALU.mult,
                        [f"ps{b1}", "PRM", ("RS", 1)], [("G", ct, tc)])
            if stop_after == "qk":
                return
            for ti in range(3):
                wt, wres = WS.get(("win", s, l, wi)); wi += 1
                for t16 in range(16):
                    tc = t16 // 4
                    pb = bank()
                    for kc in range(8):
                        mm(PS[pb][:, 0:128], XN[:, kc, t16 * 128:(t16 + 1) * 128], wt[:, kc * 128:(kc + 1) * 128], kc == 0, kc == 7,
                           [wres, ("XN", kc, tc)], [f"ps{pb}"])
                    if ti == 0:
                        cpy("act", VSW[:, t16, 0, 0:64], PS[pb][:, 0:64], [f"ps{pb}"], ["VSW"])
                        cpy("dve", VSW[:, t16, 1, 0:64], PS[pb][:, 64:128], [f"ps{pb}"], ["VSW"])
                    elif ti == 1:
                        cpy("act", CV[:, t16, 0, 0:64], PS[pb][:, 0:64], [f"ps{pb}"], ["CV"])
                        cpy("dve", CV[:, t16, 1, 0:64], PS[pb][:, 64:128], [f"ps{pb}"], ["CV"])
                    else:
                        act(GT[:, t16, :], PS[pb][:, 0:12], AF.Sigmoid, [f"ps{pb}"], ["GT"])
            if stop_after == "tm":
                return
            bA, bB = bank(), bank()
            kvr = [("G", S_KVC, tc) for tc in range(4)]
            for q in range(4):
                wt, wres = WS.get(("cw1", s, l, q))
                for l8 in range(8):
                    ll = 8 * q + l8
                    for pr, bk in ((slice(0, 64), bA), (slice(64, 128), bB)):
                        mm(PS[bk][:, 0:127], wt[pr, l8 * 128:(l8 + 1) * 128], Gf[pr, S_KVC * T + ll: S_KVC * T + ll + 2017: 16],
                           ll == 0, False, [wres] + kvr, [f"ps{bk}"])
                        mm(PS[bk][:, 127:128], wt[pr, l8 * 128:(l8 + 1) * 128], POSB[pr, ll:ll + 1],
                           False, ll == 31, [wres, "PRB"], [f"ps{bk}"])
            for i, bk in enumerate((bA, bB)):
                X, X2, X3 = SQ[:, 0, 0:127], SQ[:, 0, 128:255], SQ[:, 0, 256:383]
                cpy("act", SM[:, 50 + i:51 + i], PS[bk][:, 127:128], [f"ps{bk}"], [("SM", 50 + i)])
                tsc(X, PS[bk][:, 0:127], SM[:, 50 + i:51 + i], ALU.add, [f"ps{bk}", ("SM", 50 + i)], [("SQ", 0)])
                tt(X2, X, X, ALU.mult, [("SQ", 0)], [("SQ", 0)])
                tsc(X2, X2, 0.044715, ALU.mult, [("SQ", 0)], [("SQ", 0)], s2=1.0, op1=ALU.add)
                tt(X2, X2, X, ALU.mult, [("SQ", 0), ("SQ", 0)], [("SQ", 0)])
                act(X3, X2, AF.Sigmoid, [("SQ", 0)], [("SQ", 0)], scale=1.5957691216057308)
                tt(HKV[:, i, 0:127], X, X3, ALU.mult, [("SQ", 0), ("SQ", 0)], [("HKV", i)])
            b1, b2, b3 = bank(), bank(), bank()
            mm(PS[b1][:, 0:127], CW2[:, 0:128], HKV[:, 0, 0:127], True, True, ["PRB", ("HKV", 0)], [f"ps{b1}"])
            act(SQ[:, 1, 0:127], PS[b1][:, 0:127], AF.Square, [f"ps{b1}"], [("SQ", 1)])
            P.op("dve", lambda e: e.memset(SQ[:, 1, 127:128], 1.0), [("SQ", 1)], [("SQ", 1)])
            mm(PS[b2][:, 0:128], BD64, SQ[:, 1, 0:128], True, True, [("SQ", 1), "CF"], [f"ps{b2}"])
            rstd_from(PS[b2][:, 0:128], f"ps{b2}", 1.0 / 64, n=128)
            for hf in range(2):
                pr = slice(hf * 64, hf * 64 + 64)
                stt(KCP[pr, hf, 0:127], PS[b1][pr, 0:127], PRM[pr, 8:9], RS[pr, 1, 0:127], ALU.mult, ALU.mult,
                    [f"ps{b1}", "PRM", ("RS", 1)], ["KCP"])
            mm(PS[b3][0:127, 0:64], HKV[:, 1, 0:127], CW2[:, 128:192], True, True, ["PRB", ("HKV", 1)], [f"ps{b3}"])
            cpy("act", VCA[0:127, 0:64], PS[b3][0:127, 0:64], [f"ps{b3}"], ["VCA"])
            if stop_after == "cmp":
                return
            gn_fm(l, (S_YB0, S_YB1), (17, 18), (4, 5))
            gn_fm(l, (S_YD0, S_YD1), (19, 20), (6, 7))
            if stop_after == "gn":
                return
            def zpad(dst, src, hf):
                pr, po = slice(hf * 64, hf * 64 + 64), slice((1 - hf) * 64, (1 - hf) * 64 + 64)
                allk = lambda sl: [("G", sl, t_) for t_ in range(4)]
                P.op("dve", lambda e: e.memset(Gf[po, dst * T:(dst + 1) * T], 0.0), [], allk(dst))
                P.op("act", lambda e: e.copy(out=Gf[pr, dst * T:(dst + 1) * T], in_=Gf[pr, src * T:(src + 1) * T]),
                     allk(src), allk(dst))
            zpad(S_YB0, S_KS, 0); zpad(S_YB1, S_KS, 1)
            zpad(S_YD0, S_KW, 0); zpad(S_YD1, S_KW, 1)
            K_SLC, K_WIN, K_SWA = S_YB0, S_YD0, S_KS
            GNA = PRM[:, 25:281]
            GNC = PRM[:, 281:537]

            def evac_a(qs, t16, ncol, br, first):
                ps = PS[qs]
                pr = f"ps{qs}"
                S = SM2[:, qs, :]
                sk = ("SM2", qs)
                tsc(S[:, 0:4], ps[:, 64:64 + 3 * ncol + 1:ncol], 1e-30, ALU.max, [pr], [sk])
                recip(S[:, 4:8], S[:, 0:4], [sk], [sk])
                tt(S[:, 8:12], S[:, 4:8], GT[:, t16, br:12:3], ALU.mult, [sk, "GT"], [sk])
                psv = ps[:, 0:4 * ncol].rearrange("p (h c) -> p h c", c=ncol)
                oav = OA[:, qs, :].rearrange("p (h d) -> p h d", d=64)
                facb = S[:, 8:12].unsqueeze(2).broadcast_to([128, 4, 64])
                if first:
                    tt(oav, psv[:, :, 0:64], facb, ALU.mult, [pr, sk], [("OA", qs)])
                else:
                    tt(YT[:].rearrange("p (h d) -> p h d", d=64), psv[:, :, 0:64], facb, ALU.mult, [pr, sk], ["YT"])
                    tt(OA[:, qs, :], OA[:, qs, :], YT[:], ALU.add, ["YT", ("OA", qs)], [("OA", qs)])
                if br == 0:
                    IMP = SCI[:, qs, :]
                    rdb = S[:, 4:8].unsqueeze(2).broadcast_to([128, 4, 32])
                    tt(YT[:, 0:128].rearrange("p (h j) -> p h j", j=32), psv[:, :, 65:97], rdb, ALU.mult, [pr, sk], ["YT"])
                    P.op("dve", lambda e: e.tensor_reduce(out=IMP, in_=YT[:, 0:128].rearrange("p (h j) -> p j h", j=32),
                                                         axis=mybir.AxisListType.X, op=ALU.add), ["YT"], [("IMP", qs)])

            def evac_b(qs, t16, br, last):
                if br == 0:
                    IMP, SCR, SELM, M8 = SCI[:, qs, :], SC[:, 1, :], SC[:, 2, :], SC[:, 3, 0:8]
                    tt(SCR, IMP, CB[:, CB_SELA + t16 * 32:CB_SELA + (t16 + 1) * 32], ALU.mult, [("IMP", qs)] + CBK, ["SCR"])
                    tt(SCR, SCR, CB[:, CB_SELB + t16 * 32:CB_SELB + (t16 + 1) * 32], ALU.add, ["SCR"] + CBK, ["SCR"])
                    P.op("dve", lambda e: e.max(out=M8, in_=SCR), ["SCR"], ["M8"])
                    tsc(SELM, SCR, SC[:, 3, 7:8], ALU.is_ge, ["SCR", "M8"], ["SELM"], s2=-1.0, op1=ALU.add)
                    pb = 6 + cnt["m"] % 2
                    cnt["m"] += 1
                    P.op("pe", lambda e: e.transpose(out=PS[pb][0:32, 0:128], in_=SELM, identity=IDF), ["SELM", "CF"], [f"ps{pb}"])
                    cpy("act", SELT[0:32, qs * 128:(qs + 1) * 128], PS[pb][0:32, 0:128], [f"ps{pb}"], ["SELT"])
                if last:
                    gn_tm(OA[:, qs, :], [("OA", qs)], GNA, 0, t16)

            def evac_all(c, ncol, br, first, last):
                for qs in range(4):
                    evac_a(qs, 4 * c + qs, ncol, br, first)
                for qs in range(4):
                    evac_b(qs, 4 * c + qs, br, last)

            def sbank():
                b = 4 + cnt["s"] % 2
                cnt["s"] += 1
                return b

            def etbuf():
                j = cnt["et"] % 3
                cnt["et"] += 1
                return j

            for c in range(4):
                ca, cbb = c * 512, (c + 1) * 512
                def pipeline(units):
                    n = len(units)
                    bks = [sbank()]
                    units[0][0](bks[0])
                    for i in range(n):
                        if i + 1 < n:
                            bks.append(sbank())
                            units[i + 1][0](bks[i + 1])
                        j = etbuf()
                        units[i][1](bks[i], j)
                        units[i][2](j)

                units = []
                for h in range(4):
                    aq = S_AQ0 + h // 2

                    def sc(sbk, h=h, aq=aq):
                        mm(PS[sbk][0:127, :], KCP[:, h % 2, 0:127], Gs(aq, ca, cbb), True, True,
                           ["KCP", ("G", aq, c)], [f"ps{sbk}"])

                    def ex(sbk, j):
                        act(ET[0:127, j, :], PS[sbk][0:127, :], AF.Exp, [f"ps{sbk}"], [("ET", j)], scale=0.125)
                        P.op("pool", lambda e: e.affine_select(out=ET[0:127, j, :], in_=ET[0:127, j, :], pattern=[[1, 512]],
                                                               compare_op=ALU.is_ge, fill=0.0, base=512 * c - 31, channel_multiplier=-16),
                             [("ET", j)], [("ET", j)])

                    def pv(j, h=h):
                        for qs in range(4):
                            mm(PS[qs][:, h * 97:(h + 1) * 97], ET[0:127, j, qs * 128:(qs + 1) * 128], VCA[0:127, 0:97], h == 0, True,
                               [("ET", j), "VCA"], [f"ps{qs}"])
                    units.append((sc, ex, pv))
                pipeline(units)
                evac_all(c, 97, 0, True, False)

                def ex_full(sbk, j):
                    act(ET[:, j, :], PS[sbk][:], AF.Exp, [f"ps{sbk}"], [("ET", j)], scale=0.125)
                units = []
                for h in range(4):
                    aq = S_AQ0 + h // 2
                    for kt in range(4 * c + 4):
                        r = 4 * c - kt

                        def sc(sbk, h=h, aq=aq, kt=kt, r=r):
                            mm(PS[sbk][:], Gs(K_SLC + h % 2, kt * 128, (kt + 1) * 128), Gs(aq, ca, cbb), True, False,
                               [("G", K_SLC + h % 2, kt // 4), ("G", aq, c)], [f"ps{sbk}"])
                            mm(PS[sbk][:], CB[:, CB_EXP + kt * 128:CB_EXP + (kt + 1) * 128], SELT[:, :], False, r > 0,
                               CBK + ["SELT"], [f"ps{sbk}"])
                            if r <= 0:
                                mm(PS[sbk][:], IDB, CB[:, CB_CAUS + 384 + 128 * r:CB_CAUS + 384 + 128 * r + 512], False, True,
                                   CBK, [f"ps{sbk}"])

                        def pv(j, h=h, kt=kt):
                            for qs in range(4):
                                if kt > 4 * c + qs:
                                    continue
                                mm(PS[qs][:, h * 65:(h + 1) * 65], ET[:, j, qs * 128:(qs + 1) * 128], VSW[:, kt, 0, :],
                                   h == 0 and kt == 0, True, [("ET", j), "VSW"], [f"ps{qs}"])
                        units.append((sc, ex_full, pv))
                pipeline(units)
                evac_all(c, 65, 1, False, False)
                units = []
                for h in range(4):
                    aq = S_AQ0 + h // 2
                    for kt in range(max(0, 4 * c - 4), 4 * c + 4):
                        r = 4 * c - kt

                        def sc(sbk, h=h, aq=aq, kt=kt, r=r):
                            mm(PS[sbk][:], Gs(K_WIN + h % 2, kt * 128, (kt + 1) * 128), Gs(aq, ca, cbb), True, False,
                               [("G", K_WIN + h % 2, kt // 4), ("G", aq, c)], [f"ps{sbk}"])
                            if r <= 0:
                                msk = CB[:, CB_CAUS + 384 + 128 * r:CB_CAUS + 384 + 128 * r + 512]
                            else:
                                msk = CB[:, CB_WINM + 128 * (r - 1):CB_WINM + 128 * (r - 1) + 512]
                            mm(PS[sbk][:], IDB, msk, False, True, CBK, [f"ps{sbk}"])

                        def pv(j, h=h, kt=kt):
                            for qs in range(4):
                                tq = 4 * c + qs
                                if kt > tq or kt < tq - 4:
                                    continue
                                mm(PS[qs][:, h * 65:(h + 1) * 65], ET[:, j, qs * 128:(qs + 1) * 128], VSW[:, kt, 1, :],
                                   h == 0 and kt == max(0, tq - 4), True, [("ET", j), "VSW"], [f"ps{qs}"])
                        units.append((sc, ex_full, pv))
                pipeline(units)
                evac_all(c, 65, 2, False, True)
            if stop_after == "nsa":
                return
            zpad(S_KS, S_CK, 0); zpad(S_KW, S_CK, 1)
            deferred = []

            def swa_evac(kt):
                ps, pr = PS[kt % 4], f"ps{kt % 4}"
                tt(SM[:, 0:4], ps[:, 64:64 + 3 * 65 + 1:65], SINKE[:], ALU.add, [pr, "SINKE"], [("SM", 0)])
                recip(SM[:, 4:8], SM[:, 0:4], [("SM", 0)], [("SM", 4)])
                tt(OA[:, 0, :].rearrange("p (h d) -> p h d", d=64), ps[:, 0:260].rearrange("p (h c) -> p h c", c=65)[:, :, 0:64],
                   SM[:, 4:8].unsqueeze(2).broadcast_to([128, 4, 64]), ALU.mult, [pr, ("SM", 4)], [("OA", 0)])
                gn_tm(OA[:, 0, :], [("OA", 0)], GNC, 2, kt)

            units = []
            for kt in range(16):
                nq = 256 if kt < 15 else 128
                q0 = kt * 128
                qres = sorted(set([q0 // 512, (q0 + nq - 1) // 512]))
                for ui, (a_, hf) in enumerate(((0, 0), (0, 1), (1, 0), (1, 1))):
                    h = 2 * hf + a_

                    def sc(sbk, kt=kt, nq=nq, q0=q0, qres=qres, a_=a_, hf=hf):
                        mm(PS[sbk][:, 0:nq], Gs(K_SWA + hf, kt * 128, (kt + 1) * 128), Gs(S_CQ0 + a_, q0, q0 + nq),
                           True, False, [("G", K_SWA + hf, kt // 4)] + [("G", S_CQ0 + a_, t_) for t_ in qres], [f"ps{sbk}"])
                        mm(PS[sbk][:, 0:nq], IDB, CB[:, CB_BAND:CB_BAND + nq], False, True, CBK, [f"ps{sbk}"])

                    def ex(sbk, j, nq=nq):
                        act(ET[:, j, 0:nq], PS[sbk][:, 0:nq], AF.Exp, [f"ps{sbk}"], [("ET", j)], scale=0.125)

                    def pv(j, kt=kt, h=h, hf=hf, ui=ui):
                        b0, b1 = kt % 4, (kt + 1) % 4
                        mm(PS[b0][:, h * 65:(h + 1) * 65], ET[:, j, 0:128], CV[:, kt, hf, :], kt == 0 and ui == 0, True,
                           [("ET", j), "CV"], [f"ps{b0}"])
                        if kt < 15:
                            mm(PS[b1][:, h * 65:(h + 1) * 65], ET[:, j, 128:256], CV[:, kt, hf, :], ui == 0, False,
                               [("ET", j), "CV"], [f"ps{b1}"])
                        if ui == 3:
                            deferred.append(lambda kt=kt: swa_evac(kt))
                    units.append((sc, ex, pv))
            bks = [sbank()]
            units[0][0](bks[0])
            for i in range(len(units)):
                if i + 1 < len(units):
                    bks.append(sbank())
                    units[i + 1][0](bks[i + 1])
                j = etbuf()
                units[i][1](bks[i], j)
                units[i][2](j)
                if i % 4 == 1 and len(deferred) and i > 8:
                    deferred.pop(0)()
            while deferred:
                deferred.pop(0)()
            if stop_after == "swa":
                return
            ysrc = (0, 1, 4, 5, 2, 3, 6, 7)
            for dc in range(8):
                wt, wres = WS.get(("wout", s, l, dc))
                for tc in range(4):
                    ts = slice(tc * 512, (tc + 1) * 512)
                    pb = bank()
                    for kc in range(8):
                        mm(PS[pb][:], wt[:, kc * 128:(kc + 1) * 128], XN[:, ysrc[kc], ts], kc == 0, kc == 7,
                           [wres, ("XN", ysrc[kc], tc)], [f"ps{pb}"])
                    tt(XT[:, dc, ts], PS[pb][:], XT[:, dc, ts], ALU.add, [f"ps{pb}", ("XT", dc, tc)], [("XT", dc, tc)])

        for s in range(nseq):
            for kc in range(8):
                P.dma("sp", XT[:, kc, :], xT[s, :, kc, :],
                      writes=[("XT", kc, tc) for tc in range(4)], semkey=("XT", kc))
            for l in layers:
                if do_ffn:
                    ffn(s, l, 0)
                if do_mixer:
                    mixer(s, l)
                if do_ffn:
                    ffn(s, l, 1)
            for kc in range(8):
                P.dma("sp", yT[s, :, kc, :], XT[:, kc, :],
                      reads=[("XT", kc, tc) for tc in range(4)], semkey=("XT", kc))
        P.finish()
        print("instructions", P.ninst, "waits", P.nwait)
    return nc


def prep_inputs(inp):
    f = lambda a: np.ascontiguousarray(np.asarray(a, dtype=np.float32))
    x = f(inp["x"])
    B = x.shape[0]
    xTh = np.ascontiguousarray(x.reshape(B, T, 8, 128).transpose(0, 3, 2, 1))

    def w13(w):
        w = f(w).reshape(L, 8, 128, NFC, 128)
        return np.ascontiguousarray(w.transpose(0, 3, 2, 1, 4)).reshape(L, NFC, 128, 1024)

    def w2(w):
        w = f(w).reshape(L, NG, GSZ, 128, 8, 128)
        return np.ascontiguousarray(w.transpose(0, 1, 4, 3, 2, 5)).reshape(L, NG, 8, 128, GSZ * 128)

    w1r = np.stack([w13(inp["ffn1_w1"]), w13(inp["ffn2_w1"])], axis=1)
    w3r = np.stack([w13(inp["ffn1_w3"]), w13(inp["ffn2_w3"])], axis=1)
    w2r = np.stack([w2(inp["ffn1_w2"]), w2(inp["ffn2_w2"])], axis=1)
    nrm = np.stack([f(inp["ffn1_norm"]), f(inp["mix_norm"]), f(inp["ffn2_norm"])], axis=1)
    fnorm = np.ascontiguousarray(nrm.reshape(L, 3, 8, 128).transpose(3, 0, 1, 2)).reshape(128, L * 3 * 8)

    o = {}
    off = 0
    for name, sz in (("a_q", 256), ("a_kc", 64), ("a_vc", 64), ("a_ks", 64), ("a_vs", 64), ("a_kw", 64), ("a_vw", 64),
                     ("a_g", 12), ("b_b", 256), ("b_c", 256), ("b_x", 256), ("c_q", 256), ("c_k", 128), ("c_v", 128), ("d_v", 256)):
        o[name] = off
        off += sz
    r = lambda name, a, n: list(range(o[name] + a, o[name] + a + n))
    tiles = [r("b_c", 0, 128), r("b_x", 0, 128), r("b_b", 0, 128), r("b_c", 128, 128), r("b_x", 128, 128), r("b_b", 128, 128),
             r("d_v", 0, 128), r("d_v", 128, 128),
             r("a_q", 0, 128), r("a_q", 128, 128), r("a_ks", 0, 64) * 2, r("a_kw", 0, 64) * 2,
             r("a_kc", 0, 64) + r("a_vc", 0, 64),
             r("c_q", 0, 64) + r("c_q", 128, 64), r("c_q", 64, 64) + r("c_q", 192, 64), r("c_k", 0, 128),
             r("a_vs", 0, 64) + r("a_vw", 0, 64), r("c_v", 0, 128), r("a_g", 0, 12) + [-1] * 116]
    w_in = f(inp["w_in"])
    w_in_p = np.concatenate([w_in, np.zeros((L, D, 1), np.float32)], axis=2)
    winr = np.stack([w_in_p[:, :, cols].reshape(L, 8, 128, 128).transpose(0, 2, 1, 3).reshape(L, 128, 1024)
                     for cols in tiles], axis=1)
    winr = np.ascontiguousarray(winr)
    ck = f(inp["cmp_w1_k"]).reshape(L, 4, 8, 64, 128)
    cvv = f(inp["cmp_w1_v"]).reshape(L, 4, 8, 64, 128)
    cw1r = np.ascontiguousarray(np.concatenate([ck, cvv], axis=3).transpose(0, 1, 3, 2, 4)).reshape(L, 4, 128, 1024)
    woutr = np.ascontiguousarray(f(inp["w_out"]).reshape(L, 8, 128, 8, 128).transpose(0, 3, 2, 1, 4)).reshape(L, 8, 128, 1024)
    prm = np.zeros((L, 128, NPRM), np.float32)
    p64 = np.arange(128) % 64
    qk_gain = {0: "nsa_q_norm", 1: "nsa_q_norm", 2: "nsa_ks_norm", 3: "nsa_kw_norm", 5: "swa_q_norm", 6: "swa_q_norm", 7: "swa_k_norm"}
    for ct, nm in qk_gain.items():
        prm[:, :, ct] = f(inp[nm])[:, p64]
    prm[:, :, 4] = 1.0
    prm[:, :, 8] = f(inp["nsa_kc_norm"])[:, p64]
    cwv = f(inp["conv_w"])
    for cb in range(2):
        for k in range(3):
            prm[:, :, 9 + cb * 3 + k] = cwv[:, k, cb * 128:(cb + 1) * 128]
    psc = f(inp["pool_scale"])
    gnv = f(inp["group_norm"])
    for c in range(2):
        prm[:, :, 15 + c] = psc[:, c * 128:(c + 1) * 128]
        prm[:, :, 17 + c] = gnv[:, 256 + c * 128:256 + (c + 1) * 128]
        prm[:, :, 19 + c] = gnv[:, 768 + c * 128:768 + (c + 1) * 128]
    prm[:, :, 21:25] = f(inp["swa_sinks"])[:, None, :]
    prm[:, :, 25:281] = gnv[:, None, 0:256]
    prm[:, :, 281:537] = gnv[:, None, 512:768]
    prb = np.zeros((L, 128, NPRB), np.float32)
    prb[:, 0:64, 0:32] = f(inp["cmp_pos_k"]).transpose(0, 2, 1)
    prb[:, 64:128, 0:32] = f(inp["cmp_pos_v"]).transpose(0, 2, 1)
    prb[:, :, 32:96] = f(inp["cmp_w2_k"])
    prb[:, :, 96:160] = f(inp["cmp_w2_k"])
    prb[:, :, 160:224] = f(inp["cmp_w2_v"])
    pw = f(inp["pool_w"])
    for c in range(2):
        prb[:, 0:64, 224 + c * 128:224 + c * 128 + 64] = pw[:, 2 * c]
        prb[:, 64:128, 224 + c * 128 + 64:224 + (c + 1) * 128] = pw[:, 2 * c + 1]
    cstf = np.zeros((128, NCF), np.float32)
    cstf[:, 0:128] = 1.0
    pp = np.arange(128)
    cstf[:, 128:256] = (pp[:, None] // 64 == pp[None, :] // 64)
    cstf[:, 256:384] = np.eye(128)
    for c in range(2):
        wp = np.where(pp < 64, POOLWIN[2 * c], POOLWIN[2 * c + 1]).astype(np.float32)
        cstf[:, 384 + c] = 1.0 / wp
        tcol = np.arange(16)[None, :]
        cstf[:, 386 + c * 16:386 + (c + 1) * 16] = wp[:, None] / np.minimum(tcol + 1, wp[:, None])
    cstb = np.zeros((128, NCB), np.float32)
    cstb[:, CB_IDB:CB_IDB + 128] = np.eye(128)
    k = pp[:, None]
    xx = np.arange(896)[None, :]
    cstb[:, CB_CAUS:CB_CAUS + 896] = np.where(xx - 384 - k >= 0, 0.0, NEGB)
    cstb[:, CB_WINM:CB_WINM + 896] = np.where(k - xx + 383 >= 0, 0.0, NEGB)
    xb = np.arange(256)[None, :]
    cstb[:, CB_BAND:CB_BAND + 256] = np.where((xb - k >= 0) & (xb - k < 128), 0.0, NEGB)
    tglob = (np.arange(16)[None, :, None] * 128 + pp[:, None, None])
    jj = np.arange(32)[None, None, :]
    cur = tglob // 64
    forced = (jj == 0) | (jj == cur) | (jj == cur - 1)
    future = jj * 64 > tglob
    cstb[:, CB_SELA:CB_SELA + 512] = np.where(forced | future, 0.0, 1.0).reshape(128, 512)
    cstb[:, CB_SELB:CB_SELB + 512] = np.where(forced, 1e4, np.where(future, -1.0, 0.0)).reshape(128, 512)
    ex = np.zeros((128, 16, 128), np.float32)
    for kt in range(16):
        ex[2 * kt, kt, 0:64] = -NEGB
        ex[2 * kt + 1, kt, 64:128] = -NEGB
    cstb[:, CB_EXP:CB_EXP + 2048] = ex.reshape(128, 2048)
    covl = np.zeros((128, 33), np.float32)
    covl[:, 0] = 1.0
    ci = np.arange(127)[:, None] * 16
    sj = np.arange(32)[None, :] * 64
    covl[0:127, 1:33] = ((ci <= sj + 63) & (ci + 31 >= sj))
    shared = dict(w1r=w1r, w3r=w3r, w2r=w2r, fnorm=fnorm, winr=winr, cw1r=cw1r, woutr=woutr, prm=prm, prb=prb,
                  cstf=cstf, cstb=cstb, covl=covl)
    return xTh, shared


def kernel(**inputs):
    xTh, shared = prep_inputs(inputs)
    nc = build_program()
    in_maps = []
    for c in range(NCORES):
        m = dict(shared)
        m["xT"] = np.ascontiguousarray(xTh[c * SEQ_PER_CORE:(c + 1) * SEQ_PER_CORE])
        in_maps.append(m)
    res = run_bass_kernel_spmd(nc, in_maps, core_ids=list(range(NCORES)))
    yT = np.concatenate([r["yT"] for r in res.results], axis=0)
    out = np.ascontiguousarray(yT.transpose(0, 3, 2, 1)).reshape(-1, T, D)
    return out.astype(np.float32)
```

```python
import numpy as np
from contextlib import ExitStack
import concourse.bass as bass
import concourse.mybir as mybir
from concourse.bass_utils import run_bass_kernel_spmd

F32 = mybir.dt.float32
BF16 = mybir.dt.bfloat16
AF = mybir.ActivationFunctionType
ALU = mybir.AluOpType

NCORES = 8
L = 2
D = 1024
T = 2048
DFF = 2816
NFC = DFF // 128
NG = 2
GSZ = NFC // NG
SEQ_PER_CORE = 2
EPS = 1e-6
SELF_SYNC = True


class Prog:
    EPOCH = 2000

    def __init__(self, nc):
        self.nc = nc
        self.eng = dict(pe=nc.tensor, dve=nc.vector, act=nc.scalar,
                        pool=nc.gpsimd, sp=nc.sync)
        self.sems = {}
        self.epoch = {}
        self.cnt = {}
        self.seen = {k: {} for k in self.eng}
        self.lastw = {}
        self.reads = {}
        self.nwait = 0
        self.ninst = 0

    def _cur(self, base, step):
        ep = self.epoch.get(base, 0)
        sid = (base, ep)
        if sid in self.sems and self.cnt[sid] + step > self.EPOCH:
            ep += 1
            self.epoch[base] = ep
            sid = (base, ep)
        if sid not in self.sems:
            self.sems[sid] = self.nc.alloc_semaphore(name=f"s{len(self.sems)}")
            self.cnt[sid] = 0
        return sid

    def _deps(self, reads, writes):
        deps = []
        for r in reads:
            if r in self.lastw:
                deps.append(self.lastw[r])
        for w in writes:
            if w in self.lastw:
                deps.append(self.lastw[w])
            deps.extend(self.reads.get(w, {}).items())
        return deps

    def _wait(self, e, deps):
        need = {}
        for sid, v in deps:
            if sid[0] == e and (e in ("pe", "sp") or not SELF_SYNC):
                continue
            if self.seen[e].get(sid, 0) >= v:
                continue
            if need.get(sid, 0) < v:
                need[sid] = v
        for sid, v in need.items():
            self.eng[e].wait_ge(self.sems[sid], v)
            self.seen[e][sid] = v
            self.nwait += 1

    def _record(self, ev, reads, writes):
        for r in reads:
            d = self.reads.setdefault(r, {})
            if d.get(ev[0], 0) < ev[1]:
                d[ev[0]] = ev[1]
        for w in writes:
            self.lastw[w] = ev
            self.reads[w] = {}

    def op(self, e, fn, reads=(), writes=()):
        writes = list(writes) + [r for r in reads if isinstance(r, str) and r.startswith("ps")]
        self._wait(e, self._deps(reads, writes))
        inst = fn(self.eng[e])
        sid = self._cur(e, 1)
        self.cnt[sid] += 1
        inst.then_inc(self.sems[sid], 1)
        self.ninst += 1
        self._record((sid, self.cnt[sid]), reads, writes)

    def dma(self, q, out, in_, reads=(), writes=(), semkey=None, **kw):
        self._wait(q, self._deps(reads, writes))
        sid = self._cur(("d", semkey), 16)
        inst = self.eng[q].dma_start(out=out, in_=in_, **kw)
        self.cnt[sid] += 16
        inst.then_inc(self.sems[sid], 16)
        self.ninst += 1
        self._record((sid, self.cnt[sid]), reads, writes)

    def finish(self, e="sp"):
        deps = [(sid, v) for sid, v in self.cnt.items() if v > 0 and sid[0] != e]
        self._wait(e, deps)
        print("semaphores used", len(self.sems))


class WStream:
    def __init__(self, P, ring, nslots, plan, lookahead):
        self.P, self.ring, self.n, self.plan, self.la = P, ring, nslots, plan, lookahead
        self.pos = 0
        self.issued = 0

    def _issue(self, j):
        key, src, width = self.plan[j]
        slot = j % self.n
        self.P.dma("pool", self.ring[:, slot, 0:width], src,
                   writes=[("W", slot)], semkey=("W", slot))

    def get(self, key):
        k, src, width = self.plan[self.pos]
        assert k == key, (k, key)
        hi = min(len(self.plan), self.pos + self.la + 1)
        while self.issued < hi:
            self._issue(self.issued)
            self.issued += 1
        slot = self.pos % self.n
        self.pos += 1
        return self.ring[:, slot, 0:width], ("W", slot)


POOLWIN = (2, 4, 8, 16)
S_AQ0, S_AQ1, S_KS, S_KW, S_KVC, S_CQ0, S_CQ1, S_CK, S_YB0, S_YB1, S_YD0, S_YD1 = range(12)
NPRM = 537
NPRB = 480
NCF = 162
CB_IDB, CB_CAUS, CB_WINM, CB_BAND, CB_SELA, CB_SELB, CB_ZERO, CB_EXP, CB_ONES, CB_BD64, NCB = 0, 128, 1024, 1920, 2176, 2688, 3200, 3712, 5760, 5888, 6016
NEGB = -30000.0


def build_program(layers=(0, 1), nseq=SEQ_PER_CORE, do_mixer=True, do_ffn=True, stop_after=None):
    nc = bass.Bass("TRN2", target_bir_lowering=False)
    din = lambda n, s: nc.dram_tensor(n, list(s), F32, kind="ExternalInput").ap()
    xT = din("xT", [nseq, 128, 8, T])
    if do_ffn:
        w1r = din("w1r", [L, 2, NFC, 128, 1024])
        w3r = din("w3r", [L, 2, NFC, 128, 1024])
        w2r = din("w2r", [L, 2, NG, 8, 128, GSZ * 128])
    fnorm = din("fnorm", [128, L * 3 * 8])
    winr = din("winr", [L, 19, 128, 1024])
    cw1r = din("cw1r", [L, 4, 128, 1024])
    woutr = din("woutr", [L, 8, 128, 1024])
    prm = din("prm", [L, 128, NPRM])
    prb = din("prb", [L, 128, NPRB])
    cstf = din("cstf", [128, NCF])
    cstb = din("cstb", [128, NCB])
    covl = din("covl", [128, 33])
    yT = nc.dram_tensor("yT", [nseq, 128, 8, T], F32, kind="ExternalOutput").ap()

    with ExitStack() as es:
        sb = lambda n, s, d: es.enter_context(nc.sbuf_tensor(n, list(s), d))
        XT = sb("XT", [128, 8, T], F32)
        XN = sb("XN", [128, 8, T], BF16)
        Gf = sb("G", [128, 12 * T], BF16)
        NW = 6
        WR = sb("WR", [128, NW, 1024], BF16)
        SQ = sb("SQ", [128, 2, 512], F32)
        RS = sb("RS", [128, 1, 512], F32)
        SL = sb("SL", [128, 2, 512], BF16)
        SQB = sb("SQB", [128, 2, 512], BF16)
        GN = sb("GN", [128, L * 3 * 8], F32)
        CF = sb("CF", [128, NCF], F32)
        CB = sb("CB", [128, NCB], BF16)
        PRM = sb("PRM", [128, NPRM], F32)
        PRB = sb("PRB", [128, NPRB], BF16)
        EPSB = sb("EPSB", [128, 1], F32)
        VSW = sb("VSW", [128, 16, 2, 65], BF16)
        CV = sb("CV", [128, 16, 2, 65], BF16)
        GT = sb("GT", [128, 16, 12], F32)
        ET = sb("ET", [128, 3, 512], BF16)
        OA = sb("OA", [128, 2, 4, 256], F32)
        YT = sb("YT", [128, 256], F32)
        SM = sb("SM", [128, 64], F32)
        SC = sb("SC", [128, 4, 32], F32)
        SCI = sb("SCI", [128, 4, 32], F32)
        SM2 = sb("SM2", [128, 4, 12], F32)
        SELT = sb("SELT", [128, 512], BF16)
        KCP = sb("KCP", [128, 2, 128], BF16)
        VCA = sb("VCA", [128, 97], BF16)
        HKV = sb("HKV", [128, 2, 128], BF16)
        SINKE = sb("SINKE", [128, 4], F32)
        PS = [es.enter_context(nc.psum_tensor(f"ps{i}", [128, 512], F32)) for i in range(8)]
        IDF = CF[:, 0:128]
        IDB = CB[:, CB_IDB:CB_IDB + 128]
        ONESB = CB[:, CB_ONES:CB_ONES + 128]
        BD64B = CB[:, CB_BD64:CB_BD64 + 128]

        P = Prog(nc)

        def Gs(slot, a=0, b=T, p0=0, p1=128):
            return Gf[p0:p1, slot * T + a: slot * T + b]

        def mm(out, lhsT, rhs, start, stop, reads, writes):
            P.op("pe", lambda e: e.matmul(out, lhsT=lhsT, rhs=rhs, start=start, stop=stop, skip_group_check=True),
                 reads, writes)

        def act(out, in_, func, reads, writes, **kw):
            P.op("act", lambda e: e.activation(out=out, in_=in_, func=func, **kw), reads, writes)

        def tt(out, in0, in1, op, reads, writes):
            P.op("dve", lambda e: e.tensor_tensor(out=out, in0=in0, in1=in1, op=op), reads, writes)

        def tsc(out, in0, s1, op0, reads, writes, s2=None, op1=None):
            if op1 is None:
                P.op("dve", lambda e: e.tensor_scalar(out=out, in0=in0, scalar1=s1, scalar2=None, op0=op0), reads, writes)
            else:
                P.op("dve", lambda e: e.tensor_scalar(out=out, in0=in0, scalar1=s1, scalar2=s2, op0=op0, op1=op1), reads, writes)

        def stt(out, in0, scalar, in1, op0, op1, reads, writes):
            P.op("dve", lambda e: e.scalar_tensor_tensor(out=out, in0=in0, scalar=scalar, in1=in1, op0=op0, op1=op1),
                 reads, writes)

        def recip(out, in_, reads, writes):
            P.op("dve", lambda e: e.reciprocal(out=out, in_=in_), reads, writes)

        def cpy(eng, out, in_, reads, writes):
            if eng == "act":
                P.op("act", lambda e: e.copy(out=out, in_=in_), reads, writes)
            else:
                P.op(eng, lambda e: e.tensor_copy(out=out, in_=in_), reads, writes)

        plan = []
        for s in range(nseq):
            for l in layers:
                for fi in range(2):
                    if fi == 1 and do_mixer:
                        for i in range(19):
                            plan.append((("win", s, l, i), winr[l, i], 1024))
                        for q in range(4):
                            plan.append((("cw1", s, l, q), cw1r[l, q], 1024))
                        for dc in range(8):
                            plan.append((("wout", s, l, dc), woutr[l, dc], 1024))
                    if not do_ffn:
                        continue
                    for g in range(NG):
                        for i in range(GSZ):
                            fc = g * GSZ + i
                            plan.append((("w1", s, l, fi, fc), w1r[l, fi, fc], 1024))
                            plan.append((("w3", s, l, fi, fc), w3r[l, fi, fc], 1024))
                        for dc in range(8):
                            plan.append((("w2", s, l, fi, g, dc, 0), w2r[l, fi, g, dc][:, 0:768], 768))
                            plan.append((("w2", s, l, fi, g, dc, 1), w2r[l, fi, g, dc][:, 768:GSZ * 128], GSZ * 128 - 768))
        WS = WStream(P, WR, NW, plan, NW - 3)

        P.dma("sp", GN[:], fnorm, writes=["GN"], semkey="GN")
        P.dma("sp", CF[:], cstf, writes=["CF"], semkey="CF")
        for i in range(0, NCB, 1920):
            P.dma("pool", CB[:, i:min(NCB, i + 1920)], cstb[:, i:min(NCB, i + 1920)], writes=[("CB", i)], semkey=("CB", i))
        CBK = [("CB", i) for i in range(0, NCB, 1920)]
        P.dma("pool", VCA[:, 64:97], covl, writes=["VCA"], semkey="VCA")
        P.op("dve", lambda e: e.memset(EPSB[:], EPS), writes=["EPSB"])
        P.op("pool", lambda e: e.memset(VSW[:, :, :, 64:65], 1.0), writes=["VSW"])
        P.op("pool", lambda e: e.memset(CV[:, :, :, 64:65], 1.0), writes=["CV"])
        P.op("pool", lambda e: e.memset(SELT[:], 0.0), writes=["SELT"])
        P.op("pool", lambda e: e.memset(KCP[:], 0.0), writes=["KCP"])

        cnt = {"h": 0, "o": 0, "sl": 0, "sq": 0, "pb": 0, "s": 0, "et": 0, "m": 0, "cp": 0, "stg": 0}

        def sqbuf():
            j = cnt["sq"] % 2
            cnt["sq"] += 1
            return j

        def slbuf():
            j = cnt["sl"] % 2
            cnt["sl"] += 1
            return j

        def bank(lo=0, hi=8):
            b = lo + cnt["pb"] % (hi - lo)
            cnt["pb"] += 1
            return b

        def cpeng():
            cnt["cp"] += 1
            return "act" if cnt["cp"] % 2 else "dve"

        def rstd_from(pss_ap, pres, scale, n=512):
            act(RS[:, 0, 0:n], pss_ap, AF.Ln, [pres, "EPSB"], [("RS", 0)], scale=scale, bias=EPSB[:])
            act(RS[:, 0, 0:n], RS[:, 0, 0:n], AF.Exp, [("RS", 0)], [("RS", 0)], scale=-0.5)

        def rmsnorm(l, ni):
            for tc in range(4):
                ts = slice(tc * 512, (tc + 1) * 512)
                pb = bank()
                for kc in range(8):
                    j = sqbuf()
                    act(SQB[:, j, :], XT[:, kc, ts], AF.Square, [("XT", kc, tc)], [("SQB", j)])
                    mm(PS[pb][:], ONESB, SQB[:, j, :], kc == 0, kc == 7, [("SQB", j)] + CBK, [f"ps{pb}"])
                rstd_from(PS[pb][:], f"ps{pb}", 1.0 / D)
                for kc in range(8):
                    gi = (l * 3 + ni) * 8 + kc
                    stt(XN[:, kc, ts], XT[:, kc, ts], GN[:, gi:gi + 1], RS[:, 0, :], ALU.mult, ALU.mult,
                        [("XT", kc, tc), ("RS", 0), "GN"], [("XN", kc, tc)])

        def ffn(s, l, fi):
            rmsnorm(l, 0 if fi == 0 else 2)
            for g in range(NG):
                for i in range(GSZ):
                    fc = g * GSZ + i
                    w1t, w1res = WS.get(("w1", s, l, fi, fc))
                    w3t, w3res = WS.get(("w3", s, l, fi, fc))
                    for tc in range(4):
                        ts = slice(tc * 512, (tc + 1) * 512)
                        hb = (cnt["h"] % 2) * 2
                        cnt["h"] += 1
                        p1, p3 = PS[hb], PS[hb + 1]
                        for kc in range(8):
                            mm(p1[:], w1t[:, kc * 128:(kc + 1) * 128], XN[:, kc, ts], kc == 0, kc == 7,
                               [w1res, ("XN", kc, tc)], [f"ps{hb}"])
                        for kc in range(8):
                            mm(p3[:], w3t[:, kc * 128:(kc + 1) * 128], XN[:, kc, ts], kc == 0, kc == 7,
                               [w3res, ("XN", kc, tc)], [f"ps{hb + 1}"])
                        j = slbuf()
                        act(SL[:, j, :], p1[:], AF.Silu, [f"ps{hb}"], [("SL", j)])
                        tt(Gs(i, tc * 512, (tc + 1) * 512), SL[:, j, :], p3[:], ALU.mult,
                           [("SL", j), f"ps{hb + 1}"], [("G", i, tc)])
                for dc in range(8):
                    w2a, w2ares = WS.get(("w2", s, l, fi, g, dc, 0))
                    w2b, w2bres = WS.get(("w2", s, l, fi, g, dc, 1))
                    for tc in range(4):
                        ts = slice(tc * 512, (tc + 1) * 512)
                        ob = 4 + cnt["o"] % 2
                        cnt["o"] += 1
                        po = PS[ob]
                        for i in range(GSZ):
                            w2t, w2res, ii = (w2a, w2ares, i) if i < 6 else (w2b, w2bres, i - 6)
                            mm(po[:], w2t[:, ii * 128:(ii + 1) * 128], Gs(i, tc * 512, (tc + 1) * 512), i == 0, i == GSZ - 1,
                               [w2res, ("G", i, tc)], [f"ps{ob}"])
                        stt(XT[:, dc, ts], po[:], 0.5, XT[:, dc, ts], ALU.mult, ALU.add,
                            [f"ps{ob}", ("XT", dc, tc)], [("XT", dc, tc)])

        ZB = Gf[:, 0:2 * T].bitcast(F32)

        def zkeys(tc):
            return [("G", tc // 2, 2 * (tc % 2)), ("G", tc // 2, 2 * (tc % 2) + 1)]

        def proj_fm(wt, wres, tc, pb):
            ts = slice(tc * 512, (tc + 1) * 512)
            for kc in range(8):
                mm(PS[pb][:], wt[:, kc * 128:(kc + 1) * 128], XN[:, kc, ts], kc == 0, kc == 7,
                   [wres, ("XN", kc, tc)], [f"ps{pb}"])

        def gn_fm(l, slots, gcols, dests):
            for tc in range(4):
                a, b = tc * 512, (tc + 1) * 512
                pb = bank()
                for i, sl in enumerate(slots):
                    j = sqbuf()
                    act(SQB[:, j, :], Gs(sl, a, b), AF.Square, [("G", sl, tc)], [("SQB", j)])
                    mm(PS[pb][:], ONESB, SQB[:, j, :], i == 0, i == 1, [("SQB", j)] + CBK, [f"ps{pb}"])
                rstd_from(PS[pb][:], f"ps{pb}", 1.0 / 256)
                for i, sl in enumerate(slots):
                    stt(XN[:, dests[i], a:b], Gs(sl, a, b), PRM[:, gcols[i]:gcols[i] + 1], RS[:, 0, :], ALU.mult, ALU.mult,
                        [("G", sl, tc), ("RS", 0), "PRM"], [("XN", dests[i], tc)])

        def gn_tm(oav, oares, gain, dest, tt_):
            tc = tt_ // 4
            tt(YT[:], oav, oav, ALU.mult, oares, ["YT"])
            P.op("dve", lambda e: e.reduce_sum(out=SM[:, 40:41], in_=YT[:], axis=mybir.AxisListType.X), ["YT"], [("SM", 40)])
            act(SM[:, 41:42], SM[:, 40:41], AF.Ln, [("SM", 40), "EPSB"], [("SM", 41)], scale=1.0 / 256, bias=EPSB[:])
            act(SM[:, 42:43], SM[:, 41:42], AF.Exp, [("SM", 41)], [("SM", 42)], scale=-0.5)
            stt(YT[:], oav, SM[:, 42:43], gain, ALU.mult, ALU.mult, oares + [("SM", 42), "PRM"], ["YT"])
            for i in range(2):
                pb = 6 + cnt["m"] % 2
                cnt["m"] += 1
                P.op("pe", lambda e: e.transpose(out=PS[pb][:, 0:128], in_=YT[:, i * 128:(i + 1) * 128], identity=IDF),
                     ["YT", "CF"], [f"ps{pb}"])
                cpy("dve", XN[:, dest + i, tt_ * 128:(tt_ + 1) * 128], PS[pb][:, 0:128], [f"ps{pb}"], [("XN", dest + i, tc)])

        def mixer(s, l):
            rmsnorm(l, 1)
            P.dma("sp", PRM[:], prm[l], writes=["PRM"], semkey="PRM")
            P.dma("pool", PRB[:], prb[l], writes=["PRB"], semkey="PRB")
            act(SINKE[:], PRM[:, 21:25], AF.Exp, ["PRM"], ["SINKE"])
            POSB = PRB[:, 0:32]
            CW2 = PRB[:, 32:224]
            PW = PRB[:, 224:480]
            wi = 0
            for cb in range(2):
                wc, rc = WS.get(("win", s, l, wi)); wx, rx = WS.get(("win", s, l, wi + 1)); wb, rb = WS.get(("win", s, l, wi + 2))
                wi += 3
                for tc in range(4):
                    a, b = tc * 512, (tc + 1) * 512
                    b1, b2, b3 = bank(), bank(), bank()
                    proj_fm(wc, rc, tc, b1); proj_fm(wx, rx, tc, b2); proj_fm(wb, rb, tc, b3)
                    j = sqbuf()
                    cpy("act", SQ[:, j, :], PS[b1][:], [f"ps{b1}"], [("SQ", j)])
                    tt(ZB[:, a:b], SQ[:, j, :], PS[b2][:], ALU.mult, [("SQ", j), f"ps{b2}"], zkeys(tc))
                    j2 = sqbuf()
                    zk = zkeys(tc) + (zkeys(tc - 1) if tc else [])
                    cw = lambda k: PRM[:, 9 + cb * 3 + k: 10 + cb * 3 + k]
                    tsc(SQ[:, j2, :], ZB[:, a:b], cw(2), ALU.mult, zk + ["PRM"], [("SQ", j2)])
                    for k, sh in ((1, 1), (0, 2)):
                        lo = max(a, sh)
                        stt(SQ[:, j2, lo - a:512], ZB[:, lo - sh:b - sh], cw(k), SQ[:, j2, lo - a:512], ALU.mult, ALU.add,
                            zk + ["PRM", ("SQ", j2)], [("SQ", j2)])
                    tt(Gs(S_YB0 + cb, a, b), SQ[:, j2, :], PS[b3][:], ALU.mult, [("SQ", j2), f"ps{b3}"], [("G", S_YB0 + cb, tc)])
            if stop_after == "B":
                return
            for cd in range(2):
                wd, rd = WS.get(("win", s, l, wi)); wi += 1
                for tc in range(4):
                    a, b = tc * 512, (tc + 1) * 512
                    b1, b2 = bank(), bank()
                    proj_fm(wd, rd, tc, b1)
                    zk = zkeys(tc) + (zkeys(tc - 1) if tc else [])
                    init = 0.0 if tc == 0 else ZB[:, a - 1:a]
                    P.op("dve", lambda e: e.tensor_tensor_scan(out=ZB[:, a:b], data0=CB[:, CB_ZERO:CB_ZERO + 512], data1=PS[b1][:],
                                                               initial=init, op0=ALU.add, op1=ALU.add),
                         [f"ps{b1}"] + CBK + zk, zkeys(tc))
                    j = sqbuf()
                    for half in range(2):
                        w = POOLWIN[2 * cd + half]
                        pr = slice(half * 64, half * 64 + 64)
                        lo = max(a, w)
                        tt(SQ[pr, j, lo - a:512], ZB[pr, lo:b], ZB[pr, lo - w:b - w], ALU.subtract, zk, [("SQ", j)])
                        if tc == 0:
                            cpy("dve", SQ[pr, j, 0:w], ZB[pr, 0:w], zk, [("SQ", j)])
                    if tc == 0:
                        tt(SQ[:, j, 0:16], SQ[:, j, 0:16], CF[:, 130 + cd * 16:130 + cd * 16 + 16], ALU.mult, [("SQ", j), "CF"], [("SQ", j)])
                    js = slbuf()
                    stt(SL[:, js, :], SQ[:, j, :], CF[:, 128 + cd:129 + cd], PS[b1][:], ALU.mult, ALU.subtract,
                        [("SQ", j), "CF", f"ps{b1}"], [("SL", js)])
                    mm(PS[b2][:], PW[:, cd * 128:(cd + 1) * 128], SL[:, js, :], True, True, ["PRB", ("SL", js)], [f"ps{b2}"])
                    tsc(Gs(S_YD0 + cd, a, b), PS[b2][:], PRM[:, 15 + cd:16 + cd], ALU.mult, [f"ps{b2}", "PRM"], [("G", S_YD0 + cd, tc)])
            if stop_after == "D":
                return
            for ct in range(8):
                wt, wres = WS.get(("win", s, l, wi)); wi += 1
                for tc in range(4):
                    a, b = tc * 512, (tc + 1) * 512
                    b1 = bank()
                    proj_fm(wt, wres, tc, b1)
                    if ct == S_KVC:
                        cpy(cpeng(), Gs(ct, a, b), PS[b1][:], [f"ps{b1}"], [("G", ct, tc)])
                        continue
                    j = sqbuf()
                    act(SQB[:, j, :], PS[b1][:], AF.Square, [f"ps{b1}"], [("SQB", j)])
                    b2 = bank()
                    mm(PS[b2][:], BD64B, SQB[:, j, :], True, True, [("SQB", j)] + CBK, [f"ps{b2}"])
                    rstd_from(PS[b2][:], f"ps{b2}", 1.0 / 64)
                    stt(Gs(ct, a, b), PS[b1][:], PRM[:, ct:ct + 1], RS[:, 0, :], ALU.mult, ALU.mult,
                        [f"ps{b1}", "PRM", ("RS", 0)], [("G", ct, tc)])
            if stop_after == "qk":
                return
            for ti in range(3):
                wt, wres = WS.get(("win", s, l, wi)); wi += 1
                for t16 in range(16):
                    tc = t16 // 4
                    pb = bank()
                    for kc in range(8):
                        mm(PS[pb][:, 0:128], XN[:, kc, t16 * 128:(t16 + 1) * 128], wt[:, kc * 128:(kc + 1) * 128], kc == 0, kc == 7,
                           [wres, ("XN", kc, tc)], [f"ps{pb}"])
                    if ti == 0:
                        cpy("act", VSW[:, t16, 0, 0:64], PS[pb][:, 0:64], [f"ps{pb}"], ["VSW"])
                        cpy("dve", VSW[:, t16, 1, 0:64], PS[pb][:, 64:128], [f"ps{pb}"], ["VSW"])
                    elif ti == 1:
                        cpy("act", CV[:, t16, 0, 0:64], PS[pb][:, 0:64], [f"ps{pb}"], ["CV"])
                        cpy("dve", CV[:, t16, 1, 0:64], PS[pb][:, 64:128], [f"ps{pb}"], ["CV"])
                    else:
                        act(GT[:, t16, :], PS[pb][:, 0:12], AF.Sigmoid, [f"ps{pb}"], ["GT"])
            if stop_after == "tm":
                return
            bA, bB = bank(), bank()
            kvr = [("G", S_KVC, tc) for tc in range(4)]
            for q in range(4):
                wt, wres = WS.get(("cw1", s, l, q))
                for l8 in range(8):
                    ll = 8 * q + l8
                    for pr, bk in ((slice(0, 64), bA), (slice(64, 128), bB)):
                        mm(PS[bk][:, 0:127], wt[pr, l8 * 128:(l8 + 1) * 128], Gf[pr, S_KVC * T + ll: S_KVC * T + ll + 2017: 16],
                           ll == 0, False, [wres] + kvr, [f"ps{bk}"])
                        mm(PS[bk][:, 127:128], wt[pr, l8 * 128:(l8 + 1) * 128], POSB[pr, ll:ll + 1],
                           False, ll == 31, [wres, "PRB"], [f"ps{bk}"])
            for i, bk in enumerate((bA, bB)):
                X, X2, X3 = SQ[:, 0, 0:127], SQ[:, 0, 128:255], SQ[:, 0, 256:383]
                cpy("act", SM[:, 50 + i:51 + i], PS[bk][:, 127:128], [f"ps{bk}"], [("SM", 50 + i)])
                tsc(X, PS[bk][:, 0:127], SM[:, 50 + i:51 + i], ALU.add, [f"ps{bk}", ("SM", 50 + i)], [("SQ", 0)])
                tt(X2, X, X, ALU.mult, [("SQ", 0)], [("SQ", 0)])
                tsc(X2, X2, 0.044715, ALU.mult, [("SQ", 0)], [("SQ", 0)], s2=1.0, op1=ALU.add)
                tt(X2, X2, X, ALU.mult, [("SQ", 0), ("SQ", 0)], [("SQ", 0)])
                act(X3, X2, AF.Sigmoid, [("SQ", 0)], [("SQ", 0)], scale=1.5957691216057308)
                tt(HKV[:, i, 0:127], X, X3, ALU.mult, [("SQ", 0), ("SQ", 0)], [("HKV", i)])
            b1, b2, b3 = bank(), bank(), bank()
            mm(PS[b1][:, 0:127], CW2[:, 0:128], HKV[:, 0, 0:127], True, True, ["PRB", ("HKV", 0)], [f"ps{b1}"])
            act(SQB[:, 1, 0:127], PS[b1][:, 0:127], AF.Square, [f"ps{b1}"], [("SQB", 1)])
            P.op("dve", lambda e: e.memset(SQB[:, 1, 127:128], 1.0), [("SQB", 1)], [("SQB", 1)])
            mm(PS[b2][:, 0:128], BD64B, SQB[:, 1, 0:128], True, True, [("SQB", 1)] + CBK, [f"ps{b2}"])
            rstd_from(PS[b2][:, 0:128], f"ps{b2}", 1.0 / 64, n=128)
            for hf in range(2):
                pr = slice(hf * 64, hf * 64 + 64)
                stt(KCP[pr, hf, 0:127], PS[b1][pr, 0:127], PRM[pr, 8:9], RS[pr, 0, 0:127], ALU.mult, ALU.mult,
                    [f"ps{b1}", "PRM", ("RS", 0)], ["KCP"])
            mm(PS[b3][0:127, 0:64], HKV[:, 1, 0:127], CW2[:, 128:192], True, True, ["PRB", ("HKV", 1)], [f"ps{b3}"])
            cpy("act", VCA[0:127, 0:64], PS[b3][0:127, 0:64], [f"ps{b3}"], ["VCA"])
            if stop_after == "cmp":
                return
            gn_fm(l, (S_YB0, S_YB1), (17, 18), (4, 5))
            gn_fm(l, (S_YD0, S_YD1), (19, 20), (6, 7))
            if stop_after == "gn":
                return
            def zpad(dst, src, hf):
                pr, po = slice(hf * 64, hf * 64 + 64), slice((1 - hf) * 64, (1 - hf) * 64 + 64)
                allk = lambda sl: [("G", sl, t_) for t_ in range(4)]
                P.op("dve", lambda e: e.memset(Gf[po, dst * T:(dst + 1) * T], 0.0), [], allk(dst))
                P.op("act", lambda e: e.copy(out=Gf[pr, dst * T:(dst + 1) * T], in_=Gf[pr, src * T:(src + 1) * T]),
                     allk(src), allk(dst))
            zpad(S_YB0, S_KS, 0); zpad(S_YB1, S_KS, 1)
            zpad(S_YD0, S_KW, 0); zpad(S_YD1, S_KW, 1)
            K_SLC, K_WIN, K_SWA = S_YB0, S_YD0, S_KS
            GNA = PRM[:, 25:281]
            GNC = PRM[:, 281:537]

            def evac_a(qs, t16, ncol, br, first):
                ob = (t16 // 4) % 2
                ps = PS[qs]
                pr = f"ps{qs}"
                S = SM2[:, qs, :]
                sk = ("SM2", qs)
                tsc(S[:, 0:4], ps[:, 64:64 + 3 * ncol + 1:ncol], 1e-30, ALU.max, [pr], [sk])
                recip(S[:, 4:8], S[:, 0:4], [sk], [sk])
                tt(S[:, 8:12], S[:, 4:8], GT[:, t16, br:12:3], ALU.mult, [sk, "GT"], [sk])
                psv = ps[:, 0:4 * ncol].rearrange("p (h c) -> p h c", c=ncol)
                oav = OA[:, ob, qs, :].rearrange("p (h d) -> p h d", d=64)
                okey = ("OA", ob, qs)
                facb = S[:, 8:12].unsqueeze(2).broadcast_to([128, 4, 64])
                if first:
                    tt(oav, psv[:, :, 0:64], facb, ALU.mult, [pr, sk], [okey])
                else:
                    tt(YT[:].rearrange("p (h d) -> p h d", d=64), psv[:, :, 0:64], facb, ALU.mult, [pr, sk], ["YT"])
                    tt(OA[:, ob, qs, :], OA[:, ob, qs, :], YT[:], ALU.add, ["YT", okey], [okey])
                if br == 0:
                    IMP = SCI[:, qs, :]
                    rdb = S[:, 4:8].unsqueeze(2).broadcast_to([128, 4, 32])
                    tt(YT[:, 0:128].rearrange("p (h j) -> p h j", j=32), psv[:, :, 65:97], rdb, ALU.mult, [pr, sk], ["YT"])
                    P.op("dve", lambda e: e.tensor_reduce(out=IMP, in_=YT[:, 0:128].rearrange("p (h j) -> p j h", j=32),
                                                         axis=mybir.AxisListType.X, op=ALU.add), ["YT"], [("IMP", qs)])

            def evac_b(qs, t16, br, last):
                if br == 0:
                    IMP, SCR, SELM, M8 = SCI[:, qs, :], SC[:, 1, :], SC[:, 2, :], SC[:, 3, 0:8]
                    tt(SCR, IMP, CB[:, CB_SELA + t16 * 32:CB_SELA + (t16 + 1) * 32], ALU.mult, [("IMP", qs)] + CBK, ["SCR"])
                    tt(SCR, SCR, CB[:, CB_SELB + t16 * 32:CB_SELB + (t16 + 1) * 32], ALU.add, ["SCR"] + CBK, ["SCR"])
                    P.op("dve", lambda e: e.max(out=M8, in_=SCR), ["SCR"], ["M8"])
                    tsc(SELM, SCR, SC[:, 3, 7:8], ALU.is_ge, ["SCR", "M8"], ["SELM"], s2=-1.0, op1=ALU.add)
                    pb = 6 + cnt["m"] % 2
                    cnt["m"] += 1
                    P.op("pe", lambda e: e.transpose(out=PS[pb][0:32, 0:128], in_=SELM, identity=IDF), ["SELM", "CF"], [f"ps{pb}"])
                    cpy("act", SELT[0:32, qs * 128:(qs + 1) * 128], PS[pb][0:32, 0:128], [f"ps{pb}"], ["SELT"])
                if last:
                    ob = (t16 // 4) % 2
                    gn_tm(OA[:, ob, qs, :], [("OA", ob, qs)], GNA, 0, t16)

            def evac_all(c, ncol, br, first, last):
                for qs in range(4):
                    evac_a(qs, 4 * c + qs, ncol, br, first)
                for qs in range(4):
                    evac_b(qs, 4 * c + qs, br, last)

            def sbank():
                b = 4 + cnt["s"] % 2
                cnt["s"] += 1
                return b

            def etbuf():
                j = cnt["et"] % 3
                cnt["et"] += 1
                return j

            for c in range(4):
                ca, cbb = c * 512, (c + 1) * 512
                def pipeline(units):
                    n = len(units)
                    bks = [sbank()]
                    units[0][0](bks[0])
                    for i in range(n):
                        if i + 1 < n:
                            bks.append(sbank())
                            units[i + 1][0](bks[i + 1])
                        j = etbuf()
                        units[i][1](bks[i], j)
                        units[i][2](j)

                units = []
                for h in range(4):
                    aq = S_AQ0 + h // 2

                    def sc(sbk, h=h, aq=aq):
                        mm(PS[sbk][0:127, :], KCP[:, h % 2, 0:127], Gs(aq, ca, cbb), True, True,
                           ["KCP", ("G", aq, c)], [f"ps{sbk}"])

                    def ex(sbk, j):
                        act(ET[0:127, j, :], PS[sbk][0:127, :], AF.Exp, [f"ps{sbk}"], [("ET", j)], scale=0.125)
                        P.op("pool", lambda e: e.affine_select(out=ET[0:127, j, :], in_=ET[0:127, j, :], pattern=[[1, 512]],
                                                               compare_op=ALU.is_ge, fill=0.0, base=512 * c - 31, channel_multiplier=-16),
                             [("ET", j)], [("ET", j)])

                    def pv(j, h=h):
                        for qs in range(4):
                            mm(PS[qs][:, h * 97:(h + 1) * 97], ET[0:127, j, qs * 128:(qs + 1) * 128], VCA[0:127, 0:97], h == 0, True,
                               [("ET", j), "VCA"], [f"ps{qs}"])
                    units.append((sc, ex, pv))
                pipeline(units)
                for qs in range(4):
                    evac_a(qs, 4 * c + qs, 97, 0, True)

                def ex_full(sbk, j):
                    act(ET[:, j, :], PS[sbk][:], AF.Exp, [f"ps{sbk}"], [("ET", j)], scale=0.125)
                units = []
                for h in range(4):
                    aq = S_AQ0 + h // 2
                    for kt in range(max(0, 4 * c - 4), 4 * c + 4):
                        r = 4 * c - kt

                        def sc(sbk, h=h, aq=aq, kt=kt, r=r):
                            mm(PS[sbk][:], Gs(K_WIN + h % 2, kt * 128, (kt + 1) * 128), Gs(aq, ca, cbb), True, False,
                               [("G", K_WIN + h % 2, kt // 4), ("G", aq, c)], [f"ps{sbk}"])
                            if r <= 0:
                                msk = CB[:, CB_CAUS + 384 + 128 * r:CB_CAUS + 384 + 128 * r + 512]
                            else:
                                msk = CB[:, CB_WINM + 128 * (r - 1):CB_WINM + 128 * (r - 1) + 512]
                            mm(PS[sbk][:], IDB, msk, False, True, CBK, [f"ps{sbk}"])

                        def pv(j, h=h, kt=kt):
                            for qs in range(4):
                                tq = 4 * c + qs
                                if kt > tq or kt < tq - 4:
                                    continue
                                mm(PS[qs][:, h * 65:(h + 1) * 65], ET[:, j, qs * 128:(qs + 1) * 128], VSW[:, kt, 1, :],
                                   h == 0 and kt == max(0, tq - 4), True, [("ET", j), "VSW"], [f"ps{qs}"])
                        units.append((sc, ex_full, pv))
                pipeline(units)
                if c > 0:
                    for qs in range(4):
                        evac_b(qs, 4 * (c - 1) + qs, 2, True)
                for qs in range(4):
                    evac_b(qs, 4 * c + qs, 0, False)
                for qs in range(4):
                    evac_a(qs, 4 * c + qs, 65, 2, False)
                units = []
                for h in range(4):
                    aq = S_AQ0 + h // 2
                    for kt in range(4 * c + 4):
                        r = 4 * c - kt

                        def sc(sbk, h=h, aq=aq, kt=kt, r=r):
                            mm(PS[sbk][:], Gs(K_SLC + h % 2, kt * 128, (kt + 1) * 128), Gs(aq, ca, cbb), True, False,
                               [("G", K_SLC + h % 2, kt // 4), ("G", aq, c)], [f"ps{sbk}"])
                            mm(PS[sbk][:], CB[:, CB_EXP + kt * 128:CB_EXP + (kt + 1) * 128], SELT[:, :], False, r > 0,
                               CBK + ["SELT"], [f"ps{sbk}"])
                            if r <= 0:
                                mm(PS[sbk][:], IDB, CB[:, CB_CAUS + 384 + 128 * r:CB_CAUS + 384 + 128 * r + 512], False, True,
                                   CBK, [f"ps{sbk}"])

                        def pv(j, h=h, kt=kt):
                            for qs in range(4):
                                if kt > 4 * c + qs:
                                    continue
                                mm(PS[qs][:, h * 65:(h + 1) * 65], ET[:, j, qs * 128:(qs + 1) * 128], VSW[:, kt, 0, :],
                                   h == 0 and kt == 0, True, [("ET", j), "VSW"], [f"ps{qs}"])
                        units.append((sc, ex_full, pv))
                pipeline(units)
                for qs in range(4):
                    evac_a(qs, 4 * c + qs, 65, 1, False)
            for qs in range(4):
                evac_b(qs, 12 + qs, 2, True)
            if stop_after == "nsa":
                return
            zpad(S_KS, S_CK, 0); zpad(S_KW, S_CK, 1)
            deferred = []

            def swa_evac(kt):
                ps, pr = PS[kt % 4], f"ps{kt % 4}"
                tt(SM[:, 0:4], ps[:, 64:64 + 3 * 65 + 1:65], SINKE[:], ALU.add, [pr, "SINKE"], [("SM", 0)])
                recip(SM[:, 4:8], SM[:, 0:4], [("SM", 0)], [("SM", 4)])
                tt(OA[:, 0, 0, :].rearrange("p (h d) -> p h d", d=64), ps[:, 0:260].rearrange("p (h c) -> p h c", c=65)[:, :, 0:64],
                   SM[:, 4:8].unsqueeze(2).broadcast_to([128, 4, 64]), ALU.mult, [pr, ("SM", 4)], [("OA", 0, 0)])
                gn_tm(OA[:, 0, 0, :], [("OA", 0, 0)], GNC, 2, kt)

            units = []
            for kt in range(16):
                nq = 256 if kt < 15 else 128
                q0 = kt * 128
                qres = sorted(set([q0 // 512, (q0 + nq - 1) // 512]))
                for ui, (a_, hf) in enumerate(((0, 0), (0, 1), (1, 0), (1, 1))):
                    h = 2 * hf + a_

                    def sc(sbk, kt=kt, nq=nq, q0=q0, qres=qres, a_=a_, hf=hf):
                        mm(PS[sbk][:, 0:nq], Gs(K_SWA + hf, kt * 128, (kt + 1) * 128), Gs(S_CQ0 + a_, q0, q0 + nq),
                           True, False, [("G", K_SWA + hf, kt // 4)] + [("G", S_CQ0 + a_, t_) for t_ in qres], [f"ps{sbk}"])
                        mm(PS[sbk][:, 0:nq], IDB, CB[:, CB_BAND:CB_BAND + nq], False, True, CBK, [f"ps{sbk}"])

                    def ex(sbk, j, nq=nq):
                        act(ET[:, j, 0:nq], PS[sbk][:, 0:nq], AF.Exp, [f"ps{sbk}"], [("ET", j)], scale=0.125)

                    def pv(j, kt=kt, h=h, hf=hf, ui=ui):
                        b0, b1 = kt % 4, (kt + 1) % 4
                        mm(PS[b0][:, h * 65:(h + 1) * 65], ET[:, j, 0:128], CV[:, kt, hf, :], kt == 0 and ui == 0, True,
                           [("ET", j), "CV"], [f"ps{b0}"])
                        if kt < 15:
                            mm(PS[b1][:, h * 65:(h + 1) * 65], ET[:, j, 128:256], CV[:, kt, hf, :], ui == 0, False,
                               [("ET", j), "CV"], [f"ps{b1}"])
                        if ui == 3:
                            deferred.append(lambda kt=kt: swa_evac(kt))
                    units.append((sc, ex, pv))
            bks = [sbank()]
            units[0][0](bks[0])
            for i in range(len(units)):
                if i + 1 < len(units):
                    bks.append(sbank())
                    units[i + 1][0](bks[i + 1])
                j = etbuf()
                units[i][1](bks[i], j)
                units[i][2](j)
                if i % 4 == 1 and len(deferred) and i > 8:
                    deferred.pop(0)()
            while deferred:
                deferred.pop(0)()
            if stop_after == "swa":
                return
            ysrc = (0, 1, 4, 5, 2, 3, 6, 7)
            for dc in range(8):
                wt, wres = WS.get(("wout", s, l, dc))
                for tc in range(4):
                    ts = slice(tc * 512, (tc + 1) * 512)
                    pb = bank()
                    for kc in range(8):
                        mm(PS[pb][:], wt[:, kc * 128:(kc + 1) * 128], XN[:, ysrc[kc], ts], kc == 0, kc == 7,
                           [wres, ("XN", ysrc[kc], tc)], [f"ps{pb}"])
                    tt(XT[:, dc, ts], PS[pb][:], XT[:, dc, ts], ALU.add, [f"ps{pb}", ("XT", dc, tc)], [("XT", dc, tc)])

        for s in range(nseq):
            for kc in range(8):
                P.dma("sp", XT[:, kc, :], xT[s, :, kc, :],
                      writes=[("XT", kc, tc) for tc in range(4)], semkey=("XT", kc))
            for l in layers:
                if do_ffn:
                    ffn(s, l, 0)
                if do_mixer:
                    mixer(s, l)
                if do_ffn:
                    ffn(s, l, 1)
            for kc in range(8):
                P.dma("sp", yT[s, :, kc, :], XT[:, kc, :],
                      reads=[("XT", kc, tc) for tc in range(4)], semkey=("XT", kc))
        P.finish()
        print("instructions", P.ninst, "waits", P.nwait)
    return nc


def prep_inputs(inp):
    f = lambda a: np.ascontiguousarray(np.asarray(a, dtype=np.float32))
    x = f(inp["x"])
    B = x.shape[0]
    xTh = np.ascontiguousarray(x.reshape(B, T, 8, 128).transpose(0, 3, 2, 1))

    def w13(w):
        w = f(w).reshape(L, 8, 128, NFC, 128)
        return np.ascontiguousarray(w.transpose(0, 3, 2, 1, 4)).reshape(L, NFC, 128, 1024)

    def w2(w):
        w = f(w).reshape(L, NG, GSZ, 128, 8, 128)
        return np.ascontiguousarray(w.transpose(0, 1, 4, 3, 2, 5)).reshape(L, NG, 8, 128, GSZ * 128)

    w1r = np.stack([w13(inp["ffn1_w1"]), w13(inp["ffn2_w1"])], axis=1)
    w3r = np.stack([w13(inp["ffn1_w3"]), w13(inp["ffn2_w3"])], axis=1)
    w2r = np.stack([w2(inp["ffn1_w2"]), w2(inp["ffn2_w2"])], axis=1)
    nrm = np.stack([f(inp["ffn1_norm"]), f(inp["mix_norm"]), f(inp["ffn2_norm"])], axis=1)
    fnorm = np.ascontiguousarray(nrm.reshape(L, 3, 8, 128).transpose(3, 0, 1, 2)).reshape(128, L * 3 * 8)

    o = {}
    off = 0
    for name, sz in (("a_q", 256), ("a_kc", 64), ("a_vc", 64), ("a_ks", 64), ("a_vs", 64), ("a_kw", 64), ("a_vw", 64),
                     ("a_g", 12), ("b_b", 256), ("b_c", 256), ("b_x", 256), ("c_q", 256), ("c_k", 128), ("c_v", 128), ("d_v", 256)):
        o[name] = off
        off += sz
    r = lambda name, a, n: list(range(o[name] + a, o[name] + a + n))
    tiles = [r("b_c", 0, 128), r("b_x", 0, 128), r("b_b", 0, 128), r("b_c", 128, 128), r("b_x", 128, 128), r("b_b", 128, 128),
             r("d_v", 0, 128), r("d_v", 128, 128),
             r("a_q", 0, 128), r("a_q", 128, 128), r("a_ks", 0, 64) * 2, r("a_kw", 0, 64) * 2,
             r("a_kc", 0, 64) + r("a_vc", 0, 64),
             r("c_q", 0, 64) + r("c_q", 128, 64), r("c_q", 64, 64) + r("c_q", 192, 64), r("c_k", 0, 128),
             r("a_vs", 0, 64) + r("a_vw", 0, 64), r("c_v", 0, 128), r("a_g", 0, 12) + [-1] * 116]
    w_in = f(inp["w_in"])
    w_in_p = np.concatenate([w_in, np.zeros((L, D, 1), np.float32)], axis=2)
    winr = np.stack([w_in_p[:, :, cols].reshape(L, 8, 128, 128).transpose(0, 2, 1, 3).reshape(L, 128, 1024)
                     for cols in tiles], axis=1)
    winr = np.ascontiguousarray(winr)
    ck = f(inp["cmp_w1_k"]).reshape(L, 4, 8, 64, 128)
    cvv = f(inp["cmp_w1_v"]).reshape(L, 4, 8, 64, 128)
    cw1r = np.ascontiguousarray(np.concatenate([ck, cvv], axis=3).transpose(0, 1, 3, 2, 4)).reshape(L, 4, 128, 1024)
    woutr = np.ascontiguousarray(f(inp["w_out"]).reshape(L, 8, 128, 8, 128).transpose(0, 3, 2, 1, 4)).reshape(L, 8, 128, 1024)
    prm = np.zeros((L, 128, NPRM), np.float32)
    p64 = np.arange(128) % 64
    qk_gain = {0: "nsa_q_norm", 1: "nsa_q_norm", 2: "nsa_ks_norm", 3: "nsa_kw_norm", 5: "swa_q_norm", 6: "swa_q_norm", 7: "swa_k_norm"}
    for ct, nm in qk_gain.items():
        prm[:, :, ct] = f(inp[nm])[:, p64]
    prm[:, :, 4] = 1.0
    prm[:, :, 8] = f(inp["nsa_kc_norm"])[:, p64]
    cwv = f(inp["conv_w"])
    for cb in range(2):
        for k in range(3):
            prm[:, :, 9 + cb * 3 + k] = cwv[:, k, cb * 128:(cb + 1) * 128]
    psc = f(inp["pool_scale"])
    gnv = f(inp["group_norm"])
    for c in range(2):
        prm[:, :, 15 + c] = psc[:, c * 128:(c + 1) * 128]
        prm[:, :, 17 + c] = gnv[:, 256 + c * 128:256 + (c + 1) * 128]
        prm[:, :, 19 + c] = gnv[:, 768 + c * 128:768 + (c + 1) * 128]
    prm[:, :, 21:25] = f(inp["swa_sinks"])[:, None, :]
    prm[:, :, 25:281] = gnv[:, None, 0:256]
    prm[:, :, 281:537] = gnv[:, None, 512:768]
    prb = np.zeros((L, 128, NPRB), np.float32)
    prb[:, 0:64, 0:32] = f(inp["cmp_pos_k"]).transpose(0, 2, 1)
    prb[:, 64:128, 0:32] = f(inp["cmp_pos_v"]).transpose(0, 2, 1)
    prb[:, :, 32:96] = f(inp["cmp_w2_k"])
    prb[:, :, 96:160] = f(inp["cmp_w2_k"])
    prb[:, :, 160:224] = f(inp["cmp_w2_v"])
    pw = f(inp["pool_w"])
    for c in range(2):
        prb[:, 0:64, 224 + c * 128:224 + c * 128 + 64] = pw[:, 2 * c]
        prb[:, 64:128, 224 + c * 128 + 64:224 + (c + 1) * 128] = pw[:, 2 * c + 1]
    cstf = np.zeros((128, NCF), np.float32)
    pp = np.arange(128)
    cstf[:, 0:128] = np.eye(128)
    for c in range(2):
        wp = np.where(pp < 64, POOLWIN[2 * c], POOLWIN[2 * c + 1]).astype(np.float32)
        cstf[:, 128 + c] = 1.0 / wp
        tcol = np.arange(16)[None, :]
        cstf[:, 130 + c * 16:130 + (c + 1) * 16] = wp[:, None] / np.minimum(tcol + 1, wp[:, None])
    cstb = np.zeros((128, NCB), np.float32)
    cstb[:, CB_IDB:CB_IDB + 128] = np.eye(128)
    k = pp[:, None]
    xx = np.arange(896)[None, :]
    cstb[:, CB_CAUS:CB_CAUS + 896] = np.where(xx - 384 - k >= 0, 0.0, NEGB)
    cstb[:, CB_WINM:CB_WINM + 896] = np.where(k - xx + 383 >= 0, 0.0, NEGB)
    xb = np.arange(256)[None, :]
    cstb[:, CB_BAND:CB_BAND + 256] = np.where((xb - k >= 0) & (xb - k < 128), 0.0, NEGB)
    tglob = (np.arange(16)[None, :, None] * 128 + pp[:, None, None])
    jj = np.arange(32)[None, None, :]
    cur = tglob // 64
    forced = (jj == 0) | (jj == cur) | (jj == cur - 1)
    future = jj * 64 > tglob
    cstb[:, CB_SELA:CB_SELA + 512] = np.where(forced | future, 0.0, 1.0).reshape(128, 512)
    cstb[:, CB_SELB:CB_SELB + 512] = np.where(forced, 1e4, np.where(future, -1.0, 0.0)).reshape(128, 512)
    ex = np.zeros((128, 16, 128), np.float32)
    for kt in range(16):
        ex[2 * kt, kt, 0:64] = -NEGB
        ex[2 * kt + 1, kt, 64:128] = -NEGB
    cstb[:, CB_EXP:CB_EXP + 2048] = ex.reshape(128, 2048)
    cstb[:, CB_ONES:CB_ONES + 128] = 1.0
    cstb[:, CB_BD64:CB_BD64 + 128] = (pp[:, None] // 64 == pp[None, :] // 64)
    covl = np.zeros((128, 33), np.float32)
    covl[:, 0] = 1.0
    ci = np.arange(127)[:, None] * 16
    sj = np.arange(32)[None, :] * 64
    covl[0:127, 1:33] = ((ci <= sj + 63) & (ci + 31 >= sj))
    shared = dict(w1r=w1r, w3r=w3r, w2r=w2r, fnorm=fnorm, winr=winr, cw1r=cw1r, woutr=woutr, prm=prm, prb=prb,
                  cstf=cstf, cstb=cstb, covl=covl)
    return xTh, shared


def kernel(**inputs):
    xTh, shared = prep_inputs(inputs)
    nc = build_program()
    in_maps = []
    for c in range(NCORES):
        m = dict(shared)
        m["xT"] = np.ascontiguousarray(xTh[c * SEQ_PER_CORE:(c + 1) * SEQ_PER_CORE])
        in_maps.append(m)
    res = run_bass_kernel_spmd(nc, in_maps, core_ids=list(range(NCORES)))
    yT = np.concatenate([r["yT"] for r in res.results], axis=0)
    out = np.ascontiguousarray(yT.transpose(0, 3, 2, 1)).reshape(-1, T, D)
    return out.astype(np.float32)
```

```python
import numpy as np
from contextlib import ExitStack
import concourse.bass as bass
import concourse.mybir as mybir
from concourse.bass_utils import run_bass_kernel_spmd

F32 = mybir.dt.float32
BF16 = mybir.dt.bfloat16
AF = mybir.ActivationFunctionType
ALU = mybir.AluOpType

NCORES = 8
L = 2
D = 1024
T = 2048
DFF = 2816
NFC = DFF // 128
NG = 2
GSZ = NFC // NG
SEQ_PER_CORE = 2
EPS = 1e-6
SELF_SYNC = True


class Prog:
    EPOCH = 2000

    def __init__(self, nc):
        self.nc = nc
        self.eng = dict(pe=nc.tensor, dve=nc.vector, act=nc.scalar,
                        pool=nc.gpsimd, sp=nc.sync)
        self.sems = {}
        self.epoch = {}
        self.cnt = {}
        self.seen = {k: {} for k in self.eng}
        self.lastw = {}
        self.reads = {}
        self.nwait = 0
        self.ninst = 0

    def _cur(self, base, step):
        ep = self.epoch.get(base, 0)
        sid = (base, ep)
        if sid in self.sems and self.cnt[sid] + step > self.EPOCH:
            ep += 1
            self.epoch[base] = ep
            sid = (base, ep)
        if sid not in self.sems:
            self.sems[sid] = self.nc.alloc_semaphore(name=f"s{len(self.sems)}")
            self.cnt[sid] = 0
        return sid

    def _deps(self, reads, writes):
        deps = []
        for r in reads:
            if r in self.lastw:
                deps.append(self.lastw[r])
        for w in writes:
            if w in self.lastw:
                deps.append(self.lastw[w])
            deps.extend(self.reads.get(w, {}).items())
        return deps

    def _wait(self, e, deps):
        need = {}
        for sid, v in deps:
            if sid[0] == e and (e in ("pe", "sp") or not SELF_SYNC):
                continue
            if self.seen[e].get(sid, 0) >= v:
                continue
            if need.get(sid, 0) < v:
                need[sid] = v
        for sid, v in need.items():
            self.eng[e].wait_ge(self.sems[sid], v)
            self.seen[e][sid] = v
            self.nwait += 1

    def _record(self, ev, reads, writes):
        for r in reads:
            d = self.reads.setdefault(r, {})
            if d.get(ev[0], 0) < ev[1]:
                d[ev[0]] = ev[1]
        for w in writes:
            self.lastw[w] = ev
            self.reads[w] = {}

    def op(self, e, fn, reads=(), writes=()):
        writes = list(writes) + [r for r in reads if isinstance(r, str) and r.startswith("ps")]
        self._wait(e, self._deps(reads, writes))
        inst = fn(self.eng[e])
        sid = self._cur(e, 1)
        self.cnt[sid] += 1
        inst.then_inc(self.sems[sid], 1)
        self.ninst += 1
        self._record((sid, self.cnt[sid]), reads, writes)

    def dma(self, q, out, in_, reads=(), writes=(), semkey=None, **kw):
        self._wait(q, self._deps(reads, writes))
        sid = self._cur(("d", semkey), 16)
        inst = self.eng[q].dma_start(out=out, in_=in_, **kw)
        self.cnt[sid] += 16
        inst.then_inc(self.sems[sid], 16)
        self.ninst += 1
        self._record((sid, self.cnt[sid]), reads, writes)

    def finish(self, e="sp"):
        deps = [(sid, v) for sid, v in self.cnt.items() if v > 0 and sid[0] != e]
        self._wait(e, deps)
        print("semaphores used", len(self.sems))


class WStream:
    def __init__(self, P, ring, nslots, plan, lookahead):
        self.P, self.ring, self.n, self.plan, self.la = P, ring, nslots, plan, lookahead
        self.pos = 0
        self.issued = 0

    def _issue(self, j):
        key, src, width = self.plan[j]
        slot = j % self.n
        self.P.dma("pool", self.ring[:, slot, 0:width], src,
                   writes=[("W", slot)], semkey=("W", slot))

    def get(self, key):
        k, src, width = self.plan[self.pos]
        assert k == key, (k, key)
        hi = min(len(self.plan), self.pos + self.la + 1)
        while self.issued < hi:
            self._issue(self.issued)
            self.issued += 1
        slot = self.pos % self.n
        self.pos += 1
        return self.ring[:, slot, 0:width], ("W", slot)


POOLWIN = (2, 4, 8, 16)
S_AQ0, S_AQ1, S_KS, S_KW, S_KVC, S_CQ0, S_CQ1, S_CK, S_YB0, S_YB1, S_YD0, S_YD1 = range(12)
NPRM = 537
NPRB = 480
NCF = 162
CB_IDB, CB_CAUS, CB_WINM, CB_BAND, CB_SELA, CB_SELB, CB_ZERO, CB_EXP, CB_ONES, CB_BD64, NCB = 0, 128, 1024, 1920, 2176, 2688, 3200, 3712, 5760, 5888, 6016
NEGB = -30000.0


def build_program(layers=(0, 1), nseq=SEQ_PER_CORE, do_mixer=True, do_ffn=True, stop_after=None):
    nc = bass.Bass("TRN2", target_bir_lowering=False)
    din = lambda n, s: nc.dram_tensor(n, list(s), F32, kind="ExternalInput").ap()
    xT = din("xT", [nseq, 128, 8, T])
    if do_ffn:
        w1r = din("w1r", [L, 2, NFC, 128, 1024])
        w3r = din("w3r", [L, 2, NFC, 128, 1024])
        w2r = din("w2r", [L, 2, NG, 8, 128, GSZ * 128])
    fnorm = din("fnorm", [128, L * 3 * 8])
    winr = din("winr", [L, 19, 128, 1024])
    cw1r = din("cw1r", [L, 4, 128, 1024])
    woutr = din("woutr", [L, 8, 128, 1024])
    prm = din("prm", [L, 128, NPRM])
    prb = din("prb", [L, 128, NPRB])
    cstf = din("cstf", [128, NCF])
    cstb = din("cstb", [128, NCB])
    covl = din("covl", [128, 33])
    yT = nc.dram_tensor("yT", [nseq, 128, 8, T], F32, kind="ExternalOutput").ap()

    with ExitStack() as es:
        sb = lambda n, s, d: es.enter_context(nc.sbuf_tensor(n, list(s), d))
        XT = sb("XT", [128, 8, T], F32)
        XN = sb("XN", [128, 8, T], BF16)
        Gf = sb("G", [128, 12 * T], BF16)
        NW = 6
        WR = sb("WR", [128, NW, 1024], BF16)
        SQ = sb("SQ", [128, 2, 512], F32)
        RS = sb("RS", [128, 1, 512], F32)
        SL = sb("SL", [128, 2, 512], BF16)
        SQB = sb("SQB", [128, 2, 512], BF16)
        GN = sb("GN", [128, L * 3 * 8], F32)
        CF = sb("CF", [128, NCF], F32)
        CB = sb("CB", [128, NCB], BF16)
        PRM = sb("PRM", [128, NPRM], F32)
        PRB = sb("PRB", [128, NPRB], BF16)
        EPSB = sb("EPSB", [128, 1], F32)
        VSW = sb("VSW", [128, 16, 2, 65], BF16)
        CV = sb("CV", [128, 16, 2, 65], BF16)
        GT = sb("GT", [128, 16, 12], F32)
        ET = sb("ET", [128, 3, 512], BF16)
        OA = sb("OA", [128, 2, 4, 256], F32)
        YT = sb("YT", [128, 256], F32)
        SM = sb("SM", [128, 64], F32)
        SC = sb("SC", [128, 4, 32], F32)
        SCI = sb("SCI", [128, 4, 32], F32)
        SM2 = sb("SM2", [128, 4, 12], F32)
        SELT = sb("SELT", [128, 512], BF16)
        KCP = sb("KCP", [128, 2, 128], BF16)
        VCA = sb("VCA", [128, 97], BF16)
        HKV = sb("HKV", [128, 2, 128], BF16)
        SINKE = sb("SINKE", [128, 4], F32)
        PS = [es.enter_context(nc.psum_tensor(f"ps{i}", [128, 512], F32)) for i in range(8)]
        IDF = CF[:, 0:128]
        IDB = CB[:, CB_IDB:CB_IDB + 128]
        ONESB = CB[:, CB_ONES:CB_ONES + 128]
        BD64B = CB[:, CB_BD64:CB_BD64 + 128]

        P = Prog(nc)

        def Gs(slot, a=0, b=T, p0=0, p1=128):
            return Gf[p0:p1, slot * T + a: slot * T + b]

        def mm(out, lhsT, rhs, start, stop, reads, writes):
            P.op("pe", lambda e: e.matmul(out, lhsT=lhsT, rhs=rhs, start=start, stop=stop, skip_group_check=True),
                 reads, writes)

        def act(out, in_, func, reads, writes, **kw):
            P.op("act", lambda e: e.activation(out=out, in_=in_, func=func, **kw), reads, writes)

        def tt(out, in0, in1, op, reads, writes):
            P.op("dve", lambda e: e.tensor_tensor(out=out, in0=in0, in1=in1, op=op), reads, writes)

        def tsc(out, in0, s1, op0, reads, writes, s2=None, op1=None):
            if op1 is None:
                P.op("dve", lambda e: e.tensor_scalar(out=out, in0=in0, scalar1=s1, scalar2=None, op0=op0), reads, writes)
            else:
                P.op("dve", lambda e: e.tensor_scalar(out=out, in0=in0, scalar1=s1, scalar2=s2, op0=op0, op1=op1), reads, writes)

        def stt(out, in0, scalar, in1, op0, op1, reads, writes):
            P.op("dve", lambda e: e.scalar_tensor_tensor(out=out, in0=in0, scalar=scalar, in1=in1, op0=op0, op1=op1),
                 reads, writes)

        def recip(out, in_, reads, writes):
            P.op("dve", lambda e: e.reciprocal(out=out, in_=in_), reads, writes)

        def cpy(eng, out, in_, reads, writes):
            if eng == "act":
                P.op("act", lambda e: e.copy(out=out, in_=in_), reads, writes)
            else:
                P.op(eng, lambda e: e.tensor_copy(out=out, in_=in_), reads, writes)

        plan = []
        for s in range(nseq):
            for l in layers:
                for fi in range(2):
                    if fi == 1 and do_mixer:
                        for i in range(19):
                            plan.append((("win", s, l, i), winr[l, i], 1024))
                        for q in range(4):
                            plan.append((("cw1", s, l, q), cw1r[l, q], 1024))
                        for dc in range(8):
                            plan.append((("wout", s, l, dc), woutr[l, dc], 1024))
                    if not do_ffn:
                        continue
                    for g in range(NG):
                        for i in range(GSZ):
                            fc = g * GSZ + i
                            plan.append((("w1", s, l, fi, fc), w1r[l, fi, fc], 1024))
                            plan.append((("w3", s, l, fi, fc), w3r[l, fi, fc], 1024))
                        for dc in range(8):
                            plan.append((("w2", s, l, fi, g, dc, 0), w2r[l, fi, g, dc][:, 0:768], 768))
                            plan.append((("w2", s, l, fi, g, dc, 1), w2r[l, fi, g, dc][:, 768:GSZ * 128], GSZ * 128 - 768))
        WS = WStream(P, WR, NW, plan, NW - 3)

        P.dma("sp", GN[:], fnorm, writes=["GN"], semkey="GN")
        P.dma("sp", CF[:], cstf, writes=["CF"], semkey="CF")
        for i in range(0, NCB, 1920):
            P.dma("pool", CB[:, i:min(NCB, i + 1920)], cstb[:, i:min(NCB, i + 1920)], writes=[("CB", i)], semkey=("CB", i))
        CBK = [("CB", i) for i in range(0, NCB, 1920)]
        P.dma("pool", VCA[:, 64:97], covl, writes=["VCA"], semkey="VCA")
        P.op("dve", lambda e: e.memset(EPSB[:], EPS), writes=["EPSB"])
        P.op("pool", lambda e: e.memset(VSW[:, :, :, 64:65], 1.0), writes=["VSW"])
        P.op("pool", lambda e: e.memset(CV[:, :, :, 64:65], 1.0), writes=["CV"])
        P.op("pool", lambda e: e.memset(SELT[:], 0.0), writes=["SELT"])
        P.op("pool", lambda e: e.memset(KCP[:], 0.0), writes=["KCP"])

        cnt = {"h": 0, "o": 0, "sl": 0, "sq": 0, "pb": 0, "s": 0, "et": 0, "m": 0, "cp": 0, "stg": 0}

        def sqbuf():
            j = cnt["sq"] % 2
            cnt["sq"] += 1
            return j

        def slbuf():
            j = cnt["sl"] % 2
            cnt["sl"] += 1
            return j

        def bank(lo=0, hi=8):
            b = lo + cnt["pb"] % (hi - lo)
            cnt["pb"] += 1
            return b

        def cpeng():
            cnt["cp"] += 1
            return "act" if cnt["cp"] % 2 else "dve"

        def rstd_from(pss_ap, pres, scale, n=512):
            act(RS[:, 0, 0:n], pss_ap, AF.Ln, [pres, "EPSB"], [("RS", 0)], scale=scale, bias=EPSB[:])
            act(RS[:, 0, 0:n], RS[:, 0, 0:n], AF.Exp, [("RS", 0)], [("RS", 0)], scale=-0.5)

        def rmsnorm(l, ni):
            for tc in range(4):
                ts = slice(tc * 512, (tc + 1) * 512)
                pb = bank()
                for kc in range(8):
                    j = sqbuf()
                    act(SQB[:, j, :], XT[:, kc, ts], AF.Square, [("XT", kc, tc)], [("SQB", j)])
                    mm(PS[pb][:], ONESB, SQB[:, j, :], kc == 0, kc == 7, [("SQB", j)] + CBK, [f"ps{pb}"])
                rstd_from(PS[pb][:], f"ps{pb}", 1.0 / D)
                for kc in range(8):
                    gi = (l * 3 + ni) * 8 + kc
                    stt(XN[:, kc, ts], XT[:, kc, ts], GN[:, gi:gi + 1], RS[:, 0, :], ALU.mult, ALU.mult,
                        [("XT", kc, tc), ("RS", 0), "GN"], [("XN", kc, tc)])

        def ffn(s, l, fi):
            rmsnorm(l, 0 if fi == 0 else 2)
            for g in range(NG):
                for i in range(GSZ):
                    fc = g * GSZ + i
                    w1t, w1res = WS.get(("w1", s, l, fi, fc))
                    w3t, w3res = WS.get(("w3", s, l, fi, fc))
                    for tc in range(4):
                        ts = slice(tc * 512, (tc + 1) * 512)
                        hb = (cnt["h"] % 2) * 2
                        cnt["h"] += 1
                        p1, p3 = PS[hb], PS[hb + 1]
                        for kc in range(8):
                            mm(p1[:], w1t[:, kc * 128:(kc + 1) * 128], XN[:, kc, ts], kc == 0, kc == 7,
                               [w1res, ("XN", kc, tc)], [f"ps{hb}"])
                        for kc in range(8):
                            mm(p3[:], w3t[:, kc * 128:(kc + 1) * 128], XN[:, kc, ts], kc == 0, kc == 7,
                               [w3res, ("XN", kc, tc)], [f"ps{hb + 1}"])
                        j = slbuf()
                        act(SL[:, j, :], p1[:], AF.Silu, [f"ps{hb}"], [("SL", j)])
                        tt(Gs(i, tc * 512, (tc + 1) * 512), SL[:, j, :], p3[:], ALU.mult,
                           [("SL", j), f"ps{hb + 1}"], [("G", i, tc)])
                for dc in range(8):
                    w2a, w2ares = WS.get(("w2", s, l, fi, g, dc, 0))
                    w2b, w2bres = WS.get(("w2", s, l, fi, g, dc, 1))
                    for tc in range(4):
                        ts = slice(tc * 512, (tc + 1) * 512)
                        ob = 4 + cnt["o"] % 2
                        cnt["o"] += 1
                        po = PS[ob]
                        for i in range(GSZ):
                            w2t, w2res, ii = (w2a, w2ares, i) if i < 6 else (w2b, w2bres, i - 6)
                            mm(po[:], w2t[:, ii * 128:(ii + 1) * 128], Gs(i, tc * 512, (tc + 1) * 512), i == 0, i == GSZ - 1,
                               [w2res, ("G", i, tc)], [f"ps{ob}"])
                        stt(XT[:, dc, ts], po[:], 0.5, XT[:, dc, ts], ALU.mult, ALU.add,
                            [f"ps{ob}", ("XT", dc, tc)], [("XT", dc, tc)])

        ZB = Gf[:, 0:2 * T].bitcast(F32)

        def zkeys(tc):
            return [("G", tc // 2, 2 * (tc % 2)), ("G", tc // 2, 2 * (tc % 2) + 1)]

        def proj_fm(wt, wres, tc, pb):
            ts = slice(tc * 512, (tc + 1) * 512)
            for kc in range(8):
                mm(PS[pb][:], wt[:, kc * 128:(kc + 1) * 128], XN[:, kc, ts], kc == 0, kc == 7,
                   [wres, ("XN", kc, tc)], [f"ps{pb}"])

        def gn_fm(l, slots, gcols, dests):
            for tc in range(4):
                a, b = tc * 512, (tc + 1) * 512
                pb = bank()
                for i, sl in enumerate(slots):
                    j = sqbuf()
                    act(SQB[:, j, :], Gs(sl, a, b), AF.Square, [("G", sl, tc)], [("SQB", j)])
                    mm(PS[pb][:], ONESB, SQB[:, j, :], i == 0, i == 1, [("SQB", j)] + CBK, [f"ps{pb}"])
                rstd_from(PS[pb][:], f"ps{pb}", 1.0 / 256)
                for i, sl in enumerate(slots):
                    stt(XN[:, dests[i], a:b], Gs(sl, a, b), PRM[:, gcols[i]:gcols[i] + 1], RS[:, 0, :], ALU.mult, ALU.mult,
                        [("G", sl, tc), ("RS", 0), "PRM"], [("XN", dests[i], tc)])

        def gn_tm(oav, oares, gain, dest, tt_):
            tc = tt_ // 4
            tt(YT[:], oav, oav, ALU.mult, oares, ["YT"])
            P.op("dve", lambda e: e.reduce_sum(out=SM[:, 40:41], in_=YT[:], axis=mybir.AxisListType.X), ["YT"], [("SM", 40)])
            act(SM[:, 41:42], SM[:, 40:41], AF.Ln, [("SM", 40), "EPSB"], [("SM", 41)], scale=1.0 / 256, bias=EPSB[:])
            act(SM[:, 42:43], SM[:, 41:42], AF.Exp, [("SM", 41)], [("SM", 42)], scale=-0.5)
            stt(YT[:], oav, SM[:, 42:43], gain, ALU.mult, ALU.mult, oares + [("SM", 42), "PRM"], ["YT"])
            for i in range(2):
                pb = 7
                cnt["m"] += 1
                P.op("pe", lambda e: e.transpose(out=PS[pb][:, 0:128], in_=YT[:, i * 128:(i + 1) * 128], identity=IDF),
                     ["YT", "CF"], [f"ps{pb}"])
                cpy("dve", XN[:, dest + i, tt_ * 128:(tt_ + 1) * 128], PS[pb][:, 0:128], [f"ps{pb}"], [("XN", dest + i, tc)])

        def gn_a(oav, okey, gain):
            tt(YT[:], oav, oav, ALU.mult, [okey], ["YT"])
            P.op("dve", lambda e: e.reduce_sum(out=SM[:, 40:41], in_=YT[:], axis=mybir.AxisListType.X), ["YT"], [("SM", 40)])
            act(SM[:, 41:42], SM[:, 40:41], AF.Ln, [("SM", 40), "EPSB"], [("SM", 41)], scale=1.0 / 256, bias=EPSB[:])
            act(SM[:, 42:43], SM[:, 41:42], AF.Exp, [("SM", 41)], [("SM", 42)], scale=-0.5)
            stt(oav, oav, SM[:, 42:43], gain, ALU.mult, ALU.mult, [okey, ("SM", 42), "PRM"], [okey])

        def gn_b(oav, okey, dest, tt_):
            tc = tt_ // 4
            for i in range(2):
                P.op("pe", lambda e: e.transpose(out=PS[7][:, 0:128], in_=oav[:, i * 128:(i + 1) * 128], identity=IDF),
                     [okey, "CF"], ["ps7"])
                cpy("dve", XN[:, dest + i, tt_ * 128:(tt_ + 1) * 128], PS[7][:, 0:128], ["ps7"], [("XN", dest + i, tc)])

        def mixer(s, l):
            rmsnorm(l, 1)
            P.dma("sp", PRM[:], prm[l], writes=["PRM"], semkey="PRM")
            P.dma("pool", PRB[:], prb[l], writes=["PRB"], semkey="PRB")
            act(SINKE[:], PRM[:, 21:25], AF.Exp, ["PRM"], ["SINKE"])
            POSB = PRB[:, 0:32]
            CW2 = PRB[:, 32:224]
            PW = PRB[:, 224:480]
            wi = 0
            for cb in range(2):
                wc, rc = WS.get(("win", s, l, wi)); wx, rx = WS.get(("win", s, l, wi + 1)); wb, rb = WS.get(("win", s, l, wi + 2))
                wi += 3
                for tc in range(4):
                    a, b = tc * 512, (tc + 1) * 512
                    b1, b2, b3 = bank(), bank(), bank()
                    proj_fm(wc, rc, tc, b1); proj_fm(wx, rx, tc, b2); proj_fm(wb, rb, tc, b3)
                    j = sqbuf()
                    cpy("act", SQ[:, j, :], PS[b1][:], [f"ps{b1}"], [("SQ", j)])
                    tt(ZB[:, a:b], SQ[:, j, :], PS[b2][:], ALU.mult, [("SQ", j), f"ps{b2}"], zkeys(tc))
                    j2 = sqbuf()
                    zk = zkeys(tc) + (zkeys(tc - 1) if tc else [])
                    cw = lambda k: PRM[:, 9 + cb * 3 + k: 10 + cb * 3 + k]
                    tsc(SQ[:, j2, :], ZB[:, a:b], cw(2), ALU.mult, zk + ["PRM"], [("SQ", j2)])
                    for k, sh in ((1, 1), (0, 2)):
                        lo = max(a, sh)
                        stt(SQ[:, j2, lo - a:512], ZB[:, lo - sh:b - sh], cw(k), SQ[:, j2, lo - a:512], ALU.mult, ALU.add,
                            zk + ["PRM", ("SQ", j2)], [("SQ", j2)])
                    tt(Gs(S_YB0 + cb, a, b), SQ[:, j2, :], PS[b3][:], ALU.mult, [("SQ", j2), f"ps{b3}"], [("G", S_YB0 + cb, tc)])
            if stop_after == "B":
                return
            for cd in range(2):
                wd, rd = WS.get(("win", s, l, wi)); wi += 1
                for tc in range(4):
                    a, b = tc * 512, (tc + 1) * 512
                    b1, b2 = bank(), bank()
                    proj_fm(wd, rd, tc, b1)
                    zk = zkeys(tc) + (zkeys(tc - 1) if tc else [])
                    init = 0.0 if tc == 0 else ZB[:, a - 1:a]
                    P.op("dve", lambda e: e.tensor_tensor_scan(out=ZB[:, a:b], data0=CB[:, CB_ZERO:CB_ZERO + 512], data1=PS[b1][:],
                                                               initial=init, op0=ALU.add, op1=ALU.add),
                         [f"ps{b1}"] + CBK + zk, zkeys(tc))
                    j = sqbuf()
                    for half in range(2):
                        w = POOLWIN[2 * cd + half]
                        pr = slice(half * 64, half * 64 + 64)
                        lo = max(a, w)
                        tt(SQ[pr, j, lo - a:512], ZB[pr, lo:b], ZB[pr, lo - w:b - w], ALU.subtract, zk, [("SQ", j)])
                        if tc == 0:
                            cpy("dve", SQ[pr, j, 0:w], ZB[pr, 0:w], zk, [("SQ", j)])
                    if tc == 0:
                        tt(SQ[:, j, 0:16], SQ[:, j, 0:16], CF[:, 130 + cd * 16:130 + cd * 16 + 16], ALU.mult, [("SQ", j), "CF"], [("SQ", j)])
                    js = slbuf()
                    stt(SL[:, js, :], SQ[:, j, :], CF[:, 128 + cd:129 + cd], PS[b1][:], ALU.mult, ALU.subtract,
                        [("SQ", j), "CF", f"ps{b1}"], [("SL", js)])
                    mm(PS[b2][:], PW[:, cd * 128:(cd + 1) * 128], SL[:, js, :], True, True, ["PRB", ("SL", js)], [f"ps{b2}"])
                    tsc(Gs(S_YD0 + cd, a, b), PS[b2][:], PRM[:, 15 + cd:16 + cd], ALU.mult, [f"ps{b2}", "PRM"], [("G", S_YD0 + cd, tc)])
            if stop_after == "D":
                return
            for ct in range(8):
                wt, wres = WS.get(("win", s, l, wi)); wi += 1
                for tc in range(4):
                    a, b = tc * 512, (tc + 1) * 512
                    b1 = bank()
                    proj_fm(wt, wres, tc, b1)
                    if ct == S_KVC:
                        cpy(cpeng(), Gs(ct, a, b), PS[b1][:], [f"ps{b1}"], [("G", ct, tc)])
                        continue
                    j = sqbuf()
                    act(SQB[:, j, :], PS[b1][:], AF.Square, [f"ps{b1}"], [("SQB", j)])
                    b2 = bank()
                    mm(PS[b2][:], BD64B, SQB[:, j, :], True, True, [("SQB", j)] + CBK, [f"ps{b2}"])
                    rstd_from(PS[b2][:], f"ps{b2}", 1.0 / 64)
                    stt(Gs(ct, a, b), PS[b1][:], PRM[:, ct:ct + 1], RS[:, 0, :], ALU.mult, ALU.mult,
                        [f"ps{b1}", "PRM", ("RS", 0)], [("G", ct, tc)])
            if stop_after == "qk":
                return
            for ti in range(3):
                wt, wres = WS.get(("win", s, l, wi)); wi += 1
                for t16 in range(16):
                    tc = t16 // 4
                    pb = bank()
                    for kc in range(8):
                        mm(PS[pb][:, 0:128], XN[:, kc, t16 * 128:(t16 + 1) * 128], wt[:, kc * 128:(kc + 1) * 128], kc == 0, kc == 7,
                           [wres, ("XN", kc, tc)], [f"ps{pb}"])
                    if ti == 0:
                        cpy("act", VSW[:, t16, 0, 0:64], PS[pb][:, 0:64], [f"ps{pb}"], ["VSW"])
                        cpy("dve", VSW[:, t16, 1, 0:64], PS[pb][:, 64:128], [f"ps{pb}"], ["VSW"])
                    elif ti == 1:
                        cpy("act", CV[:, t16, 0, 0:64], PS[pb][:, 0:64], [f"ps{pb}"], ["CV"])
                        cpy("dve", CV[:, t16, 1, 0:64], PS[pb][:, 64:128], [f"ps{pb}"], ["CV"])
                    else:
                        act(GT[:, t16, :], PS[pb][:, 0:12], AF.Sigmoid, [f"ps{pb}"], ["GT"])
            if stop_after == "tm":
                return
            bA, bB = bank(), bank()
            kvr = [("G", S_KVC, tc) for tc in range(4)]
            for q in range(4):
                wt, wres = WS.get(("cw1", s, l, q))
                for l8 in range(8):
                    ll = 8 * q + l8
                    for pr, bk in ((slice(0, 64), bA), (slice(64, 128), bB)):
                        mm(PS[bk][:, 0:127], wt[pr, l8 * 128:(l8 + 1) * 128], Gf[pr, S_KVC * T + ll: S_KVC * T + ll + 2017: 16],
                           ll == 0, False, [wres] + kvr, [f"ps{bk}"])
                        mm(PS[bk][:, 127:128], wt[pr, l8 * 128:(l8 + 1) * 128], POSB[pr, ll:ll + 1],
                           False, ll == 31, [wres, "PRB"], [f"ps{bk}"])
            for i, bk in enumerate((bA, bB)):
                X, X2, X3 = SQ[:, 0, 0:127], SQ[:, 0, 128:255], SQ[:, 0, 256:383]
                cpy("act", SM[:, 50 + i:51 + i], PS[bk][:, 127:128], [f"ps{bk}"], [("SM", 50 + i)])
                tsc(X, PS[bk][:, 0:127], SM[:, 50 + i:51 + i], ALU.add, [f"ps{bk}", ("SM", 50 + i)], [("SQ", 0)])
                tt(X2, X, X, ALU.mult, [("SQ", 0)], [("SQ", 0)])
                tsc(X2, X2, 0.044715, ALU.mult, [("SQ", 0)], [("SQ", 0)], s2=1.0, op1=ALU.add)
                tt(X2, X2, X, ALU.mult, [("SQ", 0), ("SQ", 0)], [("SQ", 0)])
                act(X3, X2, AF.Sigmoid, [("SQ", 0)], [("SQ", 0)], scale=1.5957691216057308)
                tt(HKV[:, i, 0:127], X, X3, ALU.mult, [("SQ", 0), ("SQ", 0)], [("HKV", i)])
            b1, b2, b3 = bank(), bank(), bank()
            mm(PS[b1][:, 0:127], CW2[:, 0:128], HKV[:, 0, 0:127], True, True, ["PRB", ("HKV", 0)], [f"ps{b1}"])
            act(SQB[:, 1, 0:127], PS[b1][:, 0:127], AF.Square, [f"ps{b1}"], [("SQB", 1)])
            P.op("dve", lambda e: e.memset(SQB[:, 1, 127:128], 1.0), [("SQB", 1)], [("SQB", 1)])
            mm(PS[b2][:, 0:128], BD64B, SQB[:, 1, 0:128], True, True, [("SQB", 1)] + CBK, [f"ps{b2}"])
            rstd_from(PS[b2][:, 0:128], f"ps{b2}", 1.0 / 64, n=128)
            for hf in range(2):
                pr = slice(hf * 64, hf * 64 + 64)
                stt(KCP[pr, hf, 0:127], PS[b1][pr, 0:127], PRM[pr, 8:9], RS[pr, 0, 0:127], ALU.mult, ALU.mult,
                    [f"ps{b1}", "PRM", ("RS", 0)], ["KCP"])
            mm(PS[b3][0:127, 0:64], HKV[:, 1, 0:127], CW2[:, 128:192], True, True, ["PRB", ("HKV", 1)], [f"ps{b3}"])
            cpy("act", VCA[0:127, 0:64], PS[b3][0:127, 0:64], [f"ps{b3}"], ["VCA"])
            if stop_after == "cmp":
                return
            gn_fm(l, (S_YB0, S_YB1), (17, 18), (4, 5))
            gn_fm(l, (S_YD0, S_YD1), (19, 20), (6, 7))
            if stop_after == "gn":
                return
            def zpad(dst, src, hf):
                pr, po = slice(hf * 64, hf * 64 + 64), slice((1 - hf) * 64, (1 - hf) * 64 + 64)
                allk = lambda sl: [("G", sl, t_) for t_ in range(4)]
                P.op("dve", lambda e: e.memset(Gf[po, dst * T:(dst + 1) * T], 0.0), [], allk(dst))
                P.op("act", lambda e: e.copy(out=Gf[pr, dst * T:(dst + 1) * T], in_=Gf[pr, src * T:(src + 1) * T]),
                     allk(src), allk(dst))
            zpad(S_YB0, S_KS, 0); zpad(S_YB1, S_KS, 1)
            zpad(S_YD0, S_KW, 0); zpad(S_YD1, S_KW, 1)
            K_SLC, K_WIN, K_SWA = S_YB0, S_YD0, S_KS
            GNA = PRM[:, 25:281]
            GNC = PRM[:, 281:537]

            def evac_a(qs, t16, ncol, br, first):
                ob = (t16 // 4) % 2
                ps = PS[qs]
                pr = f"ps{qs}"
                S = SM2[:, qs, :]
                sk = ("SM2", qs)
                tsc(S[:, 0:4], ps[:, 64:64 + 3 * ncol + 1:ncol], 1e-30, ALU.max, [pr], [sk])
                recip(S[:, 4:8], S[:, 0:4], [sk], [sk])
                tt(S[:, 8:12], S[:, 4:8], GT[:, t16, br:12:3], ALU.mult, [sk, "GT"], [sk])
                psv = ps[:, 0:4 * ncol].rearrange("p (h c) -> p h c", c=ncol)
                oav = OA[:, ob, qs, :].rearrange("p (h d) -> p h d", d=64)
                okey = ("OA", ob, qs)
                facb = S[:, 8:12].unsqueeze(2).broadcast_to([128, 4, 64])
                if first:
                    tt(oav, psv[:, :, 0:64], facb, ALU.mult, [pr, sk], [okey])
                else:
                    tt(YT[:].rearrange("p (h d) -> p h d", d=64), psv[:, :, 0:64], facb, ALU.mult, [pr, sk], ["YT"])
                    tt(OA[:, ob, qs, :], OA[:, ob, qs, :], YT[:], ALU.add, ["YT", okey], [okey])
                if br == 0:
                    IMP = SCI[:, qs, :]
                    rdb = S[:, 4:8].unsqueeze(2).broadcast_to([128, 4, 32])
                    tt(YT[:, 0:128].rearrange("p (h j) -> p h j", j=32), psv[:, :, 65:97], rdb, ALU.mult, [pr, sk], ["YT"])
                    P.op("dve", lambda e: e.tensor_reduce(out=IMP, in_=YT[:, 0:128].rearrange("p (h j) -> p j h", j=32),
                                                         axis=mybir.AxisListType.X, op=ALU.add), ["YT"], [("IMP", qs)])

            def evac_b(qs, t16, br, last):
                if br == 0:
                    IMP, SCR, SELM, M8 = SCI[:, qs, :], SC[:, 1, :], SC[:, 2, :], SC[:, 3, 0:8]
                    tt(SCR, IMP, CB[:, CB_SELA + t16 * 32:CB_SELA + (t16 + 1) * 32], ALU.mult, [("IMP", qs)] + CBK, ["SCR"])
                    tt(SCR, SCR, CB[:, CB_SELB + t16 * 32:CB_SELB + (t16 + 1) * 32], ALU.add, ["SCR"] + CBK, ["SCR"])
                    P.op("dve", lambda e: e.max(out=M8, in_=SCR), ["SCR"], ["M8"])
                    tsc(SELM, SCR, SC[:, 3, 7:8], ALU.is_ge, ["SCR", "M8"], ["SELM"], s2=-1.0, op1=ALU.add)
                    pb = 7
                    cnt["m"] += 1
                    P.op("pe", lambda e: e.transpose(out=PS[pb][0:32, 0:128], in_=SELM, identity=IDF), ["SELM", "CF"], [f"ps{pb}"])
                    cpy("act", SELT[0:32, qs * 128:(qs + 1) * 128], PS[pb][0:32, 0:128], [f"ps{pb}"], ["SELT"])
                if last:
                    ob = (t16 // 4) % 2
                    gn_tm(OA[:, ob, qs, :], [("OA", ob, qs)], GNA, 0, t16)

            def evac_all(c, ncol, br, first, last):
                for qs in range(4):
                    evac_a(qs, 4 * c + qs, ncol, br, first)
                for qs in range(4):
                    evac_b(qs, 4 * c + qs, br, last)

            def sbank():
                b = 4 + cnt["s"] % 3
                cnt["s"] += 1
                return b

            def etbuf():
                j = cnt["et"] % 3
                cnt["et"] += 1
                return j

            for c in range(4):
                ca, cbb = c * 512, (c + 1) * 512
                def pipeline(units):
                    n = len(units)
                    bks = []
                    for k in range(min(2, n)):
                        bks.append(sbank())
                        units[k][0](bks[k])
                    for i in range(n):
                        if i + 2 < n:
                            bks.append(sbank())
                            units[i + 2][0](bks[i + 2])
                        j = etbuf()
                        units[i][1](bks[i], j)
                        units[i][2](j)

                units = []
                for h in range(4):
                    aq = S_AQ0 + h // 2

                    def sc(sbk, h=h, aq=aq):
                        mm(PS[sbk][0:127, :], KCP[:, h % 2, 0:127], Gs(aq, ca, cbb), True, True,
                           ["KCP", ("G", aq, c)], [f"ps{sbk}"])

                    def ex(sbk, j):
                        act(ET[0:127, j, :], PS[sbk][0:127, :], AF.Exp, [f"ps{sbk}"], [("ET", j)], scale=0.125)
                        P.op("pool", lambda e: e.affine_select(out=ET[0:127, j, :], in_=ET[0:127, j, :], pattern=[[1, 512]],
                                                               compare_op=ALU.is_ge, fill=0.0, base=512 * c - 31, channel_multiplier=-16),
                             [("ET", j)], [("ET", j)])

                    def pv(j, h=h):
                        for qs in range(4):
                            mm(PS[qs][:, h * 97:(h + 1) * 97], ET[0:127, j, qs * 128:(qs + 1) * 128], VCA[0:127, 0:97], h == 0, True,
                               [("ET", j), "VCA"], [f"ps{qs}"])
                    units.append((sc, ex, pv))
                pipeline(units)
                for qs in range(4):
                    evac_a(qs, 4 * c + qs, 97, 0, True)
                if c > 0:
                    for qs in range(4):
                        gn_a(OA[:, (c - 1) % 2, qs, :], ("OA", (c - 1) % 2, qs), GNA)

                def ex_full(sbk, j):
                    act(ET[:, j, :], PS[sbk][:], AF.Exp, [f"ps{sbk}"], [("ET", j)], scale=0.125)
                units = []
                for h in range(4):
                    aq = S_AQ0 + h // 2
                    for kt in range(max(0, 4 * c - 4), 4 * c + 4):
                        r = 4 * c - kt

                        def sc(sbk, h=h, aq=aq, kt=kt, r=r):
                            mm(PS[sbk][:], Gs(K_WIN + h % 2, kt * 128, (kt + 1) * 128), Gs(aq, ca, cbb), True, False,
                               [("G", K_WIN + h % 2, kt // 4), ("G", aq, c)], [f"ps{sbk}"])
                            if r <= 0:
                                msk = CB[:, CB_CAUS + 384 + 128 * r:CB_CAUS + 384 + 128 * r + 512]
                            else:
                                msk = CB[:, CB_WINM + 128 * (r - 1):CB_WINM + 128 * (r - 1) + 512]
                            mm(PS[sbk][:], IDB, msk, False, True, CBK, [f"ps{sbk}"])

                        def pv(j, h=h, kt=kt):
                            for qs in range(4):
                                tq = 4 * c + qs
                                if kt > tq or kt < tq - 4:
                                    continue
                                mm(PS[qs][:, h * 65:(h + 1) * 65], ET[:, j, qs * 128:(qs + 1) * 128], VSW[:, kt, 1, :],
                                   h == 0 and kt == max(0, tq - 4), True, [("ET", j), "VSW"], [f"ps{qs}"])
                        units.append((sc, ex_full, pv))
                pipeline(units)
                for qs in range(4):
                    evac_b(qs, 4 * c + qs, 0, False)
                for qs in range(4):
                    evac_a(qs, 4 * c + qs, 65, 2, False)
                units = []
                for h in range(4):
                    aq = S_AQ0 + h // 2
                    for kt in range(4 * c + 4):
                        r = 4 * c - kt

                        def sc(sbk, h=h, aq=aq, kt=kt, r=r):
                            mm(PS[sbk][:], Gs(K_SLC + h % 2, kt * 128, (kt + 1) * 128), Gs(aq, ca, cbb), True, False,
                               [("G", K_SLC + h % 2, kt // 4), ("G", aq, c)], [f"ps{sbk}"])
                            mm(PS[sbk][:], CB[:, CB_EXP + kt * 128:CB_EXP + (kt + 1) * 128], SELT[:, :], False, r > 0,
                               CBK + ["SELT"], [f"ps{sbk}"])
                            if r <= 0:
                                mm(PS[sbk][:], IDB, CB[:, CB_CAUS + 384 + 128 * r:CB_CAUS + 384 + 128 * r + 512], False, True,
                                   CBK, [f"ps{sbk}"])

                        def pv(j, h=h, kt=kt):
                            for qs in range(4):
                                if kt > 4 * c + qs:
                                    continue
                                mm(PS[qs][:, h * 65:(h + 1) * 65], ET[:, j, qs * 128:(qs + 1) * 128], VSW[:, kt, 0, :],
                                   h == 0 and kt == 0, True, [("ET", j), "VSW"], [f"ps{qs}"])
                        units.append((sc, ex_full, pv))
                pipeline(units)
                if c > 0:
                    for qs in range(4):
                        gn_b(OA[:, (c - 1) % 2, qs, :], ("OA", (c - 1) % 2, qs), 0, 4 * (c - 1) + qs)
                for qs in range(4):
                    evac_a(qs, 4 * c + qs, 65, 1, False)
            for qs in range(4):
                gn_a(OA[:, 1, qs, :], ("OA", 1, qs), GNA)
                gn_b(OA[:, 1, qs, :], ("OA", 1, qs), 0, 12 + qs)
            if stop_after == "nsa":
                return
            zpad(S_KS, S_CK, 0); zpad(S_KW, S_CK, 1)
            deferred = []

            def swa_evac(kt):
                ps, pr = PS[kt % 4], f"ps{kt % 4}"
                tt(SM[:, 0:4], ps[:, 64:64 + 3 * 65 + 1:65], SINKE[:], ALU.add, [pr, "SINKE"], [("SM", 0)])
                recip(SM[:, 4:8], SM[:, 0:4], [("SM", 0)], [("SM", 4)])
                tt(OA[:, 0, 0, :].rearrange("p (h d) -> p h d", d=64), ps[:, 0:260].rearrange("p (h c) -> p h c", c=65)[:, :, 0:64],
                   SM[:, 4:8].unsqueeze(2).broadcast_to([128, 4, 64]), ALU.mult, [pr, ("SM", 4)], [("OA", 0, 0)])
                gn_tm(OA[:, 0, 0, :], [("OA", 0, 0)], GNC, 2, kt)

            units = []
            for kt in range(16):
                nq = 256 if kt < 15 else 128
                q0 = kt * 128
                qres = sorted(set([q0 // 512, (q0 + nq - 1) // 512]))
                for ui, (a_, hf) in enumerate(((0, 0), (0, 1), (1, 0), (1, 1))):
                    h = 2 * hf + a_

                    def sc(sbk, kt=kt, nq=nq, q0=q0, qres=qres, a_=a_, hf=hf):
                        mm(PS[sbk][:, 0:nq], Gs(K_SWA + hf, kt * 128, (kt + 1) * 128), Gs(S_CQ0 + a_, q0, q0 + nq),
                           True, False, [("G", K_SWA + hf, kt // 4)] + [("G", S_CQ0 + a_, t_) for t_ in qres], [f"ps{sbk}"])
                        mm(PS[sbk][:, 0:nq], IDB, CB[:, CB_BAND:CB_BAND + nq], False, True, CBK, [f"ps{sbk}"])

                    def ex(sbk, j, nq=nq):
                        act(ET[:, j, 0:nq], PS[sbk][:, 0:nq], AF.Exp, [f"ps{sbk}"], [("ET", j)], scale=0.125)

                    def pv(j, kt=kt, h=h, hf=hf, ui=ui):
                        b0, b1 = kt % 4, (kt + 1) % 4
                        mm(PS[b0][:, h * 65:(h + 1) * 65], ET[:, j, 0:128], CV[:, kt, hf, :], kt == 0 and ui == 0, True,
                           [("ET", j), "CV"], [f"ps{b0}"])
                        if kt < 15:
                            mm(PS[b1][:, h * 65:(h + 1) * 65], ET[:, j, 128:256], CV[:, kt, hf, :], ui == 0, False,
                               [("ET", j), "CV"], [f"ps{b1}"])
                        if ui == 3:
                            deferred.append(lambda kt=kt: swa_evac(kt))
                    units.append((sc, ex, pv))
            bks = []
            for k in range(2):
                bks.append(sbank())
                units[k][0](bks[k])
            for i in range(len(units)):
                if i + 2 < len(units):
                    bks.append(sbank())
                    units[i + 2][0](bks[i + 2])
                j = etbuf()
                units[i][1](bks[i], j)
                units[i][2](j)
                if i % 4 == 1 and len(deferred) and i > 8:
                    deferred.pop(0)()
            while deferred:
                deferred.pop(0)()
            if stop_after == "swa":
                return
            ysrc = (0, 1, 4, 5, 2, 3, 6, 7)
            for dc in range(8):
                wt, wres = WS.get(("wout", s, l, dc))
                for tc in range(4):
                    ts = slice(tc * 512, (tc + 1) * 512)
                    pb = bank()
                    for kc in range(8):
                        mm(PS[pb][:], wt[:, kc * 128:(kc + 1) * 128], XN[:, ysrc[kc], ts], kc == 0, kc == 7,
                           [wres, ("XN", ysrc[kc], tc)], [f"ps{pb}"])
                    tt(XT[:, dc, ts], PS[pb][:], XT[:, dc, ts], ALU.add, [f"ps{pb}", ("XT", dc, tc)], [("XT", dc, tc)])

        for s in range(nseq):
            for kc in range(8):
                P.dma("sp", XT[:, kc, :], xT[s, :, kc, :],
                      writes=[("XT", kc, tc) for tc in range(4)], semkey=("XT", kc))
            for l in layers:
                if do_ffn:
                    ffn(s, l, 0)
                if do_mixer:
                    mixer(s, l)
                if do_ffn:
                    ffn(s, l, 1)
            for kc in range(8):
                P.dma("sp", yT[s, :, kc, :], XT[:, kc, :],
                      reads=[("XT", kc, tc) for tc in range(4)], semkey=("XT", kc))
        P.finish()
        print("instructions", P.ninst, "waits", P.nwait)
    return nc


def prep_inputs(inp):
    f = lambda a: np.ascontiguousarray(np.asarray(a, dtype=np.float32))
    x = f(inp["x"])
    B = x.shape[0]
    xTh = np.ascontiguousarray(x.reshape(B, T, 8, 128).transpose(0, 3, 2, 1))

    def w13(w):
        w = f(w).reshape(L, 8, 128, NFC, 128)
        return np.ascontiguousarray(w.transpose(0, 3, 2, 1, 4)).reshape(L, NFC, 128, 1024)

    def w2(w):
        w = f(w).reshape(L, NG, GSZ, 128, 8, 128)
        return np.ascontiguousarray(w.transpose(0, 1, 4, 3, 2, 5)).reshape(L, NG, 8, 128, GSZ * 128)

    w1r = np.stack([w13(inp["ffn1_w1"]), w13(inp["ffn2_w1"])], axis=1)
    w3r = np.stack([w13(inp["ffn1_w3"]), w13(inp["ffn2_w3"])], axis=1)
    w2r = np.stack([w2(inp["ffn1_w2"]), w2(inp["ffn2_w2"])], axis=1)
    nrm = np.stack([f(inp["ffn1_norm"]), f(inp["mix_norm"]), f(inp["ffn2_norm"])], axis=1)
    fnorm = np.ascontiguousarray(nrm.reshape(L, 3, 8, 128).transpose(3, 0, 1, 2)).reshape(128, L * 3 * 8)

    o = {}
    off = 0
    for name, sz in (("a_q", 256), ("a_kc", 64), ("a_vc", 64), ("a_ks", 64), ("a_vs", 64), ("a_kw", 64), ("a_vw", 64),
                     ("a_g", 12), ("b_b", 256), ("b_c", 256), ("b_x", 256), ("c_q", 256), ("c_k", 128), ("c_v", 128), ("d_v", 256)):
        o[name] = off
        off += sz
    r = lambda name, a, n: list(range(o[name] + a, o[name] + a + n))
    tiles = [r("b_c", 0, 128), r("b_x", 0, 128), r("b_b", 0, 128), r("b_c", 128, 128), r("b_x", 128, 128), r("b_b", 128, 128),
             r("d_v", 0, 128), r("d_v", 128, 128),
             r("a_q", 0, 128), r("a_q", 128, 128), r("a_ks", 0, 64) * 2, r("a_kw", 0, 64) * 2,
             r("a_kc", 0, 64) + r("a_vc", 0, 64),
             r("c_q", 0, 64) + r("c_q", 128, 64), r("c_q", 64, 64) + r("c_q", 192, 64), r("c_k", 0, 128),
             r("a_vs", 0, 64) + r("a_vw", 0, 64), r("c_v", 0, 128), r("a_g", 0, 12) + [-1] * 116]
    w_in = f(inp["w_in"])
    w_in_p = np.concatenate([w_in, np.zeros((L, D, 1), np.float32)], axis=2)
    winr = np.stack([w_in_p[:, :, cols].reshape(L, 8, 128, 128).transpose(0, 2, 1, 3).reshape(L, 128, 1024)
                     for cols in tiles], axis=1)
    winr = np.ascontiguousarray(winr)
    ck = f(inp["cmp_w1_k"]).reshape(L, 4, 8, 64, 128)
    cvv = f(inp["cmp_w1_v"]).reshape(L, 4, 8, 64, 128)
    cw1r = np.ascontiguousarray(np.concatenate([ck, cvv], axis=3).transpose(0, 1, 3, 2, 4)).reshape(L, 4, 128, 1024)
    woutr = np.ascontiguousarray(f(inp["w_out"]).reshape(L, 8, 128, 8, 128).transpose(0, 3, 2, 1, 4)).reshape(L, 8, 128, 1024)
    prm = np.zeros((L, 128, NPRM), np.float32)
    p64 = np.arange(128) % 64
    qk_gain = {0: "nsa_q_norm", 1: "nsa_q_norm", 2: "nsa_ks_norm", 3: "nsa_kw_norm", 5: "swa_q_norm", 6: "swa_q_norm", 7: "swa_k_norm"}
    for ct, nm in qk_gain.items():
        prm[:, :, ct] = f(inp[nm])[:, p64]
    prm[:, :, 4] = 1.0
    prm[:, :, 8] = f(inp["nsa_kc_norm"])[:, p64]
    cwv = f(inp["conv_w"])
    for cb in range(2):
        for k in range(3):
            prm[:, :, 9 + cb * 3 + k] = cwv[:, k, cb * 128:(cb + 1) * 128]
    psc = f(inp["pool_scale"])
    gnv = f(inp["group_norm"])
    for c in range(2):
        prm[:, :, 15 + c] = psc[:, c * 128:(c + 1) * 128]
        prm[:, :, 17 + c] = gnv[:, 256 + c * 128:256 + (c + 1) * 128]
        prm[:, :, 19 + c] = gnv[:, 768 + c * 128:768 + (c + 1) * 128]
    prm[:, :, 21:25] = f(inp["swa_sinks"])[:, None, :]
    prm[:, :, 25:281] = gnv[:, None, 0:256]
    prm[:, :, 281:537] = gnv[:, None, 512:768]
    prb = np.zeros((L, 128, NPRB), np.float32)
    prb[:, 0:64, 0:32] = f(inp["cmp_pos_k"]).transpose(0, 2, 1)
    prb[:, 64:128, 0:32] = f(inp["cmp_pos_v"]).transpose(0, 2, 1)
    prb[:, :, 32:96] = f(inp["cmp_w2_k"])
    prb[:, :, 96:160] = f(inp["cmp_w2_k"])
    prb[:, :, 160:224] = f(inp["cmp_w2_v"])
    pw = f(inp["pool_w"])
    for c in range(2):
        prb[:, 0:64, 224 + c * 128:224 + c * 128 + 64] = pw[:, 2 * c]
        prb[:, 64:128, 224 + c * 128 + 64:224 + (c + 1) * 128] = pw[:, 2 * c + 1]
    cstf = np.zeros((128, NCF), np.float32)
    pp = np.arange(128)
    cstf[:, 0:128] = np.eye(128)
    for c in range(2):
        wp = np.where(pp < 64, POOLWIN[2 * c], POOLWIN[2 * c + 1]).astype(np.float32)
        cstf[:, 128 + c] = 1.0 / wp
        tcol = np.arange(16)[None, :]
        cstf[:, 130 + c * 16:130 + (c + 1) * 16] = wp[:, None] / np.minimum(tcol + 1, wp[:, None])
    cstb = np.zeros((128, NCB), np.float32)
    cstb[:, CB_IDB:CB_IDB + 128] = np.eye(128)
    k = pp[:, None]
    xx = np.arange(896)[None, :]
    cstb[:, CB_CAUS:CB_CAUS + 896] = np.where(xx - 384 - k >= 0, 0.0, NEGB)
    cstb[:, CB_WINM:CB_WINM + 896] = np.where(k - xx + 383 >= 0, 0.0, NEGB)
    xb = np.arange(256)[None, :]
    cstb[:, CB_BAND:CB_BAND + 256] = np.where((xb - k >= 0) & (xb - k < 128), 0.0, NEGB)
    tglob = (np.arange(16)[None, :, None] * 128 + pp[:, None, None])
    jj = np.arange(32)[None, None, :]
    cur = tglob // 64
    forced = (jj == 0) | (jj == cur) | (jj == cur - 1)
    future = jj * 64 > tglob
    cstb[:, CB_SELA:CB_SELA + 512] = np.where(forced | future, 0.0, 1.0).reshape(128, 512)
    cstb[:, CB_SELB:CB_SELB + 512] = np.where(forced, 1e4, np.where(future, -1.0, 0.0)).reshape(128, 512)
    ex = np.zeros((128, 16, 128), np.float32)
    for kt in range(16):
        ex[2 * kt, kt, 0:64] = -NEGB
        ex[2 * kt + 1, kt, 64:128] = -NEGB
    cstb[:, CB_EXP:CB_EXP + 2048] = ex.reshape(128, 2048)
    cstb[:, CB_ONES:CB_ONES + 128] = 1.0
    cstb[:, CB_BD64:CB_BD64 + 128] = (pp[:, None] // 64 == pp[None, :] // 64)
    covl = np.zeros((128, 33), np.float32)
    covl[:, 0] = 1.0
    ci = np.arange(127)[:, None] * 16
    sj = np.arange(32)[None, :] * 64
    covl[0:127, 1:33] = ((ci <= sj + 63) & (ci + 31 >= sj))
    shared = dict(w1r=w1r, w3r=w3r, w2r=w2r, fnorm=fnorm, winr=winr, cw1r=cw1r, woutr=woutr, prm=prm, prb=prb,
                  cstf=cstf, cstb=cstb, covl=covl)
    return xTh, shared


def kernel(**inputs):
    xTh, shared = prep_inputs(inputs)
    nc = build_program()
    in_maps = []
    for c in range(NCORES):
        m = dict(shared)
        m["xT"] = np.ascontiguousarray(xTh[c * SEQ_PER_CORE:(c + 1) * SEQ_PER_CORE])
        in_maps.append(m)
    res = run_bass_kernel_spmd(nc, in_maps, core_ids=list(range(NCORES)))
    yT = np.concatenate([r["yT"] for r in res.results], axis=0)
    out = np.ascontiguousarray(yT.transpose(0, 3, 2, 1)).reshape(-1, T, D)
    return out.astype(np.float32)
```

```python
import numpy as np
from contextlib import ExitStack
import concourse.bass as bass
import concourse.mybir as mybir
from concourse.bass_utils import run_bass_kernel_spmd

F32 = mybir.dt.float32
BF16 = mybir.dt.bfloat16
AF = mybir.ActivationFunctionType
ALU = mybir.AluOpType

NCORES = 8
L = 2
D = 1024
T = 2048
DFF = 2816
NFC = DFF // 128
NG = 2
GSZ = NFC // NG
SEQ_PER_CORE = 2
EPS = 1e-6
SELF_SYNC = True


class Prog:
    EPOCH = 2000

    def __init__(self, nc):
        self.nc = nc
        self.eng = dict(pe=nc.tensor, dve=nc.vector, act=nc.scalar,
                        pool=nc.gpsimd, sp=nc.sync)
        self.sems = {}
        self.epoch = {}
        self.cnt = {}
        self.seen = {k: {} for k in self.eng}
        self.lastw = {}
        self.reads = {}
        self.nwait = 0
        self.ninst = 0

    def _cur(self, base, step):
        ep = self.epoch.get(base, 0)
        sid = (base, ep)
        if sid in self.sems and self.cnt[sid] + step > self.EPOCH:
            ep += 1
            self.epoch[base] = ep
            sid = (base, ep)
        if sid not in self.sems:
            self.sems[sid] = self.nc.alloc_semaphore(name=f"s{len(self.sems)}")
            self.cnt[sid] = 0
        return sid

    def _deps(self, reads, writes):
        deps = []
        for r in reads:
            if r in self.lastw:
                deps.append(self.lastw[r])
        for w in writes:
            if w in self.lastw:
                deps.append(self.lastw[w])
            deps.extend(self.reads.get(w, {}).items())
        return deps

    def _wait(self, e, deps):
        need = {}
        for sid, v in deps:
            if sid[0] == e and (e in ("pe", "sp") or not SELF_SYNC):
                continue
            if self.seen[e].get(sid, 0) >= v:
                continue
            if need.get(sid, 0) < v:
                need[sid] = v
        for sid, v in need.items():
            self.eng[e].wait_ge(self.sems[sid], v)
            self.seen[e][sid] = v
            self.nwait += 1

    def _record(self, ev, reads, writes):
        for r in reads:
            d = self.reads.setdefault(r, {})
            if d.get(ev[0], 0) < ev[1]:
                d[ev[0]] = ev[1]
        for w in writes:
            self.lastw[w] = ev
            self.reads[w] = {}

    def op(self, e, fn, reads=(), writes=()):
        writes = list(writes) + [r for r in reads if isinstance(r, str) and r.startswith("ps")]
        self._wait(e, self._deps(reads, writes))
        inst = fn(self.eng[e])
        sid = self._cur(e, 1)
        self.cnt[sid] += 1
        inst.then_inc(self.sems[sid], 1)
        self.ninst += 1
        self._record((sid, self.cnt[sid]), reads, writes)

    def dma(self, q, out, in_, reads=(), writes=(), semkey=None, **kw):
        self._wait(q, self._deps(reads, writes))
        sid = self._cur(("d", semkey), 16)
        inst = self.eng[q].dma_start(out=out, in_=in_, **kw)
        self.cnt[sid] += 16
        inst.then_inc(self.sems[sid], 16)
        self.ninst += 1
        self._record((sid, self.cnt[sid]), reads, writes)

    def finish(self, e="sp"):
        deps = [(sid, v) for sid, v in self.cnt.items() if v > 0 and sid[0] != e]
        self._wait(e, deps)
        print("semaphores used", len(self.sems))


class WStream:
    def __init__(self, P, ring, nslots, plan, lookahead):
        self.P, self.ring, self.n, self.plan, self.la = P, ring, nslots, plan, lookahead
        self.pos = 0
        self.issued = 0

    def _issue(self, j):
        key, src, width = self.plan[j]
        slot = j % self.n
        self.P.dma("pool", self.ring[:, slot, 0:width], src,
                   writes=[("W", slot)], semkey=("W", slot))

    def get(self, key):
        k, src, width = self.plan[self.pos]
        assert k == key, (k, key)
        hi = min(len(self.plan), self.pos + self.la + 1)
        while self.issued < hi:
            self._issue(self.issued)
            self.issued += 1
        slot = self.pos % self.n
        self.pos += 1
        return self.ring[:, slot, 0:width], ("W", slot)


POOLWIN = (2, 4, 8, 16)
S_AQ0, S_AQ1, S_KS, S_KW, S_KVC, S_CQ0, S_CQ1, S_CK, S_YB0, S_YB1, S_YD0, S_YD1 = range(12)
NPRM = 537
NPRB = 480
NCF = 162
CB_IDB, CB_CAUS, CB_WINM, CB_BAND, CB_SELA, CB_SELB, CB_ZERO, CB_EXP, CB_ONES, CB_BD64, NCB = 0, 128, 1024, 1920, 2176, 2688, 3200, 3712, 5760, 5888, 6016
NEGB = -30000.0


def build_program(layers=(0, 1), nseq=SEQ_PER_CORE, do_mixer=True, do_ffn=True, stop_after=None):
    nc = bass.Bass("TRN2", target_bir_lowering=False)
    din = lambda n, s: nc.dram_tensor(n, list(s), F32, kind="ExternalInput").ap()
    xT = din("xT", [nseq, 128, 8, T])
    if do_ffn:
        w1r = din("w1r", [L, 2, NFC, 128, 1024])
        w3r = din("w3r", [L, 2, NFC, 128, 1024])
        w2r = din("w2r", [L, 2, NG, 8, 128, GSZ * 128])
    fnorm = din("fnorm", [128, L * 3 * 8])
    winr = din("winr", [L, 19, 128, 1024])
    cw1r = din("cw1r", [L, 4, 128, 1024])
    woutr = din("woutr", [L, 8, 128, 1024])
    prm = din("prm", [L, 128, NPRM])
    prb = din("prb", [L, 128, NPRB])
    cstf = din("cstf", [128, NCF])
    cstb = din("cstb", [128, NCB])
    covl = din("covl", [128, 33])
    yT = nc.dram_tensor("yT", [nseq, 128, 8, T], F32, kind="ExternalOutput").ap()

    with ExitStack() as es:
        sb = lambda n, s, d: es.enter_context(nc.sbuf_tensor(n, list(s), d))
        XT = sb("XT", [128, 8, T], F32)
        XN = sb("XN", [128, 8, T], BF16)
        Gf = sb("G", [128, 12 * T], BF16)
        NW = 6
        WR = sb("WR", [128, NW, 1024], BF16)
        SQ = sb("SQ", [128, 2, 512], F32)
        RS = sb("RS", [128, 1, 512], F32)
        SL = sb("SL", [128, 2, 512], BF16)
        SQB = sb("SQB", [128, 2, 512], BF16)
        GN = sb("GN", [128, L * 3 * 8], F32)
        CF = sb("CF", [128, NCF], F32)
        CB = sb("CB", [128, NCB], BF16)
        PRM = sb("PRM", [128, NPRM], F32)
        PRB = sb("PRB", [128, NPRB], BF16)
        EPSB = sb("EPSB", [128, 1], F32)
        VSW = sb("VSW", [128, 16, 2, 65], BF16)
        CV = sb("CV", [128, 16, 2, 65], BF16)
        GT = sb("GT", [128, 16, 12], F32)
        ET = sb("ET", [128, 3, 512], BF16)
        OA = sb("OA", [128, 2, 4, 256], F32)
        YT = sb("YT", [128, 256], F32)
        SM = sb("SM", [128, 64], F32)
        SC = sb("SC", [128, 4, 32], F32)
        SCI = sb("SCI", [128, 4, 32], F32)
        SM2 = sb("SM2", [128, 4, 12], F32)
        SELT = sb("SELT", [128, 512], BF16)
        KCP = sb("KCP", [128, 2, 128], BF16)
        VCA = sb("VCA", [128, 97], BF16)
        HKV = sb("HKV", [128, 2, 128], BF16)
        SINKE = sb("SINKE", [128, 4], F32)
        PS = [es.enter_context(nc.psum_tensor(f"ps{i}", [128, 512], F32)) for i in range(8)]
        IDF = CF[:, 0:128]
        IDB = CB[:, CB_IDB:CB_IDB + 128]
        ONESB = CB[:, CB_ONES:CB_ONES + 128]
        BD64B = CB[:, CB_BD64:CB_BD64 + 128]

        P = Prog(nc)

        def Gs(slot, a=0, b=T, p0=0, p1=128):
            return Gf[p0:p1, slot * T + a: slot * T + b]

        def mm(out, lhsT, rhs, start, stop, reads, writes):
            P.op("pe", lambda e: e.matmul(out, lhsT=lhsT, rhs=rhs, start=start, stop=stop, skip_group_check=True),
                 reads, writes)

        def act(out, in_, func, reads, writes, **kw):
            P.op("act", lambda e: e.activation(out=out, in_=in_, func=func, **kw), reads, writes)

        def tt(out, in0, in1, op, reads, writes):
            P.op("dve", lambda e: e.tensor_tensor(out=out, in0=in0, in1=in1, op=op), reads, writes)

        def tsc(out, in0, s1, op0, reads, writes, s2=None, op1=None):
            if op1 is None:
                P.op("dve", lambda e: e.tensor_scalar(out=out, in0=in0, scalar1=s1, scalar2=None, op0=op0), reads, writes)
            else:
                P.op("dve", lambda e: e.tensor_scalar(out=out, in0=in0, scalar1=s1, scalar2=s2, op0=op0, op1=op1), reads, writes)

        def stt(out, in0, scalar, in1, op0, op1, reads, writes):
            P.op("dve", lambda e: e.scalar_tensor_tensor(out=out, in0=in0, scalar=scalar, in1=in1, op0=op0, op1=op1),
                 reads, writes)

        def recip(out, in_, reads, writes):
            P.op("dve", lambda e: e.reciprocal(out=out, in_=in_), reads, writes)

        def cpy(eng, out, in_, reads, writes):
            if eng == "act":
                P.op("act", lambda e: e.copy(out=out, in_=in_), reads, writes)
            else:
                P.op(eng, lambda e: e.tensor_copy(out=out, in_=in_), reads, writes)

        plan = []
        for s in range(nseq):
            for l in layers:
                for fi in range(2):
                    if fi == 1 and do_mixer:
                        for i in range(19):
                            plan.append((("win", s, l, i), winr[l, i], 1024))
                        for q in range(4):
                            plan.append((("cw1", s, l, q), cw1r[l, q], 1024))
                        for dc in range(8):
                            plan.append((("wout", s, l, dc), woutr[l, dc], 1024))
                    if not do_ffn:
                        continue
                    for g in range(NG):
                        for i in range(GSZ):
                            fc = g * GSZ + i
                            plan.append((("w1", s, l, fi, fc), w1r[l, fi, fc], 1024))
                            plan.append((("w3", s, l, fi, fc), w3r[l, fi, fc], 1024))
                        for dc in range(8):
                            plan.append((("w2", s, l, fi, g, dc, 0), w2r[l, fi, g, dc][:, 0:768], 768))
                            plan.append((("w2", s, l, fi, g, dc, 1), w2r[l, fi, g, dc][:, 768:GSZ * 128], GSZ * 128 - 768))
        WS = WStream(P, WR, NW, plan, NW - 3)

        P.dma("sp", GN[:], fnorm, writes=["GN"], semkey="GN")
        P.dma("sp", CF[:], cstf, writes=["CF"], semkey="CF")
        for i in range(0, NCB, 1920):
            P.dma("pool", CB[:, i:min(NCB, i + 1920)], cstb[:, i:min(NCB, i + 1920)], writes=[("CB", i)], semkey=("CB", i))
        CBK = [("CB", i) for i in range(0, NCB, 1920)]
        P.dma("pool", VCA[:, 64:97], covl, writes=["VCA"], semkey="VCA")
        P.op("dve", lambda e: e.memset(EPSB[:], EPS), writes=["EPSB"])
        P.op("pool", lambda e: e.memset(VSW[:, :, :, 64:65], 1.0), writes=["VSW"])
        P.op("pool", lambda e: e.memset(CV[:, :, :, 64:65], 1.0), writes=["CV"])
        P.op("pool", lambda e: e.memset(SELT[:], 0.0), writes=["SELT"])
        P.op("pool", lambda e: e.memset(KCP[:], 0.0), writes=["KCP"])

        cnt = {"h": 0, "o": 0, "sl": 0, "sq": 0, "pb": 0, "s": 0, "et": 0, "m": 0, "cp": 0, "stg": 0}

        def sqbuf():
            j = cnt["sq"] % 2
            cnt["sq"] += 1
            return j

        def slbuf():
            j = cnt["sl"] % 2
            cnt["sl"] += 1
            return j

        reserved = set()

        def bank(lo=0, hi=8):
            while True:
                b = lo + cnt["pb"] % (hi - lo)
                cnt["pb"] += 1
                if b not in reserved:
                    return b

        def cpeng():
            cnt["cp"] += 1
            return "act" if cnt["cp"] % 2 else "dve"

        def rstd_from(pss_ap, pres, scale, n=512):
            act(RS[:, 0, 0:n], pss_ap, AF.Ln, [pres, "EPSB"], [("RS", 0)], scale=scale, bias=EPSB[:])
            act(RS[:, 0, 0:n], RS[:, 0, 0:n], AF.Exp, [("RS", 0)], [("RS", 0)], scale=-0.5)

        def rmsnorm(l, ni):
            for tc in range(4):
                ts = slice(tc * 512, (tc + 1) * 512)
                pb = bank()
                for kc in range(8):
                    j = sqbuf()
                    act(SQB[:, j, :], XT[:, kc, ts], AF.Square, [("XT", kc, tc)], [("SQB", j)])
                    mm(PS[pb][:], ONESB, SQB[:, j, :], kc == 0, kc == 7, [("SQB", j)] + CBK, [f"ps{pb}"])
                rstd_from(PS[pb][:], f"ps{pb}", 1.0 / D)
                for kc in range(8):
                    gi = (l * 3 + ni) * 8 + kc
                    stt(XN[:, kc, ts], XT[:, kc, ts], GN[:, gi:gi + 1], RS[:, 0, :], ALU.mult, ALU.mult,
                        [("XT", kc, tc), ("RS", 0), "GN"], [("XN", kc, tc)])

        def ffn(s, l, fi):
            rmsnorm(l, 0 if fi == 0 else 2)
            for g in range(NG):
                for i in range(GSZ):
                    fc = g * GSZ + i
                    w1t, w1res = WS.get(("w1", s, l, fi, fc))
                    w3t, w3res = WS.get(("w3", s, l, fi, fc))
                    for tc in range(4):
                        ts = slice(tc * 512, (tc + 1) * 512)
                        hb = (cnt["h"] % 2) * 2
                        cnt["h"] += 1
                        p1, p3 = PS[hb], PS[hb + 1]
                        for kc in range(8):
                            mm(p1[:], w1t[:, kc * 128:(kc + 1) * 128], XN[:, kc, ts], kc == 0, kc == 7,
                               [w1res, ("XN", kc, tc)], [f"ps{hb}"])
                        for kc in range(8):
                            mm(p3[:], w3t[:, kc * 128:(kc + 1) * 128], XN[:, kc, ts], kc == 0, kc == 7,
                               [w3res, ("XN", kc, tc)], [f"ps{hb + 1}"])
                        j = slbuf()
                        act(SL[:, j, :], p1[:], AF.Silu, [f"ps{hb}"], [("SL", j)])
                        tt(Gs(i, tc * 512, (tc + 1) * 512), SL[:, j, :], p3[:], ALU.mult,
                           [("SL", j), f"ps{hb + 1}"], [("G", i, tc)])
                for dc in range(8):
                    w2a, w2ares = WS.get(("w2", s, l, fi, g, dc, 0))
                    w2b, w2bres = WS.get(("w2", s, l, fi, g, dc, 1))
                    for tc in range(4):
                        ts = slice(tc * 512, (tc + 1) * 512)
                        ob = 4 + cnt["o"] % 2
                        cnt["o"] += 1
                        po = PS[ob]
                        for i in range(GSZ):
                            w2t, w2res, ii = (w2a, w2ares, i) if i < 6 else (w2b, w2bres, i - 6)
                            mm(po[:], w2t[:, ii * 128:(ii + 1) * 128], Gs(i, tc * 512, (tc + 1) * 512), i == 0, i == GSZ - 1,
                               [w2res, ("G", i, tc)], [f"ps{ob}"])
                        stt(XT[:, dc, ts], po[:], 0.5, XT[:, dc, ts], ALU.mult, ALU.add,
                            [f"ps{ob}", ("XT", dc, tc)], [("XT", dc, tc)])

        ZB = Gf[:, 0:2 * T].bitcast(F32)

        def zkeys(tc):
            return [("G", tc // 2, 2 * (tc % 2)), ("G", tc // 2, 2 * (tc % 2) + 1)]

        def proj_fm(wt, wres, tc, pb):
            ts = slice(tc * 512, (tc + 1) * 512)
            for kc in range(8):
                mm(PS[pb][:], wt[:, kc * 128:(kc + 1) * 128], XN[:, kc, ts], kc == 0, kc == 7,
                   [wres, ("XN", kc, tc)], [f"ps{pb}"])

        def gn_fm(l, slots, gcols, dests, tcs=(0, 1, 2, 3)):
            for tc in tcs:
                a, b = tc * 512, (tc + 1) * 512
                pb = bank()
                for i, sl in enumerate(slots):
                    j = sqbuf()
                    act(SQB[:, j, :], Gs(sl, a, b), AF.Square, [("G", sl, tc)], [("SQB", j)])
                    mm(PS[pb][:], ONESB, SQB[:, j, :], i == 0, i == 1, [("SQB", j)] + CBK, [f"ps{pb}"])
                rstd_from(PS[pb][:], f"ps{pb}", 1.0 / 256)
                for i, sl in enumerate(slots):
                    stt(XN[:, dests[i], a:b], Gs(sl, a, b), PRM[:, gcols[i]:gcols[i] + 1], RS[:, 0, :], ALU.mult, ALU.mult,
                        [("G", sl, tc), ("RS", 0), "PRM"], [("XN", dests[i], tc)])

        def gn_tm(oav, oares, gain, dest, tt_):
            tc = tt_ // 4
            tt(YT[:], oav, oav, ALU.mult, oares, ["YT"])
            P.op("dve", lambda e: e.reduce_sum(out=SM[:, 40:41], in_=YT[:], axis=mybir.AxisListType.X), ["YT"], [("SM", 40)])
            act(SM[:, 41:42], SM[:, 40:41], AF.Ln, [("SM", 40), "EPSB"], [("SM", 41)], scale=1.0 / 256, bias=EPSB[:])
            act(SM[:, 42:43], SM[:, 41:42], AF.Exp, [("SM", 41)], [("SM", 42)], scale=-0.5)
            stt(YT[:], oav, SM[:, 42:43], gain, ALU.mult, ALU.mult, oares + [("SM", 42), "PRM"], ["YT"])
            for i in range(2):
                pb = 7
                cnt["m"] += 1
                P.op("pe", lambda e: e.transpose(out=PS[pb][:, 0:128], in_=YT[:, i * 128:(i + 1) * 128], identity=IDF),
                     ["YT", "CF"], [f"ps{pb}"])
                cpy("dve", XN[:, dest + i, tt_ * 128:(tt_ + 1) * 128], PS[pb][:, 0:128], [f"ps{pb}"], [("XN", dest + i, tc)])

        def gn_a(oav, okey, gain):
            tt(YT[:], oav, oav, ALU.mult, [okey], ["YT"])
            P.op("dve", lambda e: e.reduce_sum(out=SM[:, 40:41], in_=YT[:], axis=mybir.AxisListType.X), ["YT"], [("SM", 40)])
            act(SM[:, 41:42], SM[:, 40:41], AF.Ln, [("SM", 40), "EPSB"], [("SM", 41)], scale=1.0 / 256, bias=EPSB[:])
            act(SM[:, 42:43], SM[:, 41:42], AF.Exp, [("SM", 41)], [("SM", 42)], scale=-0.5)
            stt(oav, oav, SM[:, 42:43], gain, ALU.mult, ALU.mult, [okey, ("SM", 42), "PRM"], [okey])

        def gn_b(oav, okey, dest, tt_):
            tc = tt_ // 4
            for i in range(2):
                P.op("pe", lambda e: e.transpose(out=PS[7][:, 0:128], in_=oav[:, i * 128:(i + 1) * 128], identity=IDF),
                     [okey, "CF"], ["ps7"])
                cpy("dve", XN[:, dest + i, tt_ * 128:(tt_ + 1) * 128], PS[7][:, 0:128], ["ps7"], [("XN", dest + i, tc)])

        def mixer(s, l):
            rmsnorm(l, 1)
            P.dma("sp", PRM[:], prm[l], writes=["PRM"], semkey="PRM")
            P.dma("pool", PRB[:], prb[l], writes=["PRB"], semkey="PRB")
            act(SINKE[:], PRM[:, 21:25], AF.Exp, ["PRM"], ["SINKE"])
            POSB = PRB[:, 0:32]
            CW2 = PRB[:, 32:224]
            PW = PRB[:, 224:480]
            wi = 0
            for cb in range(2):
                wc, rc = WS.get(("win", s, l, wi)); wx, rx = WS.get(("win", s, l, wi + 1)); wb, rb = WS.get(("win", s, l, wi + 2))
                wi += 3
                for tc in range(4):
                    a, b = tc * 512, (tc + 1) * 512
                    b1, b2, b3 = bank(), bank(), bank()
                    proj_fm(wc, rc, tc, b1); proj_fm(wx, rx, tc, b2); proj_fm(wb, rb, tc, b3)
                    j = sqbuf()
                    cpy("act", SQ[:, j, :], PS[b1][:], [f"ps{b1}"], [("SQ", j)])
                    tt(ZB[:, a:b], SQ[:, j, :], PS[b2][:], ALU.mult, [("SQ", j), f"ps{b2}"], zkeys(tc))
                    j2 = sqbuf()
                    zk = zkeys(tc) + (zkeys(tc - 1) if tc else [])
                    cw = lambda k: PRM[:, 9 + cb * 3 + k: 10 + cb * 3 + k]
                    tsc(SQ[:, j2, :], ZB[:, a:b], cw(2), ALU.mult, zk + ["PRM"], [("SQ", j2)])
                    for k, sh in ((1, 1), (0, 2)):
                        lo = max(a, sh)
                        stt(SQ[:, j2, lo - a:512], ZB[:, lo - sh:b - sh], cw(k), SQ[:, j2, lo - a:512], ALU.mult, ALU.add,
                            zk + ["PRM", ("SQ", j2)], [("SQ", j2)])
                    tt(Gs(S_YB0 + cb, a, b), SQ[:, j2, :], PS[b3][:], ALU.mult, [("SQ", j2), f"ps{b3}"], [("G", S_YB0 + cb, tc)])
            if stop_after == "B":
                return
            dst = {}

            def d_s1(idx):
                cd, tc = idx // 4, idx % 4
                if tc == 0:
                    dst[cd] = WS.get(("win", s, l, wi + cd))
                b1 = bank()
                proj_fm(dst[cd][0], dst[cd][1], tc, b1)
                return b1

            def d_s2(idx, b1):
                cd, tc = idx // 4, idx % 4
                a, b = tc * 512, (tc + 1) * 512
                b2 = bank()
                zk = zkeys(tc) + (zkeys(tc - 1) if tc else [])
                init = 0.0 if tc == 0 else ZB[:, a - 1:a]
                P.op("dve", lambda e: e.tensor_tensor_scan(out=ZB[:, a:b], data0=CB[:, CB_ZERO:CB_ZERO + 512], data1=PS[b1][:],
                                                           initial=init, op0=ALU.add, op1=ALU.add),
                     [f"ps{b1}"] + CBK + zk, zkeys(tc))
                j = sqbuf()
                for half in range(2):
                    w = POOLWIN[2 * cd + half]
                    pr = slice(half * 64, half * 64 + 64)
                    lo = max(a, w)
                    tt(SQ[pr, j, lo - a:512], ZB[pr, lo:b], ZB[pr, lo - w:b - w], ALU.subtract, zk, [("SQ", j)])
                    if tc == 0:
                        cpy("dve", SQ[pr, j, 0:w], ZB[pr, 0:w], zk, [("SQ", j)])
                if tc == 0:
                    tt(SQ[:, j, 0:16], SQ[:, j, 0:16], CF[:, 130 + cd * 16:130 + cd * 16 + 16], ALU.mult, [("SQ", j), "CF"], [("SQ", j)])
                js = slbuf()
                stt(SL[:, js, :], SQ[:, j, :], CF[:, 128 + cd:129 + cd], PS[b1][:], ALU.mult, ALU.subtract,
                    [("SQ", j), "CF", f"ps{b1}"], [("SL", js)])
                mm(PS[b2][:], PW[:, cd * 128:(cd + 1) * 128], SL[:, js, :], True, True, ["PRB", ("SL", js)], [f"ps{b2}"])
                tsc(Gs(S_YD0 + cd, a, b), PS[b2][:], PRM[:, 15 + cd:16 + cd], ALU.mult, [f"ps{b2}", "PRM"], [("G", S_YD0 + cd, tc)])

            def run2(n, s1, s2):
                bk = {0: s1(0)}
                for i in range(n):
                    if i + 1 < n:
                        bk[i + 1] = s1(i + 1)
                    s2(i, bk[i])
            run2(8, d_s1, d_s2)
            wi += 2
            if stop_after == "D":
                return
            qst = {}

            def q_s1(idx):
                ct, tc = idx // 4, idx % 4
                if tc == 0:
                    qst[ct] = WS.get(("win", s, l, wi + ct))
                b1 = bank()
                proj_fm(qst[ct][0], qst[ct][1], tc, b1)
                return b1

            def q_s2(idx, b1):
                ct, tc = idx // 4, idx % 4
                a, b = tc * 512, (tc + 1) * 512
                if ct == S_KVC:
                    cpy(cpeng(), Gs(ct, a, b), PS[b1][:], [f"ps{b1}"], [("G", ct, tc)])
                    return
                j = sqbuf()
                act(SQB[:, j, :], PS[b1][:], AF.Square, [f"ps{b1}"], [("SQB", j)])
                b2 = bank()
                mm(PS[b2][:], BD64B, SQB[:, j, :], True, True, [("SQB", j)] + CBK, [f"ps{b2}"])
                rstd_from(PS[b2][:], f"ps{b2}", 1.0 / 64)
                stt(Gs(ct, a, b), PS[b1][:], PRM[:, ct:ct + 1], RS[:, 0, :], ALU.mult, ALU.mult,
                    [f"ps{b1}", "PRM", ("RS", 0)], [("G", ct, tc)])
            run2(32, q_s1, q_s2)
            wi += 8
            if stop_after == "qk":
                return
            for ti in range(3):
                wt, wres = WS.get(("win", s, l, wi)); wi += 1
                for t16 in range(16):
                    tc = t16 // 4
                    pb = bank()
                    for kc in range(8):
                        mm(PS[pb][:, 0:128], XN[:, kc, t16 * 128:(t16 + 1) * 128], wt[:, kc * 128:(kc + 1) * 128], kc == 0, kc == 7,
                           [wres, ("XN", kc, tc)], [f"ps{pb}"])
                    if ti == 0:
                        cpy("act", VSW[:, t16, 0, 0:64], PS[pb][:, 0:64], [f"ps{pb}"], ["VSW"])
                        cpy("dve", VSW[:, t16, 1, 0:64], PS[pb][:, 64:128], [f"ps{pb}"], ["VSW"])
                    elif ti == 1:
                        cpy("act", CV[:, t16, 0, 0:64], PS[pb][:, 0:64], [f"ps{pb}"], ["CV"])
                        cpy("dve", CV[:, t16, 1, 0:64], PS[pb][:, 64:128], [f"ps{pb}"], ["CV"])
                    else:
                        act(GT[:, t16, :], PS[pb][:, 0:12], AF.Sigmoid, [f"ps{pb}"], ["GT"])
            if stop_after == "tm":
                return
            bA, bB = bank(), bank()
            reserved.update((bA, bB))
            kvr = [("G", S_KVC, tc) for tc in range(4)]
            for q in range(4):
                wt, wres = WS.get(("cw1", s, l, q))
                for l8 in range(8):
                    ll = 8 * q + l8
                    for pr, bk in ((slice(0, 64), bA), (slice(64, 128), bB)):
                        mm(PS[bk][:, 0:127], wt[pr, l8 * 128:(l8 + 1) * 128], Gf[pr, S_KVC * T + ll: S_KVC * T + ll + 2017: 16],
                           ll == 0, False, [wres] + kvr, [f"ps{bk}"])
                        mm(PS[bk][:, 127:128], wt[pr, l8 * 128:(l8 + 1) * 128], POSB[pr, ll:ll + 1],
                           False, ll == 31, [wres, "PRB"], [f"ps{bk}"])
                gn_fm(l, (S_YB0, S_YB1), (17, 18), (4, 5), tcs=(q,))
                gn_fm(l, (S_YD0, S_YD1), (19, 20), (6, 7), tcs=(q,))
            for i, bk in enumerate((bA, bB)):
                X, X2, X3 = SQ[:, 0, 0:127], SQ[:, 0, 128:255], SQ[:, 0, 256:383]
                cpy("act", SM[:, 50 + i:51 + i], PS[bk][:, 127:128], [f"ps{bk}"], [("SM", 50 + i)])
                tsc(X, PS[bk][:, 0:127], SM[:, 50 + i:51 + i], ALU.add, [f"ps{bk}", ("SM", 50 + i)], [("SQ", 0)])
                tt(X2, X, X, ALU.mult, [("SQ", 0)], [("SQ", 0)])
                tsc(X2, X2, 0.044715, ALU.mult, [("SQ", 0)], [("SQ", 0)], s2=1.0, op1=ALU.add)
                tt(X2, X2, X, ALU.mult, [("SQ", 0), ("SQ", 0)], [("SQ", 0)])
                act(X3, X2, AF.Sigmoid, [("SQ", 0)], [("SQ", 0)], scale=1.5957691216057308)
                tt(HKV[:, i, 0:127], X, X3, ALU.mult, [("SQ", 0), ("SQ", 0)], [("HKV", i)])
            reserved.clear()
            b1, b2, b3 = bank(), bank(), bank()
            mm(PS[b1][:, 0:127], CW2[:, 0:128], HKV[:, 0, 0:127], True, True, ["PRB", ("HKV", 0)], [f"ps{b1}"])
            act(SQB[:, 1, 0:127], PS[b1][:, 0:127], AF.Square, [f"ps{b1}"], [("SQB", 1)])
            P.op("dve", lambda e: e.memset(SQB[:, 1, 127:128], 1.0), [("SQB", 1)], [("SQB", 1)])
            mm(PS[b2][:, 0:128], BD64B, SQB[:, 1, 0:128], True, True, [("SQB", 1)] + CBK, [f"ps{b2}"])
            rstd_from(PS[b2][:, 0:128], f"ps{b2}", 1.0 / 64, n=128)
            for hf in range(2):
                pr = slice(hf * 64, hf * 64 + 64)
                stt(KCP[pr, hf, 0:127], PS[b1][pr, 0:127], PRM[pr, 8:9], RS[pr, 0, 0:127], ALU.mult, ALU.mult,
                    [f"ps{b1}", "PRM", ("RS", 0)], ["KCP"])
            mm(PS[b3][0:127, 0:64], HKV[:, 1, 0:127], CW2[:, 128:192], True, True, ["PRB", ("HKV", 1)], [f"ps{b3}"])
            cpy("act", VCA[0:127, 0:64], PS[b3][0:127, 0:64], [f"ps{b3}"], ["VCA"])
            if stop_after == "cmp":
                return
            if stop_after == "gn":
                return
            def zpad(dst, src, hf):
                pr, po = slice(hf * 64, hf * 64 + 64), slice((1 - hf) * 64, (1 - hf) * 64 + 64)
                allk = lambda sl: [("G", sl, t_) for t_ in range(4)]
                P.op("dve", lambda e: e.memset(Gf[po, dst * T:(dst + 1) * T], 0.0), [], allk(dst))
                P.op("act", lambda e: e.copy(out=Gf[pr, dst * T:(dst + 1) * T], in_=Gf[pr, src * T:(src + 1) * T]),
                     allk(src), allk(dst))
            zpad(S_YB0, S_KS, 0); zpad(S_YB1, S_KS, 1)
            zpad(S_YD0, S_KW, 0); zpad(S_YD1, S_KW, 1)
            K_SLC, K_WIN, K_SWA = S_YB0, S_YD0, S_KS
            GNA = PRM[:, 25:281]
            GNC = PRM[:, 281:537]

            def evac_a(qs, t16, ncol, br, first):
                ob = (t16 // 4) % 2
                ps = PS[qs]
                pr = f"ps{qs}"
                S = SM2[:, qs, :]
                sk = ("SM2", qs)
                tsc(S[:, 0:4], ps[:, 64:64 + 3 * ncol + 1:ncol], 1e-30, ALU.max, [pr], [sk])
                recip(S[:, 4:8], S[:, 0:4], [sk], [sk])
                tt(S[:, 8:12], S[:, 4:8], GT[:, t16, br:12:3], ALU.mult, [sk, "GT"], [sk])
                psv = ps[:, 0:4 * ncol].rearrange("p (h c) -> p h c", c=ncol)
                oav = OA[:, ob, qs, :].rearrange("p (h d) -> p h d", d=64)
                okey = ("OA", ob, qs)
                facb = S[:, 8:12].unsqueeze(2).broadcast_to([128, 4, 64])
                if first:
                    tt(oav, psv[:, :, 0:64], facb, ALU.mult, [pr, sk], [okey])
                else:
                    tt(YT[:].rearrange("p (h d) -> p h d", d=64), psv[:, :, 0:64], facb, ALU.mult, [pr, sk], ["YT"])
                    tt(OA[:, ob, qs, :], OA[:, ob, qs, :], YT[:], ALU.add, ["YT", okey], [okey])
                if br == 0:
                    IMP = SCI[:, qs, :]
                    rdb = S[:, 4:8].unsqueeze(2).broadcast_to([128, 4, 32])
                    tt(YT[:, 0:128].rearrange("p (h j) -> p h j", j=32), psv[:, :, 65:97], rdb, ALU.mult, [pr, sk], ["YT"])
                    P.op("dve", lambda e: e.tensor_reduce(out=IMP, in_=YT[:, 0:128].rearrange("p (h j) -> p j h", j=32),
                                                         axis=mybir.AxisListType.X, op=ALU.add), ["YT"], [("IMP", qs)])

            def evac_b(qs, t16, br, last):
                if br == 0:
                    IMP, SCR, SELM, M8 = SCI[:, qs, :], SC[:, 1, :], SC[:, 2, :], SC[:, 3, 0:8]
                    tt(SCR, IMP, CB[:, CB_SELA + t16 * 32:CB_SELA + (t16 + 1) * 32], ALU.mult, [("IMP", qs)] + CBK, ["SCR"])
                    tt(SCR, SCR, CB[:, CB_SELB + t16 * 32:CB_SELB + (t16 + 1) * 32], ALU.add, ["SCR"] + CBK, ["SCR"])
                    P.op("dve", lambda e: e.max(out=M8, in_=SCR), ["SCR"], ["M8"])
                    tsc(SELM, SCR, SC[:, 3, 7:8], ALU.is_ge, ["SCR", "M8"], ["SELM"], s2=-1.0, op1=ALU.add)
                    pb = 7
                    cnt["m"] += 1
                    P.op("pe", lambda e: e.transpose(out=PS[pb][0:32, 0:128], in_=SELM, identity=IDF), ["SELM", "CF"], [f"ps{pb}"])
                    cpy("act", SELT[0:32, qs * 128:(qs + 1) * 128], PS[pb][0:32, 0:128], [f"ps{pb}"], ["SELT"])
                if last:
                    ob = (t16 // 4) % 2
                    gn_tm(OA[:, ob, qs, :], [("OA", ob, qs)], GNA, 0, t16)

            def evac_all(c, ncol, br, first, last):
                for qs in range(4):
                    evac_a(qs, 4 * c + qs, ncol, br, first)
                for qs in range(4):
                    evac_b(qs, 4 * c + qs, br, last)

            def sbank():
                b = 4 + cnt["s"] % 3
                cnt["s"] += 1
                return b

            def etbuf():
                j = cnt["et"] % 3
                cnt["et"] += 1
                return j

            for c in range(4):
                ca, cbb = c * 512, (c + 1) * 512
                def pipeline(units):
                    n = len(units)
                    bks = []
                    for k in range(min(2, n)):
                        bks.append(sbank())
                        units[k][0](bks[k])
                    for i in range(n):
                        if i + 2 < n:
                            bks.append(sbank())
                            units[i + 2][0](bks[i + 2])
                        j = etbuf()
                        units[i][1](bks[i], j)
                        units[i][2](j)

                units = []
                for h in range(4):
                    aq = S_AQ0 + h // 2

                    def sc(sbk, h=h, aq=aq):
                        mm(PS[sbk][0:127, :], KCP[:, h % 2, 0:127], Gs(aq, ca, cbb), True, True,
                           ["KCP", ("G", aq, c)], [f"ps{sbk}"])

                    def ex(sbk, j):
                        act(ET[0:127, j, :], PS[sbk][0:127, :], AF.Exp, [f"ps{sbk}"], [("ET", j)], scale=0.125)
                        P.op("pool", lambda e: e.affine_select(out=ET[0:127, j, :], in_=ET[0:127, j, :], pattern=[[1, 512]],
                                                               compare_op=ALU.is_ge, fill=0.0, base=512 * c - 31, channel_multiplier=-16),
                             [("ET", j)], [("ET", j)])

                    def pv(j, h=h):
                        for qs in range(4):
                            mm(PS[qs][:, h * 97:(h + 1) * 97], ET[0:127, j, qs * 128:(qs + 1) * 128], VCA[0:127, 0:97], h == 0, True,
                               [("ET", j), "VCA"], [f"ps{qs}"])
                    units.append((sc, ex, pv))
                pipeline(units)
                for qs in range(4):
                    evac_a(qs, 4 * c + qs, 97, 0, True)
                if c > 0:
                    for qs in range(4):
                        gn_a(OA[:, (c - 1) % 2, qs, :], ("OA", (c - 1) % 2, qs), GNA)

                def ex_full(sbk, j):
                    act(ET[:, j, :], PS[sbk][:], AF.Exp, [f"ps{sbk}"], [("ET", j)], scale=0.125)
                units = []
                for h in range(4):
                    aq = S_AQ0 + h // 2
                    for kt in range(max(0, 4 * c - 4), 4 * c + 4):
                        r = 4 * c - kt

                        def sc(sbk, h=h, aq=aq, kt=kt, r=r):
                            mm(PS[sbk][:], Gs(K_WIN + h % 2, kt * 128, (kt + 1) * 128), Gs(aq, ca, cbb), True, False,
                               [("G", K_WIN + h % 2, kt // 4), ("G", aq, c)], [f"ps{sbk}"])
                            if r <= 0:
                                msk = CB[:, CB_CAUS + 384 + 128 * r:CB_CAUS + 384 + 128 * r + 512]
                            else:
                                msk = CB[:, CB_WINM + 128 * (r - 1):CB_WINM + 128 * (r - 1) + 512]
                            mm(PS[sbk][:], IDB, msk, False, True, CBK, [f"ps{sbk}"])

                        def pv(j, h=h, kt=kt):
                            for qs in range(4):
                                tq = 4 * c + qs
                                if kt > tq or kt < tq - 4:
                                    continue
                                mm(PS[qs][:, h * 65:(h + 1) * 65], ET[:, j, qs * 128:(qs + 1) * 128], VSW[:, kt, 1, :],
                                   h == 0 and kt == max(0, tq - 4), True, [("ET", j), "VSW"], [f"ps{qs}"])
                        units.append((sc, ex_full, pv))
                pipeline(units)
                for qs in range(4):
                    evac_b(qs, 4 * c + qs, 0, False)
                for qs in range(4):
                    evac_a(qs, 4 * c + qs, 65, 2, False)
                units = []
                for h in range(4):
                    aq = S_AQ0 + h // 2
                    for kt in range(4 * c + 4):
                        r = 4 * c - kt

                        def sc(sbk, h=h, aq=aq, kt=kt, r=r):
                            mm(PS[sbk][:], Gs(K_SLC + h % 2, kt * 128, (kt + 1) * 128), Gs(aq, ca, cbb), True, False,
                               [("G", K_SLC + h % 2, kt // 4), ("G", aq, c)], [f"ps{sbk}"])
                            mm(PS[sbk][:], CB[:, CB_EXP + kt * 128:CB_EXP + (kt + 1) * 128], SELT[:, :], False, r > 0,
                               CBK + ["SELT"], [f"ps{sbk}"])
                            if r <= 0:
                                mm(PS[sbk][:], IDB, CB[:, CB_CAUS + 384 + 128 * r:CB_CAUS + 384 + 128 * r + 512], False, True,
                                   CBK, [f"ps{sbk}"])

                        def pv(j, h=h, kt=kt):
                            for qs in range(4):
                                if kt > 4 * c + qs:
                                    continue
                                mm(PS[qs][:, h * 65:(h + 1) * 65], ET[:, j, qs * 128:(qs + 1) * 128], VSW[:, kt, 0, :],
                                   h == 0 and kt == 0, True, [("ET", j), "VSW"], [f"ps{qs}"])
                        units.append((sc, ex_full, pv))
                pipeline(units)
                if c > 0:
                    for qs in range(4):
                        gn_b(OA[:, (c - 1) % 2, qs, :], ("OA", (c - 1) % 2, qs), 0, 4 * (c - 1) + qs)
                for qs in range(4):
                    evac_a(qs, 4 * c + qs, 65, 1, False)
            for qs in range(4):
                gn_a(OA[:, 1, qs, :], ("OA", 1, qs), GNA)
                gn_b(OA[:, 1, qs, :], ("OA", 1, qs), 0, 12 + qs)
            if stop_after == "nsa":
                return
            zpad(S_KS, S_CK, 0); zpad(S_KW, S_CK, 1)
            deferred = []

            def swa_evac(kt):
                ps, pr = PS[kt % 4], f"ps{kt % 4}"
                tt(SM[:, 0:4], ps[:, 64:64 + 3 * 65 + 1:65], SINKE[:], ALU.add, [pr, "SINKE"], [("SM", 0)])
                recip(SM[:, 4:8], SM[:, 0:4], [("SM", 0)], [("SM", 4)])
                tt(OA[:, 0, 0, :].rearrange("p (h d) -> p h d", d=64), ps[:, 0:260].rearrange("p (h c) -> p h c", c=65)[:, :, 0:64],
                   SM[:, 4:8].unsqueeze(2).broadcast_to([128, 4, 64]), ALU.mult, [pr, ("SM", 4)], [("OA", 0, 0)])
                gn_tm(OA[:, 0, 0, :], [("OA", 0, 0)], GNC, 2, kt)

            units = []
            for kt in range(16):
                nq = 256 if kt < 15 else 128
                q0 = kt * 128
                qres = sorted(set([q0 // 512, (q0 + nq - 1) // 512]))
                for ui, (a_, hf) in enumerate(((0, 0), (0, 1), (1, 0), (1, 1))):
                    h = 2 * hf + a_

                    def sc(sbk, kt=kt, nq=nq, q0=q0, qres=qres, a_=a_, hf=hf):
                        mm(PS[sbk][:, 0:nq], Gs(K_SWA + hf, kt * 128, (kt + 1) * 128), Gs(S_CQ0 + a_, q0, q0 + nq),
                           True, False, [("G", K_SWA + hf, kt // 4)] + [("G", S_CQ0 + a_, t_) for t_ in qres], [f"ps{sbk}"])
                        mm(PS[sbk][:, 0:nq], IDB, CB[:, CB_BAND:CB_BAND + nq], False, True, CBK, [f"ps{sbk}"])

                    def ex(sbk, j, nq=nq):
                        act(ET[:, j, 0:nq], PS[sbk][:, 0:nq], AF.Exp, [f"ps{sbk}"], [("ET", j)], scale=0.125)

                    def pv(j, kt=kt, h=h, hf=hf, ui=ui):
                        b0, b1 = kt % 4, (kt + 1) % 4
                        mm(PS[b0][:, h * 65:(h + 1) * 65], ET[:, j, 0:128], CV[:, kt, hf, :], kt == 0 and ui == 0, True,
                           [("ET", j), "CV"], [f"ps{b0}"])
                        if kt < 15:
                            mm(PS[b1][:, h * 65:(h + 1) * 65], ET[:, j, 128:256], CV[:, kt, hf, :], ui == 0, False,
                               [("ET", j), "CV"], [f"ps{b1}"])
                        if ui == 3:
                            deferred.append(lambda kt=kt: swa_evac(kt))
                    units.append((sc, ex, pv))
            bks = []
            for k in range(2):
                bks.append(sbank())
                units[k][0](bks[k])
            for i in range(len(units)):
                if i + 2 < len(units):
                    bks.append(sbank())
                    units[i + 2][0](bks[i + 2])
                j = etbuf()
                units[i][1](bks[i], j)
                units[i][2](j)
                if i % 4 == 1 and len(deferred) and i > 8:
                    deferred.pop(0)()
            while deferred:
                deferred.pop(0)()
            if stop_after == "swa":
                return
            ysrc = (0, 1, 4, 5, 2, 3, 6, 7)
            for dc in range(8):
                wt, wres = WS.get(("wout", s, l, dc))
                for tc in range(4):
                    ts = slice(tc * 512, (tc + 1) * 512)
                    pb = bank()
                    for kc in range(8):
                        mm(PS[pb][:], wt[:, kc * 128:(kc + 1) * 128], XN[:, ysrc[kc], ts], kc == 0, kc == 7,
                           [wres, ("XN", ysrc[kc], tc)], [f"ps{pb}"])
                    tt(XT[:, dc, ts], PS[pb][:], XT[:, dc, ts], ALU.add, [f"ps{pb}", ("XT", dc, tc)], [("XT", dc, tc)])

        for s in range(nseq):
            for kc in range(8):
                P.dma("sp", XT[:, kc, :], xT[s, :, kc, :],
                      writes=[("XT", kc, tc) for tc in range(4)], semkey=("XT", kc))
            for l in layers:
                if do_ffn:
                    ffn(s, l, 0)
                if do_mixer:
                    mixer(s, l)
                if do_ffn:
                    ffn(s, l, 1)
            for kc in range(8):
                P.dma("sp", yT[s, :, kc, :], XT[:, kc, :],
                      reads=[("XT", kc, tc) for tc in range(4)], semkey=("XT", kc))
        P.finish()
        print("instructions", P.ninst, "waits", P.nwait)
    return nc


def prep_inputs(inp):
    f = lambda a: np.ascontiguousarray(np.asarray(a, dtype=np.float32))
    x = f(inp["x"])
    B = x.shape[0]
    xTh = np.ascontiguousarray(x.reshape(B, T, 8, 128).transpose(0, 3, 2, 1))

    def w13(w):
        w = f(w).reshape(L, 8, 128, NFC, 128)
        return np.ascontiguousarray(w.transpose(0, 3, 2, 1, 4)).reshape(L, NFC, 128, 1024)

    def w2(w):
        w = f(w).reshape(L, NG, GSZ, 128, 8, 128)
        return np.ascontiguousarray(w.transpose(0, 1, 4, 3, 2, 5)).reshape(L, NG, 8, 128, GSZ * 128)

    w1r = np.stack([w13(inp["ffn1_w1"]), w13(inp["ffn2_w1"])], axis=1)
    w3r = np.stack([w13(inp["ffn1_w3"]), w13(inp["ffn2_w3"])], axis=1)
    w2r = np.stack([w2(inp["ffn1_w2"]), w2(inp["ffn2_w2"])], axis=1)
    nrm = np.stack([f(inp["ffn1_norm"]), f(inp["mix_norm"]), f(inp["ffn2_norm"])], axis=1)
    fnorm = np.ascontiguousarray(nrm.reshape(L, 3, 8, 128).transpose(3, 0, 1, 2)).reshape(128, L * 3 * 8)

    o = {}
    off = 0
    for name, sz in (("a_q", 256), ("a_kc", 64), ("a_vc", 64), ("a_ks", 64), ("a_vs", 64), ("a_kw", 64), ("a_vw", 64),
                     ("a_g", 12), ("b_b", 256), ("b_c", 256), ("b_x", 256), ("c_q", 256), ("c_k", 128), ("c_v", 128), ("d_v", 256)):
        o[name] = off
        off += sz
    r = lambda name, a, n: list(range(o[name] + a, o[name] + a + n))
    tiles = [r("b_c", 0, 128), r("b_x", 0, 128), r("b_b", 0, 128), r("b_c", 128, 128), r("b_x", 128, 128), r("b_b", 128, 128),
             r("d_v", 0, 128), r("d_v", 128, 128),
             r("a_q", 0, 128), r("a_q", 128, 128), r("a_ks", 0, 64) * 2, r("a_kw", 0, 64) * 2,
             r("a_kc", 0, 64) + r("a_vc", 0, 64),
             r("c_q", 0, 64) + r("c_q", 128, 64), r("c_q", 64, 64) + r("c_q", 192, 64), r("c_k", 0, 128),
             r("a_vs", 0, 64) + r("a_vw", 0, 64), r("c_v", 0, 128), r("a_g", 0, 12) + [-1] * 116]
    w_in = f(inp["w_in"])
    w_in_p = np.concatenate([w_in, np.zeros((L, D, 1), np.float32)], axis=2)
    winr = np.stack([w_in_p[:, :, cols].reshape(L, 8, 128, 128).transpose(0, 2, 1, 3).reshape(L, 128, 1024)
                     for cols in tiles], axis=1)
    winr = np.ascontiguousarray(winr)
    ck = f(inp["cmp_w1_k"]).reshape(L, 4, 8, 64, 128)
    cvv = f(inp["cmp_w1_v"]).reshape(L, 4, 8, 64, 128)
    cw1r = np.ascontiguousarray(np.concatenate([ck, cvv], axis=3).transpose(0, 1, 3, 2, 4)).reshape(L, 4, 128, 1024)
    woutr = np.ascontiguousarray(f(inp["w_out"]).reshape(L, 8, 128, 8, 128).transpose(0, 3, 2, 1, 4)).reshape(L, 8, 128, 1024)
    prm = np.zeros((L, 128, NPRM), np.float32)
    p64 = np.arange(128) % 64
    qk_gain = {0: "nsa_q_norm", 1: "nsa_q_norm", 2: "nsa_ks_norm", 3: "nsa_kw_norm", 5: "swa_q_norm", 6: "swa_q_norm", 7: "swa_k_norm"}
    for ct, nm in qk_gain.items():
        prm[:, :, ct] = f(inp[nm])[:, p64]
    prm[:, :, 4] = 1.0
    prm[:, :, 8] = f(inp["nsa_kc_norm"])[:, p64]
    cwv = f(inp["conv_w"])
    for cb in range(2):
        for k in range(3):
            prm[:, :, 9 + cb * 3 + k] = cwv[:, k, cb * 128:(cb + 1) * 128]
    psc = f(inp["pool_scale"])
    gnv = f(inp["group_norm"])
    for c in range(2):
        prm[:, :, 15 + c] = psc[:, c * 128:(c + 1) * 128]
        prm[:, :, 17 + c] = gnv[:, 256 + c * 128:256 + (c + 1) * 128]
        prm[:, :, 19 + c] = gnv[:, 768 + c * 128:768 + (c + 1) * 128]
    prm[:, :, 21:25] = f(inp["swa_sinks"])[:, None, :]
    prm[:, :, 25:281] = gnv[:, None, 0:256]
    prm[:, :, 281:537] = gnv[:, None, 512:768]
    prb = np.zeros((L, 128, NPRB), np.float32)
    prb[:, 0:64, 0:32] = f(inp["cmp_pos_k"]).transpose(0, 2, 1)
    prb[:, 64:128, 0:32] = f(inp["cmp_pos_v"]).transpose(0, 2, 1)
    prb[:, :, 32:96] = f(inp["cmp_w2_k"])
    prb[:, :, 96:160] = f(inp["cmp_w2_k"])
    prb[:, :, 160:224] = f(inp["cmp_w2_v"])
    pw = f(inp["pool_w"])
    for c in range(2):
        prb[:, 0:64, 224 + c * 128:224 + c * 128 + 64] = pw[:, 2 * c]
        prb[:, 64:128, 224 + c * 128 + 64:224 + (c + 1) * 128] = pw[:, 2 * c + 1]
    cstf = np.zeros((128, NCF), np.float32)
    pp = np.arange(128)
    cstf[:, 0:128] = np.eye(128)
    for c in range(2):
        wp = np.where(pp < 64, POOLWIN[2 * c], POOLWIN[2 * c + 1]).astype(np.float32)
        cstf[:, 128 + c] = 1.0 / wp
        tcol = np.arange(16)[None, :]
        cstf[:, 130 + c * 16:130 + (c + 1) * 16] = wp[:, None] / np.minimum(tcol + 1, wp[:, None])
    cstb = np.zeros((128, NCB), np.float32)
    cstb[:, CB_IDB:CB_IDB + 128] = np.eye(128)
    k = pp[:, None]
    xx = np.arange(896)[None, :]
    cstb[:, CB_CAUS:CB_CAUS + 896] = np.where(xx - 384 - k >= 0, 0.0, NEGB)
    cstb[:, CB_WINM:CB_WINM + 896] = np.where(k - xx + 383 >= 0, 0.0, NEGB)
    xb = np.arange(256)[None, :]
    cstb[:, CB_BAND:CB_BAND + 256] = np.where((xb - k >= 0) & (xb - k < 128), 0.0, NEGB)
    tglob = (np.arange(16)[None, :, None] * 128 + pp[:, None, None])
    jj = np.arange(32)[None, None, :]
    cur = tglob // 64
    forced = (jj == 0) | (jj == cur) | (jj == cur - 1)
    future = jj * 64 > tglob
    cstb[:, CB_SELA:CB_SELA + 512] = np.where(forced | future, 0.0, 1.0).reshape(128, 512)
    cstb[:, CB_SELB:CB_SELB + 512] = np.where(forced, 1e4, np.where(future, -1.0, 0.0)).reshape(128, 512)
    ex = np.zeros((128, 16, 128), np.float32)
    for kt in range(16):
        ex[2 * kt, kt, 0:64] = -NEGB
        ex[2 * kt + 1, kt, 64:128] = -NEGB
    cstb[:, CB_EXP:CB_EXP + 2048] = ex.reshape(128, 2048)
    cstb[:, CB_ONES:CB_ONES + 128] = 1.0
    cstb[:, CB_BD64:CB_BD64 + 128] = (pp[:, None] // 64 == pp[None, :] // 64)
    covl = np.zeros((128, 33), np.float32)
    covl[:, 0] = 1.0
    ci = np.arange(127)[:, None] * 16
    sj = np.arange(32)[None, :] * 64
    covl[0:127, 1:33] = ((ci <= sj + 63) & (ci + 31 >= sj))
    shared = dict(w1r=w1r, w3r=w3r, w2r=w2r, fnorm=fnorm, winr=winr, cw1r=cw1r, woutr=woutr, prm=prm, prb=prb,
                  cstf=cstf, cstb=cstb, covl=covl)
    return xTh, shared


def kernel(**inputs):
    xTh, shared = prep_inputs(inputs)
    nc = build_program()
    in_maps = []
    for c in range(NCORES):
        m = dict(shared)
        m["xT"] = np.ascontiguousarray(xTh[c * SEQ_PER_CORE:(c + 1) * SEQ_PER_CORE])
        in_maps.append(m)
    res = run_bass_kernel_spmd(nc, in_maps, core_ids=list(range(NCORES)))
    yT = np.concatenate([r["yT"] for r in res.results], axis=0)
    out = np.ascontiguousarray(yT.transpose(0, 3, 2, 1)).reshape(-1, T, D)
    return out.astype(np.float32)
```
